# Optimizing a Trainium2 kernel written in Bass

```python
import math
import jax
import jax.numpy as jnp
from jax import lax
import numpy as np

D_MODEL = 1024
BATCH = 8
SEQ = 4096
DEPTH = 2

GRID_W = 64
CTX_LEN = 256
GDN_HEAD_DIM = 128
GDN_HEADS = D_MODEL // (2 * GDN_HEAD_DIM)
GDN_WIDTH = GDN_HEADS * GDN_HEAD_DIM
CONV_W = 5
CHUNK = 64
ATT_HEAD_DIM = 128
ATT_Q_HEADS = D_MODEL // (2 * ATT_HEAD_DIM)
ATT_KV_HEADS = ATT_Q_HEADS // 2
ATT_WIDTH = ATT_Q_HEADS * ATT_HEAD_DIM
ATT_KV_WIDTH = ATT_KV_HEADS * ATT_HEAD_DIM
ATT_GROUP = ATT_Q_HEADS // ATT_KV_HEADS
ROPE_AXIS_DIM = ATT_HEAD_DIM // 2
ROPE_THETA = 10000.0
Q_BLOCK = 128
D_MIX = GDN_WIDTH + ATT_WIDTH
OFF_QKV = 0
OFF_Z = 3 * GDN_WIDTH
OFF_BA = 4 * GDN_WIDTH
OFF_ATT = OFF_BA + 4 * GDN_HEADS
N_IN = OFF_ATT + ATT_WIDTH + 2 * ATT_KV_WIDTH
N_EXPERTS = 256
TOP_K = 8
N_GROUPS = 8
TOPK_GROUPS = 4
EXPERT_DIM = 256
SHARED_DIM = 256
ROUTED_SCALE = 2.5
DISPATCH_BLOCK = 128
LN_EPS = 1e-5
RMS_EPS = 1e-6

kernel_name = 'hybrid_gdn_gqa_moe_diffusion_block'


def _standardize(x, eps):
    xf = x.astype(jnp.float32)
    mu = jnp.mean(xf, -1, keepdims=True)
    var = jnp.mean(jnp.square(xf - mu), -1, keepdims=True)
    return (xf - mu) * lax.rsqrt(var + eps)


def layer_norm(x, w, b):
    return (_standardize(x, LN_EPS) * w.astype(jnp.float32) + b.astype(jnp.float32)).astype(x.dtype)


def rms_norm(x, w):
    xf = x.astype(jnp.float32)
    y = xf * lax.rsqrt(jnp.mean(xf * xf, -1, keepdims=True) + RMS_EPS) * w.astype(jnp.float32)
    return y.astype(x.dtype)


def l2_normalize(x):
    xf = x.astype(jnp.float32)
    return xf * lax.rsqrt(jnp.sum(xf * xf, -1, keepdims=True) + RMS_EPS)


def axial_rope_tables(rows):
    row = jnp.repeat(jnp.arange(rows, dtype=jnp.float32), GRID_W)
    col = jnp.tile(jnp.arange(GRID_W, dtype=jnp.float32), rows)
    inv_freq = ROPE_THETA ** (-jnp.arange(0, ROPE_AXIS_DIM, 2, dtype=jnp.float32) / ROPE_AXIS_DIM)
    ang = jnp.stack([row[:, None] * inv_freq, col[:, None] * inv_freq], axis=1)
    return jnp.cos(ang), jnp.sin(ang)


def apply_axial_rope(x, cos, sin):
    B, L, H, hd = x.shape
    F = ROPE_AXIS_DIM // 2
    xf = x.astype(jnp.float32).reshape(B, L, H, 2, 2, F)
    x1, x2 = xf[..., 0, :], xf[..., 1, :]
    c, s = cos[None, :, None], sin[None, :, None]
    out = jnp.stack([x1 * c - x2 * s, x2 * c + x1 * s], axis=-2)
    return out.reshape(B, L, H, hd).astype(x.dtype)


def centred_depthwise_conv(x, w):
    return lax.conv_general_dilated(x, w[:, None, :].astype(x.dtype), window_strides=(1,),
                                    padding=[(CONV_W // 2, CONV_W // 2)],
                                    dimension_numbers=('NWC', 'WIO', 'NWC'),
                                    feature_group_count=x.shape[-1])


def chunk_gated_delta(q, k, v, g, beta, s0):
    f32 = jnp.float32
    B, H, L, dk = q.shape
    dv = v.shape[-1]
    n = L // CHUNK
    q, k, v = (t.astype(f32).reshape(B, H, n, CHUNK, -1) for t in (q, k, v))
    g = g.astype(f32).reshape(B, H, n, CHUNK)
    beta = beta.astype(f32).reshape(B, H, n, CHUNK)
    G = jnp.cumsum(g, axis=-1)
    idx = jnp.arange(CHUNK)
    incl = idx[:, None] >= idx[None, :]
    strict = idx[:, None] > idx[None, :]
    diff = G[..., :, None] - G[..., None, :]
    decay = jnp.where(incl, jnp.exp(jnp.where(incl, diff, 0.0)), 0.0)
    kk = jnp.einsum('bhnid,bhnjd->bhnij', k, k)
    a_mat = jnp.where(strict, beta[..., :, None] * kk * decay, 0.0)
    m = a_mat + jnp.eye(CHUNK, dtype=f32)
    rhs = jnp.concatenate([v * beta[..., None], k * (beta * jnp.exp(G))[..., None]], axis=-1)
    sol = lax.linalg.triangular_solve(m, rhs, left_side=True, lower=True, unit_diagonal=True)
    u, w = sol[..., :dv], sol[..., dv:]
    qk = jnp.einsum('bhnid,bhnjd->bhnij', q, k) * decay
    q_dec = q * jnp.exp(G)[..., None]
    k_dec = k * jnp.exp(G[..., -1:] - G)[..., None]
    c_dec = jnp.exp(G[..., -1])

    def step(s, xs):
        u_c, w_c, qk_c, qd_c, kd_c, cd_c = xs
        v_new = u_c - jnp.einsum('bhcd,bhde->bhce', w_c, s)
        o_c = jnp.einsum('bhcd,bhde->bhce', qd_c, s) + jnp.einsum('bhcj,bhje->bhce', qk_c, v_new)
        s = s * cd_c[..., None, None] + jnp.einsum('bhcd,bhce->bhde', kd_c, v_new)
        return s, o_c

    xs = tuple(jnp.moveaxis(t, 2, 0) for t in (u, w, qk, q_dec, k_dec, c_dec))
    s_final, o = lax.scan(step, s0.astype(f32), xs)
    return jnp.moveaxis(o, 0, 2).reshape(B, H, L, dv), s_final


def bidirectional_delta(q, k, v, g, beta, s0_fwd, s0_bwd):
    o_f, s_f = chunk_gated_delta(q, k, v, g[0], beta[0], s0_fwd)
    fl = lambda t: jnp.flip(t, axis=2)
    o_b, s_b = chunk_gated_delta(fl(q), fl(k), fl(v), fl(g[1]), fl(beta[1]), s0_bwd)
    return o_f + fl(o_b), s_f, s_b


def gdn_inputs(p, conv_w, a_log, dt_bias):
    f32 = jnp.float32
    B, L, _ = p.shape
    qkv = jax.nn.silu(centred_depthwise_conv(p[..., OFF_QKV:OFF_Z], conv_w))
    heads = lambda t: t.reshape(B, L, GDN_HEADS, GDN_HEAD_DIM).transpose(0, 2, 1, 3)
    q, k, v = (heads(qkv[..., i * GDN_WIDTH:(i + 1) * GDN_WIDTH]) for i in range(3))
    q = l2_normalize(q) * GDN_HEAD_DIM ** -0.5
    k = l2_normalize(k)
    ba = p[..., OFF_BA:OFF_ATT].astype(f32).reshape(B, L, 2, 2, GDN_HEADS)
    beta = jax.nn.sigmoid(ba[:, :, 0]).transpose(2, 0, 3, 1)
    g = (-jnp.exp(a_log.astype(f32)) * jax.nn.softplus(ba[:, :, 1] + dt_bias.astype(f32))).transpose(2, 0, 3, 1)
    return q, k, v, g, beta


def gdn_output(o, z, norm_w):
    B, H, L, dv = o.shape
    y = rms_norm(o.transpose(0, 2, 1, 3), norm_w) * jax.nn.silu(z.astype(jnp.float32).reshape(B, L, H, dv))
    return y.reshape(B, L, GDN_WIDTH).astype(z.dtype)


def attn_inputs(p, q_norm_w, k_norm_w):
    B, L, _ = p.shape
    a = p[..., OFF_ATT:]
    q = rms_norm(a[..., :ATT_WIDTH].reshape(B, L, ATT_Q_HEADS, ATT_HEAD_DIM), q_norm_w)
    k = rms_norm(a[..., ATT_WIDTH:ATT_WIDTH + ATT_KV_WIDTH].reshape(B, L, ATT_KV_HEADS, ATT_HEAD_DIM), k_norm_w)
    v = a[..., ATT_WIDTH + ATT_KV_WIDTH:].reshape(B, L, ATT_KV_HEADS, ATT_HEAD_DIM)
    return q, k, v


def attend(q, k, v):
    s = jnp.einsum('bqhgd,bkhd->bhgqk', q, k, preferred_element_type=jnp.float32) * ATT_HEAD_DIM ** -0.5
    p = jax.nn.softmax(s, axis=-1).astype(v.dtype)
    return jnp.einsum('bhgqk,bkhd->bqhgd', p, v)


def latent_attention(q, k, v, k_ctx, v_ctx):
    B, L, _, _ = q.shape
    kk = jnp.concatenate([k, k_ctx], axis=1)
    vv = jnp.concatenate([v, v_ctx], axis=1)
    nb = L // Q_BLOCK
    qb = q.reshape(B, nb, Q_BLOCK, ATT_KV_HEADS, ATT_GROUP, ATT_HEAD_DIM).swapaxes(0, 1)
    o = lax.map(lambda qblk: attend(qblk, kk, vv), qb)
    return o.swapaxes(0, 1).reshape(B, L, ATT_WIDTH)


def hybrid_mixer(h, hc, w_in, conv_w, a_log, dt_bias, gdn_norm_w, q_norm_w, k_norm_w, w_out, cos, sin, ctx_out):
    B, Lc, _ = hc.shape
    p = h @ w_in
    pc = hc @ w_in
    q, k, v, g, beta = gdn_inputs(p, conv_w, a_log, dt_bias)
    qc, kc, vc, gc, betac = gdn_inputs(pc, conv_w, a_log, dt_bias)
    s0 = jnp.zeros((B, GDN_HEADS, GDN_HEAD_DIM, GDN_HEAD_DIM), jnp.float32)
    oc, s_f, s_b = bidirectional_delta(qc, kc, vc, gc, betac, s0, s0)
    o, _, _ = bidirectional_delta(q, k, v, g, beta, s_f, s_b)
    gdn = gdn_output(o, p[..., OFF_Z:OFF_BA], gdn_norm_w)
    qa, ka, va = attn_inputs(p, q_norm_w, k_norm_w)
    qa, ka = apply_axial_rope(qa, cos, sin), apply_axial_rope(ka, cos, sin)
    qac, kac, vac = attn_inputs(pc, q_norm_w, k_norm_w)
    att = latent_attention(qa, ka, va, kac, vac)
    y = jnp.concatenate([gdn, att], axis=-1) @ w_out
    if not ctx_out:
        return y, None
    gdn_c = gdn_output(oc, pc[..., OFF_Z:OFF_BA], gdn_norm_w)
    att_c = attend(qac.reshape(B, Lc, ATT_KV_HEADS, ATT_GROUP, ATT_HEAD_DIM), kac, vac).reshape(B, Lc, ATT_WIDTH)
    yc = jnp.concatenate([gdn_c, att_c], axis=-1) @ w_out
    return y, yc


def moe_ffn(t, router_w, router_bias, w_gate, w_up, w_down, sh_gate, sh_up, sh_down):
    f32 = jnp.float32
    T, D = t.shape
    s = jax.nn.sigmoid(jnp.matmul(t, router_w, preferred_element_type=f32))
    sel = s + router_bias.astype(f32)
    per_group = N_EXPERTS // N_GROUPS
    grp_score = jnp.sum(lax.top_k(sel.reshape(T, N_GROUPS, per_group), 2)[0], axis=-1)
    _, top_groups = lax.top_k(grp_score, TOPK_GROUPS)
    group_mask = jnp.any(top_groups[:, :, None] == jnp.arange(N_GROUPS)[None, None, :], axis=1)
    expert_mask = jnp.repeat(group_mask, per_group, axis=1)
    _, eidx = lax.top_k(jnp.where(expert_mask, sel, -jnp.inf), TOP_K)
    wts = jnp.take_along_axis(s, eidx, axis=1)
    wts = wts / jnp.sum(wts, -1, keepdims=True) * ROUTED_SCALE
    A = T * TOP_K
    flat_e = eidx.reshape(A)
    flat_tok = jnp.repeat(jnp.arange(T, dtype=jnp.int32), TOP_K)
    flat_w = wts.reshape(A)
    order = jnp.argsort(flat_e)
    se = flat_e[order]
    counts = jnp.bincount(flat_e, length=N_EXPERTS)
    starts = jnp.cumsum(counts) - counts
    padded = (counts + DISPATCH_BLOCK - 1) // DISPATCH_BLOCK * DISPATCH_BLOCK
    pad_ends = jnp.cumsum(padded)
    pad_starts = pad_ends - padded
    dest = pad_starts[se] + (jnp.arange(A) - starts[se])
    n_blocks = -(-A // DISPATCH_BLOCK) + N_EXPERTS
    P = n_blocks * DISPATCH_BLOCK
    buf_tok = jnp.full((P,), T, jnp.int32).at[dest].set(flat_tok[order])
    buf_w = jnp.zeros((P,), f32).at[dest].set(flat_w[order])
    blk_e = jnp.minimum(jnp.searchsorted(pad_ends, jnp.arange(n_blocks) * DISPATCH_BLOCK, side='right'), N_EXPERTS - 1)
    t_pad = jnp.concatenate([t, jnp.zeros((1, D), t.dtype)], axis=0)

    def run_block(args):
        rows, wt, e = args
        xb = t_pad[rows]
        hb = jax.nn.silu(xb @ w_gate[e]) * (xb @ w_up[e])
        return (hb @ w_down[e]) * wt[:, None].astype(xb.dtype)

    out = lax.map(run_block, (buf_tok.reshape(n_blocks, DISPATCH_BLOCK), buf_w.reshape(n_blocks, DISPATCH_BLOCK), blk_e))
    routed = jnp.zeros((T + 1, D), t.dtype).at[buf_tok].add(out.reshape(P, D))[:T]
    shared = (jax.nn.silu(t @ sh_gate) * (t @ sh_up)) @ sh_down
    return routed + shared


def setup_inputs(seed: int = 0) -> dict:
    key = jax.random.key(seed)
    ks = jax.random.split(key, 26)
    f32 = jnp.float32
    nrm = lambda k, shape, scale: jax.random.normal(k, shape, f32) * scale
    out_scale = (8.0 * DEPTH) ** -0.25
    D = D_MODEL
    dt = jnp.exp(jax.random.uniform(ks[9], (DEPTH, 2, GDN_HEADS), f32, math.log(1e-3), math.log(1e-1)))
    return {
        'x': nrm(ks[0], (BATCH, SEQ, D), 1.0),
        'c': nrm(ks[1], (BATCH, D), 1.0),
        'ctx': nrm(ks[2], (BATCH, CTX_LEN, D), 1.0),
        'c_ctx': nrm(ks[3], (D,), 1.0),
        'ada_w': nrm(ks[4], (DEPTH, D, 6 * D), 0.5 * D ** -0.5),
        'ada_b': nrm(ks[5], (DEPTH, 6 * D), 0.02),
        'w_in': nrm(ks[6], (DEPTH, D, N_IN), D ** -0.5),
        'conv_w': nrm(ks[7], (DEPTH, CONV_W, 3 * GDN_WIDTH), CONV_W ** -0.5),
        'gdn_a_log': jnp.log(jax.random.uniform(ks[8], (DEPTH, 2, GDN_HEADS), f32, 1.0, 16.0)),
        'gdn_dt_bias': dt + jnp.log(-jnp.expm1(-dt)),
        'gdn_norm_w': 1.0 + nrm(ks[10], (DEPTH, GDN_HEAD_DIM), 0.1),
        'q_norm_w': 1.0 + nrm(ks[11], (DEPTH, ATT_HEAD_DIM), 0.1),
        'k_norm_w': 1.0 + nrm(ks[12], (DEPTH, ATT_HEAD_DIM), 0.1),
        'w_out': nrm(ks[13], (DEPTH, D_MIX, D), out_scale * D_MIX ** -0.5),
        'ln1_w': 1.0 + nrm(ks[14], (DEPTH, D), 0.1),
        'ln1_b': nrm(ks[15], (DEPTH, D), 0.02),
        'router_w': nrm(ks[16], (DEPTH, D, N_EXPERTS), D ** -0.5),
        'router_bias': nrm(ks[17], (DEPTH, N_EXPERTS), 0.01),
        'exp_w_gate': nrm(ks[18], (DEPTH, N_EXPERTS, D, EXPERT_DIM), D ** -0.5),
        'exp_w_up': nrm(ks[19], (DEPTH, N_EXPERTS, D, EXPERT_DIM), D ** -0.5),
        'exp_w_down': nrm(ks[20], (DEPTH, N_EXPERTS, EXPERT_DIM, D), out_scale * EXPERT_DIM ** -0.5),
        'sh_w_gate': nrm(ks[21], (DEPTH, D, SHARED_DIM), D ** -0.5),
        'sh_w_up': nrm(ks[22], (DEPTH, D, SHARED_DIM), D ** -0.5),
        'sh_w_down': nrm(ks[23], (DEPTH, SHARED_DIM, D), out_scale * SHARED_DIM ** -0.5),
        'ln2_w': 1.0 + nrm(ks[24], (DEPTH, D), 0.1),
        'ln2_b': nrm(ks[25], (DEPTH, D), 0.02),
    }


def reference(x, c, ctx, c_ctx, ada_w, ada_b, w_in, conv_w, gdn_a_log, gdn_dt_bias, gdn_norm_w, q_norm_w, k_norm_w,
              w_out, ln1_w, ln1_b, router_w, router_bias, exp_w_gate, exp_w_up, exp_w_down, sh_w_gate, sh_w_up,
              sh_w_down, ln2_w, ln2_b):
    B, L, D = x.shape
    Lc = ctx.shape[1]
    rows = L // GRID_W
    cos, sin = axial_rope_tables(rows)
    alpha = (2.0 * DEPTH) ** 0.25
    x = _standardize(x, LN_EPS).astype(x.dtype)
    xc = _standardize(ctx, LN_EPS).astype(ctx.dtype)
    for l in range(DEPTH):
        last = l == DEPTH - 1
        mod = jax.nn.silu(c) @ ada_w[l] + ada_b[l]
        mod_c = jax.nn.silu(c_ctx) @ ada_w[l] + ada_b[l]
        sh1, sc1, g1, sh2, sc2, g2 = jnp.split(mod[:, None, :], 6, axis=-1)
        sh1c, sc1c, g1c, sh2c, sc2c, g2c = jnp.split(mod_c[None, None, :], 6, axis=-1)
        y, yc = hybrid_mixer(x * (1.0 + sc1) + sh1, xc * (1.0 + sc1c) + sh1c, w_in[l], conv_w[l], gdn_a_log[l],
                             gdn_dt_bias[l], gdn_norm_w[l], q_norm_w[l], k_norm_w[l], w_out[l], cos, sin,
                             ctx_out=not last)
        x = layer_norm(alpha * x + g1 * y, ln1_w[l], ln1_b[l])
        h = x * (1.0 + sc2) + sh2
        moe_args = (router_w[l], router_bias[l], exp_w_gate[l], exp_w_up[l], exp_w_down[l],
                    sh_w_gate[l], sh_w_up[l], sh_w_down[l])
        if last:
            ff = moe_ffn(h.reshape(B * L, D), *moe_args).reshape(B, L, D)
        else:
            xc = layer_norm(alpha * xc + g1c * yc, ln1_w[l], ln1_b[l])
            hc = xc * (1.0 + sc2c) + sh2c
            ff_all = moe_ffn(jnp.concatenate([h.reshape(B * L, D), hc.reshape(B * Lc, D)], axis=0), *moe_args)
            ff = ff_all[:B * L].reshape(B, L, D)
            xc = layer_norm(alpha * xc + g2c * ff_all[B * L:].reshape(B, Lc, D), ln2_w[l], ln2_b[l])
        x = layer_norm(alpha * x + g2 * ff, ln2_w[l], ln2_b[l])
    return x
```

```python
import os
from contextlib import ExitStack
import numpy as np
import concourse.bass as bass
import concourse.mybir as mybir
from concourse.bass_utils import run_bass_kernel_spmd

F32 = mybir.dt.float32
BF16 = mybir.dt.bfloat16
U32 = mybir.dt.uint32
I32 = mybir.dt.int32
AF = mybir.ActivationFunctionType
ALU = mybir.AluOpType
AX = mybir.AxisListType

D = 1024
L = 4096
LC = 256
NT = L + LC
NTT = NT // 128
DEPTH = 2
N_IN = 3088
OFF_Z = 1536
OFF_BA = 2048
OFF_ATT = 2064
NTOKC = N_IN - OFF_Z
ALPHA = (2.0 * DEPTH) ** 0.25
LN_EPS = 1e-5
RMS_EPS = 1e-6
OPW = 5 * 128 + 8


class Res:
    __slots__ = ("name", "w", "r", "excl")

    def __init__(self, name=""):
        self.name = name
        self.w = None
        self.r = {}
        self.excl = False


class Sched:
    NDS = 8

    def __init__(self, nc, es):
        self.nc = nc
        self.engs = {"pe": nc.tensor, "dve": nc.vector, "act": nc.scalar, "pool": nc.gpsimd, "sp": nc.sync}
        self.csem = {k: es.enter_context(nc.semaphore("c_" + k)) for k in ("pe", "dve", "act", "pool")}
        self.ccnt = {k: 0 for k in self.csem}
        self.dsem = {q: [es.enter_context(nc.semaphore("d_%s%d" % (q, i))) for i in range(self.NDS)]
                     for q in ("sp", "act", "pool")}
        self.dcnt = {q: [0] * self.NDS for q in self.dsem}
        self.drr = {q: 0 for q in self.dsem}
        self.seen = {e: {} for e in self.engs}
        self.ninst = 0

    def _sem(self, key):
        return self.csem[key[1]] if key[0] == "c" else self.dsem[key[1]][key[2]]

    def _wait(self, eng, deps):
        seen = self.seen[eng]
        for key, val in sorted(deps.items(), key=lambda kv: str(kv[0])):
            if eng == "pe" and key == ("c", "pe"):
                continue
            if seen.get(key, 0) >= val:
                continue
            self.engs[eng].wait_ge(self._sem(key), val)
            seen[key] = val

    @staticmethod
    def _deps(reads, writes):
        deps = {}

        def add(ev):
            if ev is not None and deps.get(ev[0], 0) < ev[1]:
                deps[ev[0]] = ev[1]
        for r in reads:
            add(r.w)
            if r.excl:
                for k, v in r.r.items():
                    add((k, v))
        for w in writes:
            add(w.w)
            for k, v in w.r.items():
                add((k, v))
        return deps

    @staticmethod
    def _mark(ev, reads, writes):
        for r in reads:
            if r.r.get(ev[0], 0) < ev[1]:
                r.r[ev[0]] = ev[1]
        for w in writes:
            w.w = ev
            w.r = {}

    def op(self, eng, fn, reads=(), writes=()):
        self._wait(eng, self._deps(reads, writes))
        inst = fn(self.engs[eng])
        self.ccnt[eng] += 1
        inst.then_inc(self.csem[eng], 1)
        ev = (("c", eng), self.ccnt[eng])
        self._mark(ev, reads, writes)
        self.ninst += 1
        return ev

    def dma(self, q, out, in_, reads=(), writes=(), **kw):
        self._wait(q, self._deps(reads, writes))
        i = self.drr[q]
        self.drr[q] = (i + 1) % self.NDS
        inst = self.engs[q].dma_start(out=out, in_=in_, **kw)
        self.dcnt[q][i] += 16
        inst.then_inc(self.dsem[q][i], 16)
        ev = (("d", q, i), self.dcnt[q][i])
        self._mark(ev, reads, writes)
        self.ninst += 1
        return ev

    def idma(self, out, in_, out_off=None, in_off=None, reads=(), writes=(), **kw):
        q = "pool"
        self._wait(q, self._deps(reads, writes))
        i = self.drr[q]
        self.drr[q] = (i + 1) % self.NDS
        inst = self.engs[q].indirect_dma_start(
            out=out, out_offset=(bass.IndirectOffsetOnAxis(ap=out_off, axis=0) if out_off is not None else None),
            in_=in_, in_offset=(bass.IndirectOffsetOnAxis(ap=in_off, axis=0) if in_off is not None else None), **kw)
        self.dcnt[q][i] += 16
        inst.then_inc(self.dsem[q][i], 16)
        ev = (("d", q, i), self.dcnt[q][i])
        self._mark(ev, reads, writes)
        self.ninst += 1
        return ev

    def barrier(self):
        deps = {}
        for k, v in self.ccnt.items():
            if v:
                deps[("c", k)] = v
        for q in self.dcnt:
            for i, v in enumerate(self.dcnt[q]):
                if v:
                    deps[("d", q, i)] = v
        for eng in self.engs:
            d2 = dict(deps)
            self._wait_all(eng, d2)

    def _wait_all(self, eng, deps):
        seen = self.seen[eng]
        for key, val in sorted(deps.items(), key=lambda kv: str(kv[0])):
            if key == ("c", eng):
                continue
            if seen.get(key, 0) >= val:
                continue
            self.engs[eng].wait_ge(self._sem(key), val)
            seen[key] = val

    def finish(self, eng, resources):
        deps = {}
        for r in resources:
            if r.w is not None and deps.get(r.w[0], 0) < r.w[1]:
                deps[r.w[0]] = r.w[1]
        self._wait(eng, deps)


class T:
    def __init__(self, t, name=""):
        self.t = t
        self.res = Res(name)

    def __getitem__(self, k):
        return self.t[k]


class KB:
    def __init__(self, nc, es, dbg=None):
        self.nc = nc
        self.es = es
        self.s = Sched(nc, es)
        self.dbg = dbg
        self.n = 0

    def sb(self, es, shape, dt, name=None):
        self.n += 1
        name = "%s_%d" % (name or "sb", self.n)
        return T(es.enter_context(self.nc.sbuf_tensor(name, list(shape), dt)), name)

    def ps(self, es, shape, dt=F32, name=None):
        self.n += 1
        name = "%s_%d" % (name or "ps", self.n)
        return T(es.enter_context(self.nc.psum_tensor(name, list(shape), dt)), name)

    def dram(self, name, shape, dt, kind="Internal"):
        return T(self.nc.dram_tensor(name, list(shape), dt, kind=kind).ap(), name)


def rd(*ts):
    return [t.res for t in ts]


class NS:
    pass


class Cut(Exception):
    pass


def cutpt(C, k):
    return C.cut == k


def sub(bank, c0, c1, name=""):
    t = T(bank.t[:, c0:c1], name)
    t.res = bank.res
    bank.res.excl = True
    return t


def bcast_row(C, st, dst_ap, dst_t, src_ap, src_t, n, pbank):
    kb, s = C.kb, C.kb.s
    row = C.bcrow
    s.dma("sp", row[0:1, 0:n], src_ap, rd(src_t), rd(row))
    for c0 in range(0, n, 512):
        w = min(512, n - c0)
        s.op("pe", lambda e: e.matmul(out=pbank[:, 0:w], lhsT=C.ones[:], rhs=row[:, c0:c0 + w], start=True, stop=True),
             rd(C.ones, row), rd(pbank))
        s.op("dve", lambda e: e.tensor_copy(out=dst_ap[:, c0:c0 + w], in_=pbank[:, 0:w]), rd(pbank), rd(dst_t))


def stage_gdn(C, l):
    kb, s = C.kb, C.kb.s
    ident, ones, masks, psb = C.ident, C.ones, C.masks, C.psb
    CUMS = (0, 1)
    STRICT = (2, 3)
    INCLT = (0, 1)
    with ExitStack() as st:
        convw = kb.sb(st, [128, 12, 5], F32, "convw")
        for j in range(5):
            s.dma("sp", convw[:, :, j], C.conv_w[l, j].rearrange("(c p) -> p c", p=128), rd(C.conv_w), rd(convw),
                  allow_slow_non_contiguous=True)
        if C.cut == 1:
            s.dma("sp", C.o_dbg[:, 0:128], ones[:], rd(ones), rd(C.o_dbg))
            return
        dtb8 = kb.sb(st, [128, 8], F32, "dtb8")
        bcast_row(C, st, dtb8[:], dtb8, C.dt_bias[l:l + 1, :], C.dt_bias, 8, psb[0])
        negA8 = kb.sb(st, [128, 8], F32, "negA8")
        bcast_row(C, st, negA8[:], negA8, C.a_log[l:l + 1, :], C.a_log, 8, psb[0])
        s.op("act", lambda e: e.activation(out=negA8[:], in_=negA8[:], func=AF.Exp), rd(negA8), rd(negA8))
        s.op("dve", lambda e: e.tensor_scalar(out=negA8[:], in0=negA8[:], scalar1=-1.0, scalar2=None, op0=ALU.mult),
             rd(negA8), rd(negA8))
        if C.cut == 2:
            s.dma("sp", C.o_dbg[:, 0:128], ones[:], rd(ones), rd(C.o_dbg))
            return
        BETA = kb.sb(st, [128, NTT, 8], F32, "BETA")
        NBETA = kb.sb(st, [128, NTT, 8], F32, "NBETA")
        GC = kb.sb(st, [128, NTT, 8], F32, "GC")
        EG = kb.sb(st, [128, NTT, 8], F32, "EG")
        EGR = kb.sb(st, [128, NTT, 8], F32, "EGR")
        CD = kb.sb(st, [128, NTT, 8], F32, "CD")
        BEG = kb.sb(st, [128, NTT, 8], F32, "BEG")
        ba = kb.sb(st, [128, NTT, 16], F32, "ba")
        s.dma("sp", ba[:], C.PTOK.t[:, 512:528].rearrange("(n p) c -> p n c", p=128), rd(C.PTOK), rd(ba))
        s.op("act", lambda e: e.activation(out=BETA[:], in_=ba[:, :, 0:8], func=AF.Sigmoid), rd(ba), rd(BETA))
        s.op("dve", lambda e: e.tensor_scalar(out=NBETA[:], in0=BETA[:], scalar1=-1.0, scalar2=None, op0=ALU.mult),
             rd(BETA), rd(NBETA))
        if C.cut == 3:
            s.dma("sp", C.o_dbg[:, 0:128], ones[:], rd(ones), rd(C.o_dbg))
            return
        xg = kb.sb(st, [128, NTT, 8], F32, "xg")
        ag = kb.sb(st, [128, NTT, 8], F32, "ag")
        gg = kb.sb(st, [128, NTT, 8], F32, "gg")
        for n in range(NTT):
            s.op("dve", lambda e: e.tensor_tensor(out=xg[:, n, :], in0=ba[:, n, 8:16], in1=dtb8[:], op=ALU.add),
                 rd(ba, dtb8), rd(xg))
        s.op("act", lambda e: e.activation(out=ag[:], in_=xg[:], func=AF.Abs), rd(xg), rd(ag))
        s.op("act", lambda e: e.activation(out=ag[:], in_=ag[:], func=AF.Exp, scale=-1.0), rd(ag), rd(ag))
        s.op("dve", lambda e: e.tensor_scalar_add(out=ag[:], in0=ag[:], scalar1=1.0), rd(ag), rd(ag))
        s.op("act", lambda e: e.activation(out=ag[:], in_=ag[:], func=AF.Ln), rd(ag), rd(ag))
        s.op("dve", lambda e: e.tensor_scalar(out=xg[:], in0=xg[:], scalar1=0.0, scalar2=None, op0=ALU.max),
             rd(xg), rd(xg))
        s.op("dve", lambda e: e.tensor_tensor(out=gg[:], in0=xg[:], in1=ag[:], op=ALU.add), rd(xg, ag), rd(gg))
        for n in range(NTT):
            s.op("dve", lambda e: e.tensor_tensor(out=gg[:, n, :], in0=gg[:, n, :], in1=negA8[:], op=ALU.mult),
                 rd(gg, negA8), rd(gg))
        if C.cut == 4:
            s.dma("sp", C.o_dbg[:, 0:128], ones[:], rd(ones), rd(C.o_dbg))
            return
        pG = sub(psb[0], 0, NTT * 8, "pG")
        pGt = sub(psb[1], 0, NTT * 8, "pGt")
        ggv = gg[:].rearrange("p n c -> p (n c)")
        for n in range(NTT):
            for d in range(2):
                s.op("pe", lambda e: e.matmul(out=pG[:, n * 8 + d * 4:n * 8 + d * 4 + 4], lhsT=masks[:, CUMS[d], :],
                                              rhs=gg[:, n, d * 4:d * 4 + 4], start=True, stop=True),
                     rd(masks, gg), rd(pG))
        for c0 in range(0, NTT * 8, 136):
            s.op("pe", lambda e: e.matmul(out=pGt[:, c0:c0 + 136], lhsT=ones[:], rhs=ggv[:, c0:c0 + 136],
                                          start=True, stop=True), rd(ones, gg), rd(pGt))
        if C.cut == 5:
            s.dma("sp", C.o_dbg[:, 0:128], ones[:], rd(ones), rd(C.o_dbg))
            return
        GCv = GC[:].rearrange("p n c -> p (n c)")
        s.op("dve", lambda e: e.tensor_copy(out=GCv, in_=pG[:, :]), rd(pG), rd(GC))
        if C.cut == 6:
            s.dma("sp", C.o_dbg[:, 0:272], GC[:].rearrange("p n c -> p (n c)"), rd(GC), rd(C.o_dbg))
            s.dma("sp", C.o_dbg[:, 272:544], gg[:].rearrange("p n c -> p (n c)"), rd(gg), rd(C.o_dbg))
            s.dma("sp", C.o_dbg[:, 544:816], BETA[:].rearrange("p n c -> p (n c)"), rd(BETA), rd(C.o_dbg))
            return
        if os.environ.get("GSKIP") != "EG":
            if os.environ.get("GEG") == "psum":
                s.op("act", lambda e: e.activation(out=EG[:].rearrange("p n c -> p (n c)"), in_=pG[:, :], func=AF.Exp),
                     rd(pG), rd(EG))
            else:
                s.op("act", lambda e: e.activation(out=EG[:], in_=GC[:], func=AF.Exp), rd(GC), rd(EG))
        if os.environ.get("GSKIP") != "CD":
            s.op("act", lambda e: e.activation(out=CD[:].rearrange("p n c -> p (n c)"), in_=pGt[:, :], func=AF.Exp),
                 rd(pGt), rd(CD))
        if C.cut == 7:
            s.dma("sp", C.o_dbg[:, 0:128], ones[:], rd(ones), rd(C.o_dbg))
            return
        s.op("dve", lambda e: e.tensor_tensor(out=EGR[:].rearrange("p n c -> p (n c)"), in0=pGt[:, :], in1=GCv,
                                              op=ALU.subtract), rd(pGt, GC), rd(EGR))
        s.op("act", lambda e: e.activation(out=EGR[:], in_=EGR[:], func=AF.Exp), rd(EGR), rd(EGR))
        if C.cut == 8:
            s.dma("sp", C.o_dbg[:, 0:128], ones[:], rd(ones), rd(C.o_dbg))
            return
        s.op("dve", lambda e: e.tensor_tensor(out=BEG[:], in0=BETA[:], in1=EG[:], op=ALU.mult), rd(BETA, EG), rd(BEG))
        if C.cut == 9:
            s.dma("sp", C.o_dbg[:, 0:128], ones[:], rd(ones), rd(C.o_dbg))
            return

        if C.dbg_stage == "gdnA":
            s.dma("sp", C.o_dbg[:, 0:272], GC[:].rearrange("p n c -> p (n c)"), rd(GC), rd(C.o_dbg))
            s.dma("sp", C.o_dbg[:, 272:544], BEG[:].rearrange("p n c -> p (n c)"), rd(BEG), rd(C.o_dbg))
            s.dma("sp", C.o_dbg[:, 544:816], EGR[:].rearrange("p n c -> p (n c)"), rd(EGR), rd(C.o_dbg))
            return
        W = NT + 4
        xpad = [kb.sb(st, [128, NT + 8], F32, "xpad%d" % i) for i in range(2)]
        for xp in xpad:
            s.op("pool", lambda e: e.memset(xp[:], 0.0), (), rd(xp))
        cs = [kb.sb(st, [128, W], F32, "cs%d" % i) for i in range(3)]
        acc = kb.sb(st, [128, W], F32, "cacc")
        NB = 2
        vtok = [kb.sb(st, [128, 128], F32, "vtok%d" % i) for i in range(NB)]
        ktok = [kb.sb(st, [128, 128], F32, "ktok%d" % i) for i in range(NB)]
        qtok = [kb.sb(st, [128, 128], F32, "qtok%d" % i) for i in range(NB)]
        kT = [kb.sb(st, [128, 128], F32, "kT%d" % i) for i in range(NB)]
        qT = [kb.sb(st, [128, 128], F32, "qT%d" % i) for i in range(NB)]
        ssq = [kb.sb(st, [128, 2], F32, "ssq%d" % i) for i in range(NB)]
        junk = kb.sb(st, [128, 128], F32, "junk")
        NB2 = 2
        diagG = [kb.sb(st, [128, 128], F32, "diagG%d" % i) for i in range(NB2)]
        Mm = [kb.sb(st, [128, 128], F32, "Mm%d" % i) for i in range(NB2)]
        t1 = [kb.sb(st, [128, 128], F32, "t1%d" % i) for i in range(NB2)]
        t2 = [kb.sb(st, [128, 128], F32, "t2%d" % i) for i in range(NB2)]
        Xa = [kb.sb(st, [128, 128], F32, "Xa%d" % i) for i in range(NB2)]
        Xb = [kb.sb(st, [128, 128], F32, "Xb%d" % i) for i in range(NB2)]
        XTa = [kb.sb(st, [128, 128], F32, "XTa%d" % i) for i in range(NB2)]
        XTb = [kb.sb(st, [128, 128], F32, "XTb%d" % i) for i in range(NB2)]
        TT = [kb.sb(st, [128, 128], F32, "TT%d" % i) for i in range(NB2)]
        vb = [kb.sb(st, [128, 128], F32, "vb%d" % i) for i in range(NB2)]
        kbg = [kb.sb(st, [128, 128], F32, "kbg%d" % i) for i in range(NB2)]
        qd = [kb.sb(st, [128, 128], F32, "qd%d" % i) for i in range(NB2)]
        OPS = [kb.sb(st, [128, OPW], F32, "OPS%d" % i) for i in range(4)]
        pQKV = sub(psb[0], 0, 384, "pQKV")
        pKQT = sub(psb[1], 0, 256, "pKQT")
        pR = [sub(psb[2], d * 128, d * 128 + 128, "pR") for d in range(2)]
        pKK = [sub(psb[3], d * 128, d * 128 + 128, "pKK") for d in range(2)]
        pKQ = [sub(psb[4], d * 128, d * 128 + 128, "pKQ") for d in range(2)]
        pXT = [sub(psb[5], d * 256, d * 256 + 128, "pXT") for d in range(2)]
        pXT2 = [sub(psb[5], d * 256 + 128, d * 256 + 256, "pXT2") for d in range(2)]
        pX = [sub(psb[6], d * 128, d * 128 + 128, "pX") for d in range(2)]
        pT = [sub(psb[7], d * 256, d * 256 + 128, "pT") for d in range(2)]
        pO = [sub(psb[7], d * 256 + 128, d * 256 + 256, "pO") for d in range(2)]
        ci = 0
        oi = 0
        for h in range(4 if C.dbg_stage != "gdnB" else 1):
            for which in range(3):
                cc = which * 4 + h
                xp = xpad[(h * 3 + which) % 2]
                s.dma("sp", xp[:, 2:2 + L], C.QKVT[cc * 128:(cc + 1) * 128, 0:L], rd(C.QKVT), rd(xp))
                s.dma("pool", xp[:, L + 6:L + 6 + LC], C.QKVT[cc * 128:(cc + 1) * 128, L:NT], rd(C.QKVT), rd(xp))
                s.op("dve", lambda e: e.tensor_scalar(out=acc[:], in0=xp[:, 0:W], scalar1=convw[:, cc, 0:1],
                                                      scalar2=None, op0=ALU.mult), rd(xp, convw), rd(acc))
                for j in range(1, 5):
                    s.op("dve", lambda e: e.scalar_tensor_tensor(out=acc[:], in0=xp[:, j:j + W],
                                                                 scalar=convw[:, cc, j:j + 1], in1=acc[:],
                                                                 op0=ALU.mult, op1=ALU.add), rd(xp, convw, acc), rd(acc))
                s.op("act", lambda e: e.activation(out=cs[which][:], in_=acc[:], func=AF.Silu), rd(acc), rd(cs[which]))
            if cutpt(C, 20):
                return
            for n in range(NTT if C.dbg_stage != "gdnB" else 1):
                col0 = n * 128 if n < L // 128 else L + 4 + (n - L // 128) * 128
                b = ci % NB
                ci += 1
                for which in range(3):
                    s.op("pe", lambda e: e.transpose(out=pQKV[:, which * 128:(which + 1) * 128],
                                                     in_=cs[which][:, col0:col0 + 128], identity=ident[:]),
                         rd(cs[which], ident), rd(pQKV))
                s.op("act", lambda e: e.copy(out=vtok[b][:], in_=pQKV[:, 256:384]), rd(pQKV), rd(vtok[b]))
                for w_ in range(2):
                    s.op("act", lambda e: e.activation(out=junk[:], in_=pQKV[:, w_ * 128:(w_ + 1) * 128], func=AF.Square,
                                                       accum_out=ssq[b][:, w_:w_ + 1]), rd(pQKV), rd(junk, ssq[b]))
                if cutpt(C, 21):
                    return
                s.op("dve", lambda e: e.tensor_scalar_add(out=ssq[b][:], in0=ssq[b][:], scalar1=RMS_EPS), rd(ssq[b]), rd(ssq[b]))
                s.op("act", lambda e: e.sqrt(out=ssq[b][:], in_=ssq[b][:]), rd(ssq[b]), rd(ssq[b]))
                s.op("dve", lambda e: e.reciprocal(out=ssq[b][:], in_=ssq[b][:]), rd(ssq[b]), rd(ssq[b]))
                s.op("dve", lambda e: e.tensor_scalar(out=qtok[b][:], in0=pQKV[:, 0:128], scalar1=ssq[b][:, 0:1],
                                                      scalar2=128.0 ** -0.5, op0=ALU.mult, op1=ALU.mult),
                     rd(pQKV, ssq[b]), rd(qtok[b]))
                s.op("dve", lambda e: e.tensor_scalar(out=ktok[b][:], in0=pQKV[:, 128:256], scalar1=ssq[b][:, 1:2],
                                                      scalar2=None, op0=ALU.mult), rd(pQKV, ssq[b]), rd(ktok[b]))
                s.op("pe", lambda e: e.transpose(out=pKQT[:, 0:128], in_=ktok[b][:], identity=ident[:]),
                     rd(ktok[b], ident), rd(pKQT))
                s.op("pe", lambda e: e.transpose(out=pKQT[:, 128:256], in_=qtok[b][:], identity=ident[:]),
                     rd(qtok[b], ident), rd(pKQT))
                s.op("act", lambda e: e.copy(out=kT[b][:], in_=pKQT[:, 0:128]), rd(pKQT), rd(kT[b]))
                s.op("dve", lambda e: e.tensor_copy(out=qT[b][:], in_=pKQT[:, 128:256]), rd(pKQT), rd(qT[b]))
                if cutpt(C, 22):
                    return
                for d in range(2):
                    col = d * 4 + h
                    ops = OPS[oi % 4]
                    oi += 1
                    gcol = GC[:, n, col:col + 1]
                    s.op("pool", lambda e: e.tensor_scalar(out=diagG[d][:], in0=ident[:], scalar1=gcol, scalar2=None,
                                                           op0=ALU.mult), rd(ident, GC), rd(diagG[d]))
                    s.op("pe", lambda e: e.matmul(out=pR[d][:, :], lhsT=ones[:], rhs=diagG[d][:], start=True, stop=True),
                         rd(ones, diagG[d]), rd(pR[d]))
                    s.op("dve", lambda e: e.tensor_scalar(out=Mm[d][:], in0=pR[d][:, :], scalar1=gcol, scalar2=None,
                                                          op0=ALU.subtract), rd(pR[d], GC), rd(Mm[d]))
                    s.op("pool", lambda e: e.tensor_scalar(out=t1[d][:], in0=Mm[d][:], scalar1=0.0, scalar2=None,
                                                           op0=ALU.max), rd(Mm[d]), rd(t1[d]))
                    s.op("act", lambda e: e.activation(out=t1[d][:], in_=t1[d][:], func=AF.Exp, scale=-1.0), rd(t1[d]), rd(t1[d]))
                    s.op("pool", lambda e: e.tensor_tensor(out=t1[d][:], in0=t1[d][:], in1=masks[:, STRICT[d], :], op=ALU.mult),
                         rd(t1[d], masks), rd(t1[d]))
                    s.op("pool", lambda e: e.tensor_scalar(out=t2[d][:], in0=Mm[d][:], scalar1=0.0, scalar2=None,
                                                           op0=ALU.min), rd(Mm[d]), rd(t2[d]))
                    s.op("act", lambda e: e.activation(out=t2[d][:], in_=t2[d][:], func=AF.Exp), rd(t2[d]), rd(t2[d]))
                    s.op("pool", lambda e: e.tensor_tensor(out=t2[d][:], in0=t2[d][:], in1=masks[:, INCLT[d], :], op=ALU.mult),
                         rd(t2[d], masks), rd(t2[d]))
                    if cutpt(C, 23):
                        return
                    s.op("pe", lambda e: e.matmul(out=pKK[d][:, :], lhsT=kT[b][:], rhs=kT[b][:], start=True, stop=True),
                         rd(kT[b]), rd(pKK[d]))
                    s.op("pe", lambda e: e.matmul(out=pKQ[d][:, :], lhsT=kT[b][:], rhs=qT[b][:], start=True, stop=True),
                         rd(kT[b], qT[b]), rd(pKQ[d]))
                    s.op("dve", lambda e: e.scalar_tensor_tensor(out=Xa[d][:], in0=pKK[d][:, :], scalar=NBETA[:, n, col:col + 1],
                                                                 in1=t1[d][:], op0=ALU.mult, op1=ALU.mult),
                         rd(pKK[d], NBETA, t1[d]), rd(Xa[d]))
                    s.op("dve", lambda e: e.tensor_tensor(out=ops[:, 384:512], in0=pKQ[d][:, :], in1=t2[d][:], op=ALU.mult),
                         rd(pKQ[d], t2[d]), rd(ops))
                    if cutpt(C, 24):
                        return
                    s.op("pe", lambda e: e.transpose(out=pXT[d][:, :], in_=Xa[d][:], identity=ident[:]),
                         rd(Xa[d], ident), rd(pXT[d]))
                    s.op("act", lambda e: e.copy(out=XTa[d][:], in_=pXT[d][:, :]), rd(pXT[d]), rd(XTa[d]))
                    s.op("dve", lambda e: e.tensor_tensor(out=TT[d][:], in0=pXT[d][:, :], in1=ident[:], op=ALU.add),
                         rd(pXT[d], ident), rd(TT[d]))
                    if cutpt(C, 25):
                        return
                    Xc, XTc, Xn, XTn = Xa[d], XTa[d], Xb[d], XTb[d]
                    for m in range(1, 7):
                        s.op("pe", lambda e: e.matmul(out=pX[d][:, :], lhsT=XTc[:], rhs=Xc[:], start=True, stop=True),
                             rd(XTc, Xc), rd(pX[d]))
                        if m < 6:
                            s.op("pe", lambda e: e.matmul(out=pXT2[d][:, :], lhsT=Xc[:], rhs=XTc[:], start=True, stop=True),
                                 rd(XTc, Xc), rd(pXT2[d]))
                        s.op("act", lambda e: e.copy(out=Xn[:], in_=pX[d][:, :]), rd(pX[d]), rd(Xn))
                        if m < 6:
                            s.op("dve", lambda e: e.tensor_copy(out=XTn[:], in_=pXT2[d][:, :]), rd(pXT2[d]), rd(XTn))
                        s.op("pe", lambda e: e.matmul(out=pT[d][:, :], lhsT=Xn[:], rhs=TT[d][:], start=True, stop=True),
                             rd(Xn, TT[d]), rd(pT[d]))
                        s.op("dve", lambda e: e.tensor_tensor(out=TT[d][:], in0=TT[d][:], in1=pT[d][:, :], op=ALU.add),
                             rd(TT[d], pT[d]), rd(TT[d]))
                        Xc, XTc, Xn, XTn = Xn, XTn, Xc, XTc
                    if cutpt(C, 26):
                        return
                    s.op("pool", lambda e: e.tensor_scalar(out=vb[d][:], in0=vtok[b][:], scalar1=BETA[:, n, col:col + 1],
                                                           scalar2=None, op0=ALU.mult), rd(vtok[b], BETA), rd(vb[d]))
                    s.op("pool", lambda e: e.tensor_scalar(out=kbg[d][:], in0=ktok[b][:], scalar1=BEG[:, n, col:col + 1],
                                                           scalar2=None, op0=ALU.mult), rd(ktok[b], BEG), rd(kbg[d]))
                    s.op("pool", lambda e: e.tensor_scalar(out=qd[d][:], in0=qtok[b][:], scalar1=EG[:, n, col:col + 1],
                                                           scalar2=None, op0=ALU.mult), rd(qtok[b], EG), rd(qd[d]))
                    s.op("pool", lambda e: e.tensor_scalar(out=ops[:, 512:640], in0=ktok[b][:], scalar1=EGR[:, n, col:col + 1],
                                                           scalar2=None, op0=ALU.mult), rd(ktok[b], EGR), rd(ops))
                    s.op("pool", lambda e: e.tensor_copy(out=ops[:, 640:648], in_=CD[:, n, :]), rd(CD), rd(ops))
                    s.op("pe", lambda e: e.matmul(out=pO[d][:, :], lhsT=kbg[d][:], rhs=TT[d][:], start=True, stop=True),
                         rd(kbg[d], TT[d]), rd(pO[d]))
                    s.op("act", lambda e: e.copy(out=ops[:, 0:128], in_=pO[d][:, :]), rd(pO[d]), rd(ops))
                    s.op("pe", lambda e: e.matmul(out=pX[d][:, :], lhsT=TT[d][:], rhs=vb[d][:], start=True, stop=True),
                         rd(vb[d], TT[d]), rd(pX[d]))
                    s.op("dve", lambda e: e.tensor_copy(out=ops[:, 128:256], in_=pX[d][:, :]), rd(pX[d]), rd(ops))
                    s.op("pe", lambda e: e.transpose(out=pXT2[d][:, :], in_=qd[d][:], identity=ident[:]),
                         rd(qd[d], ident), rd(pXT2[d]))
                    s.op("act", lambda e: e.copy(out=ops[:, 256:384], in_=pXT2[d][:, :]), rd(pXT2[d]), rd(ops))
                    s.dma("sp", C.OPSD[h, d, n], ops[:], rd(ops), rd(C.OPSD))
                    if C.dbg_stage == "gdnB":
                        s.dma("sp", C.o_dbgB[d], ops[:], rd(ops), rd(C.o_dbgB))
                        s.dma("sp", C.o_dbgB[2 + d, :, 0:128], TT[d][:], rd(TT[d]), rd(C.o_dbgB))
                        s.dma("sp", C.o_dbgB[2 + d, :, 128:256], Xa[d][:], rd(Xa[d]), rd(C.o_dbgB))
                        s.dma("sp", C.o_dbgB[2 + d, :, 256:384], t1[d][:], rd(t1[d]), rd(C.o_dbgB))
                        s.dma("sp", C.o_dbgB[2 + d, :, 384:512], ktok[b][:], rd(ktok[b]), rd(C.o_dbgB))
    if C.dbg_stage in ("gdnprep", "gdnB"):
        return
    s.barrier()
    with ExitStack() as st:
        S = [[kb.sb(st, [128, 128], F32, "S%d%d" % (h, d)) for d in range(2)] for h in range(4)]
        for h in range(4):
            for d in range(2):
                s.op("pool", lambda e: e.memset(S[h][d][:], 0.0), (), rd(S[h][d]))
        NOB = 16
        OB = [kb.sb(st, [128, OPW], F32, "OB%d" % i) for i in range(NOB)]
        vnew = [kb.sb(st, [128, 128], F32, "vnew%d" % i) for i in range(8)]
        oev = [kb.sb(st, [128, 128], F32, "oev%d" % i) for i in range(8)]
        order = [[32, 33] + list(range(32)), [33, 32] + list(range(31, -1, -1))]
        p1 = [sub(psb[h], d * 128, d * 128 + 128, "p1") for h in range(4) for d in range(2)]
        p2 = [sub(psb[h], 256 + d * 128, 384 + d * 128, "p2") for h in range(4) for d in range(2)]
        p3 = [sub(psb[4 + h], d * 128, d * 128 + 128, "p3") for h in range(4) for d in range(2)]
        k = 0
        for step in range(NTT):
            for d in range(2):
                n = order[d][step]
                for h in range(4):
                    c = h * 2 + d
                    ob = OB[k % NOB]
                    k += 1
                    s.dma("sp" if k % 2 == 0 else "pool", ob[:], C.OPSD[h, d, n], rd(C.OPSD), rd(ob))
                    Sd = S[h][d]
                    s.op("pe", lambda e: e.matmul(out=p1[c][:, :], lhsT=ob[:, 0:128], rhs=Sd[:], start=True, stop=True),
                         rd(ob, Sd), rd(p1[c]))
                    s.op("dve", lambda e: e.tensor_tensor(out=vnew[c][:], in0=ob[:, 128:256], in1=p1[c][:, :], op=ALU.subtract),
                         rd(ob, p1[c]), rd(vnew[c]))
                    s.op("pe", lambda e: e.matmul(out=p2[c][:, :], lhsT=ob[:, 256:384], rhs=Sd[:], start=True, stop=False),
                         rd(ob, Sd), rd(p2[c]))
                    s.op("pe", lambda e: e.matmul(out=p2[c][:, :], lhsT=ob[:, 384:512], rhs=vnew[c][:], start=False, stop=True),
                         rd(ob, vnew[c]), rd(p2[c]))
                    s.op("pe", lambda e: e.matmul(out=p3[c][:, :], lhsT=ob[:, 512:640], rhs=vnew[c][:], start=True, stop=True),
                         rd(ob, vnew[c]), rd(p3[c]))
                    s.op("act", lambda e: e.copy(out=oev[c][:], in_=p2[c][:, :]), rd(p2[c]), rd(oev[c]))
                    s.dma("sp", C.OFB[d][n * 128:(n + 1) * 128, h * 128:(h + 1) * 128], oev[c][:], rd(oev[c]), rd(C.OFB[d]))
                    s.op("dve", lambda e: e.scalar_tensor_tensor(out=Sd[:], in0=Sd[:], scalar=ob[:, 640 + c_col(d, h):641 + c_col(d, h)],
                                                                 in1=p3[c][:, :], op0=ALU.mult, op1=ALU.add),
                         rd(Sd, ob, p3[c]), rd(Sd))
    if C.dbg_stage == "gdnscan":
        return
    s.barrier()
    with ExitStack() as st:
        gnw = kb.sb(st, [128, 128], F32, "gnw")
        bcast_row(C, st, gnw[:], gnw, C.gdn_norm_w[l:l + 1, :], C.gdn_norm_w, 128, psb[2])
        NBF = 2
        of = [kb.sb(st, [128, 512], F32, "of%d" % i) for i in range(NBF)]
        obk = [kb.sb(st, [128, 512], F32, "obk%d" % i) for i in range(NBF)]
        zz = [kb.sb(st, [128, 512], F32, "zz%d" % i) for i in range(NBF)]
        yy = [kb.sb(st, [128, 512], F32, "yy%d" % i) for i in range(NBF)]
        sq = [kb.sb(st, [128, 4], F32, "sq%d" % i) for i in range(NBF)]
        junk = kb.sb(st, [128, 128], F32, "junk2")
        gT = [kb.sb(st, [128, 512], BF16, "gT%d" % i) for i in range(NBF)]
        for n in range(NTT):
            b = n % NBF
            pb = psb[n % 2]
            s.dma("sp", of[b][:], C.OFB[0][n * 128:(n + 1) * 128, :], rd(C.OFB[0]), rd(of[b]))
            s.dma("pool", obk[b][:], C.OFB[1][n * 128:(n + 1) * 128, :], rd(C.OFB[1]), rd(obk[b]))
            s.dma("sp", zz[b][:], C.PTOK[n * 128:(n + 1) * 128, 0:512], rd(C.PTOK), rd(zz[b]))
            s.op("dve", lambda e: e.tensor_tensor(out=of[b][:], in0=of[b][:], in1=obk[b][:], op=ALU.add), rd(of[b], obk[b]), rd(of[b]))
            s.op("act", lambda e: e.activation(out=zz[b][:], in_=zz[b][:], func=AF.Silu), rd(zz[b]), rd(zz[b]))
            for h in range(4):
                s.op("act", lambda e: e.activation(out=junk[:], in_=of[b][:, h * 128:(h + 1) * 128], func=AF.Square,
                                                   accum_out=sq[b][:, h:h + 1]), rd(of[b]), rd(junk, sq[b]))
            s.op("dve", lambda e: e.tensor_scalar(out=sq[b][:], in0=sq[b][:], scalar1=1.0 / 128.0, scalar2=RMS_EPS,
                                                  op0=ALU.mult, op1=ALU.add), rd(sq[b]), rd(sq[b]))
            s.op("act", lambda e: e.sqrt(out=sq[b][:], in_=sq[b][:]), rd(sq[b]), rd(sq[b]))
            s.op("dve", lambda e: e.reciprocal(out=sq[b][:], in_=sq[b][:]), rd(sq[b]), rd(sq[b]))
            for h in range(4):
                s.op("pool", lambda e: e.tensor_tensor(out=zz[b][:, h * 128:(h + 1) * 128], in0=zz[b][:, h * 128:(h + 1) * 128],
                                                       in1=gnw[:], op=ALU.mult), rd(zz[b], gnw), rd(zz[b]))
            for h in range(4):
                s.op("dve", lambda e: e.scalar_tensor_tensor(out=yy[b][:, h * 128:(h + 1) * 128], in0=of[b][:, h * 128:(h + 1) * 128],
                                                             scalar=sq[b][:, h:h + 1], in1=zz[b][:, h * 128:(h + 1) * 128],
                                                             op0=ALU.mult, op1=ALU.mult), rd(of[b], sq[b], zz[b]), rd(yy[b]))
            for h in range(4):
                s.op("pe", lambda e: e.transpose(out=pb[:, h * 128:(h + 1) * 128], in_=yy[b][:, h * 128:(h + 1) * 128],
                                                 identity=ident[:]), rd(yy[b], ident), rd(pb))
            s.op("act", lambda e: e.copy(out=gT[b][:], in_=pb[:, :]), rd(pb), rd(gT[b]))
            s.dma("sp", C.MIXT.t[0:512, n * 128:(n + 1) * 128].rearrange("(h p) t -> p h t", p=128),
                  gT[b][:].rearrange("p (h t) -> p h t", h=4), rd(gT[b]), rd(C.MIXT))
            if C.dbg_stage == "gdn":
                s.dma("sp", C.o_gdn[n * 128:(n + 1) * 128, :], yy[b][:], rd(yy[b]), rd(C.o_gdn))


def stage_att(C, l):
    kb, s = C.kb, C.kb.s
    ident, psb = C.ident, C.psb
    with ExitStack() as st:
        QT = kb.sb(st, [128, 4, NT], BF16, "QT")
        KT = kb.sb(st, [128, 2, NT], BF16, "KT")
        Vb = kb.sb(st, [128, NTT, 256], BF16, "Vb")
        onesb = kb.sb(st, [128, 128], BF16, "onesb")
        s.op("pool", lambda e: e.memset(onesb[:], 1.0), (), rd(onesb))
        w6 = kb.sb(st, [128, 2, 128], F32, "w6")
        bcast_row(C, st, w6[:, 0, :], w6, C.q_norm_w[l:l + 1, :], C.q_norm_w, 128, psb[0])
        bcast_row(C, st, w6[:, 1, :], w6, C.k_norm_w[l:l + 1, :], C.k_norm_w, 128, psb[0])
        NB = 2
        xa = [kb.sb(st, [128, 1024], F32, "xa%d" % i) for i in range(NB)]
        xn = [kb.sb(st, [128, 768], F32, "xnq%d" % i) for i in range(NB)]
        xr = [kb.sb(st, [128, 768], F32, "xr%d" % i) for i in range(NB)]
        cs_ = [kb.sb(st, [128, 2, 384], F32, "cs%d" % i) for i in range(NB)]
        tmp = [kb.sb(st, [128, 4, 384], F32, "rtmp%d" % i) for i in range(NB)]
        ssq = [kb.sb(st, [128, 6], F32, "assq%d" % i) for i in range(NB)]
        junk = kb.sb(st, [128, 128], F32, "ajunk")
        for t in range(NTT):
            b = t % NB
            lat = t < L // 128
            s.dma("sp", xa[b][:], C.PTOK[t * 128:(t + 1) * 128, 528:1552], rd(C.PTOK), rd(xa[b]))
            if lat:
                s.dma("pool", cs_[b][:], C.rope_in.t[:, t * 128:(t + 1) * 128, :].rearrange("c p f -> p c f"),
                      rd(C.rope_in), rd(cs_[b]))
            for h in range(6):
                s.op("act", lambda e: e.activation(out=junk[:], in_=xa[b][:, h * 128:(h + 1) * 128], func=AF.Square,
                                                   accum_out=ssq[b][:, h:h + 1]), rd(xa[b]), rd(junk, ssq[b]))
            s.op("dve", lambda e: e.tensor_scalar(out=ssq[b][:], in0=ssq[b][:], scalar1=1.0 / 128.0, scalar2=RMS_EPS,
                                                  op0=ALU.mult, op1=ALU.add), rd(ssq[b]), rd(ssq[b]))
            s.op("act", lambda e: e.sqrt(out=ssq[b][:], in_=ssq[b][:]), rd(ssq[b]), rd(ssq[b]))
            s.op("dve", lambda e: e.reciprocal(out=ssq[b][:], in_=ssq[b][:]), rd(ssq[b]), rd(ssq[b]))
            for h in range(6):
                s.op("dve", lambda e: e.scalar_tensor_tensor(out=xn[b][:, h * 128:(h + 1) * 128], in0=xa[b][:, h * 128:(h + 1) * 128],
                                                             scalar=ssq[b][:, h:h + 1], in1=w6[:, 0 if h < 4 else 1, :],
                                                             op0=ALU.mult, op1=ALU.mult), rd(xa[b], ssq[b], w6), rd(xn[b]))
            s.op("pool", lambda e: e.tensor_copy(out=Vb[:, t, :], in_=xa[b][:, 768:1024]), rd(xa[b]), rd(Vb))
            if lat:
                x1 = xn[b][:].rearrange("p (g two f) -> p g two f", two=2, f=32)[:, :, 0, :]
                x2 = xn[b][:].rearrange("p (g two f) -> p g two f", two=2, f=32)[:, :, 1, :]
                o1 = xr[b][:].rearrange("p (g two f) -> p g two f", two=2, f=32)[:, :, 0, :]
                o2 = xr[b][:].rearrange("p (g two f) -> p g two f", two=2, f=32)[:, :, 1, :]
                cc = cs_[b][:, 0, :].rearrange("p (g f) -> p g f", f=32)
                sn = cs_[b][:, 1, :].rearrange("p (g f) -> p g f", f=32)
                tm = [tmp[b][:, i, :].rearrange("p (g f) -> p g f", f=32) for i in range(4)]
                s.op("dve", lambda e: e.tensor_tensor(out=tm[0], in0=x1, in1=cc, op=ALU.mult), rd(xn[b], cs_[b]), rd(tmp[b]))
                s.op("pool", lambda e: e.tensor_tensor(out=tm[1], in0=x2, in1=sn, op=ALU.mult), rd(xn[b], cs_[b]), rd(tmp[b]))
                s.op("dve", lambda e: e.tensor_tensor(out=tm[2], in0=x2, in1=cc, op=ALU.mult), rd(xn[b], cs_[b]), rd(tmp[b]))
                s.op("pool", lambda e: e.tensor_tensor(out=tm[3], in0=x1, in1=sn, op=ALU.mult), rd(xn[b], cs_[b]), rd(tmp[b]))
                s.op("dve", lambda e: e.tensor_tensor(out=o1, in0=tm[0], in1=tm[1], op=ALU.subtract), rd(tmp[b]), rd(xr[b]))
                s.op("pool", lambda e: e.tensor_tensor(out=o2, in0=tm[2], in1=tm[3], op=ALU.add), rd(tmp[b]), rd(xr[b]))
                src = xr[b]
            else:
                src = xn[b]
            for h in range(6):
                pb = psb[1] if h < 4 else psb[2]
                s.op("pe", lambda e: e.transpose(out=pb[:, (h % 4) * 128:(h % 4 + 1) * 128], in_=src[:, h * 128:(h + 1) * 128],
                                                 identity=ident[:]), rd(src, ident), rd(pb))
            s.op("act", lambda e: e.copy(out=QT[:, :, t * 128:(t + 1) * 128],
                                         in_=psb[1][:, :].rearrange("p (h t) -> p h t", h=4)), rd(psb[1]), rd(QT))
            s.op("dve", lambda e: e.tensor_copy(out=KT[:, :, t * 128:(t + 1) * 128],
                                                in_=psb[2][:, 0:256].rearrange("p (h t) -> p h t", h=2)), rd(psb[2]), rd(KT))
        if C.dbg_stage == "attq":
            s.dma("sp", C.o_dbg[:, 0:512].bitcast(BF16)[:, 0:512], QT[:, 1, 0:512], rd(QT), rd(C.o_dbg))
            s.dma("sp", C.o_dbg[:, 512:1024].bitcast(BF16)[:, 0:512], KT[:, 1, 0:512], rd(KT), rd(C.o_dbg))
            return
        PT = [kb.sb(st, [128, 512], BF16, "PT%d" % i) for i in range(3)]
        rec = [kb.sb(st, [128, 512], F32, "rec%d" % i) for i in range(2)]
        aT = [kb.sb(st, [128, 512], BF16, "aT%d" % i) for i in range(2)]
        jobs = []
        for h in range(4):
            for q0 in range(0, L, 512):
                jobs.append((h, q0, 512, list(range(NTT))))
            jobs.append((h, L, LC, [32, 33]))
        scale = 128.0 ** -0.5
        k = 0
        for ji, (h, q0, qw, ktiles) in enumerate(jobs):
            kv = h // 2
            pO = psb[0] if ji % 2 == 0 else psb[5]
            pS = psb[1] if ji % 2 == 0 else psb[6]
            for i, kt in enumerate(ktiles):
                pst = psb[2 + k % 3]
                pt = PT[k % 3]
                k += 1
                s.op("pe", lambda e: e.matmul(out=pst[:, 0:qw], lhsT=KT[:, kv, kt * 128:(kt + 1) * 128], rhs=QT[:, h, q0:q0 + qw],
                                              start=True, stop=True), rd(KT, QT), rd(pst))
                s.op("act", lambda e: e.activation(out=pt[:, 0:qw], in_=pst[:, 0:qw], func=AF.Exp, scale=scale), rd(pst), rd(pt))
                s.op("pe", lambda e: e.matmul(out=pO[:, 0:qw], lhsT=Vb[:, kt, kv * 128:(kv + 1) * 128], rhs=pt[:, 0:qw],
                                              start=(i == 0), stop=(i == len(ktiles) - 1)), rd(Vb, pt), rd(pO))
                s.op("pe", lambda e: e.matmul(out=pS[:, 0:qw], lhsT=onesb[:], rhs=pt[:, 0:qw],
                                              start=(i == 0), stop=(i == len(ktiles) - 1)), rd(onesb, pt), rd(pS))
            rc = rec[ji % 2]
            at = aT[ji % 2]
            s.op("dve", lambda e: e.reciprocal(out=rc[:, 0:qw], in_=pS[:, 0:qw]), rd(pS), rd(rc))
            s.op("dve", lambda e: e.tensor_tensor(out=at[:, 0:qw], in0=pO[:, 0:qw], in1=rc[:, 0:qw], op=ALU.mult), rd(pO, rc), rd(at))
            s.dma("sp", C.MIXT[512 + h * 128:512 + (h + 1) * 128, q0:q0 + qw], at[:, 0:qw], rd(at), rd(C.MIXT))


def load_bf16_w(C, st, dram_t, src_ap_fn, kchunks, ncols, name):
    kb, s = C.kb, C.kb.s
    w = kb.sb(st, [128, kchunks, ncols], BF16, name)
    for kc in range(kchunks):
        for c0 in range(0, ncols, 2048):
            c1 = min(ncols, c0 + 2048)
            s.dma("pool", w[:, kc, c0:c1], src_ap_fn(kc, c0, c1), rd(dram_t), rd(w))
    return w


def stage_D(C, l, M):
    kb, s = C.kb, C.kb.s
    ident, psb = C.ident, C.psb
    Xsrc = C.XS if l == 0 else C.X2
    with ExitStack() as st:
        wout = load_bf16_w(C, st, C.w_out, lambda kc, c0, c1: C.w_out[l, kc * 128:(kc + 1) * 128, c0:c1], 8, D, "wout")
        wsgu = kb.sb(st, [128, 8, 512], BF16, "wsgu")
        for kc in range(8):
            s.dma("pool", wsgu[:, kc, 0:256], C.sh_w_gate[l, kc * 128:(kc + 1) * 128, :], rd(C.sh_w_gate), rd(wsgu))
            s.dma("pool", wsgu[:, kc, 256:512], C.sh_w_up[l, kc * 128:(kc + 1) * 128, :], rd(C.sh_w_up), rd(wsgu))
        wsd = load_bf16_w(C, st, C.sh_w_down, lambda kc, c0, c1: C.sh_w_down[l, kc * 128:(kc + 1) * 128, c0:c1], 2, D, "wsd")
        rw = kb.sb(st, [128, 8, 256], F32, "rw")
        s.dma("sp", rw[:], C.router_w.t[l].rearrange("(k p) e -> p k e", p=128), rd(C.router_w), rd(rw))
        G1 = kb.sb(st, [128, 2, D], F32, "G1")
        SC2 = kb.sb(st, [128, 2, D], F32, "SC2")
        SH2 = kb.sb(st, [128, 2, D], F32, "SH2")
        for r in range(2):
            bcast_row(C, st, G1[:, r, :], G1, C.MODD[l, r:r + 1, 2048:3072], C.MODD, D, psb[0])
            bcast_row(C, st, SH2[:, r, :], SH2, C.MODD[l, r:r + 1, 3072:4096], C.MODD, D, psb[0])
            bcast_row(C, st, SC2[:, r, :], SC2, C.MODD[l, r:r + 1, 4096:5120], C.MODD, D, psb[0])
        s.op("dve", lambda e: e.tensor_scalar_add(out=SC2[:], in0=SC2[:], scalar1=1.0), rd(SC2), rd(SC2))
        LNW = kb.sb(st, [128, D], F32, "LNW")
        LNB = kb.sb(st, [128, D], F32, "LNB")
        bcast_row(C, st, LNW[:], LNW, C.ln1_w[l:l + 1, :], C.ln1_w, D, psb[0])
        bcast_row(C, st, LNB[:], LNB, C.ln1_b[l:l + 1, :], C.ln1_b, D, psb[0])
        RB = kb.sb(st, [128, 256], F32, "RB")
        bcast_row(C, st, RB[:], RB, C.router_bias[l:l + 1, :], C.router_bias, 256, psb[0])
        NB = 2
        mixT = [kb.sb(st, [128, 8, 128], BF16, "mixT%d" % i) for i in range(NB)]
        xt = [kb.sb(st, [128, D], F32, "xt%d" % i) for i in range(NB)]
        tt = [kb.sb(st, [128, D], F32, "tt%d" % i) for i in range(NB)]
        x1 = [kb.sb(st, [128, D], F32, "x1%d" % i) for i in range(NB)]
        h2 = [kb.sb(st, [128, D], F32, "h2%d" % i) for i in range(NB)]
        h2Tf = [kb.sb(st, [128, 8, 128], F32, "h2Tf%d" % i) for i in range(NB)]
        h2Tb = [kb.sb(st, [128, 8, 128], BF16, "h2Tb%d" % i) for i in range(NB)]
        stats = [kb.sb(st, [128, 2, 6], F32, "dst%d" % i) for i in range(NB)]
        mv = [kb.sb(st, [128, 2], F32, "dmv%d" % i) for i in range(NB)]
        rstd = [kb.sb(st, [128, 1], F32, "drs%d" % i) for i in range(NB)]
        sg = [kb.sb(st, [128, 256], F32, "sg%d" % i) for i in range(NB)]
        sel = [kb.sb(st, [128, 256], F32, "sel%d" % i) for i in range(NB)]
        selm = [kb.sb(st, [128, 256], F32, "selm%d" % i) for i in range(NB)]
        g8 = [kb.sb(st, [128, 8, 8], F32, "g8%d" % i) for i in range(NB)]
        grp = [kb.sb(st, [128, 8], F32, "grp%d" % i) for i in range(NB)]
        gs8 = [kb.sb(st, [128, 8], F32, "gs8%d" % i) for i in range(NB)]
        gm = [kb.sb(st, [128, 8], F32, "gm%d" % i) for i in range(NB)]
        t8 = [kb.sb(st, [128, 8], F32, "t8%d" % i) for i in range(NB)]
        den = [kb.sb(st, [128, 1], F32, "den%d" % i) for i in range(NB)]
        sact = [kb.sb(st, [128, 256], F32, "sact%d" % i) for i in range(NB)]
        sact2 = [kb.sb(st, [128, 256], F32, "sactb%d" % i) for i in range(NB)]
        sactT = [kb.sb(st, [128, 2, 128], BF16, "sactT%d" % i) for i in range(NB)]
        sho = [kb.sb(st, [128, D], F32, "sho%d" % i) for i in range(NB)]
        for t in range(NTT):
            b = t % NB
            r = 0 if t < L // 128 else 1
            rows = slice(t * 128, (t + 1) * 128)
            s.dma("sp", mixT[b][:], C.MIXT.t[:, rows].rearrange("(k p) t -> p k t", p=128), rd(C.MIXT), rd(mixT[b]))
            s.dma("sp", xt[b][:], Xsrc[rows, :], rd(Xsrc), rd(xt[b]))
            for cb in range(2):
                for kc in range(8):
                    s.op("pe", lambda e: e.matmul(out=psb[cb][:, :], lhsT=mixT[b][:, kc, :], rhs=wout[:, kc, cb * 512:(cb + 1) * 512],
                                                  start=(kc == 0), stop=(kc == 7)), rd(mixT[b], wout), rd(psb[cb]))
            for cb in range(2):
                cs = slice(cb * 512, (cb + 1) * 512)
                s.op("dve", lambda e: e.tensor_tensor(out=tt[b][:, cs], in0=psb[cb][:, :], in1=G1[:, r, cs], op=ALU.mult),
                     rd(psb[cb], G1), rd(tt[b]))
            if C.dbg_stage == "D":
                s.dma("sp", C.o_y[rows, :], tt[b][:], rd(tt[b]), rd(C.o_y))
            s.op("dve", lambda e: e.scalar_tensor_tensor(out=tt[b][:], in0=xt[b][:], scalar=ALPHA, in1=tt[b][:],
                                                         op0=ALU.mult, op1=ALU.add), rd(xt[b], tt[b]), rd(tt[b]))
            layer_norm_tile(C, tt[b], x1[b], stats[b], mv[b], rstd[b], LNW, LNB)
            s.dma("sp", C.X1[rows, :], x1[b][:], rd(x1[b]), rd(C.X1))
            s.op("pool", lambda e: e.tensor_tensor(out=h2[b][:], in0=x1[b][:], in1=SC2[:, r, :], op=ALU.mult), rd(x1[b], SC2), rd(h2[b]))
            s.op("pool", lambda e: e.tensor_tensor(out=h2[b][:], in0=h2[b][:], in1=SH2[:, r, :], op=ALU.add), rd(h2[b], SH2), rd(h2[b]))
            s.dma("sp", C.H2[rows, :], h2[b][:], rd(h2[b]), rd(C.H2))
            for kc in range(8):
                pb = psb[2 + kc // 4]
                s.op("pe", lambda e: e.transpose(out=pb[:, (kc % 4) * 128:(kc % 4 + 1) * 128], in_=h2[b][:, kc * 128:(kc + 1) * 128],
                                                 identity=ident[:]), rd(h2[b], ident), rd(pb))
            for hh in range(2):
                s.op("act", lambda e: e.copy(out=h2Tf[b][:, hh * 4:hh * 4 + 4, :], in_=psb[2 + hh][:, :].rearrange("p (k t) -> p k t", k=4)),
                     rd(psb[2 + hh]), rd(h2Tf[b]))
                s.op("dve", lambda e: e.tensor_copy(out=h2Tb[b][:, hh * 4:hh * 4 + 4, :], in_=psb[2 + hh][:, :].rearrange("p (k t) -> p k t", k=4)),
                     rd(psb[2 + hh]), rd(h2Tb[b]))
            for kc in range(8):
                s.op("pe", lambda e: e.matmul(out=psb[4][:, 0:256], lhsT=h2Tf[b][:, kc, :], rhs=rw[:, kc, :],
                                              start=(kc == 0), stop=(kc == 7)), rd(h2Tf[b], rw), rd(psb[4]))
            s.op("act", lambda e: e.activation(out=sg[b][:], in_=psb[4][:, 0:256], func=AF.Sigmoid), rd(psb[4]), rd(sg[b]))
            s.op("dve", lambda e: e.tensor_tensor(out=sel[b][:], in0=sg[b][:], in1=RB[:], op=ALU.add), rd(sg[b], RB), rd(sel[b]))
            for g in range(8):
                s.op("dve", lambda e: e.max(out=g8[b][:, g, :], in_=sel[b][:, g * 32:(g + 1) * 32]), rd(sel[b]), rd(g8[b]))
            s.op("dve", lambda e: e.tensor_tensor(out=grp[b][:], in0=g8[b][:, :, 0], in1=g8[b][:, :, 1], op=ALU.add), rd(g8[b]), rd(grp[b]))
            s.op("dve", lambda e: e.max(out=gs8[b][:], in_=grp[b][:]), rd(grp[b]), rd(gs8[b]))
            s.op("dve", lambda e: e.tensor_scalar(out=gm[b][:], in0=grp[b][:], scalar1=gs8[b][:, 3:4], scalar2=None, op0=ALU.is_ge),
                 rd(grp[b], gs8[b]), rd(gm[b]))
            for g in range(8):
                s.op("dve", lambda e: e.tensor_scalar(out=selm[b][:, g * 32:(g + 1) * 32], in0=sel[b][:, g * 32:(g + 1) * 32],
                                                      scalar1=2.0, scalar2=gm[b][:, g:g + 1], op0=ALU.add, op1=ALU.mult),
                     rd(sel[b], gm[b]), rd(selm[b]))
            s.op("dve", lambda e: e.max(out=t8[b][:], in_=selm[b][:]), rd(selm[b]), rd(t8[b]))
            s.op("dve", lambda e: e.tensor_scalar(out=sel[b][:], in0=selm[b][:], scalar1=t8[b][:, 7:8], scalar2=None, op0=ALU.is_ge),
                 rd(selm[b], t8[b]), rd(sel[b]))
            s.op("dve", lambda e: e.tensor_tensor(out=M.WFULL[:, t, :], in0=sg[b][:], in1=sel[b][:], op=ALU.mult),
                 rd(sg[b], sel[b]), rd(M.WFULL))
            s.op("dve", lambda e: e.reduce_sum(out=den[b][:], in_=M.WFULL[:, t, :], axis=AX.X), rd(M.WFULL), rd(den[b]))
            s.op("dve", lambda e: e.reciprocal(out=den[b][:], in_=den[b][:]), rd(den[b]), rd(den[b]))
            s.op("dve", lambda e: e.tensor_scalar(out=M.WFULL[:, t, :], in0=M.WFULL[:, t, :], scalar1=den[b][:, 0:1], scalar2=2.5,
                                                  op0=ALU.mult, op1=ALU.mult), rd(M.WFULL, den[b]), rd(M.WFULL))
            for kc in range(8):
                s.op("pe", lambda e: e.matmul(out=psb[5][:, :], lhsT=h2Tb[b][:, kc, :], rhs=wsgu[:, kc, :],
                                              start=(kc == 0), stop=(kc == 7)), rd(h2Tb[b], wsgu), rd(psb[5]))
            s.op("act", lambda e: e.activation(out=sact[b][:], in_=psb[5][:, 0:256], func=AF.Silu), rd(psb[5]), rd(sact[b]))
            s.op("dve", lambda e: e.tensor_tensor(out=sact2[b][:], in0=sact[b][:], in1=psb[5][:, 256:512], op=ALU.mult),
                 rd(sact[b], psb[5]), rd(sact2[b]))
            for k2 in range(2):
                s.op("pe", lambda e: e.transpose(out=psb[4][:, 256 + k2 * 128:384 + k2 * 128], in_=sact2[b][:, k2 * 128:(k2 + 1) * 128],
                                                 identity=ident[:]), rd(sact2[b], ident), rd(psb[4]))
            s.op("act", lambda e: e.copy(out=sactT[b][:], in_=psb[4][:, 256:512].rearrange("p (k t) -> p k t", k=2)), rd(psb[4]), rd(sactT[b]))
            for cb in range(2):
                for k2 in range(2):
                    s.op("pe", lambda e: e.matmul(out=psb[6 + cb][:, :], lhsT=sactT[b][:, k2, :], rhs=wsd[:, k2, cb * 512:(cb + 1) * 512],
                                                  start=(k2 == 0), stop=(k2 == 1)), rd(sactT[b], wsd), rd(psb[6 + cb]))
                if cb == 0:
                    s.op("act", lambda e: e.copy(out=sho[b][:, 0:512], in_=psb[6][:, :]), rd(psb[6]), rd(sho[b]))
                else:
                    s.op("dve", lambda e: e.tensor_copy(out=sho[b][:, 512:1024], in_=psb[7][:, :]), rd(psb[7]), rd(sho[b]))
            s.dma("sp", C.SHO[rows, :], sho[b][:], rd(sho[b]), rd(C.SHO))


def layer_norm_tile(C, src, dst, stats, mv, rstd, LNW, LNB):
    s = C.kb.s
    for j in range(2):
        s.op("dve", lambda e: e.bn_stats(out=stats[:, j, :], in_=src[:, j * 512:(j + 1) * 512]), rd(src), rd(stats))
    s.op("dve", lambda e: e.bn_aggr(out=mv[:], in_=stats[:].rearrange("p a b -> p (a b)")), rd(stats), rd(mv))
    s.op("dve", lambda e: e.tensor_scalar_add(out=rstd[:], in0=mv[:, 1:2], scalar1=LN_EPS), rd(mv), rd(rstd))
    s.op("act", lambda e: e.sqrt(out=rstd[:], in_=rstd[:]), rd(rstd), rd(rstd))
    s.op("dve", lambda e: e.reciprocal(out=rstd[:], in_=rstd[:]), rd(rstd), rd(rstd))
    s.op("dve", lambda e: e.tensor_scalar(out=dst[:], in0=src[:], scalar1=mv[:, 0:1], scalar2=rstd[:, 0:1],
                                          op0=ALU.subtract, op1=ALU.mult), rd(src, mv, rstd), rd(dst))
    s.op("pool", lambda e: e.tensor_tensor(out=dst[:], in0=dst[:], in1=LNW[:], op=ALU.mult), rd(dst, LNW), rd(dst))
    s.op("dve", lambda e: e.tensor_tensor(out=dst[:], in0=dst[:], in1=LNB[:], op=ALU.add), rd(dst, LNB), rd(dst))


TAB_LAYERS = DEPTH
BS = 256
NBLK = 391
NSTL = NBLK * 2
NSLOT = NSTL * 128


def stage_E(C, l, M):
    kb, s = C.kb, C.kb.s
    ident, ones, masks, psb = C.ident, C.ones, C.masks, C.psb
    WF = M.WFULL
    with ExitStack() as st:
        iot = kb.sb(st, [128, NBLK + 1 + NTT], F32, "iot")
        s.dma("sp", iot[:], C.iotas[:], rd(C.iotas), rd(iot))
        IDXW = kb.sb(st, [128, NBLK], I32, "IDXW")
        SLOT8 = kb.sb(st, [128, NTT, 8], I32, "SLOT8")
        BT = kb.sb(st, [128, NSTL, 2], F32, "BT")
        IDXT = kb.sb(st, [128, NSTL], I32, "IDXT")
        with ExitStack() as s1:
            RANK = kb.sb(s1, [128, NTT, 256], F32, "RANK")
            cnt = kb.sb(s1, [128, 256], F32, "cnt")
            s.op("pool", lambda e: e.memset(cnt[:], 0.0), (), rd(cnt))
            mk = [kb.sb(s1, [128, 256], F32, "mk%d" % i) for i in range(2)]
            for t in range(NTT):
                b = t % 2
                s.op("dve", lambda e: e.tensor_scalar(out=mk[b][:], in0=WF[:, t, :], scalar1=0.0, scalar2=None, op0=ALU.is_gt),
                     rd(WF), rd(mk[b]))
                s.op("pe", lambda e: e.matmul(out=psb[0][:, 0:256], lhsT=masks[:, 3, :], rhs=mk[b][:], start=True, stop=True),
                     rd(masks, mk[b]), rd(psb[0]))
                s.op("pe", lambda e: e.matmul(out=psb[1][:, 0:256], lhsT=ones[:], rhs=mk[b][:], start=True, stop=True),
                     rd(ones, mk[b]), rd(psb[1]))
                s.op("dve", lambda e: e.tensor_tensor(out=RANK[:, t, :], in0=psb[0][:, 0:256], in1=cnt[:], op=ALU.add),
                     rd(psb[0], cnt), rd(RANK))
                s.op("dve", lambda e: e.tensor_tensor(out=cnt[:], in0=cnt[:], in1=psb[1][:, 0:256], op=ALU.add),
                     rd(psb[1], cnt), rd(cnt))
            ci = kb.sb(s1, [128, 256], I32, "ci")
            nblk = kb.sb(s1, [128, 256], F32, "nblk")
            blkend = kb.sb(s1, [128, 256], F32, "blkend")
            bs256 = kb.sb(s1, [128, 256], F32, "bs256")
            s.op("dve", lambda e: e.tensor_scalar(out=ci[:], in0=cnt[:], scalar1=float(BS - 1), scalar2=None, op0=ALU.add), rd(cnt), rd(ci))
            s.op("dve", lambda e: e.tensor_scalar(out=ci[:], in0=ci[:], scalar1=8, scalar2=None, op0=ALU.arith_shift_right), rd(ci), rd(ci))
            s.op("dve", lambda e: e.tensor_copy(out=nblk[:], in_=ci[:]), rd(ci), rd(nblk))
            s.op("dve", lambda e: e.tensor_tensor_scan(out=blkend[:], data0=ones[:, 0:128].to_broadcast([128, 256]) if False else C.ones256[:],
                                                       data1=nblk[:], initial=0.0, op0=ALU.mult, op1=ALU.add), rd(nblk, C.ones256), rd(blkend))
            s.op("dve", lambda e: e.tensor_tensor(out=bs256[:], in0=blkend[:], in1=nblk[:], op=ALU.subtract), rd(blkend, nblk), rd(bs256))
            s.op("dve", lambda e: e.tensor_scalar(out=bs256[:], in0=bs256[:], scalar1=float(BS), scalar2=1.0, op0=ALU.mult, op1=ALU.add),
                 rd(bs256), rd(bs256))
            bcol = kb.sb(s1, [128, 2], F32, "bcol")
            tmpd = kb.sb(s1, [128, 128], F32, "tmpd")
            cmpb = [kb.sb(s1, [128, NBLK], F32, "cmpb%d" % i) for i in range(2)]
            for c in range(2):
                s.op("dve", lambda e: e.tensor_tensor(out=tmpd[:], in0=blkend[:, c * 128:(c + 1) * 128], in1=ident[:], op=ALU.mult),
                     rd(blkend, ident), rd(tmpd))
                s.op("dve", lambda e: e.reduce_sum(out=bcol[:, c:c + 1], in_=tmpd[:], axis=AX.X), rd(tmpd), rd(bcol))
                s.op("dve", lambda e: e.tensor_scalar(out=cmpb[c][:], in0=iot[:, 0:NBLK], scalar1=bcol[:, c:c + 1], scalar2=None, op0=ALU.is_ge),
                     rd(iot, bcol), rd(cmpb[c]))
                s.op("pe", lambda e: e.matmul(out=psb[2][:, 0:NBLK], lhsT=ones[:], rhs=cmpb[c][:], start=(c == 0), stop=(c == 1)),
                     rd(ones, cmpb[c]), rd(psb[2]))
            ebf = kb.sb(s1, [128, NBLK], F32, "ebf")
            s.op("dve", lambda e: e.tensor_scalar(out=ebf[:], in0=psb[2][:, 0:NBLK], scalar1=255.0, scalar2=128.0, op0=ALU.min, op1=ALU.mult),
                 rd(psb[2]), rd(ebf))
            s.op("dve", lambda e: e.tensor_scalar(out=ebf[:], in0=ebf[:], scalar1=iot[:, NBLK:NBLK + 1], scalar2=float(l * 32768),
                                                  op0=ALU.add, op1=ALU.add), rd(ebf, iot), rd(ebf))
            s.op("dve", lambda e: e.tensor_copy(out=IDXW[:], in_=ebf[:]), rd(ebf), rd(IDXW))
            zt = kb.sb(s1, [128, NSTL * 2], F32, "zt")
            s.op("pool", lambda e: e.memset(zt[:], 0.0), (), rd(zt))
            s.dma("sp", C.BUFTW.t.rearrange("(p n) two -> p (n two)", p=128), zt[:], rd(zt), rd(C.BUFTW))
            key = [kb.sb(s1, [128, 256], F32, "key%d" % i) for i in range(2)]
            top8 = [kb.sb(s1, [128, 8], F32, "top8%d" % i) for i in range(2)]
            oh = [kb.sb(s1, [128, 256], F32, "oh%d" % i) for i in range(2)]
            tw = [kb.sb(s1, [128, 8, 2], F32, "tw%d" % i) for i in range(2)]
            si = [kb.sb(s1, [128, 8], I32, "si%d" % i) for i in range(2)]
            lo = [kb.sb(s1, [128, 8], I32, "lo%d" % i) for i in range(2)]
            hi = [kb.sb(s1, [128, 8], I32, "hi%d" % i) for i in range(2)]
            lof = [kb.sb(s1, [128, 8], F32, "lof%d" % i) for i in range(2)]
            hif = [kb.sb(s1, [128, 8], F32, "hif%d" % i) for i in range(2)]
            posi = [kb.sb(s1, [128, 8], I32, "posi%d" % i) for i in range(2)]
            for t in range(NTT):
                b = t % 2
                s.op("dve", lambda e: e.tensor_scalar(out=mk[b][:], in0=WF[:, t, :], scalar1=0.0, scalar2=None, op0=ALU.is_gt),
                     rd(WF), rd(mk[b]))
                s.op("dve", lambda e: e.tensor_tensor(out=key[b][:], in0=RANK[:, t, :], in1=bs256[:], op=ALU.add), rd(RANK, bs256), rd(key[b]))
                s.op("dve", lambda e: e.tensor_tensor(out=key[b][:], in0=key[b][:], in1=mk[b][:], op=ALU.mult), rd(key[b], mk[b]), rd(key[b]))
                s.op("dve", lambda e: e.max(out=top8[b][:], in_=key[b][:]), rd(key[b]), rd(top8[b]))
                for k in range(8):
                    s.op("dve", lambda e: e.tensor_scalar(out=oh[b][:], in0=key[b][:], scalar1=top8[b][:, k:k + 1], scalar2=None, op0=ALU.is_equal),
                         rd(key[b], top8[b]), rd(oh[b]))
                    s.op("dve", lambda e: e.tensor_tensor(out=oh[b][:], in0=oh[b][:], in1=WF[:, t, :], op=ALU.mult), rd(oh[b], WF), rd(oh[b]))
                    s.op("dve", lambda e: e.reduce_sum(out=tw[b][:, k, 1:2], in_=oh[b][:], axis=AX.X), rd(oh[b]), rd(tw[b]))
                    s.op("pool", lambda e: e.tensor_copy(out=tw[b][:, k, 0:1], in_=iot[:, NBLK + 1 + t:NBLK + 2 + t]), rd(iot), rd(tw[b]))
                s.op("dve", lambda e: e.tensor_scalar(out=si[b][:], in0=top8[b][:], scalar1=-1.0, scalar2=None, op0=ALU.add), rd(top8[b]), rd(si[b]))
                s.op("dve", lambda e: e.tensor_copy(out=SLOT8[:, t, :], in_=si[b][:]), rd(si[b]), rd(SLOT8))
                s.op("dve", lambda e: e.tensor_scalar(out=lo[b][:], in0=si[b][:], scalar1=127, scalar2=None, op0=ALU.bitwise_and), rd(si[b]), rd(lo[b]))
                s.op("dve", lambda e: e.tensor_scalar(out=hi[b][:], in0=si[b][:], scalar1=7, scalar2=None, op0=ALU.arith_shift_right), rd(si[b]), rd(hi[b]))
                s.op("dve", lambda e: e.tensor_copy(out=lof[b][:], in_=lo[b][:]), rd(lo[b]), rd(lof[b]))
                s.op("dve", lambda e: e.tensor_copy(out=hif[b][:], in_=hi[b][:]), rd(hi[b]), rd(hif[b]))
                s.op("dve", lambda e: e.scalar_tensor_tensor(out=lof[b][:], in0=lof[b][:], scalar=float(NSTL), in1=hif[b][:],
                                                             op0=ALU.mult, op1=ALU.add), rd(lof[b], hif[b]), rd(lof[b]))
                s.op("dve", lambda e: e.tensor_copy(out=posi[b][:], in_=lof[b][:]), rd(lof[b]), rd(posi[b]))
                for k in range(8):
                    s.idma(out=C.BUFTW[:, :], in_=tw[b][:, k, :], out_off=posi[b][:, k:k + 1], reads=rd(tw[b], posi[b]), writes=rd(C.BUFTW))
            s.dma("sp", BT[:], C.BUFTW.t.rearrange("(p n) two -> p n two", p=128), rd(C.BUFTW), rd(BT))
            s.op("dve", lambda e: e.tensor_copy(out=IDXT[:], in_=BT[:, :, 0]), rd(BT), rd(IDXT))
            if C.dbg_stage == "E1":
                s.dma("sp", C.o_e1[:, 0:NSTL * 2], BT[:].rearrange("p n two -> p (n two)"), rd(BT), rd(C.o_e1))
                s.dma("sp", C.o_e1[:, 2000:2000 + NBLK].bitcast(I32), IDXW[:], rd(IDXW), rd(C.o_e1))
                s.dma("sp", C.o_e1[:, 2400:2400 + NTT * 8].bitcast(I32), SLOT8[:].rearrange("p n k -> p (n k)"), rd(SLOT8), rd(C.o_e1))
                s.dma("sp", C.o_e1[:, 2700:2956], cnt[:], rd(cnt), rd(C.o_e1))
                return

        s.barrier()
        with ExitStack() as s2:
            NW = 2
            wg = [kb.sb(s2, [128, 2048], BF16, "wg%d" % i) for i in range(NW)]
            wu = [kb.sb(s2, [128, 2048], BF16, "wu%d" % i) for i in range(NW)]
            wd = [kb.sb(s2, [128, 2048], BF16, "wd%d" % i) for i in range(NW)]
            NX = 3
            xg = [kb.sb(s2, [128, D], F32, "xg%d" % i) for i in range(NX)]
            xT = [kb.sb(s2, [128, 8, 128], BF16, "xTe%d" % i) for i in range(2)]
            ga = [kb.sb(s2, [128, 256], F32, "ga%d" % i) for i in range(2)]
            a2 = [kb.sb(s2, [128, 256], F32, "a2%d" % i) for i in range(2)]
            aT = [kb.sb(s2, [128, 2, 128], BF16, "aTe%d" % i) for i in range(2)]
            eo = [kb.sb(s2, [128, D], BF16, "eo%d" % i) for i in range(2)]
            nblk_run = NBLK if C.dbg_stage != "E2s" else 8
            for blk in range(nblk_run):
                wb = blk % NW
                ix = IDXW[:, blk:blk + 1]
                s.idma(out=wg[wb][:], in_=C.WG[:, :], in_off=ix, reads=rd(C.WG, IDXW), writes=rd(wg[wb]))
                s.idma(out=wu[wb][:], in_=C.WU[:, :], in_off=ix, reads=rd(C.WU, IDXW), writes=rd(wu[wb]))
                s.idma(out=wd[wb][:], in_=C.WD[:, :], in_off=ix, reads=rd(C.WD, IDXW), writes=rd(wd[wb]))
                for sti in range(2):
                    j = blk * 2 + sti
                    xb = xg[j % NX]
                    b2 = j % 2
                    s.idma(out=xb[:], in_=C.H2[:, :], in_off=IDXT[:, j:j + 1], reads=rd(C.H2, IDXT), writes=rd(xb))
                    for kc in range(8):
                        pb = psb[kc // 4]
                        s.op("pe", lambda e: e.transpose(out=pb[:, (kc % 4) * 128:(kc % 4 + 1) * 128], in_=xb[:, kc * 128:(kc + 1) * 128],
                                                         identity=ident[:]), rd(xb, ident), rd(pb))
                    s.op("act", lambda e: e.copy(out=xT[b2][:, 0:4, :], in_=psb[0][:, :].rearrange("p (k t) -> p k t", k=4)), rd(psb[0]), rd(xT[b2]))
                    s.op("dve", lambda e: e.tensor_copy(out=xT[b2][:, 4:8, :], in_=psb[1][:, :].rearrange("p (k t) -> p k t", k=4)), rd(psb[1]), rd(xT[b2]))
                    for kc in range(8):
                        s.op("pe", lambda e: e.matmul(out=psb[2][:, 0:256], lhsT=xT[b2][:, kc, :], rhs=wg[wb][:, kc * 256:(kc + 1) * 256],
                                                      start=(kc == 0), stop=(kc == 7)), rd(xT[b2], wg[wb]), rd(psb[2]))
                    for kc in range(8):
                        s.op("pe", lambda e: e.matmul(out=psb[3][:, 0:256], lhsT=xT[b2][:, kc, :], rhs=wu[wb][:, kc * 256:(kc + 1) * 256],
                                                      start=(kc == 0), stop=(kc == 7)), rd(xT[b2], wu[wb]), rd(psb[3]))
                    s.op("act", lambda e: e.activation(out=ga[b2][:], in_=psb[2][:, 0:256], func=AF.Silu), rd(psb[2]), rd(ga[b2]))
                    s.op("dve", lambda e: e.tensor_tensor(out=a2[b2][:], in0=ga[b2][:], in1=psb[3][:, 0:256], op=ALU.mult), rd(ga[b2], psb[3]), rd(a2[b2]))
                    for k2 in range(2):
                        s.op("pe", lambda e: e.transpose(out=psb[4][:, k2 * 128:(k2 + 1) * 128], in_=a2[b2][:, k2 * 128:(k2 + 1) * 128],
                                                         identity=ident[:]), rd(a2[b2], ident), rd(psb[4]))
                    s.op("act", lambda e: e.copy(out=aT[b2][:], in_=psb[4][:, 0:256].rearrange("p (k t) -> p k t", k=2)), rd(psb[4]), rd(aT[b2]))
                    for cb in range(2):
                        for k2 in range(2):
                            s.op("pe", lambda e: e.matmul(out=psb[5 + cb][:, :], lhsT=aT[b2][:, k2, :],
                                                          rhs=wd[wb][:, k2 * 1024 + cb * 512:k2 * 1024 + (cb + 1) * 512],
                                                          start=(k2 == 0), stop=(k2 == 1)), rd(aT[b2], wd[wb]), rd(psb[5 + cb]))
                    s.op("act", lambda e: e.activation(out=eo[b2][:, 0:512], in_=psb[5][:, :], func=AF.Copy, scale=BT[:, j, 1:2]),
                         rd(psb[5], BT), rd(eo[b2]))
                    s.op("dve", lambda e: e.tensor_scalar(out=eo[b2][:, 512:1024], in0=psb[6][:, :], scalar1=BT[:, j, 1:2], scalar2=None, op0=ALU.mult),
                         rd(psb[6], BT), rd(eo[b2]))
                    s.dma("sp", C.EO[j * 128:(j + 1) * 128, :], eo[b2][:], rd(eo[b2]), rd(C.EO))
            if C.dbg_stage in ("E2", "E2s"):
                return

        s.barrier()
        with ExitStack() as s3:
            G2 = kb.sb(s3, [128, 2, D], F32, "G2")
            for r in range(2):
                bcast_row(C, s3, G2[:, r, :], G2, C.MODD[l, r:r + 1, 5120:6144], C.MODD, D, psb[7])
            LNW = kb.sb(s3, [128, D], F32, "LNW2")
            LNB = kb.sb(s3, [128, D], F32, "LNB2")
            bcast_row(C, s3, LNW[:], LNW, C.ln2_w[l:l + 1, :], C.ln2_w, D, psb[7])
            bcast_row(C, s3, LNB[:], LNB, C.ln2_b[l:l + 1, :], C.ln2_b, D, psb[7])
            gat = [kb.sb(s3, [128, D], BF16, "gat%d" % i) for i in range(4)]
            ffa = [kb.sb(s3, [128, D], F32, "ffa%d" % i) for i in range(2)]
            x1t = [kb.sb(s3, [128, D], F32, "x1t%d" % i) for i in range(2)]
            x2t = [kb.sb(s3, [128, D], F32, "x2t%d" % i) for i in range(2)]
            stats = [kb.sb(s3, [128, 2, 6], F32, "est%d" % i) for i in range(2)]
            mv = [kb.sb(s3, [128, 2], F32, "emv%d" % i) for i in range(2)]
            rstd = [kb.sb(s3, [128, 1], F32, "ers%d" % i) for i in range(2)]
            gi = 0
            for t in range(NTT):
                b = t % 2
                r = 0 if t < L // 128 else 1
                rows = slice(t * 128, (t + 1) * 128)
                s.dma("sp", ffa[b][:], C.SHO[rows, :], rd(C.SHO), rd(ffa[b]))
                s.dma("sp", x1t[b][:], C.X1[rows, :], rd(C.X1), rd(x1t[b]))
                for k in range(8):
                    g_ = gat[gi % 4]
                    gi += 1
                    s.idma(out=g_[:], in_=C.EO[:, :], in_off=SLOT8[:, t, k:k + 1], reads=rd(C.EO, SLOT8), writes=rd(g_))
                    eng = "dve" if k % 2 == 0 else "pool"
                    s.op(eng, lambda e: e.tensor_tensor(out=ffa[b][:], in0=ffa[b][:], in1=g_[:], op=ALU.add), rd(ffa[b], g_), rd(ffa[b]))
                if C.dbg_stage == "E":
                    s.dma("sp", C.o_ff[rows, :], ffa[b][:], rd(ffa[b]), rd(C.o_ff))
                s.op("pool", lambda e: e.tensor_tensor(out=ffa[b][:], in0=ffa[b][:], in1=G2[:, r, :], op=ALU.mult), rd(ffa[b], G2), rd(ffa[b]))
                s.op("dve", lambda e: e.scalar_tensor_tensor(out=ffa[b][:], in0=x1t[b][:], scalar=ALPHA, in1=ffa[b][:],
                                                             op0=ALU.mult, op1=ALU.add), rd(x1t[b], ffa[b]), rd(ffa[b]))
                layer_norm_tile(C, ffa[b], x2t[b], stats[b], mv[b], rstd[b], LNW, LNB)
                s.dma("sp", C.X2[rows, :], x2t[b][:], rd(x2t[b]), rd(C.X2))
                if l == DEPTH - 1 and t < L // 128 and C.out is not None:
                    s.dma("sp", C.out[rows, :], x2t[b][:], rd(x2t[b]), rd(C.out))


def c_col(d, h):
    return d * 4 + h


def build(dbg_stage=None):
    nc = bass.Bass("TRN2", target_bir_lowering=False)
    es = ExitStack()
    kb = KB(nc, es)
    s = kb.s
    x_in = kb.dram("x", [NT, D], F32, kind="ExternalInput")
    c_in = kb.dram("c2", [2, D], F32, kind="ExternalInput")
    ada_w = kb.dram("ada_w", [DEPTH, D, 6 * D], F32, kind="ExternalInput")
    ada_b = kb.dram("ada_b", [DEPTH, 6 * D], F32, kind="ExternalInput")
    w_in = kb.dram("w_in", [DEPTH, D, N_IN], F32, kind="ExternalInput")
    ident_in = kb.dram("ident", [128, 128], F32, kind="ExternalInput")
    masks_in = kb.dram("masks", [4, 128, 128], F32, kind="ExternalInput")
    conv_w = kb.dram("conv_w", [DEPTH, 5, OFF_Z], F32, kind="ExternalInput")
    a_log = kb.dram("gdn_a_log", [DEPTH, 8], F32, kind="ExternalInput")
    dt_bias = kb.dram("gdn_dt_bias", [DEPTH, 8], F32, kind="ExternalInput")
    gdn_norm_w = kb.dram("gdn_norm_w", [DEPTH, 128], F32, kind="ExternalInput")
    q_norm_w = kb.dram("q_norm_w", [DEPTH, 128], F32, kind="ExternalInput")
    k_norm_w = kb.dram("k_norm_w", [DEPTH, 128], F32, kind="ExternalInput")
    rope_in = kb.dram("rope", [2, L, 384], F32, kind="ExternalInput")
    w_out = kb.dram("w_out", [DEPTH, D, D], F32, kind="ExternalInput")
    ln1_w = kb.dram("ln1_w", [DEPTH, D], F32, kind="ExternalInput")
    ln1_b = kb.dram("ln1_b", [DEPTH, D], F32, kind="ExternalInput")
    ln2_w = kb.dram("ln2_w", [DEPTH, D], F32, kind="ExternalInput")
    ln2_b = kb.dram("ln2_b", [DEPTH, D], F32, kind="ExternalInput")
    router_w = kb.dram("router_w", [DEPTH, D, 256], F32, kind="ExternalInput")
    router_bias = kb.dram("router_bias", [DEPTH, 256], F32, kind="ExternalInput")
    sh_w_gate = kb.dram("sh_w_gate", [DEPTH, D, 256], F32, kind="ExternalInput")
    sh_w_up = kb.dram("sh_w_up", [DEPTH, D, 256], F32, kind="ExternalInput")
    sh_w_down = kb.dram("sh_w_down", [DEPTH, 256, D], F32, kind="ExternalInput")
    iotas = kb.dram("iotas", [128, NBLK + 1 + NTT], F32, kind="ExternalInput")
    WG = kb.dram("WG", [TAB_LAYERS * 256 * 128, 2048], F32, kind="ExternalInput")
    WU = kb.dram("WU", [TAB_LAYERS * 256 * 128, 2048], F32, kind="ExternalInput")
    WD = kb.dram("WD", [TAB_LAYERS * 256 * 128, 2048], F32, kind="ExternalInput")
    outs = {}

    def dbg_out(name, shape, dt=F32):
        t = kb.dram(name, shape, dt, kind="ExternalOutput")
        outs[name] = t
        return t

    XS = kb.dram("XS", [NT, D], F32)
    MODD = kb.dram("MODD", [DEPTH, 2, 6 * D], F32)
    QKVT = kb.dram("QKVT", [OFF_Z, NT], F32)
    PTOK = kb.dram("PTOK", [NT, NTOKC], F32)
    OPSD = kb.dram("OPSD", [4, 2, NTT, 128, OPW], F32)
    OFB = [kb.dram("OFB%d" % d, [NT, 512], F32) for d in range(2)]
    MIXT = kb.dram("MIXT", [D, NT], BF16)
    X1 = kb.dram("X1", [NT, D], F32)
    X2 = kb.dram("X2", [NT, D], F32)
    H2 = kb.dram("H2", [NT, D], F32)
    SHO = kb.dram("SHO", [NT, D], F32)
    BUFTW = kb.dram("BUFTW", [NSLOT, 2], F32)
    EO = kb.dram("EO", [NSLOT, D], BF16)

    ces = es
    ident = kb.sb(ces, [128, 128], F32, "ident")
    s.dma("sp", ident[:], ident_in[:], reads=rd(ident_in), writes=rd(ident))
    identb = kb.sb(ces, [128, 128], BF16, "identb")
    s.op("dve", lambda e: e.tensor_copy(out=identb[:], in_=ident[:]), rd(ident), rd(identb))

    psb = [kb.ps(ces, [128, 512], F32, "bank%d" % i) for i in range(8)]
    for p_ in psb:
        p_.res.excl = True
    ones = kb.sb(ces, [128, 128], F32, "ones")
    s.op("pool", lambda e: e.memset(ones[:], 1.0), (), rd(ones))
    bcrow = kb.sb(ces, [128, D], F32, "bcrow")
    s.op("pool", lambda e: e.memset(bcrow[:], 0.0), (), rd(bcrow))
    masks = kb.sb(ces, [128, 4, 128], F32, "masks")
    ones256 = kb.sb(ces, [128, 256], F32, "ones256")
    s.op("pool", lambda e: e.memset(ones256[:], 1.0), (), rd(ones256))
    s.dma("sp", masks[:], masks_in.t.rearrange("m p f -> p m f"), rd(masks_in), rd(masks))

    final_out = dbg_out("out", [L, D]) if dbg_stage is None else None
    for l in range(DEPTH):
        with ExitStack() as st:
            cT = kb.sb(st, [128, 8, 2], F32, "cT")
            craw = kb.sb(st, [2, D], F32, "craw")
            s.dma("sp", craw[:], c_in[:], rd(c_in), rd(craw))
            csil = kb.sb(st, [2, D], F32, "csil")
            s.op("act", lambda e: e.activation(out=csil[:], in_=craw[:], func=AF.Silu), rd(craw), rd(csil))
            for kc in range(8):
                s.op("pe", lambda e: e.transpose(out=psb[0][:, kc * 2:kc * 2 + 2], in_=csil[:, kc * 128:(kc + 1) * 128],
                                                 identity=ident[0:2, 0:2]), rd(csil, ident), rd(psb[0]))
            s.op("dve", lambda e: e.tensor_copy(out=cT[:].rearrange("p k r -> p (k r)"), in_=psb[0][:, 0:16]),
                 rd(psb[0]), rd(cT))
            modrow = kb.sb(st, [2, 6 * D], F32, "modrow")
            abrow = kb.sb(st, [2, 6 * D], F32, "abrow")
            for r in range(2):
                s.dma("sp", abrow[r:r + 1, :], ada_b[l:l + 1, :], rd(ada_b), rd(abrow))
            wch = [kb.sb(st, [128, 3072], F32, "adaw%d" % i) for i in range(2)]
            for half in range(2):
                for kc in range(8):
                    wt = wch[kc % 2]
                    s.dma("sp" if kc % 2 == 0 else "pool", wt[:],
                          ada_w[l, kc * 128:(kc + 1) * 128, half * 3072:(half + 1) * 3072], rd(ada_w), rd(wt))
                    for cb in range(6):
                        s.op("pe", lambda e: e.matmul(out=psb[cb][0:2, :], lhsT=cT[:, kc, :],
                                                      rhs=wt[:, cb * 512:(cb + 1) * 512],
                                                      start=(kc == 0), stop=(kc == 7)),
                             rd(cT, wt), rd(psb[cb]))
                for cb in range(6):
                    c0 = half * 3072 + cb * 512
                    s.op("dve", lambda e: e.tensor_tensor(out=modrow[:, c0:c0 + 512], in0=psb[cb][0:2, :],
                                                          in1=abrow[:, c0:c0 + 512], op=ALU.add),
                         rd(psb[cb], abrow), rd(modrow))
            s.dma("sp", MODD[l], modrow[:], rd(modrow), rd(MODD))
        if dbg_stage == "mod" and l == 0:
            break

        s.barrier()
        with ExitStack() as st:
            hT = kb.sb(st, [128, 8, NT], BF16, "hT")
            modT = kb.sb(st, [128, 48, 2], F32, "modT")
            for r in range(2):
                s.dma("sp", modT[:, :, r], MODD[l, r].rearrange("(c p) -> p c", p=128), rd(MODD), rd(modT),
                      allow_slow_non_contiguous=True)
            sc1p = kb.sb(st, [128, 8, 2], F32, "sc1p")
            s.op("dve", lambda e: e.tensor_scalar_add(out=sc1p[:], in0=modT[:, 8:16, :], scalar1=1.0), rd(modT), rd(sc1p))
            xin = [kb.sb(st, [128, D], F32, "xin%d" % i) for i in range(2)]
            xn = [kb.sb(st, [128, D], F32, "xn%d" % i) for i in range(2)]
            stats = [kb.sb(st, [128, 2, 6], F32, "bst%d" % i) for i in range(2)]
            mv = [kb.sb(st, [128, 2], F32, "mv%d" % i) for i in range(2)]
            rstd = [kb.sb(st, [128, 1], F32, "rstd%d" % i) for i in range(2)]
            src = x_in if l == 0 else X2
            for t in range(NTT):
                i = t % 2
                r = 0 if t < L // 128 else 1
                s.dma("sp", xin[i][:], src[t * 128:(t + 1) * 128, :], rd(src), rd(xin[i]))
                if l == 0:
                    for j in range(2):
                        s.op("dve", lambda e: e.bn_stats(out=stats[i][:, j, :], in_=xin[i][:, j * 512:(j + 1) * 512]),
                             rd(xin[i]), rd(stats[i]))
                    s.op("dve", lambda e: e.bn_aggr(out=mv[i][:], in_=stats[i][:].rearrange("p a b -> p (a b)")),
                         rd(stats[i]), rd(mv[i]))
                    s.op("dve", lambda e: e.tensor_scalar_add(out=rstd[i][:], in0=mv[i][:, 1:2], scalar1=LN_EPS),
                         rd(mv[i]), rd(rstd[i]))
                    s.op("act", lambda e: e.sqrt(out=rstd[i][:], in_=rstd[i][:]), rd(rstd[i]), rd(rstd[i]))
                    s.op("dve", lambda e: e.reciprocal(out=rstd[i][:], in_=rstd[i][:]), rd(rstd[i]), rd(rstd[i]))
                    s.op("dve", lambda e: e.tensor_scalar(out=xn[i][:], in0=xin[i][:], scalar1=mv[i][:, 0:1],
                                                          scalar2=rstd[i][:, 0:1], op0=ALU.subtract, op1=ALU.mult),
                         rd(xin[i], mv[i], rstd[i]), rd(xn[i]))
                    s.dma("pool", XS[t * 128:(t + 1) * 128, :], xn[i][:], rd(xn[i]), rd(XS))
                    xs_t = xn[i]
                else:
                    xs_t = xin[i]
                for kc in range(8):
                    pb = psb[kc // 4]
                    s.op("pe", lambda e: e.transpose(out=pb[:, (kc % 4) * 128:(kc % 4 + 1) * 128],
                                                     in_=xs_t[:, kc * 128:(kc + 1) * 128], identity=ident[:]),
                         rd(xs_t, ident), rd(pb))
                for kc in range(8):
                    pb = psb[kc // 4]
                    eng = "act" if kc % 2 == 0 else "dve"
                    if eng == "act":
                        s.op("act", lambda e: e.activation(out=hT[:, kc, t * 128:(t + 1) * 128],
                                                           in_=pb[:, (kc % 4) * 128:(kc % 4 + 1) * 128],
                                                           func=AF.Identity, scale=sc1p[:, kc, r:r + 1],
                                                           bias=modT[:, kc, r:r + 1]),
                             rd(pb, sc1p, modT), rd(hT))
                    else:
                        s.op("dve", lambda e: e.tensor_scalar(out=hT[:, kc, t * 128:(t + 1) * 128],
                                                              in0=pb[:, (kc % 4) * 128:(kc % 4 + 1) * 128],
                                                              scalar1=sc1p[:, kc, r:r + 1], scalar2=modT[:, kc, r:r + 1],
                                                              op0=ALU.mult, op1=ALU.add),
                             rd(pb, sc1p, modT), rd(hT))
            wbf = kb.sb(st, [128, 8, N_IN], BF16, "wbf")
            for kc in range(8):
                for (c0, c1) in ((0, 1536), (1536, N_IN)):
                    s.dma("pool", wbf[:, kc, c0:c1], w_in[l, kc * 128:(kc + 1) * 128, c0:c1], rd(w_in), rd(wbf))
            ev = [kb.sb(st, [128, 512], F32, "ev%d" % i) for i in range(4)]
            n = 0
            for cc in range(12):
                for tt in range(0, NT, 512):
                    w = min(512, NT - tt)
                    pb = psb[2 + n % 4]
                    for kc in range(8):
                        s.op("pe", lambda e: e.matmul(out=pb[:, 0:w], lhsT=wbf[:, kc, cc * 128:(cc + 1) * 128],
                                                      rhs=hT[:, kc, tt:tt + w], start=(kc == 0), stop=(kc == 7)),
                             rd(wbf, hT), rd(pb))
                    e_ = ev[n % 4]
                    if n % 2 == 0:
                        s.op("act", lambda e: e.copy(out=e_[:, 0:w], in_=pb[:, 0:w]), rd(pb), rd(e_))
                    else:
                        s.op("dve", lambda e: e.tensor_copy(out=e_[:, 0:w], in_=pb[:, 0:w]), rd(pb), rd(e_))
                    s.dma("sp", QKVT[cc * 128:(cc + 1) * 128, tt:tt + w], e_[:, 0:w], rd(e_), rd(QKVT))
                    n += 1
            for t in range(NTT):
                for c0 in range(OFF_Z, N_IN, 512):
                    w = min(512, N_IN - c0)
                    pb = psb[2 + n % 4]
                    for kc in range(8):
                        s.op("pe", lambda e: e.matmul(out=pb[:, 0:w], lhsT=hT[:, kc, t * 128:(t + 1) * 128],
                                                      rhs=wbf[:, kc, c0:c0 + w], start=(kc == 0), stop=(kc == 7)),
                             rd(wbf, hT), rd(pb))
                    e_ = ev[n % 4]
                    if n % 2 == 0:
                        s.op("act", lambda e: e.copy(out=e_[:, 0:w], in_=pb[:, 0:w]), rd(pb), rd(e_))
                    else:
                        s.op("dve", lambda e: e.tensor_copy(out=e_[:, 0:w], in_=pb[:, 0:w]), rd(pb), rd(e_))
                    s.dma("sp", PTOK[t * 128:(t + 1) * 128, c0 - OFF_Z:c0 - OFF_Z + w], e_[:, 0:w], rd(e_), rd(PTOK))
                    n += 1
        if dbg_stage == "A":
            break
        s.barrier()
        C = NS()
        C.bcrow = bcrow
        C.kb = kb; C.ident = ident; C.identb = identb; C.ones = ones; C.masks = masks; C.psb = psb
        C.conv_w = conv_w; C.a_log = a_log; C.dt_bias = dt_bias; C.gdn_norm_w = gdn_norm_w
        C.QKVT = QKVT; C.PTOK = PTOK; C.OPSD = OPSD; C.OFB = OFB; C.MIXT = MIXT; C.dbg_stage = dbg_stage
        C.cut = int(os.environ.get("GCUT", "99"))
        if dbg_stage == "gdn":
            C.o_gdn = dbg_out("o_gdn", [NT, 512])
        if dbg_stage in ("gdnA", "gdnB"):
            C.o_dbg = dbg_out("o_dbg", [128, 1024])
        if dbg_stage == "gdnB":
            C.o_dbgB = dbg_out("o_dbgB", [4, 128, OPW])
        try:
            stage_gdn(C, l)
        except Cut:
            pass
        s.barrier()
        if dbg_stage in ("gdn", "gdnprep", "gdnscan", "gdnA", "gdnB"):
            break
        C.q_norm_w = q_norm_w; C.k_norm_w = k_norm_w; C.rope_in = rope_in
        if dbg_stage == "attq":
            C.o_dbg = dbg_out("o_dbg", [128, 1024])
        stage_att(C, l)
        s.barrier()
        if dbg_stage in ("att", "attq"):
            break
        C.w_out = w_out; C.ln1_w = ln1_w; C.ln1_b = ln1_b; C.ln2_w = ln2_w; C.ln2_b = ln2_b
        C.router_w = router_w; C.router_bias = router_bias; C.sh_w_gate = sh_w_gate; C.sh_w_up = sh_w_up
        C.sh_w_down = sh_w_down; C.MODD = MODD; C.XS = XS; C.X1 = X1; C.X2 = X2; C.H2 = H2; C.SHO = SHO
        if dbg_stage == "D":
            C.o_y = dbg_out("o_y", [NT, D])
        with ExitStack() as mst:
            M = NS()
            M.WFULL = kb.sb(mst, [128, NTT, 256], F32, "WFULL")
            stage_D(C, l, M)
            s.barrier()
            if dbg_stage != "D":
                C.iotas = iotas; C.WG = WG; C.WU = WU; C.WD = WD; C.BUFTW = BUFTW; C.EO = EO; C.ones256 = ones256
                C.out = final_out
                if dbg_stage == "E1":
                    C.o_e1 = dbg_out("o_e1", [128, 3000])
                if dbg_stage == "E":
                    C.o_ff = dbg_out("o_ff", [NT, D])
                stage_E(C, l, M)
                s.barrier()
            if dbg_stage == "D":
                o2 = dbg_out("o_wfull", [128, NTT, 256])
                s.dma("sp", o2[:], M.WFULL[:], rd(M.WFULL), rd(o2))
                C.dfin = [o2]
        if dbg_stage in ("D", "E1", "E2", "E2s", "E"):
            break

    finals = []
    if dbg_stage == "mod":
        o = dbg_out("o_mod", [2, 6 * D])
        with ExitStack() as st:
            tmp = kb.sb(st, [2, 6 * D], F32, "dbgm")
            s.dma("sp", tmp[:], MODD[0], rd(MODD), rd(tmp))
            s.dma("sp", o[:], tmp[:], rd(tmp), rd(o))
        finals.append(o)
    if dbg_stage == "A":
        o1 = dbg_out("o_qkvt", [OFF_Z, NT])
        o2 = dbg_out("o_ptok", [NT, NTOKC])
        o3 = dbg_out("o_xs", [NT, D])
        s.dma("sp", o1[:], QKVT[:], rd(QKVT), rd(o1))
        s.dma("sp", o2[:], PTOK[:], rd(PTOK), rd(o2))
        s.dma("sp", o3[:], XS[:], rd(XS), rd(o3))
        finals += [o1, o2, o3]
    if dbg_stage is None:
        finals.append(final_out)
    if dbg_stage == "E1":
        finals.append(outs["o_e1"])
    if dbg_stage in ("E2", "E2s"):
        o1 = dbg_out("o_eo", [2048, D], BF16)
        s.dma("sp", o1[:], EO[0:2048, :], rd(EO), rd(o1))
        finals.append(o1)
    if dbg_stage == "E":
        o1 = dbg_out("o_x2", [NT, D])
        s.dma("sp", o1[:], X2[:], rd(X2), rd(o1))
        finals += [o1, outs["o_ff"]]
    if dbg_stage in ("gdn", "gdnscan"):
        o1 = dbg_out("o_of", [NT, 512])
        o2 = dbg_out("o_ob", [NT, 512])
        s.dma("sp", o1[:], OFB[0][:], rd(OFB[0]), rd(o1))
        s.dma("sp", o2[:], OFB[1][:], rd(OFB[1]), rd(o2))
        finals += [o1, o2]
        if dbg_stage == "gdn":
            finals.append(outs["o_gdn"])
    if dbg_stage in ("gdnA", "attq"):
        finals.append(outs["o_dbg"])
    if dbg_stage == "D":
        finals += C.dfin + [outs["o_y"]]
        for nm, tsrc in (("o_x1", X1), ("o_h2", H2), ("o_sho", SHO)):
            o1 = dbg_out(nm, [NT, D])
            s.dma("sp", o1[:], tsrc[:], rd(tsrc), rd(o1))
            finals.append(o1)
    if dbg_stage == "att":
        o1 = dbg_out("o_mixt", [D, NT], BF16)
        s.dma("sp", o1[:], MIXT[:], rd(MIXT), rd(o1))
        finals.append(o1)
    if dbg_stage == "gdnB":
        finals.append(outs["o_dbgB"])
    if dbg_stage == "gdnprep":
        o1 = dbg_out("o_ops", [4, 2, NTT, 128, OPW])
        s.dma("sp", o1[:], OPSD[:], rd(OPSD), rd(o1))
        finals.append(o1)
    s.finish("sp", [f.res for f in finals])
    es.close()
    return nc, list(outs.keys())


_r = np.arange(128)
MASKS = np.stack([(_r[:, None] <= _r[None, :]), (_r[:, None] >= _r[None, :]),
                  (_r[:, None] > _r[None, :]), (_r[:, None] < _r[None, :])]).astype(np.float32)


def _rope_tables():
    t = np.arange(L)
    row = (t // 64).astype(np.float32)
    col = (t % 64).astype(np.float32)
    inv = (np.float32(10000.0) ** (-np.arange(0, 64, 2, dtype=np.float32) / np.float32(64))).astype(np.float32)
    ang = np.stack([row[:, None] * inv, col[:, None] * inv], axis=1).astype(np.float32)
    cs = np.stack([np.cos(ang), np.sin(ang)]).reshape(2, L, 64).astype(np.float32)
    return np.ascontiguousarray(np.tile(cs, (1, 1, 6)))


ROPE = _rope_tables()
IOTAS = np.concatenate([np.tile(np.arange(NBLK, dtype=np.float32), (128, 1)), np.arange(128, dtype=np.float32)[:, None],
                        (np.arange(NTT, dtype=np.float32)[None, :] * 128 + np.arange(128, dtype=np.float32)[:, None])], axis=1)


def expert_tables(inputs):
    n = TAB_LAYERS
    g = inputs["exp_w_gate"][:n]; u = inputs["exp_w_up"][:n]; d = inputs["exp_w_down"][:n]
    WG = np.ascontiguousarray(g.reshape(n, 256, 8, 128, 256).transpose(0, 1, 3, 2, 4)).reshape(n * 256 * 128, 2048)
    WU = np.ascontiguousarray(u.reshape(n, 256, 8, 128, 256).transpose(0, 1, 3, 2, 4)).reshape(n * 256 * 128, 2048)
    WD = np.ascontiguousarray(d.reshape(n, 256, 2, 128, 1024).transpose(0, 1, 3, 2, 4)).reshape(n * 256 * 128, 2048)
    return WG, WU, WD


def make_inputs(inputs, b, tabs=None):
    xx = np.concatenate([inputs["x"][b], inputs["ctx"][b]], axis=0)
    c2 = np.stack([inputs["c"][b], inputs["c_ctx"]], axis=0)
    m = {
        "x": np.ascontiguousarray(xx, dtype=np.float32),
        "c2": np.ascontiguousarray(c2, dtype=np.float32),
        "ada_w": inputs["ada_w"], "ada_b": inputs["ada_b"], "w_in": inputs["w_in"],
        "ident": np.eye(128, dtype=np.float32),
        "masks": MASKS,
        "conv_w": inputs["conv_w"], "gdn_a_log": inputs["gdn_a_log"].reshape(DEPTH, 8),
        "gdn_dt_bias": inputs["gdn_dt_bias"].reshape(DEPTH, 8), "gdn_norm_w": inputs["gdn_norm_w"],
        "q_norm_w": inputs["q_norm_w"], "k_norm_w": inputs["k_norm_w"], "rope": ROPE,
        "w_out": inputs["w_out"], "ln1_w": inputs["ln1_w"], "ln1_b": inputs["ln1_b"],
        "ln2_w": inputs["ln2_w"], "ln2_b": inputs["ln2_b"], "router_w": inputs["router_w"],
        "router_bias": inputs["router_bias"], "sh_w_gate": inputs["sh_w_gate"], "sh_w_up": inputs["sh_w_up"],
        "sh_w_down": inputs["sh_w_down"],
        "iotas": IOTAS,
    }
    if tabs is not None:
        m["WG"], m["WU"], m["WD"] = tabs
    return m


def kernel(**inputs):
    nc, onames = build()
    tabs = expert_tables(inputs)
    in_maps = [make_inputs(inputs, b, tabs) for b in range(8)]
    res = run_bass_kernel_spmd(nc, in_maps, core_ids=list(range(8)))
    return np.stack([r["out"] for r in res.results], axis=0)
```

```python
import os
from contextlib import ExitStack
import numpy as np
import concourse.bass as bass
import concourse.mybir as mybir
from concourse.bass_utils import run_bass_kernel_spmd

F32 = mybir.dt.float32
BF16 = mybir.dt.bfloat16
U32 = mybir.dt.uint32
I32 = mybir.dt.int32
AF = mybir.ActivationFunctionType
ALU = mybir.AluOpType
AX = mybir.AxisListType

D = 1024
L = 4096
LC = 256
NT = L + LC
NTT = NT // 128
DEPTH = 2
N_IN = 3088
OFF_Z = 1536
OFF_BA = 2048
OFF_ATT = 2064
NTOKC = N_IN - OFF_Z
ALPHA = (2.0 * DEPTH) ** 0.25
LN_EPS = 1e-5
RMS_EPS = 1e-6
OPW = 5 * 128 + 8


class Res:
    __slots__ = ("name", "w", "r", "excl", "wa", "wx")

    def __init__(self, name=""):
        self.name = name
        self.w = None
        self.r = {}
        self.excl = False
        self.wa = {}
        self.wx = None


class Sched:
    NDS = 8

    def __init__(self, nc, es):
        self.nc = nc
        self.engs = {"pe": nc.tensor, "dve": nc.vector, "act": nc.scalar, "pool": nc.gpsimd, "sp": nc.sync}
        self.csem = {k: es.enter_context(nc.semaphore("c_" + k)) for k in ("pe", "dve", "act", "pool")}
        self.ccnt = {k: 0 for k in self.csem}
        self.dsem = {q: [es.enter_context(nc.semaphore("d_%s%d" % (q, i))) for i in range(self.NDS)]
                     for q in ("sp", "act", "pool")}
        self.dcnt = {q: [0] * self.NDS for q in self.dsem}
        self.drr = {q: 0 for q in self.dsem}
        self.seen = {e: {} for e in self.engs}
        self.ninst = 0

    def _sem(self, key):
        return self.csem[key[1]] if key[0] == "c" else self.dsem[key[1]][key[2]]

    def _wait(self, eng, deps):
        seen = self.seen[eng]
        for key, val in sorted(deps.items(), key=lambda kv: str(kv[0])):
            if eng == "pe" and key == ("c", "pe"):
                continue
            if seen.get(key, 0) >= val:
                continue
            self.engs[eng].wait_ge(self._sem(key), val)
            seen[key] = val

    @staticmethod
    def _deps(reads, writes, nowaw=False):
        deps = {}

        def add(ev):
            if ev is not None and deps.get(ev[0], 0) < ev[1]:
                deps[ev[0]] = ev[1]
        for r in reads:
            add(r.wx)
            for k, v in r.wa.items():
                add((k, v))
            if r.excl:
                for k, v in r.r.items():
                    add((k, v))
        for w in writes:
            add(w.wx)
            if not nowaw or w.excl:
                for k, v in w.wa.items():
                    add((k, v))
            for k, v in w.r.items():
                add((k, v))
        return deps

    @staticmethod
    def _mark(ev, reads, writes, nowaw=False):
        for r in reads:
            if r.r.get(ev[0], 0) < ev[1]:
                r.r[ev[0]] = ev[1]
        for w in writes:
            w.w = ev
            if nowaw and not w.excl:
                if w.wa.get(ev[0], 0) < ev[1]:
                    w.wa[ev[0]] = ev[1]
            else:
                w.wx = ev
                w.wa = {}
                w.r = {}

    def op(self, eng, fn, reads=(), writes=(), nowaw=False):
        self._wait(eng, self._deps(reads, writes, nowaw))
        inst = fn(self.engs[eng])
        self.ccnt[eng] += 1
        inst.then_inc(self.csem[eng], 1)
        ev = (("c", eng), self.ccnt[eng])
        self._mark(ev, reads, writes, nowaw)
        self.ninst += 1
        return ev

    def dma(self, q, out, in_, reads=(), writes=(), nowaw=False, **kw):
        self._wait(q, self._deps(reads, writes, nowaw))
        i = self.drr[q]
        self.drr[q] = (i + 1) % self.NDS
        inst = self.engs[q].dma_start(out=out, in_=in_, **kw)
        self.dcnt[q][i] += 16
        inst.then_inc(self.dsem[q][i], 16)
        ev = (("d", q, i), self.dcnt[q][i])
        self._mark(ev, reads, writes, nowaw)
        self.ninst += 1
        return ev

    def idma(self, out, in_, out_off=None, in_off=None, reads=(), writes=(), nowaw=False, **kw):
        q = "pool"
        self._wait(q, self._deps(reads, writes, nowaw))
        i = self.drr[q]
        self.drr[q] = (i + 1) % self.NDS
        inst = self.engs[q].indirect_dma_start(
            out=out, out_offset=(bass.IndirectOffsetOnAxis(ap=out_off, axis=0) if out_off is not None else None),
            in_=in_, in_offset=(bass.IndirectOffsetOnAxis(ap=in_off, axis=0) if in_off is not None else None), **kw)
        self.dcnt[q][i] += 16
        inst.then_inc(self.dsem[q][i], 16)
        ev = (("d", q, i), self.dcnt[q][i])
        self._mark(ev, reads, writes, nowaw)
        self.ninst += 1
        return ev

    def barrier(self):
        deps = {}
        for k, v in self.ccnt.items():
            if v:
                deps[("c", k)] = v
        for q in self.dcnt:
            for i, v in enumerate(self.dcnt[q]):
                if v:
                    deps[("d", q, i)] = v
        for eng in self.engs:
            d2 = dict(deps)
            self._wait_all(eng, d2)

    def _wait_all(self, eng, deps):
        seen = self.seen[eng]
        for key, val in sorted(deps.items(), key=lambda kv: str(kv[0])):
            if key == ("c", eng):
                continue
            if seen.get(key, 0) >= val:
                continue
            self.engs[eng].wait_ge(self._sem(key), val)
            seen[key] = val

    def finish(self, eng, resources):
        deps = {}
        for r in resources:
            evs = list(r.wa.items()) + ([r.wx] if r.wx is not None else [])
            for k, v in evs:
                if deps.get(k, 0) < v:
                    deps[k] = v
        self._wait(eng, deps)


class T:
    def __init__(self, t, name=""):
        self.t = t
        self.res = Res(name)

    def __getitem__(self, k):
        return self.t[k]


class KB:
    def __init__(self, nc, es, dbg=None):
        self.nc = nc
        self.es = es
        self.s = Sched(nc, es)
        self.dbg = dbg
        self.n = 0

    def sb(self, es, shape, dt, name=None):
        self.n += 1
        name = "%s_%d" % (name or "sb", self.n)
        return T(es.enter_context(self.nc.sbuf_tensor(name, list(shape), dt)), name)

    def ps(self, es, shape, dt=F32, name=None):
        self.n += 1
        name = "%s_%d" % (name or "ps", self.n)
        return T(es.enter_context(self.nc.psum_tensor(name, list(shape), dt)), name)

    def dram(self, name, shape, dt, kind="Internal"):
        return T(self.nc.dram_tensor(name, list(shape), dt, kind=kind).ap(), name)


def rd(*ts):
    return [t.res for t in ts]


class NS:
    pass


class Cut(Exception):
    pass


def cutpt(C, k):
    return C.cut == k


def sub(bank, c0, c1, name=""):
    t = T(bank.t[:, c0:c1], name)
    t.res = bank.res
    bank.res.excl = True
    return t


def bcast_row(C, st, dst_ap, dst_t, src_ap, src_t, n, pbank):
    kb, s = C.kb, C.kb.s
    row = C.bcrow
    s.dma("sp", row[0:1, 0:n], src_ap, rd(src_t), rd(row))
    for c0 in range(0, n, 512):
        w = min(512, n - c0)
        s.op("pe", lambda e: e.matmul(out=pbank[:, 0:w], lhsT=C.ones[:], rhs=row[:, c0:c0 + w], start=True, stop=True),
             rd(C.ones, row), rd(pbank))
        s.op("dve", lambda e: e.tensor_copy(out=dst_ap[:, c0:c0 + w], in_=pbank[:, 0:w]), rd(pbank), rd(dst_t))


def stage_gdn(C, l):
    kb, s = C.kb, C.kb.s
    ident, ones, masks, psb = C.ident, C.ones, C.masks, C.psb
    CUMS = (0, 1)
    STRICT = (2, 3)
    INCLT = (0, 1)
    with ExitStack() as st:
        st.enter_context(C.kb.nc.named_scope("gdnprep_l%d" % l))
        convw = kb.sb(st, [128, 12, 5], F32, "convw")
        for j in range(5):
            s.dma("sp", convw[:, :, j], C.conv_w[l, j].rearrange("(c p) -> p c", p=128), rd(C.conv_w), rd(convw),
                  allow_slow_non_contiguous=True)
        if C.cut == 1:
            s.dma("sp", C.o_dbg[:, 0:128], ones[:], rd(ones), rd(C.o_dbg))
            return
        dtb8 = kb.sb(st, [128, 8], F32, "dtb8")
        bcast_row(C, st, dtb8[:], dtb8, C.dt_bias[l:l + 1, :], C.dt_bias, 8, psb[0])
        negA8 = kb.sb(st, [128, 8], F32, "negA8")
        bcast_row(C, st, negA8[:], negA8, C.a_log[l:l + 1, :], C.a_log, 8, psb[0])
        s.op("act", lambda e: e.activation(out=negA8[:], in_=negA8[:], func=AF.Exp), rd(negA8), rd(negA8))
        s.op("dve", lambda e: e.tensor_scalar(out=negA8[:], in0=negA8[:], scalar1=-1.0, scalar2=None, op0=ALU.mult),
             rd(negA8), rd(negA8))
        if C.cut == 2:
            s.dma("sp", C.o_dbg[:, 0:128], ones[:], rd(ones), rd(C.o_dbg))
            return
        BETA = kb.sb(st, [128, NTT, 8], F32, "BETA")
        NBETA = kb.sb(st, [128, NTT, 8], F32, "NBETA")
        GC = kb.sb(st, [128, NTT, 8], F32, "GC")
        EG = kb.sb(st, [128, NTT, 8], F32, "EG")
        EGR = kb.sb(st, [128, NTT, 8], F32, "EGR")
        CD = kb.sb(st, [128, NTT, 8], F32, "CD")
        BEG = kb.sb(st, [128, NTT, 8], F32, "BEG")
        ba = kb.sb(st, [128, NTT, 16], F32, "ba")
        s.dma("sp", ba[:], C.PTOK.t[:, 512:528].rearrange("(n p) c -> p n c", p=128), rd(C.PTOK), rd(ba))
        s.op("act", lambda e: e.activation(out=BETA[:], in_=ba[:, :, 0:8], func=AF.Sigmoid), rd(ba), rd(BETA))
        s.op("dve", lambda e: e.tensor_scalar(out=NBETA[:], in0=BETA[:], scalar1=-1.0, scalar2=None, op0=ALU.mult),
             rd(BETA), rd(NBETA))
        if C.cut == 3:
            s.dma("sp", C.o_dbg[:, 0:128], ones[:], rd(ones), rd(C.o_dbg))
            return
        xg = kb.sb(st, [128, NTT, 8], F32, "xg")
        ag = kb.sb(st, [128, NTT, 8], F32, "ag")
        gg = kb.sb(st, [128, NTT, 8], F32, "gg")
        for n in range(NTT):
            s.op("dve", lambda e: e.tensor_tensor(out=xg[:, n, :], in0=ba[:, n, 8:16], in1=dtb8[:], op=ALU.add),
                 rd(ba, dtb8), rd(xg))
        s.op("act", lambda e: e.activation(out=ag[:], in_=xg[:], func=AF.Abs), rd(xg), rd(ag))
        s.op("act", lambda e: e.activation(out=ag[:], in_=ag[:], func=AF.Exp, scale=-1.0), rd(ag), rd(ag))
        s.op("dve", lambda e: e.tensor_scalar_add(out=ag[:], in0=ag[:], scalar1=1.0), rd(ag), rd(ag))
        s.op("act", lambda e: e.activation(out=ag[:], in_=ag[:], func=AF.Ln), rd(ag), rd(ag))
        s.op("dve", lambda e: e.tensor_scalar(out=xg[:], in0=xg[:], scalar1=0.0, scalar2=None, op0=ALU.max),
             rd(xg), rd(xg))
        s.op("dve", lambda e: e.tensor_tensor(out=gg[:], in0=xg[:], in1=ag[:], op=ALU.add), rd(xg, ag), rd(gg))
        for n in range(NTT):
            s.op("dve", lambda e: e.tensor_tensor(out=gg[:, n, :], in0=gg[:, n, :], in1=negA8[:], op=ALU.mult),
                 rd(gg, negA8), rd(gg))
        if C.cut == 4:
            s.dma("sp", C.o_dbg[:, 0:128], ones[:], rd(ones), rd(C.o_dbg))
            return
        pG = sub(psb[0], 0, NTT * 8, "pG")
        pGt = sub(psb[1], 0, NTT * 8, "pGt")
        ggv = gg[:].rearrange("p n c -> p (n c)")
        for n in range(NTT):
            for d in range(2):
                s.op("pe", lambda e: e.matmul(out=pG[:, n * 8 + d * 4:n * 8 + d * 4 + 4], lhsT=masks[:, CUMS[d], :],
                                              rhs=gg[:, n, d * 4:d * 4 + 4], start=True, stop=True),
                     rd(masks, gg), rd(pG))
        for c0 in range(0, NTT * 8, 136):
            s.op("pe", lambda e: e.matmul(out=pGt[:, c0:c0 + 136], lhsT=ones[:], rhs=ggv[:, c0:c0 + 136],
                                          start=True, stop=True), rd(ones, gg), rd(pGt))
        if C.cut == 5:
            s.dma("sp", C.o_dbg[:, 0:128], ones[:], rd(ones), rd(C.o_dbg))
            return
        GCv = GC[:].rearrange("p n c -> p (n c)")
        s.op("dve", lambda e: e.tensor_copy(out=GCv, in_=pG[:, :]), rd(pG), rd(GC))
        if C.cut == 6:
            s.dma("sp", C.o_dbg[:, 0:272], GC[:].rearrange("p n c -> p (n c)"), rd(GC), rd(C.o_dbg))
            s.dma("sp", C.o_dbg[:, 272:544], gg[:].rearrange("p n c -> p (n c)"), rd(gg), rd(C.o_dbg))
            s.dma("sp", C.o_dbg[:, 544:816], BETA[:].rearrange("p n c -> p (n c)"), rd(BETA), rd(C.o_dbg))
            return
        if os.environ.get("GSKIP") != "EG":
            if os.environ.get("GEG") == "psum":
                s.op("act", lambda e: e.activation(out=EG[:].rearrange("p n c -> p (n c)"), in_=pG[:, :], func=AF.Exp),
                     rd(pG), rd(EG))
            else:
                s.op("act", lambda e: e.activation(out=EG[:], in_=GC[:], func=AF.Exp), rd(GC), rd(EG))
        if os.environ.get("GSKIP") != "CD":
            s.op("act", lambda e: e.activation(out=CD[:].rearrange("p n c -> p (n c)"), in_=pGt[:, :], func=AF.Exp),
                 rd(pGt), rd(CD))
        if C.cut == 7:
            s.dma("sp", C.o_dbg[:, 0:128], ones[:], rd(ones), rd(C.o_dbg))
            return
        s.op("dve", lambda e: e.tensor_tensor(out=EGR[:].rearrange("p n c -> p (n c)"), in0=pGt[:, :], in1=GCv,
                                              op=ALU.subtract), rd(pGt, GC), rd(EGR))
        s.op("act", lambda e: e.activation(out=EGR[:], in_=EGR[:], func=AF.Exp), rd(EGR), rd(EGR))
        if C.cut == 8:
            s.dma("sp", C.o_dbg[:, 0:128], ones[:], rd(ones), rd(C.o_dbg))
            return
        s.op("dve", lambda e: e.tensor_tensor(out=BEG[:], in0=BETA[:], in1=EG[:], op=ALU.mult), rd(BETA, EG), rd(BEG))
        if C.cut == 9:
            s.dma("sp", C.o_dbg[:, 0:128], ones[:], rd(ones), rd(C.o_dbg))
            return

        if C.dbg_stage == "gdnA":
            s.dma("sp", C.o_dbg[:, 0:272], GC[:].rearrange("p n c -> p (n c)"), rd(GC), rd(C.o_dbg))
            s.dma("sp", C.o_dbg[:, 272:544], BEG[:].rearrange("p n c -> p (n c)"), rd(BEG), rd(C.o_dbg))
            s.dma("sp", C.o_dbg[:, 544:816], EGR[:].rearrange("p n c -> p (n c)"), rd(EGR), rd(C.o_dbg))
            return
        W = NT + 4
        xpad = [kb.sb(st, [128, NT + 8], F32, "xpad%d" % i) for i in range(2)]
        for xp in xpad:
            s.op("pool", lambda e: e.memset(xp[:], 0.0), (), rd(xp))
        cs = [kb.sb(st, [128, W], F32, "cs%d" % i) for i in range(3)]
        acc = kb.sb(st, [128, W], F32, "cacc")
        def mk(name, *dims):
            def rec(pref, ds):
                if not ds:
                    return kb.sb(st, [128, 128], F32, pref)
                return [rec("%s_%d" % (pref, i), ds[1:]) for i in range(ds[0])]
            return rec(name, dims)
        vtok, ktok, qtok, kT, qT, junk = (mk(nm, 2) for nm in ("vtok", "ktok", "qtok", "kT", "qT", "gjunk"))
        ssq = [kb.sb(st, [128, 2], F32, "ssq%d" % i) for i in range(2)]
        kqT = [kb.sb(st, [128, 256], F32, "kqT%d" % i) for i in range(2)]
        diagG, Mm, t1, t2, Xa, Xb, XTa, XTb, TT, vb, kbg, qd = (mk(nm, 2, 2) for nm in (
            "diagG", "Mm", "t1", "t2", "Xa", "Xb", "XTa", "XTb", "TT", "vb", "kbg", "qd"))
        OPS = [[[kb.sb(st, [128, OPW], F32, "OPS%d%d%d" % (sl, d, i)) for i in range(2)] for d in range(2)] for sl in range(2)]
        opar = [[0, 0], [0, 0]]

        def chain(h, n, sl, d, pkkq):
            col = d * 4 + h
            bank = psb[3 * sl + 1 + d]
            r0, r1, r2, r3 = (sub(bank, i * 128, (i + 1) * 128) for i in range(4))
            ops = OPS[sl][d][opar[sl][d]]
            opar[sl][d] ^= 1
            gcol = GC[:, n, col:col + 1]
            dG, M_, T1, T2, TT_ = diagG[sl][d], Mm[sl][d], t1[sl][d], t2[sl][d], TT[sl][d]
            ktk, qtk, vtk = ktok[sl], qtok[sl], vtok[sl]
            s.op("act", lambda e: e.activation(out=dG[:], in_=ident[:], func=AF.Copy, scale=gcol), rd(ident, GC), rd(dG))
            yield
            s.op("pe", lambda e: e.matmul(out=r0[:, :], lhsT=ones[:], rhs=dG[:], start=True, stop=True), rd(ones, dG), rd(r0))
            yield
            s.op("dve", lambda e: e.scalar_tensor_tensor(out=T1[:], in0=r0[:, :], scalar=gcol, in1=masks[:, 4 + d, :],
                                                         op0=ALU.subtract, op1=ALU.max), rd(r0, GC, masks), rd(T1))
            s.op("dve", lambda e: e.scalar_tensor_tensor(out=T2[:], in0=r0[:, :], scalar=gcol, in1=masks[:, 6 + d, :],
                                                         op0=ALU.subtract, op1=ALU.min), rd(r0, GC, masks), rd(T2))
            yield
            s.op("act", lambda e: e.activation(out=T1[:], in_=T1[:], func=AF.Exp, scale=-1.0), rd(T1), rd(T1))
            s.op("act", lambda e: e.activation(out=T2[:], in_=T2[:], func=AF.Exp), rd(T2), rd(T2))
            yield
            Xc, XTc, Xn, XTn = Xa[sl][d], XTa[sl][d], Xb[sl][d], XTb[sl][d]
            s.op("dve", lambda e: e.scalar_tensor_tensor(out=fr(Xc[:]), in0=pkkq[:, 0:128], scalar=NBETA[:, n, col:col + 1], in1=T1[:],
                                                         op0=ALU.mult, op1=ALU.mult), rd(pkkq, NBETA, T1), rd(Xc))
            s.op("dve", lambda e: e.tensor_tensor(out=ops[:, 384:512], in0=pkkq[:, 128:256], in1=T2[:], op=ALU.mult), rd(pkkq, T2), rd(ops), nowaw=True)
            yield
            s.op("pe", lambda e: e.transpose(out=r3[:, :], in_=Xc[:], identity=ident[:]), rd(Xc, ident), rd(r3))
            yield
            s.op("act", lambda e: e.copy(out=fr(XTc[:]), in_=r3[:, :]), rd(r3), rd(XTc))
            s.op("dve", lambda e: e.tensor_tensor(out=fr(TT_[:]), in0=r3[:, :], in1=ident[:], op=ALU.add), rd(r3, ident), rd(TT_))
            yield
            for m in range(1, 7):
                s.op("pe", lambda e: e.matmul(out=r0[:, :], lhsT=fr(XTc[:]), rhs=fr(Xc[:]), start=True, stop=True), rd(XTc, Xc), rd(r0))
                if m < 6:
                    s.op("pe", lambda e: e.matmul(out=r1[:, :], lhsT=fr(Xc[:]), rhs=fr(XTc[:]), start=True, stop=True), rd(XTc, Xc), rd(r1))
                yield
                s.op("act", lambda e: e.copy(out=fr(Xn[:]), in_=r0[:, :]), rd(r0), rd(Xn))
                if m < 6:
                    s.op("dve", lambda e: e.tensor_copy(out=fr(XTn[:]), in_=r1[:, :]), rd(r1), rd(XTn))
                yield
                s.op("pe", lambda e: e.matmul(out=r2[:, :], lhsT=fr(Xn[:]), rhs=fr(TT_[:]), start=True, stop=True), rd(Xn, TT_), rd(r2))
                yield
                s.op("dve", lambda e: e.tensor_tensor(out=fr(TT_[:]), in0=TT_[:], in1=r2[:, :], op=ALU.add), rd(TT_, r2), rd(TT_))
                yield
                Xc, XTc, Xn, XTn = Xn, XTn, Xc, XTc
            vb_, kbg_, qd_ = vb[sl][d], kbg[sl][d], qd[sl][d]
            s.op("act", lambda e: e.activation(out=vb_[:], in_=vtk[:], func=AF.Copy, scale=BETA[:, n, col:col + 1]), rd(vtk, BETA), rd(vb_))
            s.op("act", lambda e: e.activation(out=kbg_[:], in_=ktk[:], func=AF.Copy, scale=BEG[:, n, col:col + 1]), rd(ktk, BEG), rd(kbg_))
            s.op("dve", lambda e: e.tensor_scalar(out=qd_[:], in0=qtk[:], scalar1=EG[:, n, col:col + 1], scalar2=None, op0=ALU.mult), rd(qtk, EG), rd(qd_))
            s.op("pool", lambda e: e.tensor_scalar(out=ops[:, 512:640], in0=ktk[:], scalar1=EGR[:, n, col:col + 1], scalar2=0.0, op0=ALU.mult, op1=ALU.add),
                 rd(ktk, EGR), rd(ops), nowaw=True)
            s.op("pool", lambda e: e.tensor_copy(out=ops[:, 640:648], in_=CD[:, n, :]), rd(CD), rd(ops), nowaw=True)
            yield
            s.op("pe", lambda e: e.matmul(out=r3[:, :], lhsT=fr(kbg_[:]), rhs=fr(TT_[:]), start=True, stop=True), rd(kbg_, TT_), rd(r3))
            s.op("pe", lambda e: e.matmul(out=r0[:, :], lhsT=fr(TT_[:]), rhs=fr(vb_[:]), start=True, stop=True), rd(vb_, TT_), rd(r0))
            s.op("pe", lambda e: e.transpose(out=r1[:, :], in_=qd_[:], identity=ident[:]), rd(qd_, ident), rd(r1))
            yield
            s.op("act", lambda e: e.copy(out=ops[:, 0:128], in_=r3[:, :]), rd(r3), rd(ops), nowaw=True)
            s.op("dve", lambda e: e.tensor_copy(out=ops[:, 128:256], in_=r0[:, :]), rd(r0), rd(ops), nowaw=True)
            s.op("act", lambda e: e.copy(out=ops[:, 256:384], in_=r1[:, :]), rd(r1), rd(ops), nowaw=True)
            yield
            s.dma("sp", C.OPSD[h, d, n], ops[:], rd(ops), rd(C.OPSD), nowaw=True)
            if C.dbg_stage == "gdnB":
                s.dma("sp", C.o_dbgB[d], ops[:], rd(ops), rd(C.o_dbgB))
                s.dma("sp", C.o_dbgB[2 + d, :, 0:128], TT_[:], rd(TT_), rd(C.o_dbgB))
                s.dma("sp", C.o_dbgB[2 + d, :, 384:512], ktk[:], rd(ktk), rd(C.o_dbgB))

        def chunk(h, n, sl):
            col0 = n * 128 if n < L // 128 else L + 4 + (n - L // 128) * 128
            pQKV = sub(psb[3 * sl], 0, 384)
            bk0 = sub(psb[3 * sl + 1], 0, 128)
            bk1 = sub(psb[3 * sl + 2], 0, 128)
            for which in range(3):
                s.op("pe", lambda e: e.transpose(out=pQKV[:, which * 128:(which + 1) * 128], in_=cs[which][:, col0:col0 + 128],
                                                 identity=ident[:]), rd(cs[which], ident), rd(pQKV))
            yield
            s.op("act", lambda e: e.copy(out=vtok[sl][:], in_=pQKV[:, 256:384]), rd(pQKV), rd(vtok[sl]))
            for w_ in range(2):
                s.op("act", lambda e: e.activation(out=junk[sl][:], in_=pQKV[:, w_ * 128:(w_ + 1) * 128], func=AF.Square,
                                                   accum_out=ssq[sl][:, w_:w_ + 1]), rd(pQKV), rd(junk[sl], ssq[sl]))
            yield
            s.op("dve", lambda e: e.tensor_scalar_add(out=ssq[sl][:], in0=ssq[sl][:], scalar1=RMS_EPS), rd(ssq[sl]), rd(ssq[sl]))
            yield
            s.op("act", lambda e: e.sqrt(out=ssq[sl][:], in_=ssq[sl][:]), rd(ssq[sl]), rd(ssq[sl]))
            yield
            s.op("dve", lambda e: e.reciprocal(out=ssq[sl][:], in_=ssq[sl][:]), rd(ssq[sl]), rd(ssq[sl]))
            s.op("dve", lambda e: e.tensor_scalar(out=qtok[sl][:], in0=pQKV[:, 0:128], scalar1=ssq[sl][:, 0:1],
                                                  scalar2=128.0 ** -0.5, op0=ALU.mult, op1=ALU.mult), rd(pQKV, ssq[sl]), rd(qtok[sl]))
            s.op("dve", lambda e: e.tensor_scalar(out=ktok[sl][:], in0=pQKV[:, 128:256], scalar1=ssq[sl][:, 1:2],
                                                  scalar2=None, op0=ALU.mult), rd(pQKV, ssq[sl]), rd(ktok[sl]))
            yield
            s.op("pe", lambda e: e.transpose(out=bk0[:, :], in_=ktok[sl][:], identity=ident[:]), rd(ktok[sl], ident), rd(bk0))
            s.op("pe", lambda e: e.transpose(out=bk1[:, :], in_=qtok[sl][:], identity=ident[:]), rd(qtok[sl], ident), rd(bk1))
            yield
            s.op("act", lambda e: e.copy(out=kqT[sl][:, 0:128], in_=bk0[:, :]), rd(bk0), rd(kqT[sl]), nowaw=True)
            s.op("dve", lambda e: e.tensor_copy(out=kqT[sl][:, 128:256], in_=bk1[:, :]), rd(bk1), rd(kqT[sl]), nowaw=True)
            yield
            pkkq = sub(psb[3 * sl], 0, 256)
            s.op("pe", lambda e: e.matmul(out=pkkq[:, :], lhsT=kqT[sl][:, 0:128], rhs=kqT[sl][:, :], start=True, stop=True),
                 rd(kqT[sl]), rd(pkkq))
            yield
            gens = [chain(h, n, sl, 0, pkkq), chain(h, n, sl, 1, pkkq)]
            while gens:
                for g in list(gens):
                    try:
                        next(g)
                    except StopIteration:
                        gens.remove(g)
                yield

        nheads = 4 if C.dbg_stage != "gdnB" else 1
        for h in range(nheads):
            for which in range(3):
                cc = which * 4 + h
                xp = xpad[(h * 3 + which) % 2]
                s.dma("sp", xp[:, 2:2 + L], C.QKVT[cc * 128:(cc + 1) * 128, 0:L], rd(C.QKVT), rd(xp))
                s.dma("pool", xp[:, L + 6:L + 6 + LC], C.QKVT[cc * 128:(cc + 1) * 128, L:NT], rd(C.QKVT), rd(xp))
                s.op("dve", lambda e: e.tensor_scalar(out=acc[:], in0=xp[:, 0:W], scalar1=convw[:, cc, 0:1],
                                                      scalar2=None, op0=ALU.mult), rd(xp, convw), rd(acc))
                for j in range(1, 5):
                    s.op("dve", lambda e: e.scalar_tensor_tensor(out=acc[:], in0=xp[:, j:j + W],
                                                                 scalar=convw[:, cc, j:j + 1], in1=acc[:],
                                                                 op0=ALU.mult, op1=ALU.add), rd(xp, convw, acc), rd(acc))
                s.op("act", lambda e: e.activation(out=cs[which][:], in_=acc[:], func=AF.Silu), rd(acc), rd(cs[which]))
            pending = list(range(NTT if C.dbg_stage != "gdnB" else 2))
            active = {}
            while pending or active:
                for sl in (0, 1):
                    if sl not in active and pending:
                        active[sl] = chunk(h, pending.pop(0), sl)
                for sl in list(active):
                    try:
                        next(active[sl])
                    except StopIteration:
                        del active[sl]
    if C.dbg_stage in ("gdnprep", "gdnB"):
        return
    s.barrier()
    with ExitStack() as st:
        st.enter_context(C.kb.nc.named_scope("gdnscan_l%d" % l))
        S = [[kb.sb(st, [128, 128], F32, "S%d%d" % (h, d)) for d in range(2)] for h in range(4)]
        for h in range(4):
            for d in range(2):
                s.op("pool", lambda e: e.memset(S[h][d][:], 0.0), (), rd(S[h][d]))
        NOB = 16
        OB = [kb.sb(st, [128, OPW], F32, "OB%d" % i) for i in range(NOB)]
        vnew = [kb.sb(st, [128, 128], F32, "vnew%d" % i) for i in range(8)]
        oev = [kb.sb(st, [128, 128], F32, "oev%d" % i) for i in range(8)]
        order = [[32, 33] + list(range(32)), [33, 32] + list(range(31, -1, -1))]
        p1 = [sub(psb[h], d * 128, d * 128 + 128, "p1") for h in range(4) for d in range(2)]
        p2 = [sub(psb[h], 256 + d * 128, 384 + d * 128, "p2") for h in range(4) for d in range(2)]
        p3 = [sub(psb[4 + h], d * 128, d * 128 + 128, "p3") for h in range(4) for d in range(2)]
        k = 0
        for step in range(NTT):
            for d in range(2):
                n = order[d][step]
                for h in range(4):
                    c = h * 2 + d
                    ob = OB[k % NOB]
                    k += 1
                    s.dma("sp" if k % 2 == 0 else "pool", ob[:], C.OPSD[h, d, n], rd(C.OPSD), rd(ob))
                    Sd = S[h][d]
                    s.op("pe", lambda e: e.matmul(out=p1[c][:, :], lhsT=ob[:, 0:128], rhs=Sd[:], start=True, stop=True),
                         rd(ob, Sd), rd(p1[c]))
                    s.op("dve", lambda e: e.tensor_tensor(out=vnew[c][:], in0=ob[:, 128:256], in1=p1[c][:, :], op=ALU.subtract),
                         rd(ob, p1[c]), rd(vnew[c]))
                    s.op("pe", lambda e: e.matmul(out=p2[c][:, :], lhsT=ob[:, 256:384], rhs=Sd[:], start=True, stop=False),
                         rd(ob, Sd), rd(p2[c]))
                    s.op("pe", lambda e: e.matmul(out=p2[c][:, :], lhsT=ob[:, 384:512], rhs=vnew[c][:], start=False, stop=True),
                         rd(ob, vnew[c]), rd(p2[c]))
                    s.op("pe", lambda e: e.matmul(out=p3[c][:, :], lhsT=ob[:, 512:640], rhs=vnew[c][:], start=True, stop=True),
                         rd(ob, vnew[c]), rd(p3[c]))
                    s.op("act", lambda e: e.copy(out=oev[c][:], in_=p2[c][:, :]), rd(p2[c]), rd(oev[c]))
                    s.dma("sp", C.OFB[d][n * 128:(n + 1) * 128, h * 128:(h + 1) * 128], oev[c][:], rd(oev[c]), rd(C.OFB[d]), nowaw=True)
                    s.op("dve", lambda e: e.scalar_tensor_tensor(out=Sd[:], in0=Sd[:], scalar=ob[:, 640 + c_col(d, h):641 + c_col(d, h)],
                                                                 in1=p3[c][:, :], op0=ALU.mult, op1=ALU.add),
                         rd(Sd, ob, p3[c]), rd(Sd))
    if C.dbg_stage == "gdnscan":
        return
    s.barrier()
    with ExitStack() as st:
        st.enter_context(C.kb.nc.named_scope("gdnfin_l%d" % l))
        gnw = kb.sb(st, [128, 128], F32, "gnw")
        bcast_row(C, st, gnw[:], gnw, C.gdn_norm_w[l:l + 1, :], C.gdn_norm_w, 128, psb[2])
        NBF = 2
        of = [kb.sb(st, [128, 512], F32, "of%d" % i) for i in range(NBF)]
        obk = [kb.sb(st, [128, 512], F32, "obk%d" % i) for i in range(NBF)]
        zz = [kb.sb(st, [128, 512], F32, "zz%d" % i) for i in range(NBF)]
        yy = [kb.sb(st, [128, 512], F32, "yy%d" % i) for i in range(NBF)]
        sq = [kb.sb(st, [128, 4], F32, "sq%d" % i) for i in range(NBF)]
        junk = kb.sb(st, [128, 128], F32, "junk2")
        gT = [kb.sb(st, [128, 512], BF16, "gT%d" % i) for i in range(NBF)]
        for n in range(NTT):
            b = n % NBF
            pb = psb[n % 2]
            s.dma("sp", of[b][:], C.OFB[0][n * 128:(n + 1) * 128, :], rd(C.OFB[0]), rd(of[b]))
            s.dma("pool", obk[b][:], C.OFB[1][n * 128:(n + 1) * 128, :], rd(C.OFB[1]), rd(obk[b]))
            s.dma("sp", zz[b][:], C.PTOK[n * 128:(n + 1) * 128, 0:512], rd(C.PTOK), rd(zz[b]))
            s.op("dve", lambda e: e.tensor_tensor(out=of[b][:], in0=of[b][:], in1=obk[b][:], op=ALU.add), rd(of[b], obk[b]), rd(of[b]))
            s.op("act", lambda e: e.activation(out=zz[b][:], in_=zz[b][:], func=AF.Silu), rd(zz[b]), rd(zz[b]))
            for h in range(4):
                s.op("act", lambda e: e.activation(out=junk[:], in_=of[b][:, h * 128:(h + 1) * 128], func=AF.Square,
                                                   accum_out=sq[b][:, h:h + 1]), rd(of[b]), rd(junk, sq[b]))
            s.op("dve", lambda e: e.tensor_scalar(out=sq[b][:], in0=sq[b][:], scalar1=1.0 / 128.0, scalar2=RMS_EPS,
                                                  op0=ALU.mult, op1=ALU.add), rd(sq[b]), rd(sq[b]))
            s.op("act", lambda e: e.sqrt(out=sq[b][:], in_=sq[b][:]), rd(sq[b]), rd(sq[b]))
            s.op("dve", lambda e: e.reciprocal(out=sq[b][:], in_=sq[b][:]), rd(sq[b]), rd(sq[b]))
            for h in range(4):
                s.op("pool", lambda e: e.tensor_tensor(out=zz[b][:, h * 128:(h + 1) * 128], in0=zz[b][:, h * 128:(h + 1) * 128],
                                                       in1=gnw[:], op=ALU.mult), rd(zz[b], gnw), rd(zz[b]))
            for h in range(4):
                s.op("dve", lambda e: e.scalar_tensor_tensor(out=yy[b][:, h * 128:(h + 1) * 128], in0=of[b][:, h * 128:(h + 1) * 128],
                                                             scalar=sq[b][:, h:h + 1], in1=zz[b][:, h * 128:(h + 1) * 128],
                                                             op0=ALU.mult, op1=ALU.mult), rd(of[b], sq[b], zz[b]), rd(yy[b]))
            for h in range(4):
                s.op("pe", lambda e: e.transpose(out=pb[:, h * 128:(h + 1) * 128], in_=yy[b][:, h * 128:(h + 1) * 128],
                                                 identity=ident[:]), rd(yy[b], ident), rd(pb))
            s.op("act", lambda e: e.copy(out=gT[b][:], in_=pb[:, :]), rd(pb), rd(gT[b]))
            s.dma("sp", C.MIXT.t[0:512, n * 128:(n + 1) * 128].rearrange("(h p) t -> p h t", p=128),
                  gT[b][:].rearrange("p (h t) -> p h t", h=4), rd(gT[b]), rd(C.MIXT), nowaw=True)
            if C.dbg_stage == "gdn":
                s.dma("sp", C.o_gdn[n * 128:(n + 1) * 128, :], yy[b][:], rd(yy[b]), rd(C.o_gdn))


def stage_att(C, l):
    kb, s = C.kb, C.kb.s
    ident, psb = C.ident, C.psb
    with ExitStack() as st:
        st.enter_context(C.kb.nc.named_scope("att_l%d" % l))
        QT = kb.sb(st, [128, 4, NT], BF16, "QT")
        KT = kb.sb(st, [128, 2, NT], BF16, "KT")
        Vb = kb.sb(st, [128, NTT, 256], BF16, "Vb")
        onesb = kb.sb(st, [128, 128], BF16, "onesb")
        s.op("pool", lambda e: e.memset(onesb[:], 1.0), (), rd(onesb))
        w6 = kb.sb(st, [128, 2, 128], F32, "w6")
        bcast_row(C, st, w6[:, 0, :], w6, C.q_norm_w[l:l + 1, :], C.q_norm_w, 128, psb[0])
        bcast_row(C, st, w6[:, 1, :], w6, C.k_norm_w[l:l + 1, :], C.k_norm_w, 128, psb[0])
        NB = 2
        xa = [kb.sb(st, [128, 1024], F32, "xa%d" % i) for i in range(NB)]
        xn = [kb.sb(st, [128, 768], F32, "xnq%d" % i) for i in range(NB)]
        xr = [kb.sb(st, [128, 768], F32, "xr%d" % i) for i in range(NB)]
        cs_ = [kb.sb(st, [128, 2, 384], F32, "cs%d" % i) for i in range(NB)]
        tmp = [kb.sb(st, [128, 4, 384], F32, "rtmp%d" % i) for i in range(NB)]
        ssq = [kb.sb(st, [128, 6], F32, "assq%d" % i) for i in range(NB)]
        junk = kb.sb(st, [128, 128], F32, "ajunk")
        for t in range(NTT):
            b = t % NB
            lat = t < L // 128
            s.dma("sp", xa[b][:], C.PTOK[t * 128:(t + 1) * 128, 528:1552], rd(C.PTOK), rd(xa[b]))
            if lat:
                s.dma("pool", cs_[b][:], C.rope_in.t[:, t * 128:(t + 1) * 128, :].rearrange("c p f -> p c f"),
                      rd(C.rope_in), rd(cs_[b]))
            for h in range(6):
                s.op("act", lambda e: e.activation(out=junk[:], in_=xa[b][:, h * 128:(h + 1) * 128], func=AF.Square,
                                                   accum_out=ssq[b][:, h:h + 1]), rd(xa[b]), rd(junk, ssq[b]))
            s.op("dve", lambda e: e.tensor_scalar(out=ssq[b][:], in0=ssq[b][:], scalar1=1.0 / 128.0, scalar2=RMS_EPS,
                                                  op0=ALU.mult, op1=ALU.add), rd(ssq[b]), rd(ssq[b]))
            s.op("act", lambda e: e.sqrt(out=ssq[b][:], in_=ssq[b][:]), rd(ssq[b]), rd(ssq[b]))
            s.op("dve", lambda e: e.reciprocal(out=ssq[b][:], in_=ssq[b][:]), rd(ssq[b]), rd(ssq[b]))
            for h in range(6):
                s.op("dve", lambda e: e.scalar_tensor_tensor(out=xn[b][:, h * 128:(h + 1) * 128], in0=xa[b][:, h * 128:(h + 1) * 128],
                                                             scalar=ssq[b][:, h:h + 1], in1=w6[:, 0 if h < 4 else 1, :],
                                                             op0=ALU.mult, op1=ALU.mult), rd(xa[b], ssq[b], w6), rd(xn[b]))
            s.op("pool", lambda e: e.tensor_copy(out=Vb[:, t, :], in_=xa[b][:, 768:1024]), rd(xa[b]), rd(Vb), nowaw=True)
            if lat:
                x1 = xn[b][:].rearrange("p (g two f) -> p g two f", two=2, f=32)[:, :, 0, :]
                x2 = xn[b][:].rearrange("p (g two f) -> p g two f", two=2, f=32)[:, :, 1, :]
                o1 = xr[b][:].rearrange("p (g two f) -> p g two f", two=2, f=32)[:, :, 0, :]
                o2 = xr[b][:].rearrange("p (g two f) -> p g two f", two=2, f=32)[:, :, 1, :]
                cc = cs_[b][:, 0, :].rearrange("p (g f) -> p g f", f=32)
                sn = cs_[b][:, 1, :].rearrange("p (g f) -> p g f", f=32)
                tm = [tmp[b][:, i, :].rearrange("p (g f) -> p g f", f=32) for i in range(4)]
                s.op("dve", lambda e: e.tensor_tensor(out=tm[0], in0=x1, in1=cc, op=ALU.mult), rd(xn[b], cs_[b]), rd(tmp[b]))
                s.op("pool", lambda e: e.tensor_tensor(out=tm[1], in0=x2, in1=sn, op=ALU.mult), rd(xn[b], cs_[b]), rd(tmp[b]))
                s.op("dve", lambda e: e.tensor_tensor(out=tm[2], in0=x2, in1=cc, op=ALU.mult), rd(xn[b], cs_[b]), rd(tmp[b]))
                s.op("pool", lambda e: e.tensor_tensor(out=tm[3], in0=x1, in1=sn, op=ALU.mult), rd(xn[b], cs_[b]), rd(tmp[b]))
                s.op("dve", lambda e: e.tensor_tensor(out=o1, in0=tm[0], in1=tm[1], op=ALU.subtract), rd(tmp[b]), rd(xr[b]))
                s.op("pool", lambda e: e.tensor_tensor(out=o2, in0=tm[2], in1=tm[3], op=ALU.add), rd(tmp[b]), rd(xr[b]))
                src = xr[b]
            else:
                src = xn[b]
            for h in range(6):
                pb = psb[1] if h < 4 else psb[2]
                s.op("pe", lambda e: e.transpose(out=pb[:, (h % 4) * 128:(h % 4 + 1) * 128], in_=src[:, h * 128:(h + 1) * 128],
                                                 identity=ident[:]), rd(src, ident), rd(pb))
            s.op("act", lambda e: e.copy(out=QT[:, :, t * 128:(t + 1) * 128],
                                         in_=psb[1][:, :].rearrange("p (h t) -> p h t", h=4)), rd(psb[1]), rd(QT), nowaw=True)
            s.op("dve", lambda e: e.tensor_copy(out=KT[:, :, t * 128:(t + 1) * 128],
                                                in_=psb[2][:, 0:256].rearrange("p (h t) -> p h t", h=2)), rd(psb[2]), rd(KT), nowaw=True)
        if C.dbg_stage == "attq":
            s.dma("sp", C.o_dbg[:, 0:512].bitcast(BF16)[:, 0:512], QT[:, 1, 0:512], rd(QT), rd(C.o_dbg))
            s.dma("sp", C.o_dbg[:, 512:1024].bitcast(BF16)[:, 0:512], KT[:, 1, 0:512], rd(KT), rd(C.o_dbg))
            return
        PT = [kb.sb(st, [128, 512], BF16, "PT%d" % i) for i in range(3)]
        rec = [kb.sb(st, [128, 512], F32, "rec%d" % i) for i in range(2)]
        aT = [kb.sb(st, [128, 512], BF16, "aT%d" % i) for i in range(2)]
        jobs = []
        for h in range(4):
            for q0 in range(0, L, 512):
                jobs.append((h, q0, 512, list(range(NTT))))
            jobs.append((h, L, LC, [32, 33]))
        scale = 128.0 ** -0.5
        k = 0
        for ji, (h, q0, qw, ktiles) in enumerate(jobs):
            kv = h // 2
            pO = psb[0] if ji % 2 == 0 else psb[5]
            pS = psb[1] if ji % 2 == 0 else psb[6]
            nk = len(ktiles)
            slots = []
            for i in range(nk):
                slots.append((psb[2 + k % 3], PT[k % 3]))
                k += 1

            def qk(i):
                pst, _ = slots[i]
                kt = ktiles[i]
                s.op("pe", lambda e: e.matmul(out=pst[:, 0:qw], lhsT=KT[:, kv, kt * 128:(kt + 1) * 128], rhs=QT[:, h, q0:q0 + qw],
                                              start=True, stop=True), rd(KT, QT), rd(pst))
            qk(0)
            for i, kt in enumerate(ktiles):
                pst, pt = slots[i]
                if i + 1 < nk:
                    qk(i + 1)
                s.op("act", lambda e: e.activation(out=pt[:, 0:qw], in_=pst[:, 0:qw], func=AF.Exp, scale=scale), rd(pst), rd(pt))
                s.op("pe", lambda e: e.matmul(out=pO[:, 0:qw], lhsT=Vb[:, kt, kv * 128:(kv + 1) * 128], rhs=pt[:, 0:qw],
                                              start=(i == 0), stop=(i == nk - 1)), rd(Vb, pt), rd(pO))
                s.op("pe", lambda e: e.matmul(out=pS[:, 0:qw], lhsT=onesb[:], rhs=pt[:, 0:qw],
                                              start=(i == 0), stop=(i == nk - 1)), rd(onesb, pt), rd(pS))
            rc = rec[ji % 2]
            at = aT[ji % 2]
            s.op("dve", lambda e: e.reciprocal(out=rc[:, 0:qw], in_=pS[:, 0:qw]), rd(pS), rd(rc))
            s.op("dve", lambda e: e.tensor_tensor(out=at[:, 0:qw], in0=pO[:, 0:qw], in1=rc[:, 0:qw], op=ALU.mult), rd(pO, rc), rd(at))
            s.dma("sp", C.MIXT[512 + h * 128:512 + (h + 1) * 128, q0:q0 + qw], at[:, 0:qw], rd(at), rd(C.MIXT), nowaw=True)


def load_bf16_w(C, st, dram_t, src_ap_fn, kchunks, ncols, name):
    kb, s = C.kb, C.kb.s
    w = kb.sb(st, [128, kchunks, ncols], BF16, name)
    for kc in range(kchunks):
        for c0 in range(0, ncols, 2048):
            c1 = min(ncols, c0 + 2048)
            s.dma("pool", w[:, kc, c0:c1], src_ap_fn(kc, c0, c1), rd(dram_t), rd(w))
    return w


def stage_D(C, l, M):
    kb, s = C.kb, C.kb.s
    ident, psb = C.ident, C.psb
    Xsrc = C.XS if l == 0 else C.X2
    with ExitStack() as st:
        st.enter_context(C.kb.nc.named_scope("D_l%d" % l))
        wout = load_bf16_w(C, st, C.w_out, lambda kc, c0, c1: C.w_out[l, kc * 128:(kc + 1) * 128, c0:c1], 8, D, "wout")
        wsgu = kb.sb(st, [128, 8, 512], BF16, "wsgu")
        for kc in range(8):
            s.dma("pool", wsgu[:, kc, 0:256], C.sh_w_gate[l, kc * 128:(kc + 1) * 128, :], rd(C.sh_w_gate), rd(wsgu))
            s.dma("pool", wsgu[:, kc, 256:512], C.sh_w_up[l, kc * 128:(kc + 1) * 128, :], rd(C.sh_w_up), rd(wsgu))
        wsd = load_bf16_w(C, st, C.sh_w_down, lambda kc, c0, c1: C.sh_w_down[l, kc * 128:(kc + 1) * 128, c0:c1], 2, D, "wsd")
        rw = kb.sb(st, [128, 8, 256], F32, "rw")
        s.dma("sp", rw[:], C.router_w.t[l].rearrange("(k p) e -> p k e", p=128), rd(C.router_w), rd(rw))
        G1 = kb.sb(st, [128, 2, D], F32, "G1")
        SC2 = kb.sb(st, [128, 2, D], F32, "SC2")
        SH2 = kb.sb(st, [128, 2, D], F32, "SH2")
        for r in range(2):
            bcast_row(C, st, G1[:, r, :], G1, C.MODD[l, r:r + 1, 2048:3072], C.MODD, D, psb[0])
            bcast_row(C, st, SH2[:, r, :], SH2, C.MODD[l, r:r + 1, 3072:4096], C.MODD, D, psb[0])
            bcast_row(C, st, SC2[:, r, :], SC2, C.MODD[l, r:r + 1, 4096:5120], C.MODD, D, psb[0])
        s.op("dve", lambda e: e.tensor_scalar_add(out=SC2[:], in0=SC2[:], scalar1=1.0), rd(SC2), rd(SC2))
        LNW = kb.sb(st, [128, D], F32, "LNW")
        LNB = kb.sb(st, [128, D], F32, "LNB")
        bcast_row(C, st, LNW[:], LNW, C.ln1_w[l:l + 1, :], C.ln1_w, D, psb[0])
        bcast_row(C, st, LNB[:], LNB, C.ln1_b[l:l + 1, :], C.ln1_b, D, psb[0])
        RB = kb.sb(st, [128, 256], F32, "RB")
        bcast_row(C, st, RB[:], RB, C.router_bias[l:l + 1, :], C.router_bias, 256, psb[0])
        NB = 2
        mixT = [kb.sb(st, [128, 8, 128], BF16, "mixT%d" % i) for i in range(NB)]
        xt = [kb.sb(st, [128, D], F32, "xt%d" % i) for i in range(NB)]
        tt = [kb.sb(st, [128, D], F32, "tt%d" % i) for i in range(NB)]
        x1 = [kb.sb(st, [128, D], F32, "x1%d" % i) for i in range(NB)]
        h2 = [kb.sb(st, [128, D], F32, "h2%d" % i) for i in range(NB)]
        h2Tf = [kb.sb(st, [128, 8, 128], F32, "h2Tf%d" % i) for i in range(NB)]
        h2Tb = [kb.sb(st, [128, 8, 128], BF16, "h2Tb%d" % i) for i in range(NB)]
        stats = [kb.sb(st, [128, 2, 6], F32, "dst%d" % i) for i in range(NB)]
        mv = [kb.sb(st, [128, 2], F32, "dmv%d" % i) for i in range(NB)]
        rstd = [kb.sb(st, [128, 1], F32, "drs%d" % i) for i in range(NB)]
        sg = [kb.sb(st, [128, 256], F32, "sg%d" % i) for i in range(NB)]
        sel = [kb.sb(st, [128, 256], F32, "sel%d" % i) for i in range(NB)]
        selm = [kb.sb(st, [128, 256], F32, "selm%d" % i) for i in range(NB)]
        g8 = [kb.sb(st, [128, 8, 8], F32, "g8%d" % i) for i in range(NB)]
        grp = [kb.sb(st, [128, 8], F32, "grp%d" % i) for i in range(NB)]
        gs8 = [kb.sb(st, [128, 8], F32, "gs8%d" % i) for i in range(NB)]
        gm = [kb.sb(st, [128, 8], F32, "gm%d" % i) for i in range(NB)]
        t8 = [kb.sb(st, [128, 8], F32, "t8%d" % i) for i in range(NB)]
        den = [kb.sb(st, [128, 1], F32, "den%d" % i) for i in range(NB)]
        sact = [kb.sb(st, [128, 256], F32, "sact%d" % i) for i in range(NB)]
        sact2 = [kb.sb(st, [128, 256], F32, "sactb%d" % i) for i in range(NB)]
        sactT = [kb.sb(st, [128, 2, 128], BF16, "sactT%d" % i) for i in range(NB)]
        sho = [kb.sb(st, [128, D], F32, "sho%d" % i) for i in range(NB)]
        for t in range(NTT):
            b = t % NB
            r = 0 if t < L // 128 else 1
            rows = slice(t * 128, (t + 1) * 128)
            s.dma("sp", mixT[b][:], C.MIXT.t[:, rows].rearrange("(k p) t -> p k t", p=128), rd(C.MIXT), rd(mixT[b]))
            s.dma("sp", xt[b][:], Xsrc[rows, :], rd(Xsrc), rd(xt[b]))
            for cb in range(2):
                for kc in range(8):
                    s.op("pe", lambda e: e.matmul(out=psb[cb][:, :], lhsT=mixT[b][:, kc, :], rhs=wout[:, kc, cb * 512:(cb + 1) * 512],
                                                  start=(kc == 0), stop=(kc == 7)), rd(mixT[b], wout), rd(psb[cb]))
            for cb in range(2):
                cs = slice(cb * 512, (cb + 1) * 512)
                s.op("dve", lambda e: e.tensor_tensor(out=tt[b][:, cs], in0=psb[cb][:, :], in1=G1[:, r, cs], op=ALU.mult),
                     rd(psb[cb], G1), rd(tt[b]))
            if C.dbg_stage == "D":
                s.dma("sp", C.o_y[rows, :], tt[b][:], rd(tt[b]), rd(C.o_y))
            s.op("dve", lambda e: e.scalar_tensor_tensor(out=tt[b][:], in0=xt[b][:], scalar=ALPHA, in1=tt[b][:],
                                                         op0=ALU.mult, op1=ALU.add), rd(xt[b], tt[b]), rd(tt[b]))
            layer_norm_tile(C, tt[b], x1[b], stats[b], mv[b], rstd[b], LNW, LNB)
            s.dma("sp", C.X1[rows, :], x1[b][:], rd(x1[b]), rd(C.X1), nowaw=True)
            s.op("pool", lambda e: e.tensor_tensor(out=h2[b][:], in0=x1[b][:], in1=SC2[:, r, :], op=ALU.mult), rd(x1[b], SC2), rd(h2[b]))
            s.op("pool", lambda e: e.tensor_tensor(out=h2[b][:], in0=h2[b][:], in1=SH2[:, r, :], op=ALU.add), rd(h2[b], SH2), rd(h2[b]))
            s.dma("sp", C.H2[rows, :], h2[b][:], rd(h2[b]), rd(C.H2), nowaw=True)
            for kc in range(8):
                pb = psb[2 + kc // 4]
                s.op("pe", lambda e: e.transpose(out=pb[:, (kc % 4) * 128:(kc % 4 + 1) * 128], in_=h2[b][:, kc * 128:(kc + 1) * 128],
                                                 identity=ident[:]), rd(h2[b], ident), rd(pb))
            for hh in range(2):
                s.op("act", lambda e: e.copy(out=h2Tf[b][:, hh * 4:hh * 4 + 4, :], in_=psb[2 + hh][:, :].rearrange("p (k t) -> p k t", k=4)),
                     rd(psb[2 + hh]), rd(h2Tf[b]), nowaw=True)
                s.op("dve", lambda e: e.tensor_copy(out=h2Tb[b][:, hh * 4:hh * 4 + 4, :], in_=psb[2 + hh][:, :].rearrange("p (k t) -> p k t", k=4)),
                     rd(psb[2 + hh]), rd(h2Tb[b]), nowaw=True)
            for kc in range(8):
                s.op("pe", lambda e: e.matmul(out=psb[4][:, 0:256], lhsT=h2Tf[b][:, kc, :], rhs=rw[:, kc, :],
                                              start=(kc == 0), stop=(kc == 7)), rd(h2Tf[b], rw), rd(psb[4]))
            s.op("act", lambda e: e.activation(out=sg[b][:], in_=psb[4][:, 0:256], func=AF.Sigmoid), rd(psb[4]), rd(sg[b]))
            s.op("dve", lambda e: e.tensor_tensor(out=sel[b][:], in0=sg[b][:], in1=RB[:], op=ALU.add), rd(sg[b], RB), rd(sel[b]))
            for g in range(8):
                s.op("dve", lambda e: e.max(out=g8[b][:, g, :], in_=sel[b][:, g * 32:(g + 1) * 32]), rd(sel[b]), rd(g8[b]))
            s.op("dve", lambda e: e.tensor_tensor(out=grp[b][:], in0=g8[b][:, :, 0], in1=g8[b][:, :, 1], op=ALU.add), rd(g8[b]), rd(grp[b]))
            s.op("dve", lambda e: e.max(out=gs8[b][:], in_=grp[b][:]), rd(grp[b]), rd(gs8[b]))
            s.op("dve", lambda e: e.tensor_scalar(out=gm[b][:], in0=grp[b][:], scalar1=gs8[b][:, 3:4], scalar2=None, op0=ALU.is_ge),
                 rd(grp[b], gs8[b]), rd(gm[b]))
            for g in range(8):
                s.op("dve", lambda e: e.tensor_scalar(out=selm[b][:, g * 32:(g + 1) * 32], in0=sel[b][:, g * 32:(g + 1) * 32],
                                                      scalar1=2.0, scalar2=gm[b][:, g:g + 1], op0=ALU.add, op1=ALU.mult),
                     rd(sel[b], gm[b]), rd(selm[b]))
            s.op("dve", lambda e: e.max(out=t8[b][:], in_=selm[b][:]), rd(selm[b]), rd(t8[b]))
            s.op("dve", lambda e: e.tensor_scalar(out=sel[b][:], in0=selm[b][:], scalar1=t8[b][:, 7:8], scalar2=None, op0=ALU.is_ge),
                 rd(selm[b], t8[b]), rd(sel[b]))
            s.op("dve", lambda e: e.tensor_tensor(out=M.WFULL[:, t, :], in0=sg[b][:], in1=sel[b][:], op=ALU.mult),
                 rd(sg[b], sel[b]), rd(M.WFULL))
            s.op("dve", lambda e: e.reduce_sum(out=den[b][:], in_=M.WFULL[:, t, :], axis=AX.X), rd(M.WFULL), rd(den[b]))
            s.op("dve", lambda e: e.reciprocal(out=den[b][:], in_=den[b][:]), rd(den[b]), rd(den[b]))
            s.op("dve", lambda e: e.tensor_scalar(out=M.WFULL[:, t, :], in0=M.WFULL[:, t, :], scalar1=den[b][:, 0:1], scalar2=2.5,
                                                  op0=ALU.mult, op1=ALU.mult), rd(M.WFULL, den[b]), rd(M.WFULL))
            for kc in range(8):
                s.op("pe", lambda e: e.matmul(out=psb[5][:, :], lhsT=h2Tb[b][:, kc, :], rhs=wsgu[:, kc, :],
                                              start=(kc == 0), stop=(kc == 7)), rd(h2Tb[b], wsgu), rd(psb[5]))
            s.op("act", lambda e: e.activation(out=sact[b][:], in_=psb[5][:, 0:256], func=AF.Silu), rd(psb[5]), rd(sact[b]))
            s.op("dve", lambda e: e.tensor_tensor(out=sact2[b][:], in0=sact[b][:], in1=psb[5][:, 256:512], op=ALU.mult),
                 rd(sact[b], psb[5]), rd(sact2[b]))
            for k2 in range(2):
                s.op("pe", lambda e: e.transpose(out=psb[4][:, 256 + k2 * 128:384 + k2 * 128], in_=sact2[b][:, k2 * 128:(k2 + 1) * 128],
                                                 identity=ident[:]), rd(sact2[b], ident), rd(psb[4]))
            s.op("act", lambda e: e.copy(out=sactT[b][:], in_=psb[4][:, 256:512].rearrange("p (k t) -> p k t", k=2)), rd(psb[4]), rd(sactT[b]))
            for cb in range(2):
                for k2 in range(2):
                    s.op("pe", lambda e: e.matmul(out=psb[6 + cb][:, :], lhsT=sactT[b][:, k2, :], rhs=wsd[:, k2, cb * 512:(cb + 1) * 512],
                                                  start=(k2 == 0), stop=(k2 == 1)), rd(sactT[b], wsd), rd(psb[6 + cb]))
                if cb == 0:
                    s.op("act", lambda e: e.copy(out=sho[b][:, 0:512], in_=psb[6][:, :]), rd(psb[6]), rd(sho[b]), nowaw=True)
                else:
                    s.op("dve", lambda e: e.tensor_copy(out=sho[b][:, 512:1024], in_=psb[7][:, :]), rd(psb[7]), rd(sho[b]), nowaw=True)
            s.dma("sp", C.SHO[rows, :], sho[b][:], rd(sho[b]), rd(C.SHO), nowaw=True)


def layer_norm_tile(C, src, dst, stats, mv, rstd, LNW, LNB):
    s = C.kb.s
    for j in range(2):
        s.op("dve", lambda e: e.bn_stats(out=stats[:, j, :], in_=src[:, j * 512:(j + 1) * 512]), rd(src), rd(stats))
    s.op("dve", lambda e: e.bn_aggr(out=mv[:], in_=stats[:].rearrange("p a b -> p (a b)")), rd(stats), rd(mv))
    s.op("dve", lambda e: e.tensor_scalar_add(out=rstd[:], in0=mv[:, 1:2], scalar1=LN_EPS), rd(mv), rd(rstd))
    s.op("act", lambda e: e.sqrt(out=rstd[:], in_=rstd[:]), rd(rstd), rd(rstd))
    s.op("dve", lambda e: e.reciprocal(out=rstd[:], in_=rstd[:]), rd(rstd), rd(rstd))
    s.op("dve", lambda e: e.tensor_scalar(out=dst[:], in0=src[:], scalar1=mv[:, 0:1], scalar2=rstd[:, 0:1],
                                          op0=ALU.subtract, op1=ALU.mult), rd(src, mv, rstd), rd(dst))
    s.op("pool", lambda e: e.tensor_tensor(out=dst[:], in0=dst[:], in1=LNW[:], op=ALU.mult), rd(dst, LNW), rd(dst))
    s.op("dve", lambda e: e.tensor_tensor(out=dst[:], in0=dst[:], in1=LNB[:], op=ALU.add), rd(dst, LNB), rd(dst))


TAB_LAYERS = DEPTH
BS = 256
NBLK = 391
NSTL = NBLK * 2
NSLOT = NSTL * 128


def stage_E(C, l, M):
    kb, s = C.kb, C.kb.s
    ident, ones, masks, psb = C.ident, C.ones, C.masks, C.psb
    WF = M.WFULL
    with ExitStack() as st:
        iot = kb.sb(st, [128, NBLK + 1 + NTT], F32, "iot")
        s.dma("sp", iot[:], C.iotas[:], rd(C.iotas), rd(iot))
        IDXW = kb.sb(st, [128, NBLK], I32, "IDXW")
        SLOT8 = kb.sb(st, [128, NTT, 8], I32, "SLOT8")
        BT = kb.sb(st, [128, NSTL, 2], F32, "BT")
        IDXT = kb.sb(st, [128, NSTL], I32, "IDXT")
        with ExitStack() as s1:
            s1.enter_context(C.kb.nc.named_scope("E1_l%d" % l))
            RANK = kb.sb(s1, [128, NTT, 256], F32, "RANK")
            cnt = kb.sb(s1, [128, 256], F32, "cnt")
            s.op("pool", lambda e: e.memset(cnt[:], 0.0), (), rd(cnt))
            mk = [kb.sb(s1, [128, 256], F32, "mk%d" % i) for i in range(2)]
            for t in range(NTT):
                b = t % 2
                s.op("dve", lambda e: e.tensor_scalar(out=mk[b][:], in0=WF[:, t, :], scalar1=0.0, scalar2=None, op0=ALU.is_gt),
                     rd(WF), rd(mk[b]))
                s.op("pe", lambda e: e.matmul(out=psb[0][:, 0:256], lhsT=masks[:, 3, :], rhs=mk[b][:], start=True, stop=True),
                     rd(masks, mk[b]), rd(psb[0]))
                s.op("pe", lambda e: e.matmul(out=psb[1][:, 0:256], lhsT=ones[:], rhs=mk[b][:], start=True, stop=True),
                     rd(ones, mk[b]), rd(psb[1]))
                s.op("dve", lambda e: e.tensor_tensor(out=RANK[:, t, :], in0=psb[0][:, 0:256], in1=cnt[:], op=ALU.add),
                     rd(psb[0], cnt), rd(RANK))
                s.op("dve", lambda e: e.tensor_tensor(out=cnt[:], in0=cnt[:], in1=psb[1][:, 0:256], op=ALU.add),
                     rd(psb[1], cnt), rd(cnt))
            ci = kb.sb(s1, [128, 256], I32, "ci")
            nblk = kb.sb(s1, [128, 256], F32, "nblk")
            blkend = kb.sb(s1, [128, 256], F32, "blkend")
            bs256 = kb.sb(s1, [128, 256], F32, "bs256")
            s.op("dve", lambda e: e.tensor_scalar(out=ci[:], in0=cnt[:], scalar1=float(BS - 1), scalar2=None, op0=ALU.add), rd(cnt), rd(ci))
            s.op("dve", lambda e: e.tensor_scalar(out=ci[:], in0=ci[:], scalar1=8, scalar2=None, op0=ALU.arith_shift_right), rd(ci), rd(ci))
            s.op("dve", lambda e: e.tensor_copy(out=nblk[:], in_=ci[:]), rd(ci), rd(nblk))
            s.op("dve", lambda e: e.tensor_tensor_scan(out=blkend[:], data0=ones[:, 0:128].to_broadcast([128, 256]) if False else C.ones256[:],
                                                       data1=nblk[:], initial=0.0, op0=ALU.mult, op1=ALU.add), rd(nblk, C.ones256), rd(blkend))
            s.op("dve", lambda e: e.tensor_tensor(out=bs256[:], in0=blkend[:], in1=nblk[:], op=ALU.subtract), rd(blkend, nblk), rd(bs256))
            s.op("dve", lambda e: e.tensor_scalar(out=bs256[:], in0=bs256[:], scalar1=float(BS), scalar2=1.0, op0=ALU.mult, op1=ALU.add),
                 rd(bs256), rd(bs256))
            bcol = kb.sb(s1, [128, 2], F32, "bcol")
            tmpd = kb.sb(s1, [128, 128], F32, "tmpd")
            cmpb = [kb.sb(s1, [128, NBLK], F32, "cmpb%d" % i) for i in range(2)]
            for c in range(2):
                s.op("dve", lambda e: e.tensor_tensor(out=tmpd[:], in0=blkend[:, c * 128:(c + 1) * 128], in1=ident[:], op=ALU.mult),
                     rd(blkend, ident), rd(tmpd))
                s.op("dve", lambda e: e.reduce_sum(out=bcol[:, c:c + 1], in_=tmpd[:], axis=AX.X), rd(tmpd), rd(bcol))
                s.op("dve", lambda e: e.tensor_scalar(out=cmpb[c][:], in0=iot[:, 0:NBLK], scalar1=bcol[:, c:c + 1], scalar2=None, op0=ALU.is_ge),
                     rd(iot, bcol), rd(cmpb[c]))
                s.op("pe", lambda e: e.matmul(out=psb[2][:, 0:NBLK], lhsT=ones[:], rhs=cmpb[c][:], start=(c == 0), stop=(c == 1)),
                     rd(ones, cmpb[c]), rd(psb[2]))
            ebf = kb.sb(s1, [128, NBLK], F32, "ebf")
            s.op("dve", lambda e: e.tensor_scalar(out=ebf[:], in0=psb[2][:, 0:NBLK], scalar1=255.0, scalar2=128.0, op0=ALU.min, op1=ALU.mult),
                 rd(psb[2]), rd(ebf))
            s.op("dve", lambda e: e.tensor_scalar(out=ebf[:], in0=ebf[:], scalar1=iot[:, NBLK:NBLK + 1], scalar2=float(l * 32768),
                                                  op0=ALU.add, op1=ALU.add), rd(ebf, iot), rd(ebf))
            s.op("dve", lambda e: e.tensor_copy(out=IDXW[:], in_=ebf[:]), rd(ebf), rd(IDXW))
            zt = kb.sb(s1, [128, NSTL * 2], F32, "zt")
            s.op("pool", lambda e: e.memset(zt[:], 0.0), (), rd(zt))
            s.dma("sp", C.BUFTW.t.rearrange("(p n) two -> p (n two)", p=128), zt[:], rd(zt), rd(C.BUFTW))
            key = [kb.sb(s1, [128, 256], F32, "key%d" % i) for i in range(2)]
            top8 = [kb.sb(s1, [128, 8], F32, "top8%d" % i) for i in range(2)]
            oh = [kb.sb(s1, [128, 256], F32, "oh%d" % i) for i in range(2)]
            tw = [kb.sb(s1, [128, 8, 2], F32, "tw%d" % i) for i in range(2)]
            si = [kb.sb(s1, [128, 8], I32, "si%d" % i) for i in range(2)]
            lo = [kb.sb(s1, [128, 8], I32, "lo%d" % i) for i in range(2)]
            hi = [kb.sb(s1, [128, 8], I32, "hi%d" % i) for i in range(2)]
            lof = [kb.sb(s1, [128, 8], F32, "lof%d" % i) for i in range(2)]
            hif = [kb.sb(s1, [128, 8], F32, "hif%d" % i) for i in range(2)]
            posi = [kb.sb(s1, [128, 8], I32, "posi%d" % i) for i in range(2)]
            for t in range(NTT):
                b = t % 2
                s.op("dve", lambda e: e.tensor_scalar(out=mk[b][:], in0=WF[:, t, :], scalar1=0.0, scalar2=None, op0=ALU.is_gt),
                     rd(WF), rd(mk[b]))
                s.op("dve", lambda e: e.tensor_tensor(out=key[b][:], in0=RANK[:, t, :], in1=bs256[:], op=ALU.add), rd(RANK, bs256), rd(key[b]))
                s.op("dve", lambda e: e.tensor_tensor(out=key[b][:], in0=key[b][:], in1=mk[b][:], op=ALU.mult), rd(key[b], mk[b]), rd(key[b]))
                s.op("dve", lambda e: e.max(out=top8[b][:], in_=key[b][:]), rd(key[b]), rd(top8[b]))
                for k in range(8):
                    s.op("dve", lambda e: e.tensor_scalar(out=oh[b][:], in0=key[b][:], scalar1=top8[b][:, k:k + 1], scalar2=None, op0=ALU.is_equal),
                         rd(key[b], top8[b]), rd(oh[b]))
                    s.op("dve", lambda e: e.tensor_tensor(out=oh[b][:], in0=oh[b][:], in1=WF[:, t, :], op=ALU.mult), rd(oh[b], WF), rd(oh[b]))
                    s.op("dve", lambda e: e.reduce_sum(out=tw[b][:, k, 1:2], in_=oh[b][:], axis=AX.X), rd(oh[b]), rd(tw[b]))
                    s.op("pool", lambda e: e.tensor_copy(out=tw[b][:, k, 0:1], in_=iot[:, NBLK + 1 + t:NBLK + 2 + t]), rd(iot), rd(tw[b]))
                s.op("dve", lambda e: e.tensor_scalar(out=si[b][:], in0=top8[b][:], scalar1=-1.0, scalar2=None, op0=ALU.add), rd(top8[b]), rd(si[b]))
                s.op("dve", lambda e: e.tensor_copy(out=SLOT8[:, t, :], in_=si[b][:]), rd(si[b]), rd(SLOT8))
                s.op("dve", lambda e: e.tensor_scalar(out=lo[b][:], in0=si[b][:], scalar1=127, scalar2=None, op0=ALU.bitwise_and), rd(si[b]), rd(lo[b]))
                s.op("dve", lambda e: e.tensor_scalar(out=hi[b][:], in0=si[b][:], scalar1=7, scalar2=None, op0=ALU.arith_shift_right), rd(si[b]), rd(hi[b]))
                s.op("dve", lambda e: e.tensor_copy(out=lof[b][:], in_=lo[b][:]), rd(lo[b]), rd(lof[b]))
                s.op("dve", lambda e: e.tensor_copy(out=hif[b][:], in_=hi[b][:]), rd(hi[b]), rd(hif[b]))
                s.op("dve", lambda e: e.scalar_tensor_tensor(out=lof[b][:], in0=lof[b][:], scalar=float(NSTL), in1=hif[b][:],
                                                             op0=ALU.mult, op1=ALU.add), rd(lof[b], hif[b]), rd(lof[b]))
                s.op("dve", lambda e: e.tensor_copy(out=posi[b][:], in_=lof[b][:]), rd(lof[b]), rd(posi[b]))
                for k in range(8):
                    s.idma(out=C.BUFTW[:, :], in_=tw[b][:, k, :], out_off=posi[b][:, k:k + 1], reads=rd(tw[b], posi[b]), writes=rd(C.BUFTW), nowaw=True)
            s.dma("sp", BT[:], C.BUFTW.t.rearrange("(p n) two -> p n two", p=128), rd(C.BUFTW), rd(BT))
            s.op("dve", lambda e: e.tensor_copy(out=IDXT[:], in_=BT[:, :, 0]), rd(BT), rd(IDXT))
            if C.dbg_stage == "E1":
                s.dma("sp", C.o_e1[:, 0:NSTL * 2], BT[:].rearrange("p n two -> p (n two)"), rd(BT), rd(C.o_e1))
                s.dma("sp", C.o_e1[:, 2000:2000 + NBLK].bitcast(I32), IDXW[:], rd(IDXW), rd(C.o_e1))
                s.dma("sp", C.o_e1[:, 2400:2400 + NTT * 8].bitcast(I32), SLOT8[:].rearrange("p n k -> p (n k)"), rd(SLOT8), rd(C.o_e1))
                s.dma("sp", C.o_e1[:, 2700:2956], cnt[:], rd(cnt), rd(C.o_e1))
                return

        s.barrier()
        with ExitStack() as s2:
            s2.enter_context(C.kb.nc.named_scope("E2_l%d" % l))
            NW = 2
            wg = [kb.sb(s2, [128, 2048], BF16, "wg%d" % i) for i in range(NW)]
            wu = [kb.sb(s2, [128, 2048], BF16, "wu%d" % i) for i in range(NW)]
            wd = [kb.sb(s2, [128, 2048], BF16, "wd%d" % i) for i in range(NW)]
            NX = 3
            xg = [kb.sb(s2, [128, D], F32, "xg%d" % i) for i in range(NX)]
            xT = [kb.sb(s2, [128, 8, 128], BF16, "xTe%d" % i) for i in range(2)]
            ga = [kb.sb(s2, [128, 256], F32, "ga%d" % i) for i in range(2)]
            a2 = [kb.sb(s2, [128, 256], F32, "a2%d" % i) for i in range(2)]
            aT = [kb.sb(s2, [128, 2, 128], BF16, "aTe%d" % i) for i in range(2)]
            eo = [kb.sb(s2, [128, D], BF16, "eo%d" % i) for i in range(2)]
            nblk_run = NBLK if C.dbg_stage != "E2s" else 8
            for blk in range(nblk_run):
                wb = blk % NW
                ix = IDXW[:, blk:blk + 1]
                s.idma(out=wg[wb][:], in_=C.WG[:, :], in_off=ix, reads=rd(C.WG, IDXW), writes=rd(wg[wb]))
                s.idma(out=wu[wb][:], in_=C.WU[:, :], in_off=ix, reads=rd(C.WU, IDXW), writes=rd(wu[wb]))
                s.idma(out=wd[wb][:], in_=C.WD[:, :], in_off=ix, reads=rd(C.WD, IDXW), writes=rd(wd[wb]))
                for sti in range(2):
                    j = blk * 2 + sti
                    xb = xg[j % NX]
                    b2 = j % 2
                    s.idma(out=xb[:], in_=C.H2[:, :], in_off=IDXT[:, j:j + 1], reads=rd(C.H2, IDXT), writes=rd(xb))
                    for kc in range(8):
                        pb = psb[kc // 4]
                        s.op("pe", lambda e: e.transpose(out=pb[:, (kc % 4) * 128:(kc % 4 + 1) * 128], in_=xb[:, kc * 128:(kc + 1) * 128],
                                                         identity=ident[:]), rd(xb, ident), rd(pb))
                    s.op("act", lambda e: e.copy(out=xT[b2][:, 0:4, :], in_=psb[0][:, :].rearrange("p (k t) -> p k t", k=4)), rd(psb[0]), rd(xT[b2]), nowaw=True)
                    s.op("dve", lambda e: e.tensor_copy(out=xT[b2][:, 4:8, :], in_=psb[1][:, :].rearrange("p (k t) -> p k t", k=4)), rd(psb[1]), rd(xT[b2]), nowaw=True)
                    for kc in range(8):
                        s.op("pe", lambda e: e.matmul(out=psb[2][:, 0:256], lhsT=xT[b2][:, kc, :], rhs=wg[wb][:, kc * 256:(kc + 1) * 256],
                                                      start=(kc == 0), stop=(kc == 7)), rd(xT[b2], wg[wb]), rd(psb[2]))
                    for kc in range(8):
                        s.op("pe", lambda e: e.matmul(out=psb[3][:, 0:256], lhsT=xT[b2][:, kc, :], rhs=wu[wb][:, kc * 256:(kc + 1) * 256],
                                                      start=(kc == 0), stop=(kc == 7)), rd(xT[b2], wu[wb]), rd(psb[3]))
                    s.op("act", lambda e: e.activation(out=ga[b2][:], in_=psb[2][:, 0:256], func=AF.Silu), rd(psb[2]), rd(ga[b2]))
                    s.op("dve", lambda e: e.tensor_tensor(out=a2[b2][:], in0=ga[b2][:], in1=psb[3][:, 0:256], op=ALU.mult), rd(ga[b2], psb[3]), rd(a2[b2]))
                    for k2 in range(2):
                        s.op("pe", lambda e: e.transpose(out=psb[4][:, k2 * 128:(k2 + 1) * 128], in_=a2[b2][:, k2 * 128:(k2 + 1) * 128],
                                                         identity=ident[:]), rd(a2[b2], ident), rd(psb[4]))
                    s.op("act", lambda e: e.copy(out=aT[b2][:], in_=psb[4][:, 0:256].rearrange("p (k t) -> p k t", k=2)), rd(psb[4]), rd(aT[b2]))
                    for cb in range(2):
                        for k2 in range(2):
                            s.op("pe", lambda e: e.matmul(out=psb[5 + cb][:, :], lhsT=aT[b2][:, k2, :],
                                                          rhs=wd[wb][:, k2 * 1024 + cb * 512:k2 * 1024 + (cb + 1) * 512],
                                                          start=(k2 == 0), stop=(k2 == 1)), rd(aT[b2], wd[wb]), rd(psb[5 + cb]))
                    s.op("act", lambda e: e.activation(out=eo[b2][:, 0:512], in_=psb[5][:, :], func=AF.Copy, scale=BT[:, j, 1:2]),
                         rd(psb[5], BT), rd(eo[b2]), nowaw=True)
                    s.op("dve", lambda e: e.tensor_scalar(out=eo[b2][:, 512:1024], in0=psb[6][:, :], scalar1=BT[:, j, 1:2], scalar2=None, op0=ALU.mult),
                         rd(psb[6], BT), rd(eo[b2]), nowaw=True)
                    s.dma("sp", C.EO[j * 128:(j + 1) * 128, :], eo[b2][:], rd(eo[b2]), rd(C.EO), nowaw=True)
            if C.dbg_stage in ("E2", "E2s"):
                return

        s.barrier()
        with ExitStack() as s3:
            s3.enter_context(C.kb.nc.named_scope("E3_l%d" % l))
            G2 = kb.sb(s3, [128, 2, D], F32, "G2")
            for r in range(2):
                bcast_row(C, s3, G2[:, r, :], G2, C.MODD[l, r:r + 1, 5120:6144], C.MODD, D, psb[7])
            LNW = kb.sb(s3, [128, D], F32, "LNW2")
            LNB = kb.sb(s3, [128, D], F32, "LNB2")
            bcast_row(C, s3, LNW[:], LNW, C.ln2_w[l:l + 1, :], C.ln2_w, D, psb[7])
            bcast_row(C, s3, LNB[:], LNB, C.ln2_b[l:l + 1, :], C.ln2_b, D, psb[7])
            gat = [kb.sb(s3, [128, D], BF16, "gat%d" % i) for i in range(4)]
            ffa = [kb.sb(s3, [128, D], F32, "ffa%d" % i) for i in range(2)]
            x1t = [kb.sb(s3, [128, D], F32, "x1t%d" % i) for i in range(2)]
            x2t = [kb.sb(s3, [128, D], F32, "x2t%d" % i) for i in range(2)]
            stats = [kb.sb(s3, [128, 2, 6], F32, "est%d" % i) for i in range(2)]
            mv = [kb.sb(s3, [128, 2], F32, "emv%d" % i) for i in range(2)]
            rstd = [kb.sb(s3, [128, 1], F32, "ers%d" % i) for i in range(2)]
            gi = 0
            for t in range(NTT):
                b = t % 2
                r = 0 if t < L // 128 else 1
                rows = slice(t * 128, (t + 1) * 128)
                s.dma("sp", ffa[b][:], C.SHO[rows, :], rd(C.SHO), rd(ffa[b]))
                s.dma("sp", x1t[b][:], C.X1[rows, :], rd(C.X1), rd(x1t[b]))
                for k in range(8):
                    g_ = gat[gi % 4]
                    gi += 1
                    s.idma(out=g_[:], in_=C.EO[:, :], in_off=SLOT8[:, t, k:k + 1], reads=rd(C.EO, SLOT8), writes=rd(g_))
                    eng = "dve" if k % 2 == 0 else "pool"
                    s.op(eng, lambda e: e.tensor_tensor(out=ffa[b][:], in0=ffa[b][:], in1=g_[:], op=ALU.add), rd(ffa[b], g_), rd(ffa[b]))
                if C.dbg_stage == "E":
                    s.dma("sp", C.o_ff[rows, :], ffa[b][:], rd(ffa[b]), rd(C.o_ff))
                s.op("pool", lambda e: e.tensor_tensor(out=ffa[b][:], in0=ffa[b][:], in1=G2[:, r, :], op=ALU.mult), rd(ffa[b], G2), rd(ffa[b]))
                s.op("dve", lambda e: e.scalar_tensor_tensor(out=ffa[b][:], in0=x1t[b][:], scalar=ALPHA, in1=ffa[b][:],
                                                             op0=ALU.mult, op1=ALU.add), rd(x1t[b], ffa[b]), rd(ffa[b]))
                layer_norm_tile(C, ffa[b], x2t[b], stats[b], mv[b], rstd[b], LNW, LNB)
                s.dma("sp", C.X2[rows, :], x2t[b][:], rd(x2t[b]), rd(C.X2), nowaw=True)
                if l == DEPTH - 1 and t < L // 128 and C.out is not None:
                    s.dma("sp", C.out[rows, :], x2t[b][:], rd(x2t[b]), rd(C.out), nowaw=True)


def c_col(d, h):
    return d * 4 + h


USE_F32R = os.environ.get("F32R", "0") == "1"


def fr(ap):
    return ap.bitcast(mybir.dt.float32r) if USE_F32R else ap


def build(dbg_stage=None):
    nc = bass.Bass("TRN2", target_bir_lowering=False)
    es = ExitStack()
    kb = KB(nc, es)
    s = kb.s
    x_in = kb.dram("x", [NT, D], F32, kind="ExternalInput")
    c_in = kb.dram("c2", [2, D], F32, kind="ExternalInput")
    ada_w = kb.dram("ada_w", [DEPTH, D, 6 * D], F32, kind="ExternalInput")
    ada_b = kb.dram("ada_b", [DEPTH, 6 * D], F32, kind="ExternalInput")
    w_in = kb.dram("w_in", [DEPTH, D, N_IN], F32, kind="ExternalInput")
    ident_in = kb.dram("ident", [128, 128], F32, kind="ExternalInput")
    masks_in = kb.dram("masks", [8, 128, 128], F32, kind="ExternalInput")
    conv_w = kb.dram("conv_w", [DEPTH, 5, OFF_Z], F32, kind="ExternalInput")
    a_log = kb.dram("gdn_a_log", [DEPTH, 8], F32, kind="ExternalInput")
    dt_bias = kb.dram("gdn_dt_bias", [DEPTH, 8], F32, kind="ExternalInput")
    gdn_norm_w = kb.dram("gdn_norm_w", [DEPTH, 128], F32, kind="ExternalInput")
    q_norm_w = kb.dram("q_norm_w", [DEPTH, 128], F32, kind="ExternalInput")
    k_norm_w = kb.dram("k_norm_w", [DEPTH, 128], F32, kind="ExternalInput")
    rope_in = kb.dram("rope", [2, L, 384], F32, kind="ExternalInput")
    w_out = kb.dram("w_out", [DEPTH, D, D], F32, kind="ExternalInput")
    ln1_w = kb.dram("ln1_w", [DEPTH, D], F32, kind="ExternalInput")
    ln1_b = kb.dram("ln1_b", [DEPTH, D], F32, kind="ExternalInput")
    ln2_w = kb.dram("ln2_w", [DEPTH, D], F32, kind="ExternalInput")
    ln2_b = kb.dram("ln2_b", [DEPTH, D], F32, kind="ExternalInput")
    router_w = kb.dram("router_w", [DEPTH, D, 256], F32, kind="ExternalInput")
    router_bias = kb.dram("router_bias", [DEPTH, 256], F32, kind="ExternalInput")
    sh_w_gate = kb.dram("sh_w_gate", [DEPTH, D, 256], F32, kind="ExternalInput")
    sh_w_up = kb.dram("sh_w_up", [DEPTH, D, 256], F32, kind="ExternalInput")
    sh_w_down = kb.dram("sh_w_down", [DEPTH, 256, D], F32, kind="ExternalInput")
    iotas = kb.dram("iotas", [128, NBLK + 1 + NTT], F32, kind="ExternalInput")
    WG = WU = WD = None
    if dbg_stage in (None, "E1", "E2", "E2s", "E"):
        WG = kb.dram("WG", [TAB_LAYERS * 256 * 128, 2048], F32, kind="ExternalInput")
        WU = kb.dram("WU", [TAB_LAYERS * 256 * 128, 2048], F32, kind="ExternalInput")
        WD = kb.dram("WD", [TAB_LAYERS * 256 * 128, 2048], F32, kind="ExternalInput")
    outs = {}

    def dbg_out(name, shape, dt=F32):
        t = kb.dram(name, shape, dt, kind="ExternalOutput")
        outs[name] = t
        return t

    XS = kb.dram("XS", [NT, D], F32)
    MODD = kb.dram("MODD", [DEPTH, 2, 6 * D], F32)
    QKVT = kb.dram("QKVT", [OFF_Z, NT], F32)
    PTOK = kb.dram("PTOK", [NT, NTOKC], F32)
    OPSD = kb.dram("OPSD", [4, 2, NTT, 128, OPW], F32)
    OFB = [kb.dram("OFB%d" % d, [NT, 512], F32) for d in range(2)]
    MIXT = kb.dram("MIXT", [D, NT], BF16)
    X1 = kb.dram("X1", [NT, D], F32)
    X2 = kb.dram("X2", [NT, D], F32)
    H2 = kb.dram("H2", [NT, D], F32)
    SHO = kb.dram("SHO", [NT, D], F32)
    BUFTW = kb.dram("BUFTW", [NSLOT, 2], F32)
    EO = kb.dram("EO", [NSLOT, D], BF16)

    ces = es
    ident = kb.sb(ces, [128, 128], F32, "ident")
    s.dma("sp", ident[:], ident_in[:], reads=rd(ident_in), writes=rd(ident))
    identb = kb.sb(ces, [128, 128], BF16, "identb")
    s.op("dve", lambda e: e.tensor_copy(out=identb[:], in_=ident[:]), rd(ident), rd(identb))

    psb = [kb.ps(ces, [128, 512], F32, "bank%d" % i) for i in range(8)]
    for p_ in psb:
        p_.res.excl = True
    ones = kb.sb(ces, [128, 128], F32, "ones")
    s.op("pool", lambda e: e.memset(ones[:], 1.0), (), rd(ones))
    bcrow = kb.sb(ces, [128, D], F32, "bcrow")
    s.op("pool", lambda e: e.memset(bcrow[:], 0.0), (), rd(bcrow))
    masks = kb.sb(ces, [128, 8, 128], F32, "masks")
    ones256 = kb.sb(ces, [128, 256], F32, "ones256")
    s.op("pool", lambda e: e.memset(ones256[:], 1.0), (), rd(ones256))
    s.dma("sp", masks[:], masks_in.t.rearrange("m p f -> p m f"), rd(masks_in), rd(masks))

    final_out = dbg_out("out", [L, D]) if dbg_stage is None else None
    for l in range(DEPTH):
        with ExitStack() as st:
            st.enter_context(nc.named_scope("mod_l%d" % l))
            cT = kb.sb(st, [128, 8, 2], F32, "cT")
            craw = kb.sb(st, [2, D], F32, "craw")
            s.dma("sp", craw[:], c_in[:], rd(c_in), rd(craw))
            csil = kb.sb(st, [2, D], F32, "csil")
            s.op("act", lambda e: e.activation(out=csil[:], in_=craw[:], func=AF.Silu), rd(craw), rd(csil))
            for kc in range(8):
                s.op("pe", lambda e: e.transpose(out=psb[0][:, kc * 2:kc * 2 + 2], in_=csil[:, kc * 128:(kc + 1) * 128],
                                                 identity=ident[0:2, 0:2]), rd(csil, ident), rd(psb[0]))
            s.op("dve", lambda e: e.tensor_copy(out=cT[:].rearrange("p k r -> p (k r)"), in_=psb[0][:, 0:16]),
                 rd(psb[0]), rd(cT))
            modrow = kb.sb(st, [2, 6 * D], F32, "modrow")
            abrow = kb.sb(st, [2, 6 * D], F32, "abrow")
            for r in range(2):
                s.dma("sp", abrow[r:r + 1, :], ada_b[l:l + 1, :], rd(ada_b), rd(abrow))
            wch = [kb.sb(st, [128, 3072], F32, "adaw%d" % i) for i in range(2)]
            for half in range(2):
                for kc in range(8):
                    wt = wch[kc % 2]
                    s.dma("sp" if kc % 2 == 0 else "pool", wt[:],
                          ada_w[l, kc * 128:(kc + 1) * 128, half * 3072:(half + 1) * 3072], rd(ada_w), rd(wt))
                    for cb in range(6):
                        s.op("pe", lambda e: e.matmul(out=psb[cb][0:2, :], lhsT=cT[:, kc, :],
                                                      rhs=wt[:, cb * 512:(cb + 1) * 512],
                                                      start=(kc == 0), stop=(kc == 7)),
                             rd(cT, wt), rd(psb[cb]))
                for cb in range(6):
                    c0 = half * 3072 + cb * 512
                    s.op("dve", lambda e: e.tensor_tensor(out=modrow[:, c0:c0 + 512], in0=psb[cb][0:2, :],
                                                          in1=abrow[:, c0:c0 + 512], op=ALU.add),
                         rd(psb[cb], abrow), rd(modrow))
            s.dma("sp", MODD[l], modrow[:], rd(modrow), rd(MODD))
        if dbg_stage == "mod" and l == 0:
            break

        s.barrier()
        with ExitStack() as st:
            st.enter_context(nc.named_scope("A_l%d" % l))
            hT = kb.sb(st, [128, 8, NT], BF16, "hT")
            modT = kb.sb(st, [128, 48, 2], F32, "modT")
            for r in range(2):
                s.dma("sp", modT[:, :, r], MODD[l, r].rearrange("(c p) -> p c", p=128), rd(MODD), rd(modT),
                      allow_slow_non_contiguous=True)
            sc1p = kb.sb(st, [128, 8, 2], F32, "sc1p")
            s.op("dve", lambda e: e.tensor_scalar_add(out=sc1p[:], in0=modT[:, 8:16, :], scalar1=1.0), rd(modT), rd(sc1p))
            xin = [kb.sb(st, [128, D], F32, "xin%d" % i) for i in range(2)]
            xn = [kb.sb(st, [128, D], F32, "xn%d" % i) for i in range(2)]
            stats = [kb.sb(st, [128, 2, 6], F32, "bst%d" % i) for i in range(2)]
            mv = [kb.sb(st, [128, 2], F32, "mv%d" % i) for i in range(2)]
            rstd = [kb.sb(st, [128, 1], F32, "rstd%d" % i) for i in range(2)]
            src = x_in if l == 0 else X2
            for t in range(NTT):
                i = t % 2
                r = 0 if t < L // 128 else 1
                s.dma("sp", xin[i][:], src[t * 128:(t + 1) * 128, :], rd(src), rd(xin[i]))
                if l == 0:
                    for j in range(2):
                        s.op("dve", lambda e: e.bn_stats(out=stats[i][:, j, :], in_=xin[i][:, j * 512:(j + 1) * 512]),
                             rd(xin[i]), rd(stats[i]))
                    s.op("dve", lambda e: e.bn_aggr(out=mv[i][:], in_=stats[i][:].rearrange("p a b -> p (a b)")),
                         rd(stats[i]), rd(mv[i]))
                    s.op("dve", lambda e: e.tensor_scalar_add(out=rstd[i][:], in0=mv[i][:, 1:2], scalar1=LN_EPS),
                         rd(mv[i]), rd(rstd[i]))
                    s.op("act", lambda e: e.sqrt(out=rstd[i][:], in_=rstd[i][:]), rd(rstd[i]), rd(rstd[i]))
                    s.op("dve", lambda e: e.reciprocal(out=rstd[i][:], in_=rstd[i][:]), rd(rstd[i]), rd(rstd[i]))
                    s.op("dve", lambda e: e.tensor_scalar(out=xn[i][:], in0=xin[i][:], scalar1=mv[i][:, 0:1],
                                                          scalar2=rstd[i][:, 0:1], op0=ALU.subtract, op1=ALU.mult),
                         rd(xin[i], mv[i], rstd[i]), rd(xn[i]))
                    s.dma("pool", XS[t * 128:(t + 1) * 128, :], xn[i][:], rd(xn[i]), rd(XS), nowaw=True)
                    xs_t = xn[i]
                else:
                    xs_t = xin[i]
                for kc in range(8):
                    pb = psb[kc // 4]
                    s.op("pe", lambda e: e.transpose(out=pb[:, (kc % 4) * 128:(kc % 4 + 1) * 128],
                                                     in_=xs_t[:, kc * 128:(kc + 1) * 128], identity=ident[:]),
                         rd(xs_t, ident), rd(pb))
                for kc in range(8):
                    pb = psb[kc // 4]
                    eng = "act" if kc % 2 == 0 else "dve"
                    if eng == "act":
                        s.op("act", lambda e: e.activation(out=hT[:, kc, t * 128:(t + 1) * 128],
                                                           in_=pb[:, (kc % 4) * 128:(kc % 4 + 1) * 128],
                                                           func=AF.Identity, scale=sc1p[:, kc, r:r + 1],
                                                           bias=modT[:, kc, r:r + 1]),
                             rd(pb, sc1p, modT), rd(hT), nowaw=True)
                    else:
                        s.op("dve", lambda e: e.tensor_scalar(out=hT[:, kc, t * 128:(t + 1) * 128],
                                                              in0=pb[:, (kc % 4) * 128:(kc % 4 + 1) * 128],
                                                              scalar1=sc1p[:, kc, r:r + 1], scalar2=modT[:, kc, r:r + 1],
                                                              op0=ALU.mult, op1=ALU.add),
                             rd(pb, sc1p, modT), rd(hT), nowaw=True)
            wbf = kb.sb(st, [128, 8, N_IN], BF16, "wbf")
            for kc in range(8):
                for (c0, c1) in ((0, 1536), (1536, N_IN)):
                    s.dma("pool", wbf[:, kc, c0:c1], w_in[l, kc * 128:(kc + 1) * 128, c0:c1], rd(w_in), rd(wbf))
            ev = [kb.sb(st, [128, 512], F32, "ev%d" % i) for i in range(4)]
            n = 0
            for cc in range(12):
                for tt in range(0, NT, 512):
                    w = min(512, NT - tt)
                    pb = psb[2 + n % 4]
                    for kc in range(8):
                        s.op("pe", lambda e: e.matmul(out=pb[:, 0:w], lhsT=wbf[:, kc, cc * 128:(cc + 1) * 128],
                                                      rhs=hT[:, kc, tt:tt + w], start=(kc == 0), stop=(kc == 7)),
                             rd(wbf, hT), rd(pb))
                    e_ = ev[n % 4]
                    if n % 2 == 0:
                        s.op("act", lambda e: e.copy(out=e_[:, 0:w], in_=pb[:, 0:w]), rd(pb), rd(e_))
                    else:
                        s.op("dve", lambda e: e.tensor_copy(out=e_[:, 0:w], in_=pb[:, 0:w]), rd(pb), rd(e_))
                    s.dma("sp", QKVT[cc * 128:(cc + 1) * 128, tt:tt + w], e_[:, 0:w], rd(e_), rd(QKVT), nowaw=True)
                    n += 1
            for t in range(NTT):
                for c0 in range(OFF_Z, N_IN, 512):
                    w = min(512, N_IN - c0)
                    pb = psb[2 + n % 4]
                    for kc in range(8):
                        s.op("pe", lambda e: e.matmul(out=pb[:, 0:w], lhsT=hT[:, kc, t * 128:(t + 1) * 128],
                                                      rhs=wbf[:, kc, c0:c0 + w], start=(kc == 0), stop=(kc == 7)),
                             rd(wbf, hT), rd(pb))
                    e_ = ev[n % 4]
                    if n % 2 == 0:
                        s.op("act", lambda e: e.copy(out=e_[:, 0:w], in_=pb[:, 0:w]), rd(pb), rd(e_))
                    else:
                        s.op("dve", lambda e: e.tensor_copy(out=e_[:, 0:w], in_=pb[:, 0:w]), rd(pb), rd(e_))
                    s.dma("sp", PTOK[t * 128:(t + 1) * 128, c0 - OFF_Z:c0 - OFF_Z + w], e_[:, 0:w], rd(e_), rd(PTOK), nowaw=True)
                    n += 1
        if dbg_stage == "A":
            break
        s.barrier()
        C = NS()
        C.bcrow = bcrow
        C.kb = kb; C.ident = ident; C.identb = identb; C.ones = ones; C.masks = masks; C.psb = psb
        C.conv_w = conv_w; C.a_log = a_log; C.dt_bias = dt_bias; C.gdn_norm_w = gdn_norm_w
        C.QKVT = QKVT; C.PTOK = PTOK; C.OPSD = OPSD; C.OFB = OFB; C.MIXT = MIXT; C.dbg_stage = dbg_stage
        C.cut = int(os.environ.get("GCUT", "99"))
        if dbg_stage == "gdn":
            C.o_gdn = dbg_out("o_gdn", [NT, 512])
        if dbg_stage in ("gdnA", "gdnB"):
            C.o_dbg = dbg_out("o_dbg", [128, 1024])
        if dbg_stage == "gdnB":
            C.o_dbgB = dbg_out("o_dbgB", [4, 128, OPW])
        try:
            stage_gdn(C, l)
        except Cut:
            pass
        s.barrier()
        if dbg_stage in ("gdn", "gdnprep", "gdnscan", "gdnA", "gdnB"):
            break
        C.q_norm_w = q_norm_w; C.k_norm_w = k_norm_w; C.rope_in = rope_in
        if dbg_stage == "attq":
            C.o_dbg = dbg_out("o_dbg", [128, 1024])
        stage_att(C, l)
        s.barrier()
        if dbg_stage in ("att", "attq"):
            break
        C.w_out = w_out; C.ln1_w = ln1_w; C.ln1_b = ln1_b; C.ln2_w = ln2_w; C.ln2_b = ln2_b
        C.router_w = router_w; C.router_bias = router_bias; C.sh_w_gate = sh_w_gate; C.sh_w_up = sh_w_up
        C.sh_w_down = sh_w_down; C.MODD = MODD; C.XS = XS; C.X1 = X1; C.X2 = X2; C.H2 = H2; C.SHO = SHO
        if dbg_stage == "D":
            C.o_y = dbg_out("o_y", [NT, D])
        with ExitStack() as mst:
            M = NS()
            M.WFULL = kb.sb(mst, [128, NTT, 256], F32, "WFULL")
            stage_D(C, l, M)
            s.barrier()
            if dbg_stage != "D":
                C.iotas = iotas; C.WG = WG; C.WU = WU; C.WD = WD; C.BUFTW = BUFTW; C.EO = EO; C.ones256 = ones256
                C.out = final_out
                if dbg_stage == "E1":
                    C.o_e1 = dbg_out("o_e1", [128, 3000])
                if dbg_stage == "E":
                    C.o_ff = dbg_out("o_ff", [NT, D])
                stage_E(C, l, M)
                s.barrier()
            if dbg_stage == "D":
                o2 = dbg_out("o_wfull", [128, NTT, 256])
                s.dma("sp", o2[:], M.WFULL[:], rd(M.WFULL), rd(o2))
                C.dfin = [o2]
        if dbg_stage in ("D", "E1", "E2", "E2s", "E"):
            break

    finals = []
    if dbg_stage == "mod":
        o = dbg_out("o_mod", [2, 6 * D])
        with ExitStack() as st:
            tmp = kb.sb(st, [2, 6 * D], F32, "dbgm")
            s.dma("sp", tmp[:], MODD[0], rd(MODD), rd(tmp))
            s.dma("sp", o[:], tmp[:], rd(tmp), rd(o))
        finals.append(o)
    if dbg_stage == "A":
        o1 = dbg_out("o_qkvt", [OFF_Z, NT])
        o2 = dbg_out("o_ptok", [NT, NTOKC])
        o3 = dbg_out("o_xs", [NT, D])
        s.dma("sp", o1[:], QKVT[:], rd(QKVT), rd(o1))
        s.dma("sp", o2[:], PTOK[:], rd(PTOK), rd(o2))
        s.dma("sp", o3[:], XS[:], rd(XS), rd(o3))
        finals += [o1, o2, o3]
    if dbg_stage is None:
        finals.append(final_out)
    if dbg_stage == "E1":
        finals.append(outs["o_e1"])
    if dbg_stage in ("E2", "E2s"):
        o1 = dbg_out("o_eo", [2048, D], BF16)
        s.dma("sp", o1[:], EO[0:2048, :], rd(EO), rd(o1))
        finals.append(o1)
    if dbg_stage == "E":
        o1 = dbg_out("o_x2", [NT, D])
        s.dma("sp", o1[:], X2[:], rd(X2), rd(o1))
        finals += [o1, outs["o_ff"]]
    if dbg_stage in ("gdn", "gdnscan"):
        o1 = dbg_out("o_of", [NT, 512])
        o2 = dbg_out("o_ob", [NT, 512])
        s.dma("sp", o1[:], OFB[0][:], rd(OFB[0]), rd(o1))
        s.dma("sp", o2[:], OFB[1][:], rd(OFB[1]), rd(o2))
        finals += [o1, o2]
        if dbg_stage == "gdn":
            finals.append(outs["o_gdn"])
    if dbg_stage in ("gdnA", "attq"):
        finals.append(outs["o_dbg"])
    if dbg_stage == "D":
        finals += C.dfin + [outs["o_y"]]
        for nm, tsrc in (("o_x1", X1), ("o_h2", H2), ("o_sho", SHO)):
            o1 = dbg_out(nm, [NT, D])
            s.dma("sp", o1[:], tsrc[:], rd(tsrc), rd(o1))
            finals.append(o1)
    if dbg_stage == "att":
        o1 = dbg_out("o_mixt", [D, NT], BF16)
        s.dma("sp", o1[:], MIXT[:], rd(MIXT), rd(o1))
        finals.append(o1)
    if dbg_stage == "gdnB":
        finals.append(outs["o_dbgB"])
    if dbg_stage == "gdnprep":
        o1 = dbg_out("o_ops", [4, 2, NTT, 128, OPW])
        s.dma("sp", o1[:], OPSD[:], rd(OPSD), rd(o1))
        finals.append(o1)
    s.finish("sp", [f.res for f in finals])
    es.close()
    return nc, list(outs.keys())


_r = np.arange(128)
_m4 = np.stack([(_r[:, None] <= _r[None, :]), (_r[:, None] >= _r[None, :]),
                (_r[:, None] > _r[None, :]), (_r[:, None] < _r[None, :])]).astype(np.float32)
MASKS = np.concatenate([_m4, (1.0 - _m4[2:4]) * 3.0e4, -(1.0 - _m4[0:2]) * 3.0e4]).astype(np.float32)


def _rope_tables():
    t = np.arange(L)
    row = (t // 64).astype(np.float32)
    col = (t % 64).astype(np.float32)
    inv = (np.float32(10000.0) ** (-np.arange(0, 64, 2, dtype=np.float32) / np.float32(64))).astype(np.float32)
    ang = np.stack([row[:, None] * inv, col[:, None] * inv], axis=1).astype(np.float32)
    cs = np.stack([np.cos(ang), np.sin(ang)]).reshape(2, L, 64).astype(np.float32)
    return np.ascontiguousarray(np.tile(cs, (1, 1, 6)))


ROPE = _rope_tables()
IOTAS = np.concatenate([np.tile(np.arange(NBLK, dtype=np.float32), (128, 1)), np.arange(128, dtype=np.float32)[:, None],
                        (np.arange(NTT, dtype=np.float32)[None, :] * 128 + np.arange(128, dtype=np.float32)[:, None])], axis=1)


def expert_tables(inputs):
    n = TAB_LAYERS
    g = inputs["exp_w_gate"][:n]; u = inputs["exp_w_up"][:n]; d = inputs["exp_w_down"][:n]
    WG = np.ascontiguousarray(g.reshape(n, 256, 8, 128, 256).transpose(0, 1, 3, 2, 4)).reshape(n * 256 * 128, 2048)
    WU = np.ascontiguousarray(u.reshape(n, 256, 8, 128, 256).transpose(0, 1, 3, 2, 4)).reshape(n * 256 * 128, 2048)
    WD = np.ascontiguousarray(d.reshape(n, 256, 2, 128, 1024).transpose(0, 1, 3, 2, 4)).reshape(n * 256 * 128, 2048)
    return WG, WU, WD


def make_inputs(inputs, b, tabs=None):
    xx = np.concatenate([inputs["x"][b], inputs["ctx"][b]], axis=0)
    c2 = np.stack([inputs["c"][b], inputs["c_ctx"]], axis=0)
    m = {
        "x": np.ascontiguousarray(xx, dtype=np.float32),
        "c2": np.ascontiguousarray(c2, dtype=np.float32),
        "ada_w": inputs["ada_w"], "ada_b": inputs["ada_b"], "w_in": inputs["w_in"],
        "ident": np.eye(128, dtype=np.float32),
        "masks": MASKS,
        "conv_w": inputs["conv_w"], "gdn_a_log": inputs["gdn_a_log"].reshape(DEPTH, 8),
        "gdn_dt_bias": inputs["gdn_dt_bias"].reshape(DEPTH, 8), "gdn_norm_w": inputs["gdn_norm_w"],
        "q_norm_w": inputs["q_norm_w"], "k_norm_w": inputs["k_norm_w"], "rope": ROPE,
        "w_out": inputs["w_out"], "ln1_w": inputs["ln1_w"], "ln1_b": inputs["ln1_b"],
        "ln2_w": inputs["ln2_w"], "ln2_b": inputs["ln2_b"], "router_w": inputs["router_w"],
        "router_bias": inputs["router_bias"], "sh_w_gate": inputs["sh_w_gate"], "sh_w_up": inputs["sh_w_up"],
        "sh_w_down": inputs["sh_w_down"],
        "iotas": IOTAS,
    }
    if tabs is not None:
        m["WG"], m["WU"], m["WD"] = tabs
    return m


def kernel(**inputs):
    nc, onames = build()
    tabs = expert_tables(inputs)
    in_maps = [make_inputs(inputs, b, tabs) for b in range(8)]
    res = run_bass_kernel_spmd(nc, in_maps, core_ids=list(range(8)))
    return np.stack([r["out"] for r in res.results], axis=0)
```

```python
import os
from contextlib import ExitStack
import numpy as np
import concourse.bass as bass
import concourse.mybir as mybir
from concourse.bass_utils import run_bass_kernel_spmd

F32 = mybir.dt.float32
BF16 = mybir.dt.bfloat16
U32 = mybir.dt.uint32
I32 = mybir.dt.int32
AF = mybir.ActivationFunctionType
ALU = mybir.AluOpType
AX = mybir.AxisListType

D = 1024
L = 4096
LC = 256
NT = L + LC
NTT = NT // 128
DEPTH = 2
N_IN = 3088
OFF_Z = 1536
OFF_BA = 2048
OFF_ATT = 2064
NTOKC = N_IN - OFF_Z
ALPHA = (2.0 * DEPTH) ** 0.25
LN_EPS = 1e-5
RMS_EPS = 1e-6
OPW = 5 * 128 + 8


class Res:
    __slots__ = ("name", "w", "r", "excl", "wa", "wx")

    def __init__(self, name=""):
        self.name = name
        self.w = None
        self.r = {}
        self.excl = False
        self.wa = {}
        self.wx = None


class Sched:
    NDS = 8

    def __init__(self, nc, es):
        self.nc = nc
        self.engs = {"pe": nc.tensor, "dve": nc.vector, "act": nc.scalar, "pool": nc.gpsimd, "sp": nc.sync}
        self.csem = {k: es.enter_context(nc.semaphore("c_" + k)) for k in ("pe", "dve", "act", "pool")}
        self.ccnt = {k: 0 for k in self.csem}
        self.dsem = {q: [es.enter_context(nc.semaphore("d_%s%d" % (q, i))) for i in range(self.NDS)]
                     for q in ("sp", "act", "pool")}
        self.dcnt = {q: [0] * self.NDS for q in self.dsem}
        self.drr = {q: 0 for q in self.dsem}
        self.seen = {e: {} for e in self.engs}
        self.ninst = 0

    def _sem(self, key):
        return self.csem[key[1]] if key[0] == "c" else self.dsem[key[1]][key[2]]

    def _wait(self, eng, deps):
        seen = self.seen[eng]
        for key, val in sorted(deps.items(), key=lambda kv: str(kv[0])):
            if eng == "pe" and key == ("c", "pe"):
                continue
            if seen.get(key, 0) >= val:
                continue
            self.engs[eng].wait_ge(self._sem(key), val)
            seen[key] = val

    @staticmethod
    def _deps(reads, writes, nowaw=False):
        deps = {}

        def add(ev):
            if ev is not None and deps.get(ev[0], 0) < ev[1]:
                deps[ev[0]] = ev[1]
        for r in reads:
            add(r.wx)
            for k, v in r.wa.items():
                add((k, v))
            if r.excl:
                for k, v in r.r.items():
                    add((k, v))
        for w in writes:
            add(w.wx)
            if not nowaw or w.excl:
                for k, v in w.wa.items():
                    add((k, v))
            for k, v in w.r.items():
                add((k, v))
        return deps

    @staticmethod
    def _mark(ev, reads, writes, nowaw=False):
        for r in reads:
            if r.r.get(ev[0], 0) < ev[1]:
                r.r[ev[0]] = ev[1]
        for w in writes:
            w.w = ev
            if nowaw and not w.excl:
                if w.wa.get(ev[0], 0) < ev[1]:
                    w.wa[ev[0]] = ev[1]
            else:
                w.wx = ev
                w.wa = {}
                w.r = {}

    def op(self, eng, fn, reads=(), writes=(), nowaw=False):
        self._wait(eng, self._deps(reads, writes, nowaw))
        inst = fn(self.engs[eng])
        self.ccnt[eng] += 1
        inst.then_inc(self.csem[eng], 1)
        ev = (("c", eng), self.ccnt[eng])
        self._mark(ev, reads, writes, nowaw)
        self.ninst += 1
        return ev

    def dma(self, q, out, in_, reads=(), writes=(), nowaw=False, **kw):
        self._wait(q, self._deps(reads, writes, nowaw))
        i = self.drr[q]
        self.drr[q] = (i + 1) % self.NDS
        inst = self.engs[q].dma_start(out=out, in_=in_, **kw)
        self.dcnt[q][i] += 16
        inst.then_inc(self.dsem[q][i], 16)
        ev = (("d", q, i), self.dcnt[q][i])
        self._mark(ev, reads, writes, nowaw)
        self.ninst += 1
        return ev

    def idma(self, out, in_, out_off=None, in_off=None, reads=(), writes=(), nowaw=False, **kw):
        q = "pool"
        self._wait(q, self._deps(reads, writes, nowaw))
        i = self.drr[q]
        self.drr[q] = (i + 1) % self.NDS
        inst = self.engs[q].indirect_dma_start(
            out=out, out_offset=(bass.IndirectOffsetOnAxis(ap=out_off, axis=0) if out_off is not None else None),
            in_=in_, in_offset=(bass.IndirectOffsetOnAxis(ap=in_off, axis=0) if in_off is not None else None), **kw)
        self.dcnt[q][i] += 16
        inst.then_inc(self.dsem[q][i], 16)
        ev = (("d", q, i), self.dcnt[q][i])
        self._mark(ev, reads, writes, nowaw)
        self.ninst += 1
        return ev

    def barrier(self):
        deps = {}
        for k, v in self.ccnt.items():
            if v:
                deps[("c", k)] = v
        for q in self.dcnt:
            for i, v in enumerate(self.dcnt[q]):
                if v:
                    deps[("d", q, i)] = v
        for eng in self.engs:
            d2 = dict(deps)
            self._wait_all(eng, d2)

    def _wait_all(self, eng, deps):
        seen = self.seen[eng]
        for key, val in sorted(deps.items(), key=lambda kv: str(kv[0])):
            if key == ("c", eng):
                continue
            if seen.get(key, 0) >= val:
                continue
            self.engs[eng].wait_ge(self._sem(key), val)
            seen[key] = val

    def finish(self, eng, resources):
        deps = {}
        for r in resources:
            evs = list(r.wa.items()) + ([r.wx] if r.wx is not None else [])
            for k, v in evs:
                if deps.get(k, 0) < v:
                    deps[k] = v
        self._wait(eng, deps)


class T:
    def __init__(self, t, name=""):
        self.t = t
        self.res = Res(name)

    def __getitem__(self, k):
        return self.t[k]


class KB:
    def __init__(self, nc, es, dbg=None):
        self.nc = nc
        self.es = es
        self.s = Sched(nc, es)
        self.dbg = dbg
        self.n = 0

    def sb(self, es, shape, dt, name=None):
        self.n += 1
        name = "%s_%d" % (name or "sb", self.n)
        return T(es.enter_context(self.nc.sbuf_tensor(name, list(shape), dt)), name)

    def ps(self, es, shape, dt=F32, name=None):
        self.n += 1
        name = "%s_%d" % (name or "ps", self.n)
        return T(es.enter_context(self.nc.psum_tensor(name, list(shape), dt)), name)

    def dram(self, name, shape, dt, kind="Internal"):
        return T(self.nc.dram_tensor(name, list(shape), dt, kind=kind).ap(), name)


def rd(*ts):
    return [t.res for t in ts]


class NS:
    pass


class Cut(Exception):
    pass


def cutpt(C, k):
    return C.cut == k


def sub(bank, c0, c1, name=""):
    t = T(bank.t[:, c0:c1], name)
    t.res = bank.res
    bank.res.excl = True
    return t


def bcast_row(C, st, dst_ap, dst_t, src_ap, src_t, n, pbank):
    kb, s = C.kb, C.kb.s
    row = C.bcrow
    s.dma("sp", row[0:1, 0:n], src_ap, rd(src_t), rd(row))
    for c0 in range(0, n, 512):
        w = min(512, n - c0)
        s.op("pe", lambda e: e.matmul(out=pbank[:, 0:w], lhsT=C.ones[:], rhs=row[:, c0:c0 + w], start=True, stop=True),
             rd(C.ones, row), rd(pbank))
        s.op("dve", lambda e: e.tensor_copy(out=dst_ap[:, c0:c0 + w], in_=pbank[:, 0:w]), rd(pbank), rd(dst_t))


def stage_gdn(C, l):
    kb, s = C.kb, C.kb.s
    ident, ones, masks, psb = C.ident, C.ones, C.masks, C.psb
    CUMS = (0, 1)
    STRICT = (2, 3)
    INCLT = (0, 1)
    with ExitStack() as st:
        st.enter_context(C.kb.nc.named_scope("gdnprep_l%d" % l))
        convw = kb.sb(st, [128, 12, 5], F32, "convw")
        for j in range(5):
            s.dma("sp", convw[:, :, j], C.conv_w[l, j].rearrange("(c p) -> p c", p=128), rd(C.conv_w), rd(convw),
                  allow_slow_non_contiguous=True)
        if C.cut == 1:
            s.dma("sp", C.o_dbg[:, 0:128], ones[:], rd(ones), rd(C.o_dbg))
            return
        dtb8 = kb.sb(st, [128, 8], F32, "dtb8")
        bcast_row(C, st, dtb8[:], dtb8, C.dt_bias[l:l + 1, :], C.dt_bias, 8, psb[0])
        negA8 = kb.sb(st, [128, 8], F32, "negA8")
        bcast_row(C, st, negA8[:], negA8, C.a_log[l:l + 1, :], C.a_log, 8, psb[0])
        s.op("act", lambda e: e.activation(out=negA8[:], in_=negA8[:], func=AF.Exp), rd(negA8), rd(negA8))
        s.op("dve", lambda e: e.tensor_scalar(out=negA8[:], in0=negA8[:], scalar1=-1.0, scalar2=None, op0=ALU.mult),
             rd(negA8), rd(negA8))
        if C.cut == 2:
            s.dma("sp", C.o_dbg[:, 0:128], ones[:], rd(ones), rd(C.o_dbg))
            return
        BETA = kb.sb(st, [128, NTT, 8], F32, "BETA")
        NBETA = kb.sb(st, [128, NTT, 8], F32, "NBETA")
        GC = kb.sb(st, [128, NTT, 8], F32, "GC")
        EG = kb.sb(st, [128, NTT, 8], F32, "EG")
        EGR = kb.sb(st, [128, NTT, 8], F32, "EGR")
        CD = kb.sb(st, [128, NTT, 8], F32, "CD")
        BEG = kb.sb(st, [128, NTT, 8], F32, "BEG")
        ba = kb.sb(st, [128, NTT, 16], F32, "ba")
        s.dma("sp", ba[:], C.PTOK.t[:, 512:528].rearrange("(n p) c -> p n c", p=128), rd(C.PTOK), rd(ba))
        s.op("act", lambda e: e.activation(out=BETA[:], in_=ba[:, :, 0:8], func=AF.Sigmoid), rd(ba), rd(BETA))
        s.op("dve", lambda e: e.tensor_scalar(out=NBETA[:], in0=BETA[:], scalar1=-1.0, scalar2=None, op0=ALU.mult),
             rd(BETA), rd(NBETA))
        if C.cut == 3:
            s.dma("sp", C.o_dbg[:, 0:128], ones[:], rd(ones), rd(C.o_dbg))
            return
        xg = kb.sb(st, [128, NTT, 8], F32, "xg")
        ag = kb.sb(st, [128, NTT, 8], F32, "ag")
        gg = kb.sb(st, [128, NTT, 8], F32, "gg")
        for n in range(NTT):
            s.op("dve", lambda e: e.tensor_tensor(out=xg[:, n, :], in0=ba[:, n, 8:16], in1=dtb8[:], op=ALU.add),
                 rd(ba, dtb8), rd(xg))
        s.op("act", lambda e: e.activation(out=ag[:], in_=xg[:], func=AF.Abs), rd(xg), rd(ag))
        s.op("act", lambda e: e.activation(out=ag[:], in_=ag[:], func=AF.Exp, scale=-1.0), rd(ag), rd(ag))
        s.op("dve", lambda e: e.tensor_scalar_add(out=ag[:], in0=ag[:], scalar1=1.0), rd(ag), rd(ag))
        s.op("act", lambda e: e.activation(out=ag[:], in_=ag[:], func=AF.Ln), rd(ag), rd(ag))
        s.op("dve", lambda e: e.tensor_scalar(out=xg[:], in0=xg[:], scalar1=0.0, scalar2=None, op0=ALU.max),
             rd(xg), rd(xg))
        s.op("dve", lambda e: e.tensor_tensor(out=gg[:], in0=xg[:], in1=ag[:], op=ALU.add), rd(xg, ag), rd(gg))
        for n in range(NTT):
            s.op("dve", lambda e: e.tensor_tensor(out=gg[:, n, :], in0=gg[:, n, :], in1=negA8[:], op=ALU.mult),
                 rd(gg, negA8), rd(gg))
        if C.cut == 4:
            s.dma("sp", C.o_dbg[:, 0:128], ones[:], rd(ones), rd(C.o_dbg))
            return
        pG = sub(psb[0], 0, NTT * 8, "pG")
        pGt = sub(psb[1], 0, NTT * 8, "pGt")
        ggv = gg[:].rearrange("p n c -> p (n c)")
        for n in range(NTT):
            for d in range(2):
                s.op("pe", lambda e: e.matmul(out=pG[:, n * 8 + d * 4:n * 8 + d * 4 + 4], lhsT=masks[:, CUMS[d], :],
                                              rhs=gg[:, n, d * 4:d * 4 + 4], start=True, stop=True),
                     rd(masks, gg), rd(pG))
        for c0 in range(0, NTT * 8, 136):
            s.op("pe", lambda e: e.matmul(out=pGt[:, c0:c0 + 136], lhsT=ones[:], rhs=ggv[:, c0:c0 + 136],
                                          start=True, stop=True), rd(ones, gg), rd(pGt))
        if C.cut == 5:
            s.dma("sp", C.o_dbg[:, 0:128], ones[:], rd(ones), rd(C.o_dbg))
            return
        GCv = GC[:].rearrange("p n c -> p (n c)")
        s.op("dve", lambda e: e.tensor_copy(out=GCv, in_=pG[:, :]), rd(pG), rd(GC))
        if C.cut == 6:
            s.dma("sp", C.o_dbg[:, 0:272], GC[:].rearrange("p n c -> p (n c)"), rd(GC), rd(C.o_dbg))
            s.dma("sp", C.o_dbg[:, 272:544], gg[:].rearrange("p n c -> p (n c)"), rd(gg), rd(C.o_dbg))
            s.dma("sp", C.o_dbg[:, 544:816], BETA[:].rearrange("p n c -> p (n c)"), rd(BETA), rd(C.o_dbg))
            return
        if os.environ.get("GSKIP") != "EG":
            if os.environ.get("GEG") == "psum":
                s.op("act", lambda e: e.activation(out=EG[:].rearrange("p n c -> p (n c)"), in_=pG[:, :], func=AF.Exp),
                     rd(pG), rd(EG))
            else:
                s.op("act", lambda e: e.activation(out=EG[:], in_=GC[:], func=AF.Exp), rd(GC), rd(EG))
        if os.environ.get("GSKIP") != "CD":
            s.op("act", lambda e: e.activation(out=CD[:].rearrange("p n c -> p (n c)"), in_=pGt[:, :], func=AF.Exp),
                 rd(pGt), rd(CD))
        if C.cut == 7:
            s.dma("sp", C.o_dbg[:, 0:128], ones[:], rd(ones), rd(C.o_dbg))
            return
        s.op("dve", lambda e: e.tensor_tensor(out=EGR[:].rearrange("p n c -> p (n c)"), in0=pGt[:, :], in1=GCv,
                                              op=ALU.subtract), rd(pGt, GC), rd(EGR))
        s.op("act", lambda e: e.activation(out=EGR[:], in_=EGR[:], func=AF.Exp), rd(EGR), rd(EGR))
        if C.cut == 8:
            s.dma("sp", C.o_dbg[:, 0:128], ones[:], rd(ones), rd(C.o_dbg))
            return
        s.op("dve", lambda e: e.tensor_tensor(out=BEG[:], in0=BETA[:], in1=EG[:], op=ALU.mult), rd(BETA, EG), rd(BEG))
        if C.cut == 9:
            s.dma("sp", C.o_dbg[:, 0:128], ones[:], rd(ones), rd(C.o_dbg))
            return

        if C.dbg_stage == "gdnA":
            s.dma("sp", C.o_dbg[:, 0:272], GC[:].rearrange("p n c -> p (n c)"), rd(GC), rd(C.o_dbg))
            s.dma("sp", C.o_dbg[:, 272:544], BEG[:].rearrange("p n c -> p (n c)"), rd(BEG), rd(C.o_dbg))
            s.dma("sp", C.o_dbg[:, 544:816], EGR[:].rearrange("p n c -> p (n c)"), rd(EGR), rd(C.o_dbg))
            return
        W = NT + 4
        xpad = [kb.sb(st, [128, NT + 8], F32, "xpad%d" % i) for i in range(2)]
        for xp in xpad:
            s.op("pool", lambda e: e.memset(xp[:], 0.0), (), rd(xp))
        cs = [kb.sb(st, [128, W], F32, "cs%d" % i) for i in range(3)]
        acc = kb.sb(st, [128, W], F32, "cacc")
        def mk(name, *dims):
            def rec(pref, ds):
                if not ds:
                    return kb.sb(st, [128, 128], F32, pref)
                return [rec("%s_%d" % (pref, i), ds[1:]) for i in range(ds[0])]
            return rec(name, dims)
        vtok, ktok, qtok, kT, qT, junk = (mk(nm, 2) for nm in ("vtok", "ktok", "qtok", "kT", "qT", "gjunk"))
        ssq = [kb.sb(st, [128, 2], F32, "ssq%d" % i) for i in range(2)]
        kqT = [kb.sb(st, [128, 256], F32, "kqT%d" % i) for i in range(2)]
        diagG, Mm, t1, t2, Xa, Xb, XTa, XTb, TT, vb, kbg, qd = (mk(nm, 2, 2) for nm in (
            "diagG", "Mm", "t1", "t2", "Xa", "Xb", "XTa", "XTb", "TT", "vb", "kbg", "qd"))
        OPS = [[[kb.sb(st, [128, OPW], F32, "OPS%d%d%d" % (sl, d, i)) for i in range(2)] for d in range(2)] for sl in range(2)]
        opar = [[0, 0], [0, 0]]

        def chain(h, n, sl, d, pkkq):
            col = d * 4 + h
            bank = psb[3 * sl + 1 + d]
            r0, r1, r2, r3 = (sub(bank, i * 128, (i + 1) * 128) for i in range(4))
            ops = OPS[sl][d][opar[sl][d]]
            opar[sl][d] ^= 1
            gcol = GC[:, n, col:col + 1]
            dG, M_, T1, T2, TT_ = diagG[sl][d], Mm[sl][d], t1[sl][d], t2[sl][d], TT[sl][d]
            ktk, qtk, vtk = ktok[sl], qtok[sl], vtok[sl]
            s.op("act", lambda e: e.activation(out=dG[:], in_=ident[:], func=AF.Copy, scale=gcol), rd(ident, GC), rd(dG))
            yield
            s.op("pe", lambda e: e.matmul(out=r0[:, :], lhsT=ones[:], rhs=dG[:], start=True, stop=True), rd(ones, dG), rd(r0))
            yield
            s.op("dve", lambda e: e.scalar_tensor_tensor(out=T1[:], in0=r0[:, :], scalar=gcol, in1=masks[:, 4 + d, :],
                                                         op0=ALU.subtract, op1=ALU.max), rd(r0, GC, masks), rd(T1))
            s.op("dve", lambda e: e.scalar_tensor_tensor(out=T2[:], in0=r0[:, :], scalar=gcol, in1=masks[:, 6 + d, :],
                                                         op0=ALU.subtract, op1=ALU.min), rd(r0, GC, masks), rd(T2))
            yield
            s.op("act", lambda e: e.activation(out=T1[:], in_=T1[:], func=AF.Exp, scale=-1.0), rd(T1), rd(T1))
            s.op("act", lambda e: e.activation(out=T2[:], in_=T2[:], func=AF.Exp), rd(T2), rd(T2))
            yield
            Xc, XTc, Xn, XTn = Xa[sl][d], XTa[sl][d], Xb[sl][d], XTb[sl][d]
            s.op("dve", lambda e: e.scalar_tensor_tensor(out=fr(Xc[:]), in0=pkkq[:, 0:128], scalar=NBETA[:, n, col:col + 1], in1=T1[:],
                                                         op0=ALU.mult, op1=ALU.mult), rd(pkkq, NBETA, T1), rd(Xc))
            s.op("dve", lambda e: e.tensor_tensor(out=ops[:, 384:512], in0=pkkq[:, 128:256], in1=T2[:], op=ALU.mult), rd(pkkq, T2), rd(ops), nowaw=True)
            yield
            s.op("pe", lambda e: e.transpose(out=r3[:, :], in_=Xc[:], identity=ident[:]), rd(Xc, ident), rd(r3))
            yield
            s.op("act", lambda e: e.copy(out=fr(XTc[:]), in_=r3[:, :]), rd(r3), rd(XTc))
            s.op("dve", lambda e: e.tensor_tensor(out=fr(TT_[:]), in0=r3[:, :], in1=ident[:], op=ALU.add), rd(r3, ident), rd(TT_))
            yield
            for m in range(1, 7):
                s.op("pe", lambda e: e.matmul(out=r0[:, :], lhsT=fr(XTc[:]), rhs=fr(Xc[:]), start=True, stop=True), rd(XTc, Xc), rd(r0))
                if m < 6:
                    s.op("pe", lambda e: e.matmul(out=r1[:, :], lhsT=fr(Xc[:]), rhs=fr(XTc[:]), start=True, stop=True), rd(XTc, Xc), rd(r1))
                yield
                s.op("act", lambda e: e.copy(out=fr(Xn[:]), in_=r0[:, :]), rd(r0), rd(Xn))
                if m < 6:
                    s.op("dve", lambda e: e.tensor_copy(out=fr(XTn[:]), in_=r1[:, :]), rd(r1), rd(XTn))
                yield
                s.op("pe", lambda e: e.matmul(out=r2[:, :], lhsT=fr(Xn[:]), rhs=fr(TT_[:]), start=True, stop=True), rd(Xn, TT_), rd(r2))
                yield
                s.op("dve", lambda e: e.tensor_tensor(out=fr(TT_[:]), in0=TT_[:], in1=r2[:, :], op=ALU.add), rd(TT_, r2), rd(TT_))
                yield
                Xc, XTc, Xn, XTn = Xn, XTn, Xc, XTc
            vb_, kbg_, qd_ = vb[sl][d], kbg[sl][d], qd[sl][d]
            s.op("act", lambda e: e.activation(out=vb_[:], in_=vtk[:], func=AF.Copy, scale=BETA[:, n, col:col + 1]), rd(vtk, BETA), rd(vb_))
            s.op("act", lambda e: e.activation(out=kbg_[:], in_=ktk[:], func=AF.Copy, scale=BEG[:, n, col:col + 1]), rd(ktk, BEG), rd(kbg_))
            s.op("dve", lambda e: e.tensor_scalar(out=qd_[:], in0=qtk[:], scalar1=EG[:, n, col:col + 1], scalar2=None, op0=ALU.mult), rd(qtk, EG), rd(qd_))
            s.op("pool", lambda e: e.tensor_scalar(out=ops[:, 512:640], in0=ktk[:], scalar1=EGR[:, n, col:col + 1], scalar2=0.0, op0=ALU.mult, op1=ALU.add),
                 rd(ktk, EGR), rd(ops), nowaw=True)
            s.op("pool", lambda e: e.tensor_copy(out=ops[:, 640:648], in_=CD[:, n, :]), rd(CD), rd(ops), nowaw=True)
            yield
            s.op("pe", lambda e: e.matmul(out=r3[:, :], lhsT=fr(kbg_[:]), rhs=fr(TT_[:]), start=True, stop=True), rd(kbg_, TT_), rd(r3))
            s.op("pe", lambda e: e.matmul(out=r0[:, :], lhsT=fr(TT_[:]), rhs=fr(vb_[:]), start=True, stop=True), rd(vb_, TT_), rd(r0))
            s.op("pe", lambda e: e.transpose(out=r1[:, :], in_=qd_[:], identity=ident[:]), rd(qd_, ident), rd(r1))
            yield
            s.op("act", lambda e: e.copy(out=ops[:, 0:128], in_=r3[:, :]), rd(r3), rd(ops), nowaw=True)
            s.op("dve", lambda e: e.tensor_copy(out=ops[:, 128:256], in_=r0[:, :]), rd(r0), rd(ops), nowaw=True)
            s.op("act", lambda e: e.copy(out=ops[:, 256:384], in_=r1[:, :]), rd(r1), rd(ops), nowaw=True)
            yield
            s.dma("sp", C.OPSD[h, d, n], ops[:], rd(ops), rd(C.OPSD), nowaw=True)
            if C.dbg_stage == "gdnB":
                s.dma("sp", C.o_dbgB[d], ops[:], rd(ops), rd(C.o_dbgB))
                s.dma("sp", C.o_dbgB[2 + d, :, 0:128], TT_[:], rd(TT_), rd(C.o_dbgB))
                s.dma("sp", C.o_dbgB[2 + d, :, 384:512], ktk[:], rd(ktk), rd(C.o_dbgB))

        def chunk(h, n, sl):
            col0 = n * 128 if n < L // 128 else L + 4 + (n - L // 128) * 128
            pQKV = sub(psb[3 * sl], 0, 384)
            bk0 = sub(psb[3 * sl + 1], 0, 128)
            bk1 = sub(psb[3 * sl + 2], 0, 128)
            for which in range(3):
                s.op("pe", lambda e: e.transpose(out=pQKV[:, which * 128:(which + 1) * 128], in_=cs[which][:, col0:col0 + 128],
                                                 identity=ident[:]), rd(cs[which], ident), rd(pQKV))
            yield
            s.op("act", lambda e: e.copy(out=vtok[sl][:], in_=pQKV[:, 256:384]), rd(pQKV), rd(vtok[sl]))
            for w_ in range(2):
                s.op("act", lambda e: e.activation(out=junk[sl][:], in_=pQKV[:, w_ * 128:(w_ + 1) * 128], func=AF.Square,
                                                   accum_out=ssq[sl][:, w_:w_ + 1]), rd(pQKV), rd(junk[sl], ssq[sl]))
            yield
            s.op("dve", lambda e: e.tensor_scalar_add(out=ssq[sl][:], in0=ssq[sl][:], scalar1=RMS_EPS), rd(ssq[sl]), rd(ssq[sl]))
            yield
            s.op("act", lambda e: e.sqrt(out=ssq[sl][:], in_=ssq[sl][:]), rd(ssq[sl]), rd(ssq[sl]))
            yield
            s.op("dve", lambda e: e.reciprocal(out=ssq[sl][:], in_=ssq[sl][:]), rd(ssq[sl]), rd(ssq[sl]))
            s.op("dve", lambda e: e.tensor_scalar(out=qtok[sl][:], in0=pQKV[:, 0:128], scalar1=ssq[sl][:, 0:1],
                                                  scalar2=128.0 ** -0.5, op0=ALU.mult, op1=ALU.mult), rd(pQKV, ssq[sl]), rd(qtok[sl]))
            s.op("dve", lambda e: e.tensor_scalar(out=ktok[sl][:], in0=pQKV[:, 128:256], scalar1=ssq[sl][:, 1:2],
                                                  scalar2=None, op0=ALU.mult), rd(pQKV, ssq[sl]), rd(ktok[sl]))
            yield
            s.op("pe", lambda e: e.transpose(out=bk0[:, :], in_=ktok[sl][:], identity=ident[:]), rd(ktok[sl], ident), rd(bk0))
            s.op("pe", lambda e: e.transpose(out=bk1[:, :], in_=qtok[sl][:], identity=ident[:]), rd(qtok[sl], ident), rd(bk1))
            yield
            s.op("act", lambda e: e.copy(out=kqT[sl][:, 0:128], in_=bk0[:, :]), rd(bk0), rd(kqT[sl]), nowaw=True)
            s.op("dve", lambda e: e.tensor_copy(out=kqT[sl][:, 128:256], in_=bk1[:, :]), rd(bk1), rd(kqT[sl]), nowaw=True)
            yield
            pkkq = sub(psb[3 * sl], 0, 256)
            s.op("pe", lambda e: e.matmul(out=pkkq[:, :], lhsT=kqT[sl][:, 0:128], rhs=kqT[sl][:, :], start=True, stop=True),
                 rd(kqT[sl]), rd(pkkq))
            yield
            gens = [chain(h, n, sl, 0, pkkq), chain(h, n, sl, 1, pkkq)]
            while gens:
                for g in list(gens):
                    try:
                        next(g)
                    except StopIteration:
                        gens.remove(g)
                yield

        nheads = 4 if C.dbg_stage != "gdnB" else 1
        for h in range(nheads):
            for which in range(3):
                cc = which * 4 + h
                xp = xpad[(h * 3 + which) % 2]
                s.dma("sp", xp[:, 2:2 + L], C.QKVT[cc * 128:(cc + 1) * 128, 0:L], rd(C.QKVT), rd(xp))
                s.dma("pool", xp[:, L + 6:L + 6 + LC], C.QKVT[cc * 128:(cc + 1) * 128, L:NT], rd(C.QKVT), rd(xp))
                s.op("dve", lambda e: e.tensor_scalar(out=acc[:], in0=xp[:, 0:W], scalar1=convw[:, cc, 0:1],
                                                      scalar2=None, op0=ALU.mult), rd(xp, convw), rd(acc))
                for j in range(1, 5):
                    s.op("dve", lambda e: e.scalar_tensor_tensor(out=acc[:], in0=xp[:, j:j + W],
                                                                 scalar=convw[:, cc, j:j + 1], in1=acc[:],
                                                                 op0=ALU.mult, op1=ALU.add), rd(xp, convw, acc), rd(acc))
                s.op("act", lambda e: e.activation(out=cs[which][:], in_=acc[:], func=AF.Silu), rd(acc), rd(cs[which]))
            pending = list(range(NTT if C.dbg_stage != "gdnB" else 2))
            active = {}
            while pending or active:
                for sl in (0, 1):
                    if sl not in active and pending:
                        active[sl] = chunk(h, pending.pop(0), sl)
                for sl in list(active):
                    try:
                        next(active[sl])
                    except StopIteration:
                        del active[sl]
    if C.dbg_stage in ("gdnprep", "gdnB"):
        return
    s.barrier()
    with ExitStack() as st:
        st.enter_context(C.kb.nc.named_scope("gdnscan_l%d" % l))
        S = [[kb.sb(st, [128, 128], F32, "S%d%d" % (h, d)) for d in range(2)] for h in range(4)]
        for h in range(4):
            for d in range(2):
                s.op("pool", lambda e: e.memset(S[h][d][:], 0.0), (), rd(S[h][d]))
        NOB = 16
        OB = [kb.sb(st, [128, OPW], F32, "OB%d" % i) for i in range(NOB)]
        vnew = [kb.sb(st, [128, 128], F32, "vnew%d" % i) for i in range(8)]
        oev = [kb.sb(st, [128, 128], F32, "oev%d" % i) for i in range(8)]
        order = [[32, 33] + list(range(32)), [33, 32] + list(range(31, -1, -1))]
        p1 = [sub(psb[h], d * 128, d * 128 + 128, "p1") for h in range(4) for d in range(2)]
        p2 = [sub(psb[h], 256 + d * 128, 384 + d * 128, "p2") for h in range(4) for d in range(2)]
        p3 = [sub(psb[4 + h], d * 128, d * 128 + 128, "p3") for h in range(4) for d in range(2)]
        k = 0
        for step in range(NTT):
            for d in range(2):
                n = order[d][step]
                for h in range(4):
                    c = h * 2 + d
                    ob = OB[k % NOB]
                    k += 1
                    s.dma("sp" if k % 2 == 0 else "pool", ob[:], C.OPSD[h, d, n], rd(C.OPSD), rd(ob))
                    Sd = S[h][d]
                    s.op("pe", lambda e: e.matmul(out=p1[c][:, :], lhsT=ob[:, 0:128], rhs=Sd[:], start=True, stop=True),
                         rd(ob, Sd), rd(p1[c]))
                    s.op("dve", lambda e: e.tensor_tensor(out=vnew[c][:], in0=ob[:, 128:256], in1=p1[c][:, :], op=ALU.subtract),
                         rd(ob, p1[c]), rd(vnew[c]))
                    s.op("pe", lambda e: e.matmul(out=p2[c][:, :], lhsT=ob[:, 256:384], rhs=Sd[:], start=True, stop=False),
                         rd(ob, Sd), rd(p2[c]))
                    s.op("pe", lambda e: e.matmul(out=p2[c][:, :], lhsT=ob[:, 384:512], rhs=vnew[c][:], start=False, stop=True),
                         rd(ob, vnew[c]), rd(p2[c]))
                    s.op("pe", lambda e: e.matmul(out=p3[c][:, :], lhsT=ob[:, 512:640], rhs=vnew[c][:], start=True, stop=True),
                         rd(ob, vnew[c]), rd(p3[c]))
                    s.op("act", lambda e: e.copy(out=oev[c][:], in_=p2[c][:, :]), rd(p2[c]), rd(oev[c]))
                    s.dma("sp", C.OFB[d][n * 128:(n + 1) * 128, h * 128:(h + 1) * 128], oev[c][:], rd(oev[c]), rd(C.OFB[d]), nowaw=True)
                    s.op("dve", lambda e: e.scalar_tensor_tensor(out=Sd[:], in0=Sd[:], scalar=ob[:, 640 + c_col(d, h):641 + c_col(d, h)],
                                                                 in1=p3[c][:, :], op0=ALU.mult, op1=ALU.add),
                         rd(Sd, ob, p3[c]), rd(Sd))
    if C.dbg_stage == "gdnscan":
        return
    s.barrier()
    with ExitStack() as st:
        st.enter_context(C.kb.nc.named_scope("gdnfin_l%d" % l))
        gnw = kb.sb(st, [128, 128], F32, "gnw")
        bcast_row(C, st, gnw[:], gnw, C.gdn_norm_w[l:l + 1, :], C.gdn_norm_w, 128, psb[2])
        NBF = 2
        of = [kb.sb(st, [128, 512], F32, "of%d" % i) for i in range(NBF)]
        obk = [kb.sb(st, [128, 512], F32, "obk%d" % i) for i in range(NBF)]
        zz = [kb.sb(st, [128, 512], F32, "zz%d" % i) for i in range(NBF)]
        yy = [kb.sb(st, [128, 512], F32, "yy%d" % i) for i in range(NBF)]
        sq = [kb.sb(st, [128, 4], F32, "sq%d" % i) for i in range(NBF)]
        junk = kb.sb(st, [128, 128], F32, "junk2")
        gT = [kb.sb(st, [128, 512], BF16, "gT%d" % i) for i in range(NBF)]
        for n in range(NTT):
            b = n % NBF
            pb = psb[n % 2]
            s.dma("sp", of[b][:], C.OFB[0][n * 128:(n + 1) * 128, :], rd(C.OFB[0]), rd(of[b]))
            s.dma("pool", obk[b][:], C.OFB[1][n * 128:(n + 1) * 128, :], rd(C.OFB[1]), rd(obk[b]))
            s.dma("sp", zz[b][:], C.PTOK[n * 128:(n + 1) * 128, 0:512], rd(C.PTOK), rd(zz[b]))
            s.op("dve", lambda e: e.tensor_tensor(out=of[b][:], in0=of[b][:], in1=obk[b][:], op=ALU.add), rd(of[b], obk[b]), rd(of[b]))
            s.op("act", lambda e: e.activation(out=zz[b][:], in_=zz[b][:], func=AF.Silu), rd(zz[b]), rd(zz[b]))
            for h in range(4):
                s.op("act", lambda e: e.activation(out=junk[:], in_=of[b][:, h * 128:(h + 1) * 128], func=AF.Square,
                                                   accum_out=sq[b][:, h:h + 1]), rd(of[b]), rd(junk, sq[b]))
            s.op("dve", lambda e: e.tensor_scalar(out=sq[b][:], in0=sq[b][:], scalar1=1.0 / 128.0, scalar2=RMS_EPS,
                                                  op0=ALU.mult, op1=ALU.add), rd(sq[b]), rd(sq[b]))
            s.op("act", lambda e: e.sqrt(out=sq[b][:], in_=sq[b][:]), rd(sq[b]), rd(sq[b]))
            s.op("dve", lambda e: e.reciprocal(out=sq[b][:], in_=sq[b][:]), rd(sq[b]), rd(sq[b]))
            for h in range(4):
                s.op("pool", lambda e: e.tensor_tensor(out=zz[b][:, h * 128:(h + 1) * 128], in0=zz[b][:, h * 128:(h + 1) * 128],
                                                       in1=gnw[:], op=ALU.mult), rd(zz[b], gnw), rd(zz[b]))
            for h in range(4):
                s.op("dve", lambda e: e.scalar_tensor_tensor(out=yy[b][:, h * 128:(h + 1) * 128], in0=of[b][:, h * 128:(h + 1) * 128],
                                                             scalar=sq[b][:, h:h + 1], in1=zz[b][:, h * 128:(h + 1) * 128],
                                                             op0=ALU.mult, op1=ALU.mult), rd(of[b], sq[b], zz[b]), rd(yy[b]))
            for h in range(4):
                s.op("pe", lambda e: e.transpose(out=pb[:, h * 128:(h + 1) * 128], in_=yy[b][:, h * 128:(h + 1) * 128],
                                                 identity=ident[:]), rd(yy[b], ident), rd(pb))
            s.op("act", lambda e: e.copy(out=gT[b][:], in_=pb[:, :]), rd(pb), rd(gT[b]))
            s.dma("sp", C.MIXT.t[0:512, n * 128:(n + 1) * 128].rearrange("(h p) t -> p h t", p=128),
                  gT[b][:].rearrange("p (h t) -> p h t", h=4), rd(gT[b]), rd(C.MIXT), nowaw=True)
            if C.dbg_stage == "gdn":
                s.dma("sp", C.o_gdn[n * 128:(n + 1) * 128, :], yy[b][:], rd(yy[b]), rd(C.o_gdn))


def stage_att(C, l):
    kb, s = C.kb, C.kb.s
    ident, psb = C.ident, C.psb
    with ExitStack() as st:
        st.enter_context(C.kb.nc.named_scope("att_l%d" % l))
        QT = kb.sb(st, [128, 4, NT], BF16, "QT")
        KT = kb.sb(st, [128, 2, NT], BF16, "KT")
        Vb = kb.sb(st, [128, NTT, 256], BF16, "Vb")
        onesb = kb.sb(st, [128, 128], BF16, "onesb")
        s.op("pool", lambda e: e.memset(onesb[:], 1.0), (), rd(onesb))
        w6 = kb.sb(st, [128, 2, 128], F32, "w6")
        bcast_row(C, st, w6[:, 0, :], w6, C.q_norm_w[l:l + 1, :], C.q_norm_w, 128, psb[0])
        bcast_row(C, st, w6[:, 1, :], w6, C.k_norm_w[l:l + 1, :], C.k_norm_w, 128, psb[0])
        NB = 2
        xa = [kb.sb(st, [128, 1024], F32, "xa%d" % i) for i in range(NB)]
        xn = [kb.sb(st, [128, 768], F32, "xnq%d" % i) for i in range(NB)]
        xr = [kb.sb(st, [128, 768], F32, "xr%d" % i) for i in range(NB)]
        cs_ = [kb.sb(st, [128, 2, 384], F32, "cs%d" % i) for i in range(NB)]
        tmp = [kb.sb(st, [128, 4, 384], F32, "rtmp%d" % i) for i in range(NB)]
        ssq = [kb.sb(st, [128, 6], F32, "assq%d" % i) for i in range(NB)]
        junk = kb.sb(st, [128, 128], F32, "ajunk")
        for t in range(NTT):
            b = t % NB
            lat = t < L // 128
            s.dma("sp", xa[b][:], C.PTOK[t * 128:(t + 1) * 128, 528:1552], rd(C.PTOK), rd(xa[b]))
            if lat:
                s.dma("pool", cs_[b][:], C.rope_in.t[:, t * 128:(t + 1) * 128, :].rearrange("c p f -> p c f"),
                      rd(C.rope_in), rd(cs_[b]))
            for h in range(6):
                s.op("act", lambda e: e.activation(out=junk[:], in_=xa[b][:, h * 128:(h + 1) * 128], func=AF.Square,
                                                   accum_out=ssq[b][:, h:h + 1]), rd(xa[b]), rd(junk, ssq[b]))
            s.op("dve", lambda e: e.tensor_scalar(out=ssq[b][:], in0=ssq[b][:], scalar1=1.0 / 128.0, scalar2=RMS_EPS,
                                                  op0=ALU.mult, op1=ALU.add), rd(ssq[b]), rd(ssq[b]))
            s.op("act", lambda e: e.sqrt(out=ssq[b][:], in_=ssq[b][:]), rd(ssq[b]), rd(ssq[b]))
            s.op("dve", lambda e: e.reciprocal(out=ssq[b][:], in_=ssq[b][:]), rd(ssq[b]), rd(ssq[b]))
            for h in range(6):
                s.op("dve", lambda e: e.scalar_tensor_tensor(out=xn[b][:, h * 128:(h + 1) * 128], in0=xa[b][:, h * 128:(h + 1) * 128],
                                                             scalar=ssq[b][:, h:h + 1], in1=w6[:, 0 if h < 4 else 1, :],
                                                             op0=ALU.mult, op1=ALU.mult), rd(xa[b], ssq[b], w6), rd(xn[b]))
            s.op("pool", lambda e: e.tensor_copy(out=Vb[:, t, :], in_=xa[b][:, 768:1024]), rd(xa[b]), rd(Vb), nowaw=True)
            if lat:
                x1 = xn[b][:].rearrange("p (g two f) -> p g two f", two=2, f=32)[:, :, 0, :]
                x2 = xn[b][:].rearrange("p (g two f) -> p g two f", two=2, f=32)[:, :, 1, :]
                o1 = xr[b][:].rearrange("p (g two f) -> p g two f", two=2, f=32)[:, :, 0, :]
                o2 = xr[b][:].rearrange("p (g two f) -> p g two f", two=2, f=32)[:, :, 1, :]
                cc = cs_[b][:, 0, :].rearrange("p (g f) -> p g f", f=32)
                sn = cs_[b][:, 1, :].rearrange("p (g f) -> p g f", f=32)
                tm = [tmp[b][:, i, :].rearrange("p (g f) -> p g f", f=32) for i in range(4)]
                s.op("dve", lambda e: e.tensor_tensor(out=tm[0], in0=x1, in1=cc, op=ALU.mult), rd(xn[b], cs_[b]), rd(tmp[b]))
                s.op("pool", lambda e: e.tensor_tensor(out=tm[1], in0=x2, in1=sn, op=ALU.mult), rd(xn[b], cs_[b]), rd(tmp[b]))
                s.op("dve", lambda e: e.tensor_tensor(out=tm[2], in0=x2, in1=cc, op=ALU.mult), rd(xn[b], cs_[b]), rd(tmp[b]))
                s.op("pool", lambda e: e.tensor_tensor(out=tm[3], in0=x1, in1=sn, op=ALU.mult), rd(xn[b], cs_[b]), rd(tmp[b]))
                s.op("dve", lambda e: e.tensor_tensor(out=o1, in0=tm[0], in1=tm[1], op=ALU.subtract), rd(tmp[b]), rd(xr[b]))
                s.op("pool", lambda e: e.tensor_tensor(out=o2, in0=tm[2], in1=tm[3], op=ALU.add), rd(tmp[b]), rd(xr[b]))
                src = xr[b]
            else:
                src = xn[b]
            for h in range(6):
                pb = psb[1] if h < 4 else psb[2]
                s.op("pe", lambda e: e.transpose(out=pb[:, (h % 4) * 128:(h % 4 + 1) * 128], in_=src[:, h * 128:(h + 1) * 128],
                                                 identity=ident[:]), rd(src, ident), rd(pb))
            s.op("act", lambda e: e.copy(out=QT[:, :, t * 128:(t + 1) * 128],
                                         in_=psb[1][:, :].rearrange("p (h t) -> p h t", h=4)), rd(psb[1]), rd(QT), nowaw=True)
            s.op("dve", lambda e: e.tensor_copy(out=KT[:, :, t * 128:(t + 1) * 128],
                                                in_=psb[2][:, 0:256].rearrange("p (h t) -> p h t", h=2)), rd(psb[2]), rd(KT), nowaw=True)
        if C.dbg_stage == "attq":
            s.dma("sp", C.o_dbg[:, 0:512].bitcast(BF16)[:, 0:512], QT[:, 1, 0:512], rd(QT), rd(C.o_dbg))
            s.dma("sp", C.o_dbg[:, 512:1024].bitcast(BF16)[:, 0:512], KT[:, 1, 0:512], rd(KT), rd(C.o_dbg))
            return
        PT = [kb.sb(st, [128, 512], BF16, "PT%d" % i) for i in range(3)]
        rec = [kb.sb(st, [128, 512], F32, "rec%d" % i) for i in range(2)]
        aT = [kb.sb(st, [128, 512], BF16, "aT%d" % i) for i in range(2)]
        jobs = []
        for h in range(4):
            for q0 in range(0, L, 512):
                jobs.append((h, q0, 512, list(range(NTT))))
            jobs.append((h, L, LC, [32, 33]))
        scale = 128.0 ** -0.5
        k = 0
        for ji, (h, q0, qw, ktiles) in enumerate(jobs):
            kv = h // 2
            pO = psb[0] if ji % 2 == 0 else psb[5]
            pS = psb[1] if ji % 2 == 0 else psb[6]
            nk = len(ktiles)
            slots = []
            for i in range(nk):
                slots.append((psb[2 + k % 3], PT[k % 3]))
                k += 1

            def qk(i):
                pst, _ = slots[i]
                kt = ktiles[i]
                s.op("pe", lambda e: e.matmul(out=pst[:, 0:qw], lhsT=KT[:, kv, kt * 128:(kt + 1) * 128], rhs=QT[:, h, q0:q0 + qw],
                                              start=True, stop=True), rd(KT, QT), rd(pst))
            qk(0)
            for i, kt in enumerate(ktiles):
                pst, pt = slots[i]
                if i + 1 < nk:
                    qk(i + 1)
                s.op("act", lambda e: e.activation(out=pt[:, 0:qw], in_=pst[:, 0:qw], func=AF.Exp, scale=scale), rd(pst), rd(pt))
                s.op("pe", lambda e: e.matmul(out=pO[:, 0:qw], lhsT=Vb[:, kt, kv * 128:(kv + 1) * 128], rhs=pt[:, 0:qw],
                                              start=(i == 0), stop=(i == nk - 1)), rd(Vb, pt), rd(pO))
                s.op("pe", lambda e: e.matmul(out=pS[:, 0:qw], lhsT=onesb[:], rhs=pt[:, 0:qw],
                                              start=(i == 0), stop=(i == nk - 1)), rd(onesb, pt), rd(pS))
            rc = rec[ji % 2]
            at = aT[ji % 2]
            s.op("dve", lambda e: e.reciprocal(out=rc[:, 0:qw], in_=pS[:, 0:qw]), rd(pS), rd(rc))
            s.op("dve", lambda e: e.tensor_tensor(out=at[:, 0:qw], in0=pO[:, 0:qw], in1=rc[:, 0:qw], op=ALU.mult), rd(pO, rc), rd(at))
            s.dma("sp", C.MIXT[512 + h * 128:512 + (h + 1) * 128, q0:q0 + qw], at[:, 0:qw], rd(at), rd(C.MIXT), nowaw=True)


def load_bf16_w(C, st, dram_t, src_ap_fn, kchunks, ncols, name):
    kb, s = C.kb, C.kb.s
    w = kb.sb(st, [128, kchunks, ncols], BF16, name)
    for kc in range(kchunks):
        for c0 in range(0, ncols, 2048):
            c1 = min(ncols, c0 + 2048)
            s.dma("pool", w[:, kc, c0:c1], src_ap_fn(kc, c0, c1), rd(dram_t), rd(w))
    return w


def stage_D(C, l, M):
    kb, s = C.kb, C.kb.s
    ident, psb = C.ident, C.psb
    Xsrc = C.XS if l == 0 else C.X2
    with ExitStack() as st:
        st.enter_context(C.kb.nc.named_scope("D_l%d" % l))
        wout = load_bf16_w(C, st, C.w_out, lambda kc, c0, c1: C.w_out[l, kc * 128:(kc + 1) * 128, c0:c1], 8, D, "wout")
        wsgu = kb.sb(st, [128, 8, 512], BF16, "wsgu")
        for kc in range(8):
            s.dma("pool", wsgu[:, kc, 0:256], C.sh_w_gate[l, kc * 128:(kc + 1) * 128, :], rd(C.sh_w_gate), rd(wsgu))
            s.dma("pool", wsgu[:, kc, 256:512], C.sh_w_up[l, kc * 128:(kc + 1) * 128, :], rd(C.sh_w_up), rd(wsgu))
        wsd = load_bf16_w(C, st, C.sh_w_down, lambda kc, c0, c1: C.sh_w_down[l, kc * 128:(kc + 1) * 128, c0:c1], 2, D, "wsd")
        rw = kb.sb(st, [128, 8, 256], F32, "rw")
        s.dma("sp", rw[:], C.router_w.t[l].rearrange("(k p) e -> p k e", p=128), rd(C.router_w), rd(rw))
        G1 = kb.sb(st, [128, 2, D], F32, "G1")
        SC2 = kb.sb(st, [128, 2, D], F32, "SC2")
        SH2 = kb.sb(st, [128, 2, D], F32, "SH2")
        for r in range(2):
            bcast_row(C, st, G1[:, r, :], G1, C.MODD[l, r:r + 1, 2048:3072], C.MODD, D, psb[0])
            bcast_row(C, st, SH2[:, r, :], SH2, C.MODD[l, r:r + 1, 3072:4096], C.MODD, D, psb[0])
            bcast_row(C, st, SC2[:, r, :], SC2, C.MODD[l, r:r + 1, 4096:5120], C.MODD, D, psb[0])
        s.op("dve", lambda e: e.tensor_scalar_add(out=SC2[:], in0=SC2[:], scalar1=1.0), rd(SC2), rd(SC2))
        LNW = kb.sb(st, [128, D], F32, "LNW")
        LNB = kb.sb(st, [128, D], F32, "LNB")
        bcast_row(C, st, LNW[:], LNW, C.ln1_w[l:l + 1, :], C.ln1_w, D, psb[0])
        bcast_row(C, st, LNB[:], LNB, C.ln1_b[l:l + 1, :], C.ln1_b, D, psb[0])
        RB = kb.sb(st, [128, 256], F32, "RB")
        bcast_row(C, st, RB[:], RB, C.router_bias[l:l + 1, :], C.router_bias, 256, psb[0])
        NB = 2
        mixT = [kb.sb(st, [128, 8, 128], BF16, "mixT%d" % i) for i in range(NB)]
        xt = [kb.sb(st, [128, D], F32, "xt%d" % i) for i in range(NB)]
        tt = [kb.sb(st, [128, D], F32, "tt%d" % i) for i in range(NB)]
        x1 = [kb.sb(st, [128, D], F32, "x1%d" % i) for i in range(NB)]
        h2 = [kb.sb(st, [128, D], F32, "h2%d" % i) for i in range(NB)]
        h2Tf = [kb.sb(st, [128, 8, 128], F32, "h2Tf%d" % i) for i in range(NB)]
        h2Tb = [kb.sb(st, [128, 8, 128], BF16, "h2Tb%d" % i) for i in range(NB)]
        stats = [kb.sb(st, [128, 2, 6], F32, "dst%d" % i) for i in range(NB)]
        mv = [kb.sb(st, [128, 2], F32, "dmv%d" % i) for i in range(NB)]
        rstd = [kb.sb(st, [128, 1], F32, "drs%d" % i) for i in range(NB)]
        sg = [kb.sb(st, [128, 256], F32, "sg%d" % i) for i in range(NB)]
        sel = [kb.sb(st, [128, 256], F32, "sel%d" % i) for i in range(NB)]
        selm = [kb.sb(st, [128, 256], F32, "selm%d" % i) for i in range(NB)]
        g8 = [kb.sb(st, [128, 8, 8], F32, "g8%d" % i) for i in range(NB)]
        grp = [kb.sb(st, [128, 8], F32, "grp%d" % i) for i in range(NB)]
        gs8 = [kb.sb(st, [128, 8], F32, "gs8%d" % i) for i in range(NB)]
        gm = [kb.sb(st, [128, 8], F32, "gm%d" % i) for i in range(NB)]
        t8 = [kb.sb(st, [128, 8], F32, "t8%d" % i) for i in range(NB)]
        den = [kb.sb(st, [128, 1], F32, "den%d" % i) for i in range(NB)]
        sact = [kb.sb(st, [128, 256], F32, "sact%d" % i) for i in range(NB)]
        sact2 = [kb.sb(st, [128, 256], F32, "sactb%d" % i) for i in range(NB)]
        sactT = [kb.sb(st, [128, 2, 128], BF16, "sactT%d" % i) for i in range(NB)]
        sho = [kb.sb(st, [128, D], F32, "sho%d" % i) for i in range(NB)]
        for t in range(NTT):
            b = t % NB
            r = 0 if t < L // 128 else 1
            rows = slice(t * 128, (t + 1) * 128)
            s.dma("sp", mixT[b][:], C.MIXT.t[:, rows].rearrange("(k p) t -> p k t", p=128), rd(C.MIXT), rd(mixT[b]))
            s.dma("sp", xt[b][:], Xsrc[rows, :], rd(Xsrc), rd(xt[b]))
            for cb in range(2):
                for kc in range(8):
                    s.op("pe", lambda e: e.matmul(out=psb[cb][:, :], lhsT=mixT[b][:, kc, :], rhs=wout[:, kc, cb * 512:(cb + 1) * 512],
                                                  start=(kc == 0), stop=(kc == 7)), rd(mixT[b], wout), rd(psb[cb]))
            for cb in range(2):
                cs = slice(cb * 512, (cb + 1) * 512)
                s.op("dve", lambda e: e.tensor_tensor(out=tt[b][:, cs], in0=psb[cb][:, :], in1=G1[:, r, cs], op=ALU.mult),
                     rd(psb[cb], G1), rd(tt[b]))
            if C.dbg_stage == "D":
                s.dma("sp", C.o_y[rows, :], tt[b][:], rd(tt[b]), rd(C.o_y))
            s.op("dve", lambda e: e.scalar_tensor_tensor(out=tt[b][:], in0=xt[b][:], scalar=ALPHA, in1=tt[b][:],
                                                         op0=ALU.mult, op1=ALU.add), rd(xt[b], tt[b]), rd(tt[b]))
            layer_norm_tile(C, tt[b], x1[b], stats[b], mv[b], rstd[b], LNW, LNB)
            s.dma("sp", C.X1[rows, :], x1[b][:], rd(x1[b]), rd(C.X1), nowaw=True)
            s.op("dve", lambda e: e.tensor_tensor(out=h2[b][:], in0=x1[b][:], in1=SC2[:, r, :], op=ALU.mult), rd(x1[b], SC2), rd(h2[b]))
            s.op("dve", lambda e: e.tensor_tensor(out=h2[b][:], in0=h2[b][:], in1=SH2[:, r, :], op=ALU.add), rd(h2[b], SH2), rd(h2[b]))
            s.dma("sp", C.H2[rows, :], h2[b][:], rd(h2[b]), rd(C.H2), nowaw=True)
            for kc in range(8):
                pb = psb[2 + kc // 4]
                s.op("pe", lambda e: e.transpose(out=pb[:, (kc % 4) * 128:(kc % 4 + 1) * 128], in_=h2[b][:, kc * 128:(kc + 1) * 128],
                                                 identity=ident[:]), rd(h2[b], ident), rd(pb))
            for hh in range(2):
                s.op("act", lambda e: e.copy(out=h2Tf[b][:, hh * 4:hh * 4 + 4, :], in_=psb[2 + hh][:, :].rearrange("p (k t) -> p k t", k=4)),
                     rd(psb[2 + hh]), rd(h2Tf[b]), nowaw=True)
                s.op("dve", lambda e: e.tensor_copy(out=h2Tb[b][:, hh * 4:hh * 4 + 4, :], in_=psb[2 + hh][:, :].rearrange("p (k t) -> p k t", k=4)),
                     rd(psb[2 + hh]), rd(h2Tb[b]), nowaw=True)
            for kc in range(8):
                s.op("pe", lambda e: e.matmul(out=psb[4][:, 0:256], lhsT=h2Tf[b][:, kc, :], rhs=rw[:, kc, :],
                                              start=(kc == 0), stop=(kc == 7)), rd(h2Tf[b], rw), rd(psb[4]))
            s.op("act", lambda e: e.activation(out=sg[b][:], in_=psb[4][:, 0:256], func=AF.Sigmoid), rd(psb[4]), rd(sg[b]))
            s.op("dve", lambda e: e.tensor_tensor(out=sel[b][:], in0=sg[b][:], in1=RB[:], op=ALU.add), rd(sg[b], RB), rd(sel[b]))
            for g in range(8):
                s.op("dve", lambda e: e.max(out=g8[b][:, g, :], in_=sel[b][:, g * 32:(g + 1) * 32]), rd(sel[b]), rd(g8[b]))
            s.op("dve", lambda e: e.tensor_tensor(out=grp[b][:], in0=g8[b][:, :, 0], in1=g8[b][:, :, 1], op=ALU.add), rd(g8[b]), rd(grp[b]))
            s.op("dve", lambda e: e.max(out=gs8[b][:], in_=grp[b][:]), rd(grp[b]), rd(gs8[b]))
            s.op("dve", lambda e: e.tensor_scalar(out=gm[b][:], in0=grp[b][:], scalar1=gs8[b][:, 3:4], scalar2=None, op0=ALU.is_ge),
                 rd(grp[b], gs8[b]), rd(gm[b]))
            for g in range(8):
                s.op("dve", lambda e: e.tensor_scalar(out=selm[b][:, g * 32:(g + 1) * 32], in0=sel[b][:, g * 32:(g + 1) * 32],
                                                      scalar1=2.0, scalar2=gm[b][:, g:g + 1], op0=ALU.add, op1=ALU.mult),
                     rd(sel[b], gm[b]), rd(selm[b]))
            s.op("dve", lambda e: e.max(out=t8[b][:], in_=selm[b][:]), rd(selm[b]), rd(t8[b]))
            s.op("dve", lambda e: e.tensor_scalar(out=sel[b][:], in0=selm[b][:], scalar1=t8[b][:, 7:8], scalar2=None, op0=ALU.is_ge),
                 rd(selm[b], t8[b]), rd(sel[b]))
            s.op("dve", lambda e: e.tensor_tensor(out=M.WFULL[:, t, :], in0=sg[b][:], in1=sel[b][:], op=ALU.mult),
                 rd(sg[b], sel[b]), rd(M.WFULL))
            s.op("dve", lambda e: e.reduce_sum(out=den[b][:], in_=M.WFULL[:, t, :], axis=AX.X), rd(M.WFULL), rd(den[b]))
            s.op("dve", lambda e: e.reciprocal(out=den[b][:], in_=den[b][:]), rd(den[b]), rd(den[b]))
            s.op("dve", lambda e: e.tensor_scalar(out=M.WFULL[:, t, :], in0=M.WFULL[:, t, :], scalar1=den[b][:, 0:1], scalar2=2.5,
                                                  op0=ALU.mult, op1=ALU.mult), rd(M.WFULL, den[b]), rd(M.WFULL))
            for kc in range(8):
                s.op("pe", lambda e: e.matmul(out=psb[5][:, :], lhsT=h2Tb[b][:, kc, :], rhs=wsgu[:, kc, :],
                                              start=(kc == 0), stop=(kc == 7)), rd(h2Tb[b], wsgu), rd(psb[5]))
            s.op("act", lambda e: e.activation(out=sact[b][:], in_=psb[5][:, 0:256], func=AF.Silu), rd(psb[5]), rd(sact[b]))
            s.op("dve", lambda e: e.tensor_tensor(out=sact2[b][:], in0=sact[b][:], in1=psb[5][:, 256:512], op=ALU.mult),
                 rd(sact[b], psb[5]), rd(sact2[b]))
            for k2 in range(2):
                s.op("pe", lambda e: e.transpose(out=psb[4][:, 256 + k2 * 128:384 + k2 * 128], in_=sact2[b][:, k2 * 128:(k2 + 1) * 128],
                                                 identity=ident[:]), rd(sact2[b], ident), rd(psb[4]))
            s.op("act", lambda e: e.copy(out=sactT[b][:], in_=psb[4][:, 256:512].rearrange("p (k t) -> p k t", k=2)), rd(psb[4]), rd(sactT[b]))
            for cb in range(2):
                for k2 in range(2):
                    s.op("pe", lambda e: e.matmul(out=psb[6 + cb][:, :], lhsT=sactT[b][:, k2, :], rhs=wsd[:, k2, cb * 512:(cb + 1) * 512],
                                                  start=(k2 == 0), stop=(k2 == 1)), rd(sactT[b], wsd), rd(psb[6 + cb]))
                if cb == 0:
                    s.op("act", lambda e: e.copy(out=sho[b][:, 0:512], in_=psb[6][:, :]), rd(psb[6]), rd(sho[b]), nowaw=True)
                else:
                    s.op("dve", lambda e: e.tensor_copy(out=sho[b][:, 512:1024], in_=psb[7][:, :]), rd(psb[7]), rd(sho[b]), nowaw=True)
            s.dma("sp", C.SHO[rows, :], sho[b][:], rd(sho[b]), rd(C.SHO), nowaw=True)


def layer_norm_tile(C, src, dst, stats, mv, rstd, LNW, LNB):
    s = C.kb.s
    for j in range(2):
        s.op("dve", lambda e: e.bn_stats(out=stats[:, j, :], in_=src[:, j * 512:(j + 1) * 512]), rd(src), rd(stats))
    s.op("dve", lambda e: e.bn_aggr(out=mv[:], in_=stats[:].rearrange("p a b -> p (a b)")), rd(stats), rd(mv))
    s.op("dve", lambda e: e.tensor_scalar_add(out=rstd[:], in0=mv[:, 1:2], scalar1=LN_EPS), rd(mv), rd(rstd))
    s.op("act", lambda e: e.sqrt(out=rstd[:], in_=rstd[:]), rd(rstd), rd(rstd))
    s.op("dve", lambda e: e.reciprocal(out=rstd[:], in_=rstd[:]), rd(rstd), rd(rstd))
    s.op("dve", lambda e: e.tensor_scalar(out=dst[:], in0=src[:], scalar1=mv[:, 0:1], scalar2=rstd[:, 0:1],
                                          op0=ALU.subtract, op1=ALU.mult), rd(src, mv, rstd), rd(dst))
    s.op("dve", lambda e: e.tensor_tensor(out=dst[:], in0=dst[:], in1=LNW[:], op=ALU.mult), rd(dst, LNW), rd(dst))
    s.op("dve", lambda e: e.tensor_tensor(out=dst[:], in0=dst[:], in1=LNB[:], op=ALU.add), rd(dst, LNB), rd(dst))


TAB_LAYERS = DEPTH
BS = 256
NBLK = 391
NSTL = NBLK * 2
NSLOT = NSTL * 128


def stage_E(C, l, M):
    kb, s = C.kb, C.kb.s
    ident, ones, masks, psb = C.ident, C.ones, C.masks, C.psb
    WF = M.WFULL
    with ExitStack() as st:
        iot = kb.sb(st, [128, NBLK + 1 + NTT], F32, "iot")
        s.dma("sp", iot[:], C.iotas[:], rd(C.iotas), rd(iot))
        IDXW = kb.sb(st, [128, NBLK], I32, "IDXW")
        SLOT8 = kb.sb(st, [128, NTT, 8], I32, "SLOT8")
        BT = kb.sb(st, [128, NSTL, 2], F32, "BT")
        IDXT = kb.sb(st, [128, NSTL], I32, "IDXT")
        with ExitStack() as s1:
            s1.enter_context(C.kb.nc.named_scope("E1_l%d" % l))
            RANK = kb.sb(s1, [128, NTT, 256], F32, "RANK")
            cnt = kb.sb(s1, [128, 256], F32, "cnt")
            s.op("pool", lambda e: e.memset(cnt[:], 0.0), (), rd(cnt))
            mk = [kb.sb(s1, [128, 256], F32, "mk%d" % i) for i in range(2)]
            for t in range(NTT):
                b = t % 2
                s.op("dve", lambda e: e.tensor_scalar(out=mk[b][:], in0=WF[:, t, :], scalar1=0.0, scalar2=None, op0=ALU.is_gt),
                     rd(WF), rd(mk[b]))
                s.op("pe", lambda e: e.matmul(out=psb[0][:, 0:256], lhsT=masks[:, 3, :], rhs=mk[b][:], start=True, stop=True),
                     rd(masks, mk[b]), rd(psb[0]))
                s.op("pe", lambda e: e.matmul(out=psb[1][:, 0:256], lhsT=ones[:], rhs=mk[b][:], start=True, stop=True),
                     rd(ones, mk[b]), rd(psb[1]))
                s.op("dve", lambda e: e.tensor_tensor(out=RANK[:, t, :], in0=psb[0][:, 0:256], in1=cnt[:], op=ALU.add),
                     rd(psb[0], cnt), rd(RANK))
                s.op("dve", lambda e: e.tensor_tensor(out=cnt[:], in0=cnt[:], in1=psb[1][:, 0:256], op=ALU.add),
                     rd(psb[1], cnt), rd(cnt))
            ci = kb.sb(s1, [128, 256], I32, "ci")
            nblk = kb.sb(s1, [128, 256], F32, "nblk")
            blkend = kb.sb(s1, [128, 256], F32, "blkend")
            bs256 = kb.sb(s1, [128, 256], F32, "bs256")
            s.op("dve", lambda e: e.tensor_scalar(out=ci[:], in0=cnt[:], scalar1=float(BS - 1), scalar2=None, op0=ALU.add), rd(cnt), rd(ci))
            s.op("dve", lambda e: e.tensor_scalar(out=ci[:], in0=ci[:], scalar1=8, scalar2=None, op0=ALU.arith_shift_right), rd(ci), rd(ci))
            s.op("dve", lambda e: e.tensor_copy(out=nblk[:], in_=ci[:]), rd(ci), rd(nblk))
            s.op("dve", lambda e: e.tensor_tensor_scan(out=blkend[:], data0=ones[:, 0:128].to_broadcast([128, 256]) if False else C.ones256[:],
                                                       data1=nblk[:], initial=0.0, op0=ALU.mult, op1=ALU.add), rd(nblk, C.ones256), rd(blkend))
            s.op("dve", lambda e: e.tensor_tensor(out=bs256[:], in0=blkend[:], in1=nblk[:], op=ALU.subtract), rd(blkend, nblk), rd(bs256))
            s.op("dve", lambda e: e.tensor_scalar(out=bs256[:], in0=bs256[:], scalar1=float(BS), scalar2=1.0, op0=ALU.mult, op1=ALU.add),
                 rd(bs256), rd(bs256))
            bcol = kb.sb(s1, [128, 2], F32, "bcol")
            tmpd = kb.sb(s1, [128, 128], F32, "tmpd")
            cmpb = [kb.sb(s1, [128, NBLK], F32, "cmpb%d" % i) for i in range(2)]
            for c in range(2):
                s.op("dve", lambda e: e.tensor_tensor(out=tmpd[:], in0=blkend[:, c * 128:(c + 1) * 128], in1=ident[:], op=ALU.mult),
                     rd(blkend, ident), rd(tmpd))
                s.op("dve", lambda e: e.reduce_sum(out=bcol[:, c:c + 1], in_=tmpd[:], axis=AX.X), rd(tmpd), rd(bcol))
                s.op("dve", lambda e: e.tensor_scalar(out=cmpb[c][:], in0=iot[:, 0:NBLK], scalar1=bcol[:, c:c + 1], scalar2=None, op0=ALU.is_ge),
                     rd(iot, bcol), rd(cmpb[c]))
                s.op("pe", lambda e: e.matmul(out=psb[2][:, 0:NBLK], lhsT=ones[:], rhs=cmpb[c][:], start=(c == 0), stop=(c == 1)),
                     rd(ones, cmpb[c]), rd(psb[2]))
            ebf = kb.sb(s1, [128, NBLK], F32, "ebf")
            oobf = kb.sb(s1, [128, NBLK], F32, "oobf")
            s.op("dve", lambda e: e.tensor_scalar(out=oobf[:], in0=psb[2][:, 0:NBLK], scalar1=255.5, scalar2=1.0e6, op0=ALU.is_ge, op1=ALU.mult),
                 rd(psb[2]), rd(oobf))
            s.op("dve", lambda e: e.tensor_scalar(out=ebf[:], in0=psb[2][:, 0:NBLK], scalar1=255.0, scalar2=128.0, op0=ALU.min, op1=ALU.mult),
                 rd(psb[2]), rd(ebf))
            s.op("dve", lambda e: e.tensor_tensor(out=ebf[:], in0=ebf[:], in1=oobf[:], op=ALU.add), rd(ebf, oobf), rd(ebf))
            s.op("dve", lambda e: e.tensor_scalar(out=ebf[:], in0=ebf[:], scalar1=iot[:, NBLK:NBLK + 1], scalar2=float(l * 32768),
                                                  op0=ALU.add, op1=ALU.add), rd(ebf, iot), rd(ebf))
            s.op("dve", lambda e: e.tensor_copy(out=IDXW[:], in_=ebf[:]), rd(ebf), rd(IDXW))
            zt = kb.sb(s1, [128, NSTL * 2], F32, "zt")
            s.op("pool", lambda e: e.memset(zt[:], 0.0), (), rd(zt))
            s.op("pool", lambda e: e.memset(zt[:].rearrange("p (n two) -> p n two", two=2)[:, :, 0], 60000.0), (), rd(zt))
            s.dma("sp", C.BUFTW.t.rearrange("(p n) two -> p (n two)", p=128), zt[:], rd(zt), rd(C.BUFTW))
            key = [kb.sb(s1, [128, 256], F32, "key%d" % i) for i in range(2)]
            top8 = [kb.sb(s1, [128, 8], F32, "top8%d" % i) for i in range(2)]
            oh = [kb.sb(s1, [128, 256], F32, "oh%d" % i) for i in range(2)]
            tw = [kb.sb(s1, [128, 8, 2], F32, "tw%d" % i) for i in range(2)]
            si = [kb.sb(s1, [128, 8], I32, "si%d" % i) for i in range(2)]
            lo = [kb.sb(s1, [128, 8], I32, "lo%d" % i) for i in range(2)]
            hi = [kb.sb(s1, [128, 8], I32, "hi%d" % i) for i in range(2)]
            lof = [kb.sb(s1, [128, 8], F32, "lof%d" % i) for i in range(2)]
            hif = [kb.sb(s1, [128, 8], F32, "hif%d" % i) for i in range(2)]
            posi = [kb.sb(s1, [128, 8], I32, "posi%d" % i) for i in range(2)]
            for t in range(NTT):
                b = t % 2
                s.op("dve", lambda e: e.tensor_scalar(out=mk[b][:], in0=WF[:, t, :], scalar1=0.0, scalar2=None, op0=ALU.is_gt),
                     rd(WF), rd(mk[b]))
                s.op("dve", lambda e: e.tensor_tensor(out=key[b][:], in0=RANK[:, t, :], in1=bs256[:], op=ALU.add), rd(RANK, bs256), rd(key[b]))
                s.op("dve", lambda e: e.tensor_tensor(out=key[b][:], in0=key[b][:], in1=mk[b][:], op=ALU.mult), rd(key[b], mk[b]), rd(key[b]))
                s.op("dve", lambda e: e.max(out=top8[b][:], in_=key[b][:]), rd(key[b]), rd(top8[b]))
                for k in range(8):
                    s.op("dve", lambda e: e.tensor_scalar(out=oh[b][:], in0=key[b][:], scalar1=top8[b][:, k:k + 1], scalar2=None, op0=ALU.is_equal),
                         rd(key[b], top8[b]), rd(oh[b]))
                    s.op("dve", lambda e: e.tensor_tensor(out=oh[b][:], in0=oh[b][:], in1=WF[:, t, :], op=ALU.mult), rd(oh[b], WF), rd(oh[b]))
                    s.op("dve", lambda e: e.reduce_sum(out=tw[b][:, k, 1:2], in_=oh[b][:], axis=AX.X), rd(oh[b]), rd(tw[b]))
                    s.op("pool", lambda e: e.tensor_copy(out=tw[b][:, k, 0:1], in_=iot[:, NBLK + 1 + t:NBLK + 2 + t]), rd(iot), rd(tw[b]))
                s.op("dve", lambda e: e.tensor_scalar(out=si[b][:], in0=top8[b][:], scalar1=-1.0, scalar2=None, op0=ALU.add), rd(top8[b]), rd(si[b]))
                s.op("dve", lambda e: e.tensor_copy(out=SLOT8[:, t, :], in_=si[b][:]), rd(si[b]), rd(SLOT8))
                s.op("dve", lambda e: e.tensor_scalar(out=lo[b][:], in0=si[b][:], scalar1=127, scalar2=None, op0=ALU.bitwise_and), rd(si[b]), rd(lo[b]))
                s.op("dve", lambda e: e.tensor_scalar(out=hi[b][:], in0=si[b][:], scalar1=7, scalar2=None, op0=ALU.arith_shift_right), rd(si[b]), rd(hi[b]))
                s.op("dve", lambda e: e.tensor_copy(out=lof[b][:], in_=lo[b][:]), rd(lo[b]), rd(lof[b]))
                s.op("dve", lambda e: e.tensor_copy(out=hif[b][:], in_=hi[b][:]), rd(hi[b]), rd(hif[b]))
                s.op("dve", lambda e: e.scalar_tensor_tensor(out=lof[b][:], in0=lof[b][:], scalar=float(NSTL), in1=hif[b][:],
                                                             op0=ALU.mult, op1=ALU.add), rd(lof[b], hif[b]), rd(lof[b]))
                s.op("dve", lambda e: e.tensor_copy(out=posi[b][:], in_=lof[b][:]), rd(lof[b]), rd(posi[b]))
                for k in range(8):
                    s.idma(out=C.BUFTW[:, :], in_=tw[b][:, k, :], out_off=posi[b][:, k:k + 1], reads=rd(tw[b], posi[b]), writes=rd(C.BUFTW), nowaw=True)
            s.dma("sp", BT[:], C.BUFTW.t.rearrange("(p n) two -> p n two", p=128), rd(C.BUFTW), rd(BT))
            s.op("dve", lambda e: e.tensor_copy(out=IDXT[:], in_=BT[:, :, 0]), rd(BT), rd(IDXT))
            if C.dbg_stage == "E1":
                s.dma("sp", C.o_e1[:, 0:NSTL * 2], BT[:].rearrange("p n two -> p (n two)"), rd(BT), rd(C.o_e1))
                s.dma("sp", C.o_e1[:, 2000:2000 + NBLK].bitcast(I32), IDXW[:], rd(IDXW), rd(C.o_e1))
                s.dma("sp", C.o_e1[:, 2400:2400 + NTT * 8].bitcast(I32), SLOT8[:].rearrange("p n k -> p (n k)"), rd(SLOT8), rd(C.o_e1))
                s.dma("sp", C.o_e1[:, 2700:2956], cnt[:], rd(cnt), rd(C.o_e1))
                return

        s.barrier()
        with ExitStack() as s2:
            s2.enter_context(C.kb.nc.named_scope("E2_l%d" % l))
            NW = 2
            wg = [kb.sb(s2, [128, 2048], BF16, "wg%d" % i) for i in range(NW)]
            wu = [kb.sb(s2, [128, 2048], BF16, "wu%d" % i) for i in range(NW)]
            wd = [kb.sb(s2, [128, 2048], BF16, "wd%d" % i) for i in range(NW)]
            NX = 3
            xg = [kb.sb(s2, [128, D], F32, "xg%d" % i) for i in range(NX)]
            for x_ in xg:
                s.op("pool", lambda e: e.memset(x_[:], 0.0), (), rd(x_))
            xT = [kb.sb(s2, [128, 8, 128], BF16, "xTe%d" % i) for i in range(2)]
            ga = [kb.sb(s2, [128, 256], F32, "ga%d" % i) for i in range(2)]
            a2 = [kb.sb(s2, [128, 256], F32, "a2%d" % i) for i in range(2)]
            aT = [kb.sb(s2, [128, 2, 128], BF16, "aTe%d" % i) for i in range(2)]
            eo = [kb.sb(s2, [128, D], BF16, "eo%d" % i) for i in range(2)]
            nblk_run = NBLK if C.dbg_stage != "E2s" else 8
            for blk in range(nblk_run):
                wb = blk % NW
                ix = IDXW[:, blk:blk + 1]
                s.idma(out=wg[wb][:], in_=C.WG[:, :], in_off=ix, reads=rd(C.WG, IDXW), writes=rd(wg[wb]), bounds_check=C.reg_tab, oob_is_err=False)
                s.idma(out=wu[wb][:], in_=C.WU[:, :], in_off=ix, reads=rd(C.WU, IDXW), writes=rd(wu[wb]), bounds_check=C.reg_tab, oob_is_err=False)
                s.idma(out=wd[wb][:], in_=C.WD[:, :], in_off=ix, reads=rd(C.WD, IDXW), writes=rd(wd[wb]), bounds_check=C.reg_tab, oob_is_err=False)
                for sti in range(2):
                    j = blk * 2 + sti
                    xb = xg[j % NX]
                    b2 = j % 2
                    s.idma(out=xb[:], in_=C.H2[:, :], in_off=IDXT[:, j:j + 1], reads=rd(C.H2, IDXT), writes=rd(xb), bounds_check=C.reg_nt, oob_is_err=False)
                    for kc in range(8):
                        pb = psb[kc // 4]
                        s.op("pe", lambda e: e.transpose(out=pb[:, (kc % 4) * 128:(kc % 4 + 1) * 128], in_=xb[:, kc * 128:(kc + 1) * 128],
                                                         identity=ident[:]), rd(xb, ident), rd(pb))
                    s.op("act", lambda e: e.copy(out=xT[b2][:, 0:4, :], in_=psb[0][:, :].rearrange("p (k t) -> p k t", k=4)), rd(psb[0]), rd(xT[b2]), nowaw=True)
                    s.op("dve", lambda e: e.tensor_copy(out=xT[b2][:, 4:8, :], in_=psb[1][:, :].rearrange("p (k t) -> p k t", k=4)), rd(psb[1]), rd(xT[b2]), nowaw=True)
                    for kc in range(8):
                        s.op("pe", lambda e: e.matmul(out=psb[2][:, 0:256], lhsT=xT[b2][:, kc, :], rhs=wg[wb][:, kc * 256:(kc + 1) * 256],
                                                      start=(kc == 0), stop=(kc == 7)), rd(xT[b2], wg[wb]), rd(psb[2]))
                    for kc in range(8):
                        s.op("pe", lambda e: e.matmul(out=psb[3][:, 0:256], lhsT=xT[b2][:, kc, :], rhs=wu[wb][:, kc * 256:(kc + 1) * 256],
                                                      start=(kc == 0), stop=(kc == 7)), rd(xT[b2], wu[wb]), rd(psb[3]))
                    s.op("act", lambda e: e.activation(out=ga[b2][:], in_=psb[2][:, 0:256], func=AF.Silu), rd(psb[2]), rd(ga[b2]))
                    s.op("dve", lambda e: e.tensor_tensor(out=a2[b2][:], in0=ga[b2][:], in1=psb[3][:, 0:256], op=ALU.mult), rd(ga[b2], psb[3]), rd(a2[b2]))
                    for k2 in range(2):
                        s.op("pe", lambda e: e.transpose(out=psb[4][:, k2 * 128:(k2 + 1) * 128], in_=a2[b2][:, k2 * 128:(k2 + 1) * 128],
                                                         identity=ident[:]), rd(a2[b2], ident), rd(psb[4]))
                    s.op("act", lambda e: e.copy(out=aT[b2][:], in_=psb[4][:, 0:256].rearrange("p (k t) -> p k t", k=2)), rd(psb[4]), rd(aT[b2]))
                    for cb in range(2):
                        for k2 in range(2):
                            s.op("pe", lambda e: e.matmul(out=psb[5 + cb][:, :], lhsT=aT[b2][:, k2, :],
                                                          rhs=wd[wb][:, k2 * 1024 + cb * 512:k2 * 1024 + (cb + 1) * 512],
                                                          start=(k2 == 0), stop=(k2 == 1)), rd(aT[b2], wd[wb]), rd(psb[5 + cb]))
                    s.op("act", lambda e: e.activation(out=eo[b2][:, 0:512], in_=psb[5][:, :], func=AF.Copy, scale=BT[:, j, 1:2]),
                         rd(psb[5], BT), rd(eo[b2]), nowaw=True)
                    s.op("dve", lambda e: e.tensor_scalar(out=eo[b2][:, 512:1024], in0=psb[6][:, :], scalar1=BT[:, j, 1:2], scalar2=None, op0=ALU.mult),
                         rd(psb[6], BT), rd(eo[b2]), nowaw=True)
                    s.dma("sp", C.EO[j * 128:(j + 1) * 128, :], eo[b2][:], rd(eo[b2]), rd(C.EO), nowaw=True)
            if C.dbg_stage in ("E2", "E2s"):
                return

        s.barrier()
        with ExitStack() as s3:
            s3.enter_context(C.kb.nc.named_scope("E3_l%d" % l))
            G2 = kb.sb(s3, [128, 2, D], F32, "G2")
            for r in range(2):
                bcast_row(C, s3, G2[:, r, :], G2, C.MODD[l, r:r + 1, 5120:6144], C.MODD, D, psb[7])
            LNW = kb.sb(s3, [128, D], F32, "LNW2")
            LNB = kb.sb(s3, [128, D], F32, "LNB2")
            bcast_row(C, s3, LNW[:], LNW, C.ln2_w[l:l + 1, :], C.ln2_w, D, psb[7])
            bcast_row(C, s3, LNB[:], LNB, C.ln2_b[l:l + 1, :], C.ln2_b, D, psb[7])
            gat = [kb.sb(s3, [128, D], BF16, "gat%d" % i) for i in range(4)]
            ffa = [kb.sb(s3, [128, D], F32, "ffa%d" % i) for i in range(2)]
            x1t = [kb.sb(s3, [128, D], F32, "x1t%d" % i) for i in range(2)]
            x2t = [kb.sb(s3, [128, D], F32, "x2t%d" % i) for i in range(2)]
            stats = [kb.sb(s3, [128, 2, 6], F32, "est%d" % i) for i in range(2)]
            mv = [kb.sb(s3, [128, 2], F32, "emv%d" % i) for i in range(2)]
            rstd = [kb.sb(s3, [128, 1], F32, "ers%d" % i) for i in range(2)]
            gi = 0
            for t in range(NTT):
                b = t % 2
                r = 0 if t < L // 128 else 1
                rows = slice(t * 128, (t + 1) * 128)
                s.dma("sp", ffa[b][:], C.SHO[rows, :], rd(C.SHO), rd(ffa[b]))
                s.dma("sp", x1t[b][:], C.X1[rows, :], rd(C.X1), rd(x1t[b]))
                for k in range(8):
                    g_ = gat[gi % 4]
                    gi += 1
                    s.idma(out=g_[:], in_=C.EO[:, :], in_off=SLOT8[:, t, k:k + 1], reads=rd(C.EO, SLOT8), writes=rd(g_))
                    eng = "dve"
                    s.op(eng, lambda e: e.tensor_tensor(out=ffa[b][:], in0=ffa[b][:], in1=g_[:], op=ALU.add), rd(ffa[b], g_), rd(ffa[b]))
                if C.dbg_stage == "E":
                    s.dma("sp", C.o_ff[rows, :], ffa[b][:], rd(ffa[b]), rd(C.o_ff))
                s.op("dve", lambda e: e.tensor_tensor(out=ffa[b][:], in0=ffa[b][:], in1=G2[:, r, :], op=ALU.mult), rd(ffa[b], G2), rd(ffa[b]))
                s.op("dve", lambda e: e.scalar_tensor_tensor(out=ffa[b][:], in0=x1t[b][:], scalar=ALPHA, in1=ffa[b][:],
                                                             op0=ALU.mult, op1=ALU.add), rd(x1t[b], ffa[b]), rd(ffa[b]))
                layer_norm_tile(C, ffa[b], x2t[b], stats[b], mv[b], rstd[b], LNW, LNB)
                s.dma("sp", C.X2[rows, :], x2t[b][:], rd(x2t[b]), rd(C.X2), nowaw=True)
                if l == DEPTH - 1 and t < L // 128 and C.out is not None:
                    s.dma("sp", C.out[rows, :], x2t[b][:], rd(x2t[b]), rd(C.out), nowaw=True)


def c_col(d, h):
    return d * 4 + h


USE_F32R = os.environ.get("F32R", "0") == "1"


def fr(ap):
    return ap.bitcast(mybir.dt.float32r) if USE_F32R else ap


def build(dbg_stage=None):
    nc = bass.Bass("TRN2", target_bir_lowering=False)
    es = ExitStack()
    kb = KB(nc, es)
    s = kb.s
    x_in = kb.dram("x", [NT, D], F32, kind="ExternalInput")
    c_in = kb.dram("c2", [2, D], F32, kind="ExternalInput")
    ada_w = kb.dram("ada_w", [DEPTH, D, 6 * D], F32, kind="ExternalInput")
    ada_b = kb.dram("ada_b", [DEPTH, 6 * D], F32, kind="ExternalInput")
    w_in = kb.dram("w_in", [DEPTH, D, N_IN], F32, kind="ExternalInput")
    ident_in = kb.dram("ident", [128, 128], F32, kind="ExternalInput")
    masks_in = kb.dram("masks", [8, 128, 128], F32, kind="ExternalInput")
    conv_w = kb.dram("conv_w", [DEPTH, 5, OFF_Z], F32, kind="ExternalInput")
    a_log = kb.dram("gdn_a_log", [DEPTH, 8], F32, kind="ExternalInput")
    dt_bias = kb.dram("gdn_dt_bias", [DEPTH, 8], F32, kind="ExternalInput")
    gdn_norm_w = kb.dram("gdn_norm_w", [DEPTH, 128], F32, kind="ExternalInput")
    q_norm_w = kb.dram("q_norm_w", [DEPTH, 128], F32, kind="ExternalInput")
    k_norm_w = kb.dram("k_norm_w", [DEPTH, 128], F32, kind="ExternalInput")
    rope_in = kb.dram("rope", [2, L, 384], F32, kind="ExternalInput")
    w_out = kb.dram("w_out", [DEPTH, D, D], F32, kind="ExternalInput")
    ln1_w = kb.dram("ln1_w", [DEPTH, D], F32, kind="ExternalInput")
    ln1_b = kb.dram("ln1_b", [DEPTH, D], F32, kind="ExternalInput")
    ln2_w = kb.dram("ln2_w", [DEPTH, D], F32, kind="ExternalInput")
    ln2_b = kb.dram("ln2_b", [DEPTH, D], F32, kind="ExternalInput")
    router_w = kb.dram("router_w", [DEPTH, D, 256], F32, kind="ExternalInput")
    router_bias = kb.dram("router_bias", [DEPTH, 256], F32, kind="ExternalInput")
    sh_w_gate = kb.dram("sh_w_gate", [DEPTH, D, 256], F32, kind="ExternalInput")
    sh_w_up = kb.dram("sh_w_up", [DEPTH, D, 256], F32, kind="ExternalInput")
    sh_w_down = kb.dram("sh_w_down", [DEPTH, 256, D], F32, kind="ExternalInput")
    iotas = kb.dram("iotas", [128, NBLK + 1 + NTT], F32, kind="ExternalInput")
    WG = WU = WD = None
    if dbg_stage in (None, "E1", "E2", "E2s", "E"):
        WG = kb.dram("WG", [TAB_LAYERS * 256 * 128, 2048], F32, kind="ExternalInput")
        WU = kb.dram("WU", [TAB_LAYERS * 256 * 128, 2048], F32, kind="ExternalInput")
        WD = kb.dram("WD", [TAB_LAYERS * 256 * 128, 2048], F32, kind="ExternalInput")
    outs = {}

    def dbg_out(name, shape, dt=F32):
        t = kb.dram(name, shape, dt, kind="ExternalOutput")
        outs[name] = t
        return t

    XS = kb.dram("XS", [NT, D], F32)
    MODD = kb.dram("MODD", [DEPTH, 2, 6 * D], F32)
    QKVT = kb.dram("QKVT", [OFF_Z, NT], F32)
    PTOK = kb.dram("PTOK", [NT, NTOKC], F32)
    OPSD = kb.dram("OPSD", [4, 2, NTT, 128, OPW], F32)
    OFB = [kb.dram("OFB%d" % d, [NT, 512], F32) for d in range(2)]
    MIXT = kb.dram("MIXT", [D, NT], BF16)
    X1 = kb.dram("X1", [NT, D], F32)
    X2 = kb.dram("X2", [NT, D], F32)
    H2 = kb.dram("H2", [NT, D], F32)
    SHO = kb.dram("SHO", [NT, D], F32)
    BUFTW = kb.dram("BUFTW", [NSLOT, 2], F32)
    EO = kb.dram("EO", [NSLOT, D], BF16)

    ces = es
    ident = kb.sb(ces, [128, 128], F32, "ident")
    s.dma("sp", ident[:], ident_in[:], reads=rd(ident_in), writes=rd(ident))
    identb = kb.sb(ces, [128, 128], BF16, "identb")
    s.op("dve", lambda e: e.tensor_copy(out=identb[:], in_=ident[:]), rd(ident), rd(identb))

    psb = [kb.ps(ces, [128, 512], F32, "bank%d" % i) for i in range(8)]
    for p_ in psb:
        p_.res.excl = True
    ones = kb.sb(ces, [128, 128], F32, "ones")
    s.op("pool", lambda e: e.memset(ones[:], 1.0), (), rd(ones))
    bcrow = kb.sb(ces, [128, D], F32, "bcrow")
    s.op("pool", lambda e: e.memset(bcrow[:], 0.0), (), rd(bcrow))
    masks = kb.sb(ces, [128, 8, 128], F32, "masks")
    ones256 = kb.sb(ces, [128, 256], F32, "ones256")
    s.op("pool", lambda e: e.memset(ones256[:], 1.0), (), rd(ones256))
    s.dma("sp", masks[:], masks_in.t.rearrange("m p f -> p m f"), rd(masks_in), rd(masks))

    final_out = dbg_out("out", [L, D]) if dbg_stage is None else None
    for l in range(DEPTH):
        with ExitStack() as st:
            st.enter_context(nc.named_scope("mod_l%d" % l))
            cT = kb.sb(st, [128, 8, 2], F32, "cT")
            craw = kb.sb(st, [2, D], F32, "craw")
            s.dma("sp", craw[:], c_in[:], rd(c_in), rd(craw))
            csil = kb.sb(st, [2, D], F32, "csil")
            s.op("act", lambda e: e.activation(out=csil[:], in_=craw[:], func=AF.Silu), rd(craw), rd(csil))
            for kc in range(8):
                s.op("pe", lambda e: e.transpose(out=psb[0][:, kc * 2:kc * 2 + 2], in_=csil[:, kc * 128:(kc + 1) * 128],
                                                 identity=ident[0:2, 0:2]), rd(csil, ident), rd(psb[0]))
            s.op("dve", lambda e: e.tensor_copy(out=cT[:].rearrange("p k r -> p (k r)"), in_=psb[0][:, 0:16]),
                 rd(psb[0]), rd(cT))
            modrow = kb.sb(st, [2, 6 * D], F32, "modrow")
            abrow = kb.sb(st, [2, 6 * D], F32, "abrow")
            for r in range(2):
                s.dma("sp", abrow[r:r + 1, :], ada_b[l:l + 1, :], rd(ada_b), rd(abrow))
            wch = [kb.sb(st, [128, 3072], F32, "adaw%d" % i) for i in range(2)]
            for half in range(2):
                for kc in range(8):
                    wt = wch[kc % 2]
                    s.dma("sp" if kc % 2 == 0 else "pool", wt[:],
                          ada_w[l, kc * 128:(kc + 1) * 128, half * 3072:(half + 1) * 3072], rd(ada_w), rd(wt))
                    for cb in range(6):
                        s.op("pe", lambda e: e.matmul(out=psb[cb][0:2, :], lhsT=cT[:, kc, :],
                                                      rhs=wt[:, cb * 512:(cb + 1) * 512],
                                                      start=(kc == 0), stop=(kc == 7)),
                             rd(cT, wt), rd(psb[cb]))
                for cb in range(6):
                    c0 = half * 3072 + cb * 512
                    s.op("dve", lambda e: e.tensor_tensor(out=modrow[:, c0:c0 + 512], in0=psb[cb][0:2, :],
                                                          in1=abrow[:, c0:c0 + 512], op=ALU.add),
                         rd(psb[cb], abrow), rd(modrow))
            s.dma("sp", MODD[l], modrow[:], rd(modrow), rd(MODD))
        if dbg_stage == "mod" and l == 0:
            break

        s.barrier()
        with ExitStack() as st:
            st.enter_context(nc.named_scope("A_l%d" % l))
            hT = kb.sb(st, [128, 8, NT], BF16, "hT")
            modT = kb.sb(st, [128, 48, 2], F32, "modT")
            for r in range(2):
                s.dma("sp", modT[:, :, r], MODD[l, r].rearrange("(c p) -> p c", p=128), rd(MODD), rd(modT),
                      allow_slow_non_contiguous=True)
            sc1p = kb.sb(st, [128, 8, 2], F32, "sc1p")
            s.op("dve", lambda e: e.tensor_scalar_add(out=sc1p[:], in0=modT[:, 8:16, :], scalar1=1.0), rd(modT), rd(sc1p))
            xin = [kb.sb(st, [128, D], F32, "xin%d" % i) for i in range(2)]
            xn = [kb.sb(st, [128, D], F32, "xn%d" % i) for i in range(2)]
            stats = [kb.sb(st, [128, 2, 6], F32, "bst%d" % i) for i in range(2)]
            mv = [kb.sb(st, [128, 2], F32, "mv%d" % i) for i in range(2)]
            rstd = [kb.sb(st, [128, 1], F32, "rstd%d" % i) for i in range(2)]
            src = x_in if l == 0 else X2
            for t in range(NTT):
                i = t % 2
                r = 0 if t < L // 128 else 1
                s.dma("sp", xin[i][:], src[t * 128:(t + 1) * 128, :], rd(src), rd(xin[i]))
                if l == 0:
                    for j in range(2):
                        s.op("dve", lambda e: e.bn_stats(out=stats[i][:, j, :], in_=xin[i][:, j * 512:(j + 1) * 512]),
                             rd(xin[i]), rd(stats[i]))
                    s.op("dve", lambda e: e.bn_aggr(out=mv[i][:], in_=stats[i][:].rearrange("p a b -> p (a b)")),
                         rd(stats[i]), rd(mv[i]))
                    s.op("dve", lambda e: e.tensor_scalar_add(out=rstd[i][:], in0=mv[i][:, 1:2], scalar1=LN_EPS),
                         rd(mv[i]), rd(rstd[i]))
                    s.op("act", lambda e: e.sqrt(out=rstd[i][:], in_=rstd[i][:]), rd(rstd[i]), rd(rstd[i]))
                    s.op("dve", lambda e: e.reciprocal(out=rstd[i][:], in_=rstd[i][:]), rd(rstd[i]), rd(rstd[i]))
                    s.op("dve", lambda e: e.tensor_scalar(out=xn[i][:], in0=xin[i][:], scalar1=mv[i][:, 0:1],
                                                          scalar2=rstd[i][:, 0:1], op0=ALU.subtract, op1=ALU.mult),
                         rd(xin[i], mv[i], rstd[i]), rd(xn[i]))
                    s.dma("pool", XS[t * 128:(t + 1) * 128, :], xn[i][:], rd(xn[i]), rd(XS), nowaw=True)
                    xs_t = xn[i]
                else:
                    xs_t = xin[i]
                for kc in range(8):
                    pb = psb[kc // 4]
                    s.op("pe", lambda e: e.transpose(out=pb[:, (kc % 4) * 128:(kc % 4 + 1) * 128],
                                                     in_=xs_t[:, kc * 128:(kc + 1) * 128], identity=ident[:]),
                         rd(xs_t, ident), rd(pb))
                for kc in range(8):
                    pb = psb[kc // 4]
                    eng = "act" if kc % 2 == 0 else "dve"
                    if eng == "act":
                        s.op("act", lambda e: e.activation(out=hT[:, kc, t * 128:(t + 1) * 128],
                                                           in_=pb[:, (kc % 4) * 128:(kc % 4 + 1) * 128],
                                                           func=AF.Identity, scale=sc1p[:, kc, r:r + 1],
                                                           bias=modT[:, kc, r:r + 1]),
                             rd(pb, sc1p, modT), rd(hT), nowaw=True)
                    else:
                        s.op("dve", lambda e: e.tensor_scalar(out=hT[:, kc, t * 128:(t + 1) * 128],
                                                              in0=pb[:, (kc % 4) * 128:(kc % 4 + 1) * 128],
                                                              scalar1=sc1p[:, kc, r:r + 1], scalar2=modT[:, kc, r:r + 1],
                                                              op0=ALU.mult, op1=ALU.add),
                             rd(pb, sc1p, modT), rd(hT), nowaw=True)
            wbf = kb.sb(st, [128, 8, N_IN], BF16, "wbf")
            for kc in range(8):
                for (c0, c1) in ((0, 1536), (1536, N_IN)):
                    s.dma("pool", wbf[:, kc, c0:c1], w_in[l, kc * 128:(kc + 1) * 128, c0:c1], rd(w_in), rd(wbf))
            ev = [kb.sb(st, [128, 512], F32, "ev%d" % i) for i in range(4)]
            n = 0
            for cc in range(12):
                for tt in range(0, NT, 512):
                    w = min(512, NT - tt)
                    pb = psb[2 + n % 4]
                    for kc in range(8):
                        s.op("pe", lambda e: e.matmul(out=pb[:, 0:w], lhsT=wbf[:, kc, cc * 128:(cc + 1) * 128],
                                                      rhs=hT[:, kc, tt:tt + w], start=(kc == 0), stop=(kc == 7)),
                             rd(wbf, hT), rd(pb))
                    e_ = ev[n % 4]
                    if n % 2 == 0:
                        s.op("act", lambda e: e.copy(out=e_[:, 0:w], in_=pb[:, 0:w]), rd(pb), rd(e_))
                    else:
                        s.op("dve", lambda e: e.tensor_copy(out=e_[:, 0:w], in_=pb[:, 0:w]), rd(pb), rd(e_))
                    s.dma("sp", QKVT[cc * 128:(cc + 1) * 128, tt:tt + w], e_[:, 0:w], rd(e_), rd(QKVT), nowaw=True)
                    n += 1
            for t in range(NTT):
                for c0 in range(OFF_Z, N_IN, 512):
                    w = min(512, N_IN - c0)
                    pb = psb[2 + n % 4]
                    for kc in range(8):
                        s.op("pe", lambda e: e.matmul(out=pb[:, 0:w], lhsT=hT[:, kc, t * 128:(t + 1) * 128],
                                                      rhs=wbf[:, kc, c0:c0 + w], start=(kc == 0), stop=(kc == 7)),
                             rd(wbf, hT), rd(pb))
                    e_ = ev[n % 4]
                    if n % 2 == 0:
                        s.op("act", lambda e: e.copy(out=e_[:, 0:w], in_=pb[:, 0:w]), rd(pb), rd(e_))
                    else:
                        s.op("dve", lambda e: e.tensor_copy(out=e_[:, 0:w], in_=pb[:, 0:w]), rd(pb), rd(e_))
                    s.dma("sp", PTOK[t * 128:(t + 1) * 128, c0 - OFF_Z:c0 - OFF_Z + w], e_[:, 0:w], rd(e_), rd(PTOK), nowaw=True)
                    n += 1
        if dbg_stage == "A":
            break
        s.barrier()
        C = NS()
        C.bcrow = bcrow
        C.kb = kb; C.ident = ident; C.identb = identb; C.ones = ones; C.masks = masks; C.psb = psb
        C.conv_w = conv_w; C.a_log = a_log; C.dt_bias = dt_bias; C.gdn_norm_w = gdn_norm_w
        C.QKVT = QKVT; C.PTOK = PTOK; C.OPSD = OPSD; C.OFB = OFB; C.MIXT = MIXT; C.dbg_stage = dbg_stage
        C.cut = int(os.environ.get("GCUT", "99"))
        if dbg_stage == "gdn":
            C.o_gdn = dbg_out("o_gdn", [NT, 512])
        if dbg_stage in ("gdnA", "gdnB"):
            C.o_dbg = dbg_out("o_dbg", [128, 1024])
        if dbg_stage == "gdnB":
            C.o_dbgB = dbg_out("o_dbgB", [4, 128, OPW])
        try:
            stage_gdn(C, l)
        except Cut:
            pass
        s.barrier()
        if dbg_stage in ("gdn", "gdnprep", "gdnscan", "gdnA", "gdnB"):
            break
        C.q_norm_w = q_norm_w; C.k_norm_w = k_norm_w; C.rope_in = rope_in
        if dbg_stage == "attq":
            C.o_dbg = dbg_out("o_dbg", [128, 1024])
        stage_att(C, l)
        s.barrier()
        if dbg_stage in ("att", "attq"):
            break
        C.w_out = w_out; C.ln1_w = ln1_w; C.ln1_b = ln1_b; C.ln2_w = ln2_w; C.ln2_b = ln2_b
        C.router_w = router_w; C.router_bias = router_bias; C.sh_w_gate = sh_w_gate; C.sh_w_up = sh_w_up
        C.sh_w_down = sh_w_down; C.MODD = MODD; C.XS = XS; C.X1 = X1; C.X2 = X2; C.H2 = H2; C.SHO = SHO
        if dbg_stage == "D":
            C.o_y = dbg_out("o_y", [NT, D])
        with ExitStack() as mst:
            M = NS()
            M.WFULL = kb.sb(mst, [128, NTT, 256], F32, "WFULL")
            stage_D(C, l, M)
            s.barrier()
            if dbg_stage != "D":
                C.iotas = iotas; C.WG = WG; C.WU = WU; C.WD = WD; C.BUFTW = BUFTW; C.EO = EO; C.ones256 = ones256
                C.out = final_out
                if not hasattr(kb, "reg_tab"):
                    kb.reg_tab = nc.gpsimd.to_reg(TAB_LAYERS * 32768 - 1)
                    kb.reg_nt = nc.gpsimd.to_reg(NT - 1)
                C.reg_tab = kb.reg_tab; C.reg_nt = kb.reg_nt
                if dbg_stage == "E1":
                    C.o_e1 = dbg_out("o_e1", [128, 3000])
                if dbg_stage == "E":
                    C.o_ff = dbg_out("o_ff", [NT, D])
                stage_E(C, l, M)
                s.barrier()
            if dbg_stage == "D":
                o2 = dbg_out("o_wfull", [128, NTT, 256])
                s.dma("sp", o2[:], M.WFULL[:], rd(M.WFULL), rd(o2))
                C.dfin = [o2]
        if dbg_stage in ("D", "E1", "E2", "E2s", "E"):
            break

    finals = []
    if dbg_stage == "mod":
        o = dbg_out("o_mod", [2, 6 * D])
        with ExitStack() as st:
            tmp = kb.sb(st, [2, 6 * D], F32, "dbgm")
            s.dma("sp", tmp[:], MODD[0], rd(MODD), rd(tmp))
            s.dma("sp", o[:], tmp[:], rd(tmp), rd(o))
        finals.append(o)
    if dbg_stage == "A":
        o1 = dbg_out("o_qkvt", [OFF_Z, NT])
        o2 = dbg_out("o_ptok", [NT, NTOKC])
        o3 = dbg_out("o_xs", [NT, D])
        s.dma("sp", o1[:], QKVT[:], rd(QKVT), rd(o1))
        s.dma("sp", o2[:], PTOK[:], rd(PTOK), rd(o2))
        s.dma("sp", o3[:], XS[:], rd(XS), rd(o3))
        finals += [o1, o2, o3]
    if dbg_stage is None:
        finals.append(final_out)
    if dbg_stage == "E1":
        finals.append(outs["o_e1"])
    if dbg_stage in ("E2", "E2s"):
        o1 = dbg_out("o_eo", [2048, D], BF16)
        s.dma("sp", o1[:], EO[0:2048, :], rd(EO), rd(o1))
        finals.append(o1)
    if dbg_stage == "E":
        o1 = dbg_out("o_x2", [NT, D])
        s.dma("sp", o1[:], X2[:], rd(X2), rd(o1))
        finals += [o1, outs["o_ff"]]
    if dbg_stage in ("gdn", "gdnscan"):
        o1 = dbg_out("o_of", [NT, 512])
        o2 = dbg_out("o_ob", [NT, 512])
        s.dma("sp", o1[:], OFB[0][:], rd(OFB[0]), rd(o1))
        s.dma("sp", o2[:], OFB[1][:], rd(OFB[1]), rd(o2))
        finals += [o1, o2]
        if dbg_stage == "gdn":
            finals.append(outs["o_gdn"])
    if dbg_stage in ("gdnA", "attq"):
        finals.append(outs["o_dbg"])
    if dbg_stage == "D":
        finals += C.dfin + [outs["o_y"]]
        for nm, tsrc in (("o_x1", X1), ("o_h2", H2), ("o_sho", SHO)):
            o1 = dbg_out(nm, [NT, D])
            s.dma("sp", o1[:], tsrc[:], rd(tsrc), rd(o1))
            finals.append(o1)
    if dbg_stage == "att":
        o1 = dbg_out("o_mixt", [D, NT], BF16)
        s.dma("sp", o1[:], MIXT[:], rd(MIXT), rd(o1))
        finals.append(o1)
    if dbg_stage == "gdnB":
        finals.append(outs["o_dbgB"])
    if dbg_stage == "gdnprep":
        o1 = dbg_out("o_ops", [4, 2, NTT, 128, OPW])
        s.dma("sp", o1[:], OPSD[:], rd(OPSD), rd(o1))
        finals.append(o1)
    s.finish("sp", [f.res for f in finals])
    es.close()
    return nc, list(outs.keys())


_r = np.arange(128)
_m4 = np.stack([(_r[:, None] <= _r[None, :]), (_r[:, None] >= _r[None, :]),
                (_r[:, None] > _r[None, :]), (_r[:, None] < _r[None, :])]).astype(np.float32)
MASKS = np.concatenate([_m4, (1.0 - _m4[2:4]) * 3.0e4, -(1.0 - _m4[0:2]) * 3.0e4]).astype(np.float32)


def _rope_tables():
    t = np.arange(L)
    row = (t // 64).astype(np.float32)
    col = (t % 64).astype(np.float32)
    inv = (np.float32(10000.0) ** (-np.arange(0, 64, 2, dtype=np.float32) / np.float32(64))).astype(np.float32)
    ang = np.stack([row[:, None] * inv, col[:, None] * inv], axis=1).astype(np.float32)
    cs = np.stack([np.cos(ang), np.sin(ang)]).reshape(2, L, 64).astype(np.float32)
    return np.ascontiguousarray(np.tile(cs, (1, 1, 6)))


ROPE = _rope_tables()
IOTAS = np.concatenate([np.tile(np.arange(NBLK, dtype=np.float32), (128, 1)), np.arange(128, dtype=np.float32)[:, None],
                        (np.arange(NTT, dtype=np.float32)[None, :] * 128 + np.arange(128, dtype=np.float32)[:, None])], axis=1)


def expert_tables(inputs):
    n = TAB_LAYERS
    g = inputs["exp_w_gate"][:n]; u = inputs["exp_w_up"][:n]; d = inputs["exp_w_down"][:n]
    WG = np.ascontiguousarray(g.reshape(n, 256, 8, 128, 256).transpose(0, 1, 3, 2, 4)).reshape(n * 256 * 128, 2048)
    WU = np.ascontiguousarray(u.reshape(n, 256, 8, 128, 256).transpose(0, 1, 3, 2, 4)).reshape(n * 256 * 128, 2048)
    WD = np.ascontiguousarray(d.reshape(n, 256, 2, 128, 1024).transpose(0, 1, 3, 2, 4)).reshape(n * 256 * 128, 2048)
    return WG, WU, WD


def make_inputs(inputs, b, tabs=None):
    xx = np.concatenate([inputs["x"][b], inputs["ctx"][b]], axis=0)
    c2 = np.stack([inputs["c"][b], inputs["c_ctx"]], axis=0)
    m = {
        "x": np.ascontiguousarray(xx, dtype=np.float32),
        "c2": np.ascontiguousarray(c2, dtype=np.float32),
        "ada_w": inputs["ada_w"], "ada_b": inputs["ada_b"], "w_in": inputs["w_in"],
        "ident": np.eye(128, dtype=np.float32),
        "masks": MASKS,
        "conv_w": inputs["conv_w"], "gdn_a_log": inputs["gdn_a_log"].reshape(DEPTH, 8),
        "gdn_dt_bias": inputs["gdn_dt_bias"].reshape(DEPTH, 8), "gdn_norm_w": inputs["gdn_norm_w"],
        "q_norm_w": inputs["q_norm_w"], "k_norm_w": inputs["k_norm_w"], "rope": ROPE,
        "w_out": inputs["w_out"], "ln1_w": inputs["ln1_w"], "ln1_b": inputs["ln1_b"],
        "ln2_w": inputs["ln2_w"], "ln2_b": inputs["ln2_b"], "router_w": inputs["router_w"],
        "router_bias": inputs["router_bias"], "sh_w_gate": inputs["sh_w_gate"], "sh_w_up": inputs["sh_w_up"],
        "sh_w_down": inputs["sh_w_down"],
        "iotas": IOTAS,
    }
    if tabs is not None:
        m["WG"], m["WU"], m["WD"] = tabs
    return m


def kernel(**inputs):
    nc, onames = build()
    tabs = expert_tables(inputs)
    in_maps = [make_inputs(inputs, b, tabs) for b in range(8)]
    res = run_bass_kernel_spmd(nc, in_maps, core_ids=list(range(8)))
    return np.stack([r["out"] for r in res.results], axis=0)
```

```python
import os
from contextlib import ExitStack
import numpy as np
import concourse.bass as bass
import concourse.mybir as mybir
from concourse.bass_utils import run_bass_kernel_spmd

F32 = mybir.dt.float32
BF16 = mybir.dt.bfloat16
U32 = mybir.dt.uint32
I32 = mybir.dt.int32
AF = mybir.ActivationFunctionType
ALU = mybir.AluOpType
AX = mybir.AxisListType

D = 1024
L = 4096
LC = 256
NT = L + LC
NTT = NT // 128
DEPTH = 2
N_IN = 3088
OFF_Z = 1536
OFF_BA = 2048
OFF_ATT = 2064
NTOKC = N_IN - OFF_Z
ALPHA = (2.0 * DEPTH) ** 0.25
LN_EPS = 1e-5
RMS_EPS = 1e-6
OPW = 5 * 128 + 8


class Res:
    __slots__ = ("name", "w", "r", "excl", "wa", "wx")

    def __init__(self, name=""):
        self.name = name
        self.w = None
        self.r = {}
        self.excl = False
        self.wa = {}
        self.wx = None


class Sched:
    NDS = 8

    def __init__(self, nc, es):
        self.nc = nc
        self.engs = {"pe": nc.tensor, "dve": nc.vector, "act": nc.scalar, "pool": nc.gpsimd, "sp": nc.sync}
        self.csem = {k: es.enter_context(nc.semaphore("c_" + k)) for k in ("pe", "dve", "act", "pool")}
        self.ccnt = {k: 0 for k in self.csem}
        self.dsem = {q: [es.enter_context(nc.semaphore("d_%s%d" % (q, i))) for i in range(self.NDS)]
                     for q in ("sp", "act", "pool")}
        self.dcnt = {q: [0] * self.NDS for q in self.dsem}
        self.drr = {q: 0 for q in self.dsem}
        self.seen = {e: {} for e in self.engs}
        self.ninst = 0

    def _sem(self, key):
        return self.csem[key[1]] if key[0] == "c" else self.dsem[key[1]][key[2]]

    def _wait(self, eng, deps):
        seen = self.seen[eng]
        for key, val in sorted(deps.items(), key=lambda kv: str(kv[0])):
            if eng == "pe" and key == ("c", "pe"):
                continue
            if seen.get(key, 0) >= val:
                continue
            self.engs[eng].wait_ge(self._sem(key), val)
            seen[key] = val

    @staticmethod
    def _deps(reads, writes, nowaw=False):
        deps = {}

        def add(ev):
            if ev is not None and deps.get(ev[0], 0) < ev[1]:
                deps[ev[0]] = ev[1]
        for r in reads:
            add(r.wx)
            for k, v in r.wa.items():
                add((k, v))
            if r.excl:
                for k, v in r.r.items():
                    add((k, v))
        for w in writes:
            add(w.wx)
            if not nowaw or w.excl:
                for k, v in w.wa.items():
                    add((k, v))
            for k, v in w.r.items():
                add((k, v))
        return deps

    @staticmethod
    def _mark(ev, reads, writes, nowaw=False):
        for r in reads:
            if r.r.get(ev[0], 0) < ev[1]:
                r.r[ev[0]] = ev[1]
        for w in writes:
            w.w = ev
            if nowaw and not w.excl:
                if w.wa.get(ev[0], 0) < ev[1]:
                    w.wa[ev[0]] = ev[1]
            else:
                w.wx = ev
                w.wa = {}
                w.r = {}

    def op(self, eng, fn, reads=(), writes=(), nowaw=False):
        self._wait(eng, self._deps(reads, writes, nowaw))
        inst = fn(self.engs[eng])
        self.ccnt[eng] += 1
        inst.then_inc(self.csem[eng], 1)
        ev = (("c", eng), self.ccnt[eng])
        self._mark(ev, reads, writes, nowaw)
        self.ninst += 1
        return ev

    def dma(self, q, out, in_, reads=(), writes=(), nowaw=False, **kw):
        self._wait(q, self._deps(reads, writes, nowaw))
        i = self.drr[q]
        self.drr[q] = (i + 1) % self.NDS
        inst = self.engs[q].dma_start(out=out, in_=in_, **kw)
        self.dcnt[q][i] += 16
        inst.then_inc(self.dsem[q][i], 16)
        ev = (("d", q, i), self.dcnt[q][i])
        self._mark(ev, reads, writes, nowaw)
        self.ninst += 1
        return ev

    def idma(self, out, in_, out_off=None, in_off=None, reads=(), writes=(), nowaw=False, **kw):
        q = "pool"
        self._wait(q, self._deps(reads, writes, nowaw))
        i = self.drr[q]
        self.drr[q] = (i + 1) % self.NDS
        inst = self.engs[q].indirect_dma_start(
            out=out, out_offset=(bass.IndirectOffsetOnAxis(ap=out_off, axis=0) if out_off is not None else None),
            in_=in_, in_offset=(bass.IndirectOffsetOnAxis(ap=in_off, axis=0) if in_off is not None else None), **kw)
        self.dcnt[q][i] += 16
        inst.then_inc(self.dsem[q][i], 16)
        ev = (("d", q, i), self.dcnt[q][i])
        self._mark(ev, reads, writes, nowaw)
        self.ninst += 1
        return ev

    def barrier(self):
        deps = {}
        for k, v in self.ccnt.items():
            if v:
                deps[("c", k)] = v
        for q in self.dcnt:
            for i, v in enumerate(self.dcnt[q]):
                if v:
                    deps[("d", q, i)] = v
        for eng in self.engs:
            d2 = dict(deps)
            self._wait_all(eng, d2)

    def _wait_all(self, eng, deps):
        seen = self.seen[eng]
        for key, val in sorted(deps.items(), key=lambda kv: str(kv[0])):
            if key == ("c", eng):
                continue
            if seen.get(key, 0) >= val:
                continue
            self.engs[eng].wait_ge(self._sem(key), val)
            seen[key] = val

    def finish(self, eng, resources):
        deps = {}
        for r in resources:
            evs = list(r.wa.items()) + ([r.wx] if r.wx is not None else [])
            for k, v in evs:
                if deps.get(k, 0) < v:
                    deps[k] = v
        self._wait(eng, deps)


class T:
    def __init__(self, t, name=""):
        self.t = t
        self.res = Res(name)

    def __getitem__(self, k):
        return self.t[k]


class KB:
    def __init__(self, nc, es, dbg=None):
        self.nc = nc
        self.es = es
        self.s = Sched(nc, es)
        self.dbg = dbg
        self.n = 0

    def sb(self, es, shape, dt, name=None):
        self.n += 1
        name = "%s_%d" % (name or "sb", self.n)
        return T(es.enter_context(self.nc.sbuf_tensor(name, list(shape), dt)), name)

    def ps(self, es, shape, dt=F32, name=None):
        self.n += 1
        name = "%s_%d" % (name or "ps", self.n)
        return T(es.enter_context(self.nc.psum_tensor(name, list(shape), dt)), name)

    def dram(self, name, shape, dt, kind="Internal"):
        return T(self.nc.dram_tensor(name, list(shape), dt, kind=kind).ap(), name)


def rd(*ts):
    return [t.res for t in ts]


class NS:
    pass


class Cut(Exception):
    pass


def cutpt(C, k):
    return C.cut == k


def sub(bank, c0, c1, name=""):
    t = T(bank.t[:, c0:c1], name)
    t.res = bank.res
    bank.res.excl = True
    return t


def bcast_row(C, st, dst_ap, dst_t, src_ap, src_t, n, pbank):
    kb, s = C.kb, C.kb.s
    row = C.bcrow
    s.dma("sp", row[0:1, 0:n], src_ap, rd(src_t), rd(row))
    for c0 in range(0, n, 512):
        w = min(512, n - c0)
        s.op("pe", lambda e: e.matmul(out=pbank[:, 0:w], lhsT=C.ones[:], rhs=row[:, c0:c0 + w], start=True, stop=True),
             rd(C.ones, row), rd(pbank))
        s.op("dve", lambda e: e.tensor_copy(out=dst_ap[:, c0:c0 + w], in_=pbank[:, 0:w]), rd(pbank), rd(dst_t))


def stage_gdn(C, l):
    kb, s = C.kb, C.kb.s
    ident, ones, masks, psb = C.ident, C.ones, C.masks, C.psb
    CUMS = (0, 1)
    STRICT = (2, 3)
    INCLT = (0, 1)
    with ExitStack() as st:
        st.enter_context(C.kb.nc.named_scope("gdnprep_l%d" % l))
        convw = kb.sb(st, [128, 12, 5], F32, "convw")
        for j in range(5):
            s.dma("sp", convw[:, :, j], C.conv_w[l, j].rearrange("(c p) -> p c", p=128), rd(C.conv_w), rd(convw),
                  allow_slow_non_contiguous=True)
        if C.cut == 1:
            s.dma("sp", C.o_dbg[:, 0:128], ones[:], rd(ones), rd(C.o_dbg))
            return
        dtb8 = kb.sb(st, [128, 8], F32, "dtb8")
        bcast_row(C, st, dtb8[:], dtb8, C.dt_bias[l:l + 1, :], C.dt_bias, 8, psb[0])
        negA8 = kb.sb(st, [128, 8], F32, "negA8")
        bcast_row(C, st, negA8[:], negA8, C.a_log[l:l + 1, :], C.a_log, 8, psb[0])
        s.op("act", lambda e: e.activation(out=negA8[:], in_=negA8[:], func=AF.Exp), rd(negA8), rd(negA8))
        s.op("dve", lambda e: e.tensor_scalar(out=negA8[:], in0=negA8[:], scalar1=-1.0, scalar2=None, op0=ALU.mult),
             rd(negA8), rd(negA8))
        if C.cut == 2:
            s.dma("sp", C.o_dbg[:, 0:128], ones[:], rd(ones), rd(C.o_dbg))
            return
        BETA = kb.sb(st, [128, NTT, 8], F32, "BETA")
        NBETA = kb.sb(st, [128, NTT, 8], F32, "NBETA")
        GC = kb.sb(st, [128, NTT, 8], F32, "GC")
        EG = kb.sb(st, [128, NTT, 8], F32, "EG")
        EGR = kb.sb(st, [128, NTT, 8], F32, "EGR")
        CD = kb.sb(st, [128, NTT, 8], F32, "CD")
        BEG = kb.sb(st, [128, NTT, 8], F32, "BEG")
        ba = kb.sb(st, [128, NTT, 16], F32, "ba")
        s.dma("sp", ba[:], C.PTOK.t[:, 512:528].rearrange("(n p) c -> p n c", p=128), rd(C.PTOK), rd(ba))
        s.op("act", lambda e: e.activation(out=BETA[:], in_=ba[:, :, 0:8], func=AF.Sigmoid), rd(ba), rd(BETA))
        s.op("dve", lambda e: e.tensor_scalar(out=NBETA[:], in0=BETA[:], scalar1=-1.0, scalar2=None, op0=ALU.mult),
             rd(BETA), rd(NBETA))
        if C.cut == 3:
            s.dma("sp", C.o_dbg[:, 0:128], ones[:], rd(ones), rd(C.o_dbg))
            return
        xg = kb.sb(st, [128, NTT, 8], F32, "xg")
        ag = kb.sb(st, [128, NTT, 8], F32, "ag")
        gg = kb.sb(st, [128, NTT, 8], F32, "gg")
        for n in range(NTT):
            s.op("dve", lambda e: e.tensor_tensor(out=xg[:, n, :], in0=ba[:, n, 8:16], in1=dtb8[:], op=ALU.add),
                 rd(ba, dtb8), rd(xg))
        s.op("act", lambda e: e.activation(out=ag[:], in_=xg[:], func=AF.Abs), rd(xg), rd(ag))
        s.op("act", lambda e: e.activation(out=ag[:], in_=ag[:], func=AF.Exp, scale=-1.0), rd(ag), rd(ag))
        s.op("dve", lambda e: e.tensor_scalar_add(out=ag[:], in0=ag[:], scalar1=1.0), rd(ag), rd(ag))
        s.op("act", lambda e: e.activation(out=ag[:], in_=ag[:], func=AF.Ln), rd(ag), rd(ag))
        s.op("dve", lambda e: e.tensor_scalar(out=xg[:], in0=xg[:], scalar1=0.0, scalar2=None, op0=ALU.max),
             rd(xg), rd(xg))
        s.op("dve", lambda e: e.tensor_tensor(out=gg[:], in0=xg[:], in1=ag[:], op=ALU.add), rd(xg, ag), rd(gg))
        for n in range(NTT):
            s.op("dve", lambda e: e.tensor_tensor(out=gg[:, n, :], in0=gg[:, n, :], in1=negA8[:], op=ALU.mult),
                 rd(gg, negA8), rd(gg))
        if C.cut == 4:
            s.dma("sp", C.o_dbg[:, 0:128], ones[:], rd(ones), rd(C.o_dbg))
            return
        pG = sub(psb[0], 0, NTT * 8, "pG")
        pGt = sub(psb[1], 0, NTT * 8, "pGt")
        ggv = gg[:].rearrange("p n c -> p (n c)")
        for n in range(NTT):
            for d in range(2):
                s.op("pe", lambda e: e.matmul(out=pG[:, n * 8 + d * 4:n * 8 + d * 4 + 4], lhsT=masks[:, CUMS[d], :],
                                              rhs=gg[:, n, d * 4:d * 4 + 4], start=True, stop=True),
                     rd(masks, gg), rd(pG))
        for c0 in range(0, NTT * 8, 136):
            s.op("pe", lambda e: e.matmul(out=pGt[:, c0:c0 + 136], lhsT=ones[:], rhs=ggv[:, c0:c0 + 136],
                                          start=True, stop=True), rd(ones, gg), rd(pGt))
        if C.cut == 5:
            s.dma("sp", C.o_dbg[:, 0:128], ones[:], rd(ones), rd(C.o_dbg))
            return
        GCv = GC[:].rearrange("p n c -> p (n c)")
        s.op("dve", lambda e: e.tensor_copy(out=GCv, in_=pG[:, :]), rd(pG), rd(GC))
        if C.cut == 6:
            s.dma("sp", C.o_dbg[:, 0:272], GC[:].rearrange("p n c -> p (n c)"), rd(GC), rd(C.o_dbg))
            s.dma("sp", C.o_dbg[:, 272:544], gg[:].rearrange("p n c -> p (n c)"), rd(gg), rd(C.o_dbg))
            s.dma("sp", C.o_dbg[:, 544:816], BETA[:].rearrange("p n c -> p (n c)"), rd(BETA), rd(C.o_dbg))
            return
        if os.environ.get("GSKIP") != "EG":
            if os.environ.get("GEG") == "psum":
                s.op("act", lambda e: e.activation(out=EG[:].rearrange("p n c -> p (n c)"), in_=pG[:, :], func=AF.Exp),
                     rd(pG), rd(EG))
            else:
                s.op("act", lambda e: e.activation(out=EG[:], in_=GC[:], func=AF.Exp), rd(GC), rd(EG))
        if os.environ.get("GSKIP") != "CD":
            s.op("act", lambda e: e.activation(out=CD[:].rearrange("p n c -> p (n c)"), in_=pGt[:, :], func=AF.Exp),
                 rd(pGt), rd(CD))
        if C.cut == 7:
            s.dma("sp", C.o_dbg[:, 0:128], ones[:], rd(ones), rd(C.o_dbg))
            return
        s.op("dve", lambda e: e.tensor_tensor(out=EGR[:].rearrange("p n c -> p (n c)"), in0=pGt[:, :], in1=GCv,
                                              op=ALU.subtract), rd(pGt, GC), rd(EGR))
        s.op("act", lambda e: e.activation(out=EGR[:], in_=EGR[:], func=AF.Exp), rd(EGR), rd(EGR))
        if C.cut == 8:
            s.dma("sp", C.o_dbg[:, 0:128], ones[:], rd(ones), rd(C.o_dbg))
            return
        s.op("dve", lambda e: e.tensor_tensor(out=BEG[:], in0=BETA[:], in1=EG[:], op=ALU.mult), rd(BETA, EG), rd(BEG))
        if C.cut == 9:
            s.dma("sp", C.o_dbg[:, 0:128], ones[:], rd(ones), rd(C.o_dbg))
            return

        if C.dbg_stage == "gdnA":
            s.dma("sp", C.o_dbg[:, 0:272], GC[:].rearrange("p n c -> p (n c)"), rd(GC), rd(C.o_dbg))
            s.dma("sp", C.o_dbg[:, 272:544], BEG[:].rearrange("p n c -> p (n c)"), rd(BEG), rd(C.o_dbg))
            s.dma("sp", C.o_dbg[:, 544:816], EGR[:].rearrange("p n c -> p (n c)"), rd(EGR), rd(C.o_dbg))
            return
        W = NT + 4
        xpad = [kb.sb(st, [128, NT + 8], F32, "xpad%d" % i) for i in range(2)]
        for xp in xpad:
            s.op("pool", lambda e: e.memset(xp[:], 0.0), (), rd(xp))
        cs = [kb.sb(st, [128, W], F32, "cs%d" % i) for i in range(3)]
        acc = kb.sb(st, [128, W], F32, "cacc")
        def mk(name, *dims):
            def rec(pref, ds):
                if not ds:
                    return kb.sb(st, [128, 128], F32, pref)
                return [rec("%s_%d" % (pref, i), ds[1:]) for i in range(ds[0])]
            return rec(name, dims)
        vtok, ktok, qtok, kT, qT, junk = (mk(nm, 2) for nm in ("vtok", "ktok", "qtok", "kT", "qT", "gjunk"))
        ssq = [kb.sb(st, [128, 2], F32, "ssq%d" % i) for i in range(2)]
        kqT = [kb.sb(st, [128, 256], F32, "kqT%d" % i) for i in range(2)]
        diagG, Mm, t1, t2, Xa, Xb, XTa, XTb, TT, vb, kbg, qd = (mk(nm, 2, 2) for nm in (
            "diagG", "Mm", "t1", "t2", "Xa", "Xb", "XTa", "XTb", "TT", "vb", "kbg", "qd"))
        OPS = [[[kb.sb(st, [128, OPW], F32, "OPS%d%d%d" % (sl, d, i)) for i in range(2)] for d in range(2)] for sl in range(2)]
        opar = [[0, 0], [0, 0]]

        def chain(h, n, sl, d, pkkq):
            col = d * 4 + h
            bank = psb[3 * sl + 1 + d]
            r0, r1, r2, r3 = (sub(bank, i * 128, (i + 1) * 128) for i in range(4))
            ops = OPS[sl][d][opar[sl][d]]
            opar[sl][d] ^= 1
            gcol = GC[:, n, col:col + 1]
            dG, M_, T1, T2, TT_ = diagG[sl][d], Mm[sl][d], t1[sl][d], t2[sl][d], TT[sl][d]
            ktk, qtk, vtk = ktok[sl], qtok[sl], vtok[sl]
            s.op("act", lambda e: e.activation(out=dG[:], in_=ident[:], func=AF.Copy, scale=gcol), rd(ident, GC), rd(dG))
            yield
            s.op("pe", lambda e: e.matmul(out=r0[:, :], lhsT=ones[:], rhs=dG[:], start=True, stop=True), rd(ones, dG), rd(r0))
            yield
            s.op("dve", lambda e: e.scalar_tensor_tensor(out=T1[:], in0=r0[:, :], scalar=gcol, in1=masks[:, 4 + d, :],
                                                         op0=ALU.subtract, op1=ALU.max), rd(r0, GC, masks), rd(T1))
            s.op("dve", lambda e: e.scalar_tensor_tensor(out=T2[:], in0=r0[:, :], scalar=gcol, in1=masks[:, 6 + d, :],
                                                         op0=ALU.subtract, op1=ALU.min), rd(r0, GC, masks), rd(T2))
            yield
            s.op("act", lambda e: e.activation(out=T1[:], in_=T1[:], func=AF.Exp, scale=-1.0), rd(T1), rd(T1))
            s.op("act", lambda e: e.activation(out=T2[:], in_=T2[:], func=AF.Exp), rd(T2), rd(T2))
            yield
            Xc, XTc, Xn, XTn = Xa[sl][d], XTa[sl][d], Xb[sl][d], XTb[sl][d]
            s.op("dve", lambda e: e.scalar_tensor_tensor(out=fr(Xc[:]), in0=pkkq[:, 0:128], scalar=NBETA[:, n, col:col + 1], in1=T1[:],
                                                         op0=ALU.mult, op1=ALU.mult), rd(pkkq, NBETA, T1), rd(Xc))
            s.op("dve", lambda e: e.tensor_tensor(out=ops[:, 384:512], in0=pkkq[:, 128:256], in1=T2[:], op=ALU.mult), rd(pkkq, T2), rd(ops), nowaw=True)
            yield
            s.op("pe", lambda e: e.transpose(out=r3[:, :], in_=Xc[:], identity=ident[:]), rd(Xc, ident), rd(r3))
            yield
            s.op("act", lambda e: e.copy(out=fr(XTc[:]), in_=r3[:, :]), rd(r3), rd(XTc))
            s.op("dve", lambda e: e.tensor_tensor(out=fr(TT_[:]), in0=r3[:, :], in1=ident[:], op=ALU.add), rd(r3, ident), rd(TT_))
            yield
            for m in range(1, 7):
                s.op("pe", lambda e: e.matmul(out=r0[:, :], lhsT=fr(XTc[:]), rhs=fr(Xc[:]), start=True, stop=True), rd(XTc, Xc), rd(r0))
                if m < 6:
                    s.op("pe", lambda e: e.matmul(out=r1[:, :], lhsT=fr(Xc[:]), rhs=fr(XTc[:]), start=True, stop=True), rd(XTc, Xc), rd(r1))
                yield
                s.op("act", lambda e: e.copy(out=fr(Xn[:]), in_=r0[:, :]), rd(r0), rd(Xn))
                if m < 6:
                    s.op("dve", lambda e: e.tensor_copy(out=fr(XTn[:]), in_=r1[:, :]), rd(r1), rd(XTn))
                yield
                s.op("pe", lambda e: e.matmul(out=r2[:, :], lhsT=fr(Xn[:]), rhs=fr(TT_[:]), start=True, stop=True), rd(Xn, TT_), rd(r2))
                yield
                s.op("dve", lambda e: e.tensor_tensor(out=fr(TT_[:]), in0=TT_[:], in1=r2[:, :], op=ALU.add), rd(TT_, r2), rd(TT_))
                yield
                Xc, XTc, Xn, XTn = Xn, XTn, Xc, XTc
            vb_, kbg_, qd_ = vb[sl][d], kbg[sl][d], qd[sl][d]
            s.op("act", lambda e: e.activation(out=vb_[:], in_=vtk[:], func=AF.Copy, scale=BETA[:, n, col:col + 1]), rd(vtk, BETA), rd(vb_))
            s.op("act", lambda e: e.activation(out=kbg_[:], in_=ktk[:], func=AF.Copy, scale=BEG[:, n, col:col + 1]), rd(ktk, BEG), rd(kbg_))
            s.op("dve", lambda e: e.tensor_scalar(out=qd_[:], in0=qtk[:], scalar1=EG[:, n, col:col + 1], scalar2=None, op0=ALU.mult), rd(qtk, EG), rd(qd_))
            s.op("pool", lambda e: e.tensor_scalar(out=ops[:, 512:640], in0=ktk[:], scalar1=EGR[:, n, col:col + 1], scalar2=0.0, op0=ALU.mult, op1=ALU.add),
                 rd(ktk, EGR), rd(ops), nowaw=True)
            s.op("pool", lambda e: e.tensor_copy(out=ops[:, 640:648], in_=CD[:, n, :]), rd(CD), rd(ops), nowaw=True)
            yield
            s.op("pe", lambda e: e.matmul(out=r3[:, :], lhsT=fr(kbg_[:]), rhs=fr(TT_[:]), start=True, stop=True), rd(kbg_, TT_), rd(r3))
            s.op("pe", lambda e: e.matmul(out=r0[:, :], lhsT=fr(TT_[:]), rhs=fr(vb_[:]), start=True, stop=True), rd(vb_, TT_), rd(r0))
            s.op("pe", lambda e: e.transpose(out=r1[:, :], in_=qd_[:], identity=ident[:]), rd(qd_, ident), rd(r1))
            yield
            s.op("act", lambda e: e.copy(out=ops[:, 0:128], in_=r3[:, :]), rd(r3), rd(ops), nowaw=True)
            s.op("dve", lambda e: e.tensor_copy(out=ops[:, 128:256], in_=r0[:, :]), rd(r0), rd(ops), nowaw=True)
            s.op("act", lambda e: e.copy(out=ops[:, 256:384], in_=r1[:, :]), rd(r1), rd(ops), nowaw=True)
            yield
            s.dma("sp", C.OPSD[h, d, n], ops[:], rd(ops), rd(C.OPSD), nowaw=True)
            if C.dbg_stage == "gdnB":
                s.dma("sp", C.o_dbgB[d], ops[:], rd(ops), rd(C.o_dbgB))
                s.dma("sp", C.o_dbgB[2 + d, :, 0:128], TT_[:], rd(TT_), rd(C.o_dbgB))
                s.dma("sp", C.o_dbgB[2 + d, :, 384:512], ktk[:], rd(ktk), rd(C.o_dbgB))

        def chunk(h, n, sl):
            col0 = n * 128 if n < L // 128 else L + 4 + (n - L // 128) * 128
            pQKV = sub(psb[3 * sl], 0, 384)
            bk0 = sub(psb[3 * sl + 1], 0, 128)
            bk1 = sub(psb[3 * sl + 2], 0, 128)
            for which in range(3):
                s.op("pe", lambda e: e.transpose(out=pQKV[:, which * 128:(which + 1) * 128], in_=cs[which][:, col0:col0 + 128],
                                                 identity=ident[:]), rd(cs[which], ident), rd(pQKV))
            yield
            s.op("act", lambda e: e.copy(out=vtok[sl][:], in_=pQKV[:, 256:384]), rd(pQKV), rd(vtok[sl]))
            for w_ in range(2):
                s.op("act", lambda e: e.activation(out=junk[sl][:], in_=pQKV[:, w_ * 128:(w_ + 1) * 128], func=AF.Square,
                                                   accum_out=ssq[sl][:, w_:w_ + 1]), rd(pQKV), rd(junk[sl], ssq[sl]))
            yield
            s.op("dve", lambda e: e.tensor_scalar_add(out=ssq[sl][:], in0=ssq[sl][:], scalar1=RMS_EPS), rd(ssq[sl]), rd(ssq[sl]))
            yield
            s.op("act", lambda e: e.sqrt(out=ssq[sl][:], in_=ssq[sl][:]), rd(ssq[sl]), rd(ssq[sl]))
            yield
            s.op("dve", lambda e: e.reciprocal(out=ssq[sl][:], in_=ssq[sl][:]), rd(ssq[sl]), rd(ssq[sl]))
            s.op("dve", lambda e: e.tensor_scalar(out=qtok[sl][:], in0=pQKV[:, 0:128], scalar1=ssq[sl][:, 0:1],
                                                  scalar2=128.0 ** -0.5, op0=ALU.mult, op1=ALU.mult), rd(pQKV, ssq[sl]), rd(qtok[sl]))
            s.op("dve", lambda e: e.tensor_scalar(out=ktok[sl][:], in0=pQKV[:, 128:256], scalar1=ssq[sl][:, 1:2],
                                                  scalar2=None, op0=ALU.mult), rd(pQKV, ssq[sl]), rd(ktok[sl]))
            yield
            s.op("pe", lambda e: e.transpose(out=bk0[:, :], in_=ktok[sl][:], identity=ident[:]), rd(ktok[sl], ident), rd(bk0))
            s.op("pe", lambda e: e.transpose(out=bk1[:, :], in_=qtok[sl][:], identity=ident[:]), rd(qtok[sl], ident), rd(bk1))
            yield
            s.op("act", lambda e: e.copy(out=kqT[sl][:, 0:128], in_=bk0[:, :]), rd(bk0), rd(kqT[sl]), nowaw=True)
            s.op("dve", lambda e: e.tensor_copy(out=kqT[sl][:, 128:256], in_=bk1[:, :]), rd(bk1), rd(kqT[sl]), nowaw=True)
            yield
            pkkq = sub(psb[3 * sl], 0, 256)
            s.op("pe", lambda e: e.matmul(out=pkkq[:, :], lhsT=kqT[sl][:, 0:128], rhs=kqT[sl][:, :], start=True, stop=True),
                 rd(kqT[sl]), rd(pkkq))
            yield
            gens = [chain(h, n, sl, 0, pkkq), chain(h, n, sl, 1, pkkq)]
            while gens:
                for g in list(gens):
                    try:
                        next(g)
                    except StopIteration:
                        gens.remove(g)
                yield

        nheads = 4 if C.dbg_stage != "gdnB" else 1
        for h in range(nheads):
            for which in range(3):
                cc = which * 4 + h
                xp = xpad[(h * 3 + which) % 2]
                s.dma("sp", xp[:, 2:2 + L], C.QKVT[cc * 128:(cc + 1) * 128, 0:L], rd(C.QKVT), rd(xp))
                s.dma("pool", xp[:, L + 6:L + 6 + LC], C.QKVT[cc * 128:(cc + 1) * 128, L:NT], rd(C.QKVT), rd(xp))
                s.op("dve", lambda e: e.tensor_scalar(out=acc[:], in0=xp[:, 0:W], scalar1=convw[:, cc, 0:1],
                                                      scalar2=None, op0=ALU.mult), rd(xp, convw), rd(acc))
                for j in range(1, 5):
                    s.op("dve", lambda e: e.scalar_tensor_tensor(out=acc[:], in0=xp[:, j:j + W],
                                                                 scalar=convw[:, cc, j:j + 1], in1=acc[:],
                                                                 op0=ALU.mult, op1=ALU.add), rd(xp, convw, acc), rd(acc))
                s.op("act", lambda e: e.activation(out=cs[which][:], in_=acc[:], func=AF.Silu), rd(acc), rd(cs[which]))
            pending = list(range(NTT if C.dbg_stage != "gdnB" else 2))
            active = {}
            while pending or active:
                for sl in (0, 1):
                    if sl not in active and pending:
                        active[sl] = chunk(h, pending.pop(0), sl)
                for sl in list(active):
                    try:
                        next(active[sl])
                    except StopIteration:
                        del active[sl]
    if C.dbg_stage in ("gdnprep", "gdnB"):
        return
    s.barrier()
    with ExitStack() as st:
        st.enter_context(C.kb.nc.named_scope("gdnscan_l%d" % l))
        S = [[kb.sb(st, [128, 128], F32, "S%d%d" % (h, d)) for d in range(2)] for h in range(4)]
        for h in range(4):
            for d in range(2):
                s.op("pool", lambda e: e.memset(S[h][d][:], 0.0), (), rd(S[h][d]))
        NOB = 16
        OB = [kb.sb(st, [128, OPW], F32, "OB%d" % i) for i in range(NOB)]
        vnew = [kb.sb(st, [128, 128], F32, "vnew%d" % i) for i in range(8)]
        oev = [kb.sb(st, [128, 128], F32, "oev%d" % i) for i in range(8)]
        order = [[32, 33] + list(range(32)), [33, 32] + list(range(31, -1, -1))]
        p1 = [sub(psb[c], 0, 128, "p1") for c in range(8)]
        p2 = [sub(psb[c], 128, 256, "p2") for c in range(8)]
        p3 = [sub(psb[c], 256, 384, "p3") for c in range(8)]
        k = 0
        for step in range(NTT):
            chains = []
            for d in range(2):
                n = order[d][step]
                for h in range(4):
                    c = h * 2 + d
                    ob = OB[k % NOB]
                    k += 1
                    s.dma("sp" if k % 2 == 0 else "pool", ob[:], C.OPSD[h, d, n], rd(C.OPSD), rd(ob))
                    chains.append((c, h, d, n, ob, S[h][d]))
            for (c, h, d, n, ob, Sd) in chains:
                s.op("pe", lambda e: e.matmul(out=p1[c][:, :], lhsT=ob[:, 0:128], rhs=Sd[:], start=True, stop=True),
                     rd(ob, Sd), rd(p1[c]))
            for (c, h, d, n, ob, Sd) in chains:
                s.op("dve", lambda e: e.tensor_tensor(out=vnew[c][:], in0=ob[:, 128:256], in1=p1[c][:, :], op=ALU.subtract),
                     rd(ob, p1[c]), rd(vnew[c]))
            for (c, h, d, n, ob, Sd) in chains:
                s.op("pe", lambda e: e.matmul(out=p2[c][:, :], lhsT=ob[:, 256:384], rhs=Sd[:], start=True, stop=False),
                     rd(ob, Sd), rd(p2[c]))
                s.op("pe", lambda e: e.matmul(out=p2[c][:, :], lhsT=ob[:, 384:512], rhs=vnew[c][:], start=False, stop=True),
                     rd(ob, vnew[c]), rd(p2[c]))
                s.op("pe", lambda e: e.matmul(out=p3[c][:, :], lhsT=ob[:, 512:640], rhs=vnew[c][:], start=True, stop=True),
                     rd(ob, vnew[c]), rd(p3[c]))
            for (c, h, d, n, ob, Sd) in chains:
                s.op("act", lambda e: e.copy(out=oev[c][:], in_=p2[c][:, :]), rd(p2[c]), rd(oev[c]))
                s.dma("sp", C.OFB[d][n * 128:(n + 1) * 128, h * 128:(h + 1) * 128], oev[c][:], rd(oev[c]), rd(C.OFB[d]), nowaw=True)
                s.op("dve", lambda e: e.scalar_tensor_tensor(out=Sd[:], in0=Sd[:], scalar=ob[:, 640 + c_col(d, h):641 + c_col(d, h)],
                                                             in1=p3[c][:, :], op0=ALU.mult, op1=ALU.add),
                     rd(Sd, ob, p3[c]), rd(Sd))
    if C.dbg_stage == "gdnscan":
        return
    s.barrier()
    with ExitStack() as st:
        st.enter_context(C.kb.nc.named_scope("gdnfin_l%d" % l))
        gnw = kb.sb(st, [128, 128], F32, "gnw")
        bcast_row(C, st, gnw[:], gnw, C.gdn_norm_w[l:l + 1, :], C.gdn_norm_w, 128, psb[2])
        NBF = 2
        of = [kb.sb(st, [128, 512], F32, "of%d" % i) for i in range(NBF)]
        obk = [kb.sb(st, [128, 512], F32, "obk%d" % i) for i in range(NBF)]
        zz = [kb.sb(st, [128, 512], F32, "zz%d" % i) for i in range(NBF)]
        yy = [kb.sb(st, [128, 512], F32, "yy%d" % i) for i in range(NBF)]
        sq = [kb.sb(st, [128, 4], F32, "sq%d" % i) for i in range(NBF)]
        junk = kb.sb(st, [128, 128], F32, "junk2")
        gT = [kb.sb(st, [128, 512], BF16, "gT%d" % i) for i in range(NBF)]
        for n in range(NTT):
            b = n % NBF
            pb = psb[n % 2]
            s.dma("sp", of[b][:], C.OFB[0][n * 128:(n + 1) * 128, :], rd(C.OFB[0]), rd(of[b]))
            s.dma("pool", obk[b][:], C.OFB[1][n * 128:(n + 1) * 128, :], rd(C.OFB[1]), rd(obk[b]))
            s.dma("sp", zz[b][:], C.PTOK[n * 128:(n + 1) * 128, 0:512], rd(C.PTOK), rd(zz[b]))
            s.op("dve", lambda e: e.tensor_tensor(out=of[b][:], in0=of[b][:], in1=obk[b][:], op=ALU.add), rd(of[b], obk[b]), rd(of[b]))
            s.op("act", lambda e: e.activation(out=zz[b][:], in_=zz[b][:], func=AF.Silu), rd(zz[b]), rd(zz[b]))
            for h in range(4):
                s.op("act", lambda e: e.activation(out=junk[:], in_=of[b][:, h * 128:(h + 1) * 128], func=AF.Square,
                                                   accum_out=sq[b][:, h:h + 1]), rd(of[b]), rd(junk, sq[b]))
            s.op("dve", lambda e: e.tensor_scalar(out=sq[b][:], in0=sq[b][:], scalar1=1.0 / 128.0, scalar2=RMS_EPS,
                                                  op0=ALU.mult, op1=ALU.add), rd(sq[b]), rd(sq[b]))
            s.op("act", lambda e: e.sqrt(out=sq[b][:], in_=sq[b][:]), rd(sq[b]), rd(sq[b]))
            s.op("dve", lambda e: e.reciprocal(out=sq[b][:], in_=sq[b][:]), rd(sq[b]), rd(sq[b]))
            for h in range(4):
                s.op("pool", lambda e: e.tensor_tensor(out=zz[b][:, h * 128:(h + 1) * 128], in0=zz[b][:, h * 128:(h + 1) * 128],
                                                       in1=gnw[:], op=ALU.mult), rd(zz[b], gnw), rd(zz[b]))
            for h in range(4):
                s.op("dve", lambda e: e.scalar_tensor_tensor(out=yy[b][:, h * 128:(h + 1) * 128], in0=of[b][:, h * 128:(h + 1) * 128],
                                                             scalar=sq[b][:, h:h + 1], in1=zz[b][:, h * 128:(h + 1) * 128],
                                                             op0=ALU.mult, op1=ALU.mult), rd(of[b], sq[b], zz[b]), rd(yy[b]))
            for h in range(4):
                s.op("pe", lambda e: e.transpose(out=pb[:, h * 128:(h + 1) * 128], in_=yy[b][:, h * 128:(h + 1) * 128],
                                                 identity=ident[:]), rd(yy[b], ident), rd(pb))
            s.op("act", lambda e: e.copy(out=gT[b][:], in_=pb[:, :]), rd(pb), rd(gT[b]))
            s.dma("sp", C.MIXT.t[0:512, n * 128:(n + 1) * 128].rearrange("(h p) t -> p h t", p=128),
                  gT[b][:].rearrange("p (h t) -> p h t", h=4), rd(gT[b]), rd(C.MIXT), nowaw=True)
            if C.dbg_stage == "gdn":
                s.dma("sp", C.o_gdn[n * 128:(n + 1) * 128, :], yy[b][:], rd(yy[b]), rd(C.o_gdn))


def stage_att(C, l):
    kb, s = C.kb, C.kb.s
    ident, psb = C.ident, C.psb
    with ExitStack() as st:
        st.enter_context(C.kb.nc.named_scope("att_l%d" % l))
        QT = kb.sb(st, [128, 4, NT], BF16, "QT")
        KT = kb.sb(st, [128, 2, NT], BF16, "KT")
        Vb = kb.sb(st, [128, NTT, 256], BF16, "Vb")
        onesb = kb.sb(st, [128, 128], BF16, "onesb")
        s.op("pool", lambda e: e.memset(onesb[:], 1.0), (), rd(onesb))
        w6 = kb.sb(st, [128, 2, 128], F32, "w6")
        bcast_row(C, st, w6[:, 0, :], w6, C.q_norm_w[l:l + 1, :], C.q_norm_w, 128, psb[0])
        bcast_row(C, st, w6[:, 1, :], w6, C.k_norm_w[l:l + 1, :], C.k_norm_w, 128, psb[0])
        NB = 2
        xa = [kb.sb(st, [128, 1024], F32, "xa%d" % i) for i in range(NB)]
        xn = [kb.sb(st, [128, 768], F32, "xnq%d" % i) for i in range(NB)]
        xr = [kb.sb(st, [128, 768], F32, "xr%d" % i) for i in range(NB)]
        cs_ = [kb.sb(st, [128, 2, 384], F32, "cs%d" % i) for i in range(NB)]
        tmp = [kb.sb(st, [128, 4, 384], F32, "rtmp%d" % i) for i in range(NB)]
        ssq = [kb.sb(st, [128, 6], F32, "assq%d" % i) for i in range(NB)]
        junk = kb.sb(st, [128, 128], F32, "ajunk")
        for t in range(NTT):
            b = t % NB
            lat = t < L // 128
            s.dma("sp", xa[b][:], C.PTOK[t * 128:(t + 1) * 128, 528:1552], rd(C.PTOK), rd(xa[b]))
            if lat:
                s.dma("pool", cs_[b][:], C.rope_in.t[:, t * 128:(t + 1) * 128, :].rearrange("c p f -> p c f"),
                      rd(C.rope_in), rd(cs_[b]))
            for h in range(6):
                s.op("act", lambda e: e.activation(out=junk[:], in_=xa[b][:, h * 128:(h + 1) * 128], func=AF.Square,
                                                   accum_out=ssq[b][:, h:h + 1]), rd(xa[b]), rd(junk, ssq[b]))
            s.op("dve", lambda e: e.tensor_scalar(out=ssq[b][:], in0=ssq[b][:], scalar1=1.0 / 128.0, scalar2=RMS_EPS,
                                                  op0=ALU.mult, op1=ALU.add), rd(ssq[b]), rd(ssq[b]))
            s.op("act", lambda e: e.sqrt(out=ssq[b][:], in_=ssq[b][:]), rd(ssq[b]), rd(ssq[b]))
            s.op("dve", lambda e: e.reciprocal(out=ssq[b][:], in_=ssq[b][:]), rd(ssq[b]), rd(ssq[b]))
            for h in range(6):
                s.op("dve", lambda e: e.scalar_tensor_tensor(out=xn[b][:, h * 128:(h + 1) * 128], in0=xa[b][:, h * 128:(h + 1) * 128],
                                                             scalar=ssq[b][:, h:h + 1], in1=w6[:, 0 if h < 4 else 1, :],
                                                             op0=ALU.mult, op1=ALU.mult), rd(xa[b], ssq[b], w6), rd(xn[b]))
            s.op("pool", lambda e: e.tensor_copy(out=Vb[:, t, :], in_=xa[b][:, 768:1024]), rd(xa[b]), rd(Vb), nowaw=True)
            if lat:
                x1 = xn[b][:].rearrange("p (g two f) -> p g two f", two=2, f=32)[:, :, 0, :]
                x2 = xn[b][:].rearrange("p (g two f) -> p g two f", two=2, f=32)[:, :, 1, :]
                o1 = xr[b][:].rearrange("p (g two f) -> p g two f", two=2, f=32)[:, :, 0, :]
                o2 = xr[b][:].rearrange("p (g two f) -> p g two f", two=2, f=32)[:, :, 1, :]
                cc = cs_[b][:, 0, :].rearrange("p (g f) -> p g f", f=32)
                sn = cs_[b][:, 1, :].rearrange("p (g f) -> p g f", f=32)
                tm = [tmp[b][:, i, :].rearrange("p (g f) -> p g f", f=32) for i in range(4)]
                s.op("dve", lambda e: e.tensor_tensor(out=tm[0], in0=x1, in1=cc, op=ALU.mult), rd(xn[b], cs_[b]), rd(tmp[b]))
                s.op("pool", lambda e: e.tensor_tensor(out=tm[1], in0=x2, in1=sn, op=ALU.mult), rd(xn[b], cs_[b]), rd(tmp[b]))
                s.op("dve", lambda e: e.tensor_tensor(out=tm[2], in0=x2, in1=cc, op=ALU.mult), rd(xn[b], cs_[b]), rd(tmp[b]))
                s.op("pool", lambda e: e.tensor_tensor(out=tm[3], in0=x1, in1=sn, op=ALU.mult), rd(xn[b], cs_[b]), rd(tmp[b]))
                s.op("dve", lambda e: e.tensor_tensor(out=o1, in0=tm[0], in1=tm[1], op=ALU.subtract), rd(tmp[b]), rd(xr[b]))
                s.op("pool", lambda e: e.tensor_tensor(out=o2, in0=tm[2], in1=tm[3], op=ALU.add), rd(tmp[b]), rd(xr[b]))
                src = xr[b]
            else:
                src = xn[b]
            for h in range(6):
                pb = psb[1] if h < 4 else psb[2]
                s.op("pe", lambda e: e.transpose(out=pb[:, (h % 4) * 128:(h % 4 + 1) * 128], in_=src[:, h * 128:(h + 1) * 128],
                                                 identity=ident[:]), rd(src, ident), rd(pb))
            s.op("act", lambda e: e.copy(out=QT[:, :, t * 128:(t + 1) * 128],
                                         in_=psb[1][:, :].rearrange("p (h t) -> p h t", h=4)), rd(psb[1]), rd(QT), nowaw=True)
            s.op("dve", lambda e: e.tensor_copy(out=KT[:, :, t * 128:(t + 1) * 128],
                                                in_=psb[2][:, 0:256].rearrange("p (h t) -> p h t", h=2)), rd(psb[2]), rd(KT), nowaw=True)
        if C.dbg_stage == "attq":
            s.dma("sp", C.o_dbg[:, 0:512].bitcast(BF16)[:, 0:512], QT[:, 1, 0:512], rd(QT), rd(C.o_dbg))
            s.dma("sp", C.o_dbg[:, 512:1024].bitcast(BF16)[:, 0:512], KT[:, 1, 0:512], rd(KT), rd(C.o_dbg))
            return
        PT = [kb.sb(st, [128, 512], BF16, "PT%d" % i) for i in range(3)]
        rec = [kb.sb(st, [128, 512], F32, "rec%d" % i) for i in range(2)]
        aT = [kb.sb(st, [128, 512], BF16, "aT%d" % i) for i in range(2)]
        jobs = []
        for h in range(4):
            for q0 in range(0, L, 512):
                jobs.append((h, q0, 512, list(range(NTT))))
            jobs.append((h, L, LC, [32, 33]))
        scale = 128.0 ** -0.5
        k = 0
        for ji, (h, q0, qw, ktiles) in enumerate(jobs):
            kv = h // 2
            pO = psb[0] if ji % 2 == 0 else psb[5]
            pS = psb[1] if ji % 2 == 0 else psb[6]
            nk = len(ktiles)
            slots = []
            for i in range(nk):
                slots.append((psb[2 + k % 3], PT[k % 3]))
                k += 1

            def qk(i):
                pst, _ = slots[i]
                kt = ktiles[i]
                s.op("pe", lambda e: e.matmul(out=pst[:, 0:qw], lhsT=KT[:, kv, kt * 128:(kt + 1) * 128], rhs=QT[:, h, q0:q0 + qw],
                                              start=True, stop=True), rd(KT, QT), rd(pst))
            qk(0)
            for i, kt in enumerate(ktiles):
                pst, pt = slots[i]
                if i + 1 < nk:
                    qk(i + 1)
                s.op("act", lambda e: e.activation(out=pt[:, 0:qw], in_=pst[:, 0:qw], func=AF.Exp, scale=scale), rd(pst), rd(pt))
                s.op("pe", lambda e: e.matmul(out=pO[:, 0:qw], lhsT=Vb[:, kt, kv * 128:(kv + 1) * 128], rhs=pt[:, 0:qw],
                                              start=(i == 0), stop=(i == nk - 1)), rd(Vb, pt), rd(pO))
                s.op("pe", lambda e: e.matmul(out=pS[:, 0:qw], lhsT=onesb[:], rhs=pt[:, 0:qw],
                                              start=(i == 0), stop=(i == nk - 1)), rd(onesb, pt), rd(pS))
            rc = rec[ji % 2]
            at = aT[ji % 2]
            s.op("dve", lambda e: e.reciprocal(out=rc[:, 0:qw], in_=pS[:, 0:qw]), rd(pS), rd(rc))
            s.op("dve", lambda e: e.tensor_tensor(out=at[:, 0:qw], in0=pO[:, 0:qw], in1=rc[:, 0:qw], op=ALU.mult), rd(pO, rc), rd(at))
            s.dma("sp", C.MIXT[512 + h * 128:512 + (h + 1) * 128, q0:q0 + qw], at[:, 0:qw], rd(at), rd(C.MIXT), nowaw=True)


def load_bf16_w(C, st, dram_t, src_ap_fn, kchunks, ncols, name):
    kb, s = C.kb, C.kb.s
    w = kb.sb(st, [128, kchunks, ncols], BF16, name)
    for kc in range(kchunks):
        for c0 in range(0, ncols, 2048):
            c1 = min(ncols, c0 + 2048)
            s.dma("pool", w[:, kc, c0:c1], src_ap_fn(kc, c0, c1), rd(dram_t), rd(w))
    return w


def stage_D(C, l, M):
    kb, s = C.kb, C.kb.s
    ident, psb = C.ident, C.psb
    Xsrc = C.XS if l == 0 else C.X2
    with ExitStack() as st:
        st.enter_context(C.kb.nc.named_scope("D_l%d" % l))
        wout = load_bf16_w(C, st, C.w_out, lambda kc, c0, c1: C.w_out[l, kc * 128:(kc + 1) * 128, c0:c1], 8, D, "wout")
        wsgu = kb.sb(st, [128, 8, 512], BF16, "wsgu")
        for kc in range(8):
            s.dma("pool", wsgu[:, kc, 0:256], C.sh_w_gate[l, kc * 128:(kc + 1) * 128, :], rd(C.sh_w_gate), rd(wsgu))
            s.dma("pool", wsgu[:, kc, 256:512], C.sh_w_up[l, kc * 128:(kc + 1) * 128, :], rd(C.sh_w_up), rd(wsgu))
        wsd = load_bf16_w(C, st, C.sh_w_down, lambda kc, c0, c1: C.sh_w_down[l, kc * 128:(kc + 1) * 128, c0:c1], 2, D, "wsd")
        rw = kb.sb(st, [128, 8, 256], F32, "rw")
        s.dma("sp", rw[:], C.router_w.t[l].rearrange("(k p) e -> p k e", p=128), rd(C.router_w), rd(rw))
        G1 = kb.sb(st, [128, 2, D], F32, "G1")
        SC2 = kb.sb(st, [128, 2, D], F32, "SC2")
        SH2 = kb.sb(st, [128, 2, D], F32, "SH2")
        for r in range(2):
            bcast_row(C, st, G1[:, r, :], G1, C.MODD[l, r:r + 1, 2048:3072], C.MODD, D, psb[0])
            bcast_row(C, st, SH2[:, r, :], SH2, C.MODD[l, r:r + 1, 3072:4096], C.MODD, D, psb[0])
            bcast_row(C, st, SC2[:, r, :], SC2, C.MODD[l, r:r + 1, 4096:5120], C.MODD, D, psb[0])
        s.op("dve", lambda e: e.tensor_scalar_add(out=SC2[:], in0=SC2[:], scalar1=1.0), rd(SC2), rd(SC2))
        LNW = kb.sb(st, [128, D], F32, "LNW")
        LNB = kb.sb(st, [128, D], F32, "LNB")
        bcast_row(C, st, LNW[:], LNW, C.ln1_w[l:l + 1, :], C.ln1_w, D, psb[0])
        bcast_row(C, st, LNB[:], LNB, C.ln1_b[l:l + 1, :], C.ln1_b, D, psb[0])
        RB = kb.sb(st, [128, 256], F32, "RB")
        bcast_row(C, st, RB[:], RB, C.router_bias[l:l + 1, :], C.router_bias, 256, psb[0])
        NB = 2
        mixT = [kb.sb(st, [128, 8, 128], BF16, "mixT%d" % i) for i in range(NB)]
        xt = [kb.sb(st, [128, D], F32, "xt%d" % i) for i in range(NB)]
        tt = [kb.sb(st, [128, D], F32, "tt%d" % i) for i in range(NB)]
        x1 = [kb.sb(st, [128, D], F32, "x1%d" % i) for i in range(NB)]
        h2 = [kb.sb(st, [128, D], F32, "h2%d" % i) for i in range(NB)]
        h2Tf = [kb.sb(st, [128, 8, 128], F32, "h2Tf%d" % i) for i in range(NB)]
        h2Tb = [kb.sb(st, [128, 8, 128], BF16, "h2Tb%d" % i) for i in range(NB)]
        stats = [kb.sb(st, [128, 2, 6], F32, "dst%d" % i) for i in range(NB)]
        mv = [kb.sb(st, [128, 2], F32, "dmv%d" % i) for i in range(NB)]
        rstd = [kb.sb(st, [128, 1], F32, "drs%d" % i) for i in range(NB)]
        sg = [kb.sb(st, [128, 256], F32, "sg%d" % i) for i in range(NB)]
        sel = [kb.sb(st, [128, 256], F32, "sel%d" % i) for i in range(NB)]
        selm = [kb.sb(st, [128, 256], F32, "selm%d" % i) for i in range(NB)]
        g8 = [kb.sb(st, [128, 8, 8], F32, "g8%d" % i) for i in range(NB)]
        grp = [kb.sb(st, [128, 8], F32, "grp%d" % i) for i in range(NB)]
        gs8 = [kb.sb(st, [128, 8], F32, "gs8%d" % i) for i in range(NB)]
        gm = [kb.sb(st, [128, 8], F32, "gm%d" % i) for i in range(NB)]
        t8 = [kb.sb(st, [128, 8], F32, "t8%d" % i) for i in range(NB)]
        den = [kb.sb(st, [128, 1], F32, "den%d" % i) for i in range(NB)]
        sact = [kb.sb(st, [128, 256], F32, "sact%d" % i) for i in range(NB)]
        sact2 = [kb.sb(st, [128, 256], F32, "sactb%d" % i) for i in range(NB)]
        sactT = [kb.sb(st, [128, 2, 128], BF16, "sactT%d" % i) for i in range(NB)]
        sho = [kb.sb(st, [128, D], F32, "sho%d" % i) for i in range(NB)]
        for t in range(NTT):
            b = t % NB
            r = 0 if t < L // 128 else 1
            rows = slice(t * 128, (t + 1) * 128)
            s.dma("sp", mixT[b][:], C.MIXT.t[:, rows].rearrange("(k p) t -> p k t", p=128), rd(C.MIXT), rd(mixT[b]))
            s.dma("sp", xt[b][:], Xsrc[rows, :], rd(Xsrc), rd(xt[b]))
            for cb in range(2):
                for kc in range(8):
                    s.op("pe", lambda e: e.matmul(out=psb[cb][:, :], lhsT=mixT[b][:, kc, :], rhs=wout[:, kc, cb * 512:(cb + 1) * 512],
                                                  start=(kc == 0), stop=(kc == 7)), rd(mixT[b], wout), rd(psb[cb]))
            for cb in range(2):
                cs = slice(cb * 512, (cb + 1) * 512)
                s.op("dve", lambda e: e.tensor_tensor(out=tt[b][:, cs], in0=psb[cb][:, :], in1=G1[:, r, cs], op=ALU.mult),
                     rd(psb[cb], G1), rd(tt[b]))
            if C.dbg_stage == "D":
                s.dma("sp", C.o_y[rows, :], tt[b][:], rd(tt[b]), rd(C.o_y))
            s.op("dve", lambda e: e.scalar_tensor_tensor(out=tt[b][:], in0=xt[b][:], scalar=ALPHA, in1=tt[b][:],
                                                         op0=ALU.mult, op1=ALU.add), rd(xt[b], tt[b]), rd(tt[b]))
            layer_norm_tile(C, tt[b], x1[b], stats[b], mv[b], rstd[b], LNW, LNB)
            s.dma("sp", C.X1[rows, :], x1[b][:], rd(x1[b]), rd(C.X1), nowaw=True)
            s.op("dve", lambda e: e.tensor_tensor(out=h2[b][:], in0=x1[b][:], in1=SC2[:, r, :], op=ALU.mult), rd(x1[b], SC2), rd(h2[b]))
            s.op("dve", lambda e: e.tensor_tensor(out=h2[b][:], in0=h2[b][:], in1=SH2[:, r, :], op=ALU.add), rd(h2[b], SH2), rd(h2[b]))
            s.dma("sp", C.H2[rows, :], h2[b][:], rd(h2[b]), rd(C.H2), nowaw=True)
            for kc in range(8):
                pb = psb[2 + kc // 4]
                s.op("pe", lambda e: e.transpose(out=pb[:, (kc % 4) * 128:(kc % 4 + 1) * 128], in_=h2[b][:, kc * 128:(kc + 1) * 128],
                                                 identity=ident[:]), rd(h2[b], ident), rd(pb))
            for hh in range(2):
                s.op("act", lambda e: e.copy(out=h2Tf[b][:, hh * 4:hh * 4 + 4, :], in_=psb[2 + hh][:, :].rearrange("p (k t) -> p k t", k=4)),
                     rd(psb[2 + hh]), rd(h2Tf[b]), nowaw=True)
                s.op("dve", lambda e: e.tensor_copy(out=h2Tb[b][:, hh * 4:hh * 4 + 4, :], in_=psb[2 + hh][:, :].rearrange("p (k t) -> p k t", k=4)),
                     rd(psb[2 + hh]), rd(h2Tb[b]), nowaw=True)
            for kc in range(8):
                s.op("pe", lambda e: e.matmul(out=psb[4][:, 0:256], lhsT=h2Tf[b][:, kc, :], rhs=rw[:, kc, :],
                                              start=(kc == 0), stop=(kc == 7)), rd(h2Tf[b], rw), rd(psb[4]))
            s.op("act", lambda e: e.activation(out=sg[b][:], in_=psb[4][:, 0:256], func=AF.Sigmoid), rd(psb[4]), rd(sg[b]))
            s.op("dve", lambda e: e.tensor_tensor(out=sel[b][:], in0=sg[b][:], in1=RB[:], op=ALU.add), rd(sg[b], RB), rd(sel[b]))
            for g in range(8):
                s.op("dve", lambda e: e.max(out=g8[b][:, g, :], in_=sel[b][:, g * 32:(g + 1) * 32]), rd(sel[b]), rd(g8[b]))
            s.op("dve", lambda e: e.tensor_tensor(out=grp[b][:], in0=g8[b][:, :, 0], in1=g8[b][:, :, 1], op=ALU.add), rd(g8[b]), rd(grp[b]))
            s.op("dve", lambda e: e.max(out=gs8[b][:], in_=grp[b][:]), rd(grp[b]), rd(gs8[b]))
            s.op("dve", lambda e: e.tensor_scalar(out=gm[b][:], in0=grp[b][:], scalar1=gs8[b][:, 3:4], scalar2=None, op0=ALU.is_ge),
                 rd(grp[b], gs8[b]), rd(gm[b]))
            for g in range(8):
                s.op("dve", lambda e: e.tensor_scalar(out=selm[b][:, g * 32:(g + 1) * 32], in0=sel[b][:, g * 32:(g + 1) * 32],
                                                      scalar1=2.0, scalar2=gm[b][:, g:g + 1], op0=ALU.add, op1=ALU.mult),
                     rd(sel[b], gm[b]), rd(selm[b]))
            s.op("dve", lambda e: e.max(out=t8[b][:], in_=selm[b][:]), rd(selm[b]), rd(t8[b]))
            s.op("dve", lambda e: e.tensor_scalar(out=sel[b][:], in0=selm[b][:], scalar1=t8[b][:, 7:8], scalar2=None, op0=ALU.is_ge),
                 rd(selm[b], t8[b]), rd(sel[b]))
            s.op("dve", lambda e: e.tensor_tensor(out=M.WFULL[:, t, :], in0=sg[b][:], in1=sel[b][:], op=ALU.mult),
                 rd(sg[b], sel[b]), rd(M.WFULL))
            s.op("dve", lambda e: e.reduce_sum(out=den[b][:], in_=M.WFULL[:, t, :], axis=AX.X), rd(M.WFULL), rd(den[b]))
            s.op("dve", lambda e: e.reciprocal(out=den[b][:], in_=den[b][:]), rd(den[b]), rd(den[b]))
            s.op("dve", lambda e: e.tensor_scalar(out=M.WFULL[:, t, :], in0=M.WFULL[:, t, :], scalar1=den[b][:, 0:1], scalar2=2.5,
                                                  op0=ALU.mult, op1=ALU.mult), rd(M.WFULL, den[b]), rd(M.WFULL))
            for kc in range(8):
                s.op("pe", lambda e: e.matmul(out=psb[5][:, :], lhsT=h2Tb[b][:, kc, :], rhs=wsgu[:, kc, :],
                                              start=(kc == 0), stop=(kc == 7)), rd(h2Tb[b], wsgu), rd(psb[5]))
            s.op("act", lambda e: e.activation(out=sact[b][:], in_=psb[5][:, 0:256], func=AF.Silu), rd(psb[5]), rd(sact[b]))
            s.op("dve", lambda e: e.tensor_tensor(out=sact2[b][:], in0=sact[b][:], in1=psb[5][:, 256:512], op=ALU.mult),
                 rd(sact[b], psb[5]), rd(sact2[b]))
            for k2 in range(2):
                s.op("pe", lambda e: e.transpose(out=psb[4][:, 256 + k2 * 128:384 + k2 * 128], in_=sact2[b][:, k2 * 128:(k2 + 1) * 128],
                                                 identity=ident[:]), rd(sact2[b], ident), rd(psb[4]))
            s.op("act", lambda e: e.copy(out=sactT[b][:], in_=psb[4][:, 256:512].rearrange("p (k t) -> p k t", k=2)), rd(psb[4]), rd(sactT[b]))
            for cb in range(2):
                for k2 in range(2):
                    s.op("pe", lambda e: e.matmul(out=psb[6 + cb][:, :], lhsT=sactT[b][:, k2, :], rhs=wsd[:, k2, cb * 512:(cb + 1) * 512],
                                                  start=(k2 == 0), stop=(k2 == 1)), rd(sactT[b], wsd), rd(psb[6 + cb]))
                if cb == 0:
                    s.op("act", lambda e: e.copy(out=sho[b][:, 0:512], in_=psb[6][:, :]), rd(psb[6]), rd(sho[b]), nowaw=True)
                else:
                    s.op("dve", lambda e: e.tensor_copy(out=sho[b][:, 512:1024], in_=psb[7][:, :]), rd(psb[7]), rd(sho[b]), nowaw=True)
            s.dma("sp", C.SHO[rows, :], sho[b][:], rd(sho[b]), rd(C.SHO), nowaw=True)


def layer_norm_tile(C, src, dst, stats, mv, rstd, LNW, LNB):
    s = C.kb.s
    for j in range(2):
        s.op("dve", lambda e: e.bn_stats(out=stats[:, j, :], in_=src[:, j * 512:(j + 1) * 512]), rd(src), rd(stats))
    s.op("dve", lambda e: e.bn_aggr(out=mv[:], in_=stats[:].rearrange("p a b -> p (a b)")), rd(stats), rd(mv))
    s.op("dve", lambda e: e.tensor_scalar_add(out=rstd[:], in0=mv[:, 1:2], scalar1=LN_EPS), rd(mv), rd(rstd))
    s.op("act", lambda e: e.sqrt(out=rstd[:], in_=rstd[:]), rd(rstd), rd(rstd))
    s.op("dve", lambda e: e.reciprocal(out=rstd[:], in_=rstd[:]), rd(rstd), rd(rstd))
    s.op("dve", lambda e: e.tensor_scalar(out=dst[:], in0=src[:], scalar1=mv[:, 0:1], scalar2=rstd[:, 0:1],
                                          op0=ALU.subtract, op1=ALU.mult), rd(src, mv, rstd), rd(dst))
    s.op("dve", lambda e: e.tensor_tensor(out=dst[:], in0=dst[:], in1=LNW[:], op=ALU.mult), rd(dst, LNW), rd(dst))
    s.op("dve", lambda e: e.tensor_tensor(out=dst[:], in0=dst[:], in1=LNB[:], op=ALU.add), rd(dst, LNB), rd(dst))


TAB_LAYERS = DEPTH
BS = 256
NBLK = 391
NSTL = NBLK * 2
NSLOT = NSTL * 128


def stage_E(C, l, M):
    kb, s = C.kb, C.kb.s
    ident, ones, masks, psb = C.ident, C.ones, C.masks, C.psb
    WF = M.WFULL
    with ExitStack() as st:
        iot = kb.sb(st, [128, NBLK + 1 + NTT], F32, "iot")
        s.dma("sp", iot[:], C.iotas[:], rd(C.iotas), rd(iot))
        IDXW = kb.sb(st, [128, NBLK], I32, "IDXW")
        SLOT8 = kb.sb(st, [128, NTT, 8], I32, "SLOT8")
        BT = kb.sb(st, [128, NSTL, 2], F32, "BT")
        IDXT = kb.sb(st, [128, NSTL], I32, "IDXT")
        with ExitStack() as s1:
            s1.enter_context(C.kb.nc.named_scope("E1_l%d" % l))
            RANK = kb.sb(s1, [128, NTT, 256], F32, "RANK")
            cnt = kb.sb(s1, [128, 256], F32, "cnt")
            s.op("pool", lambda e: e.memset(cnt[:], 0.0), (), rd(cnt))
            mk = [kb.sb(s1, [128, 256], F32, "mk%d" % i) for i in range(2)]
            for t in range(NTT):
                b = t % 2
                s.op("dve", lambda e: e.tensor_scalar(out=mk[b][:], in0=WF[:, t, :], scalar1=0.0, scalar2=None, op0=ALU.is_gt),
                     rd(WF), rd(mk[b]))
                s.op("pe", lambda e: e.matmul(out=psb[0][:, 0:256], lhsT=masks[:, 3, :], rhs=mk[b][:], start=True, stop=True),
                     rd(masks, mk[b]), rd(psb[0]))
                s.op("pe", lambda e: e.matmul(out=psb[1][:, 0:256], lhsT=ones[:], rhs=mk[b][:], start=True, stop=True),
                     rd(ones, mk[b]), rd(psb[1]))
                s.op("dve", lambda e: e.tensor_tensor(out=RANK[:, t, :], in0=psb[0][:, 0:256], in1=cnt[:], op=ALU.add),
                     rd(psb[0], cnt), rd(RANK))
                s.op("dve", lambda e: e.tensor_tensor(out=cnt[:], in0=cnt[:], in1=psb[1][:, 0:256], op=ALU.add),
                     rd(psb[1], cnt), rd(cnt))
            ci = kb.sb(s1, [128, 256], I32, "ci")
            nblk = kb.sb(s1, [128, 256], F32, "nblk")
            blkend = kb.sb(s1, [128, 256], F32, "blkend")
            bs256 = kb.sb(s1, [128, 256], F32, "bs256")
            s.op("dve", lambda e: e.tensor_scalar(out=ci[:], in0=cnt[:], scalar1=float(BS - 1), scalar2=None, op0=ALU.add), rd(cnt), rd(ci))
            s.op("dve", lambda e: e.tensor_scalar(out=ci[:], in0=ci[:], scalar1=8, scalar2=None, op0=ALU.arith_shift_right), rd(ci), rd(ci))
            s.op("dve", lambda e: e.tensor_copy(out=nblk[:], in_=ci[:]), rd(ci), rd(nblk))
            s.op("dve", lambda e: e.tensor_tensor_scan(out=blkend[:], data0=ones[:, 0:128].to_broadcast([128, 256]) if False else C.ones256[:],
                                                       data1=nblk[:], initial=0.0, op0=ALU.mult, op1=ALU.add), rd(nblk, C.ones256), rd(blkend))
            s.op("dve", lambda e: e.tensor_tensor(out=bs256[:], in0=blkend[:], in1=nblk[:], op=ALU.subtract), rd(blkend, nblk), rd(bs256))
            s.op("dve", lambda e: e.tensor_scalar(out=bs256[:], in0=bs256[:], scalar1=float(BS), scalar2=1.0, op0=ALU.mult, op1=ALU.add),
                 rd(bs256), rd(bs256))
            bcol = kb.sb(s1, [128, 2], F32, "bcol")
            tmpd = kb.sb(s1, [128, 128], F32, "tmpd")
            cmpb = [kb.sb(s1, [128, NBLK], F32, "cmpb%d" % i) for i in range(2)]
            for c in range(2):
                s.op("dve", lambda e: e.tensor_tensor(out=tmpd[:], in0=blkend[:, c * 128:(c + 1) * 128], in1=ident[:], op=ALU.mult),
                     rd(blkend, ident), rd(tmpd))
                s.op("dve", lambda e: e.reduce_sum(out=bcol[:, c:c + 1], in_=tmpd[:], axis=AX.X), rd(tmpd), rd(bcol))
                s.op("dve", lambda e: e.tensor_scalar(out=cmpb[c][:], in0=iot[:, 0:NBLK], scalar1=bcol[:, c:c + 1], scalar2=None, op0=ALU.is_ge),
                     rd(iot, bcol), rd(cmpb[c]))
                s.op("pe", lambda e: e.matmul(out=psb[2][:, 0:NBLK], lhsT=ones[:], rhs=cmpb[c][:], start=(c == 0), stop=(c == 1)),
                     rd(ones, cmpb[c]), rd(psb[2]))
            ebf = kb.sb(s1, [128, NBLK], F32, "ebf")
            oobf = kb.sb(s1, [128, NBLK], F32, "oobf")
            s.op("dve", lambda e: e.tensor_scalar(out=oobf[:], in0=psb[2][:, 0:NBLK], scalar1=255.5, scalar2=1.0e6, op0=ALU.is_ge, op1=ALU.mult),
                 rd(psb[2]), rd(oobf))
            s.op("dve", lambda e: e.tensor_scalar(out=ebf[:], in0=psb[2][:, 0:NBLK], scalar1=255.0, scalar2=128.0, op0=ALU.min, op1=ALU.mult),
                 rd(psb[2]), rd(ebf))
            s.op("dve", lambda e: e.tensor_tensor(out=ebf[:], in0=ebf[:], in1=oobf[:], op=ALU.add), rd(ebf, oobf), rd(ebf))
            s.op("dve", lambda e: e.tensor_scalar(out=ebf[:], in0=ebf[:], scalar1=iot[:, NBLK:NBLK + 1], scalar2=float(l * 32768),
                                                  op0=ALU.add, op1=ALU.add), rd(ebf, iot), rd(ebf))
            s.op("dve", lambda e: e.tensor_copy(out=IDXW[:], in_=ebf[:]), rd(ebf), rd(IDXW))
            zt = kb.sb(s1, [128, NSTL * 2], F32, "zt")
            s.op("pool", lambda e: e.memset(zt[:], 0.0), (), rd(zt))
            s.op("pool", lambda e: e.memset(zt[:].rearrange("p (n two) -> p n two", two=2)[:, :, 0], 60000.0), (), rd(zt))
            s.dma("sp", C.BUFTW.t.rearrange("(p n) two -> p (n two)", p=128), zt[:], rd(zt), rd(C.BUFTW))
            key = [kb.sb(s1, [128, 256], F32, "key%d" % i) for i in range(2)]
            top8 = [kb.sb(s1, [128, 8], F32, "top8%d" % i) for i in range(2)]
            oh = [kb.sb(s1, [128, 256], F32, "oh%d" % i) for i in range(2)]
            tw = [kb.sb(s1, [128, 8, 2], F32, "tw%d" % i) for i in range(2)]
            si = [kb.sb(s1, [128, 8], I32, "si%d" % i) for i in range(2)]
            lo = [kb.sb(s1, [128, 8], I32, "lo%d" % i) for i in range(2)]
            hi = [kb.sb(s1, [128, 8], I32, "hi%d" % i) for i in range(2)]
            lof = [kb.sb(s1, [128, 8], F32, "lof%d" % i) for i in range(2)]
            hif = [kb.sb(s1, [128, 8], F32, "hif%d" % i) for i in range(2)]
            posi = [kb.sb(s1, [128, 8], I32, "posi%d" % i) for i in range(2)]
            for t in range(NTT):
                b = t % 2
                s.op("dve", lambda e: e.tensor_scalar(out=mk[b][:], in0=WF[:, t, :], scalar1=0.0, scalar2=None, op0=ALU.is_gt),
                     rd(WF), rd(mk[b]))
                s.op("dve", lambda e: e.tensor_tensor(out=key[b][:], in0=RANK[:, t, :], in1=bs256[:], op=ALU.add), rd(RANK, bs256), rd(key[b]))
                s.op("dve", lambda e: e.tensor_tensor(out=key[b][:], in0=key[b][:], in1=mk[b][:], op=ALU.mult), rd(key[b], mk[b]), rd(key[b]))
                s.op("dve", lambda e: e.max(out=top8[b][:], in_=key[b][:]), rd(key[b]), rd(top8[b]))
                for k in range(8):
                    s.op("dve", lambda e: e.tensor_scalar(out=oh[b][:], in0=key[b][:], scalar1=top8[b][:, k:k + 1], scalar2=None, op0=ALU.is_equal),
                         rd(key[b], top8[b]), rd(oh[b]))
                    s.op("dve", lambda e: e.tensor_tensor(out=oh[b][:], in0=oh[b][:], in1=WF[:, t, :], op=ALU.mult), rd(oh[b], WF), rd(oh[b]))
                    s.op("dve", lambda e: e.reduce_sum(out=tw[b][:, k, 1:2], in_=oh[b][:], axis=AX.X), rd(oh[b]), rd(tw[b]))
                    s.op("pool", lambda e: e.tensor_copy(out=tw[b][:, k, 0:1], in_=iot[:, NBLK + 1 + t:NBLK + 2 + t]), rd(iot), rd(tw[b]))
                s.op("dve", lambda e: e.tensor_scalar(out=si[b][:], in0=top8[b][:], scalar1=-1.0, scalar2=None, op0=ALU.add), rd(top8[b]), rd(si[b]))
                s.op("dve", lambda e: e.tensor_copy(out=SLOT8[:, t, :], in_=si[b][:]), rd(si[b]), rd(SLOT8))
                s.op("dve", lambda e: e.tensor_scalar(out=lo[b][:], in0=si[b][:], scalar1=127, scalar2=None, op0=ALU.bitwise_and), rd(si[b]), rd(lo[b]))
                s.op("dve", lambda e: e.tensor_scalar(out=hi[b][:], in0=si[b][:], scalar1=7, scalar2=None, op0=ALU.arith_shift_right), rd(si[b]), rd(hi[b]))
                s.op("dve", lambda e: e.tensor_copy(out=lof[b][:], in_=lo[b][:]), rd(lo[b]), rd(lof[b]))
                s.op("dve", lambda e: e.tensor_copy(out=hif[b][:], in_=hi[b][:]), rd(hi[b]), rd(hif[b]))
                s.op("dve", lambda e: e.scalar_tensor_tensor(out=lof[b][:], in0=lof[b][:], scalar=float(NSTL), in1=hif[b][:],
                                                             op0=ALU.mult, op1=ALU.add), rd(lof[b], hif[b]), rd(lof[b]))
                s.op("dve", lambda e: e.tensor_copy(out=posi[b][:], in_=lof[b][:]), rd(lof[b]), rd(posi[b]))
                for k in range(8):
                    s.idma(out=C.BUFTW[:, :], in_=tw[b][:, k, :], out_off=posi[b][:, k:k + 1], reads=rd(tw[b], posi[b]), writes=rd(C.BUFTW), nowaw=True)
            s.dma("sp", BT[:], C.BUFTW.t.rearrange("(p n) two -> p n two", p=128), rd(C.BUFTW), rd(BT))
            s.op("dve", lambda e: e.tensor_copy(out=IDXT[:], in_=BT[:, :, 0]), rd(BT), rd(IDXT))
            if C.dbg_stage == "E1":
                s.dma("sp", C.o_e1[:, 0:NSTL * 2], BT[:].rearrange("p n two -> p (n two)"), rd(BT), rd(C.o_e1))
                s.dma("sp", C.o_e1[:, 2000:2000 + NBLK].bitcast(I32), IDXW[:], rd(IDXW), rd(C.o_e1))
                s.dma("sp", C.o_e1[:, 2400:2400 + NTT * 8].bitcast(I32), SLOT8[:].rearrange("p n k -> p (n k)"), rd(SLOT8), rd(C.o_e1))
                s.dma("sp", C.o_e1[:, 2700:2956], cnt[:], rd(cnt), rd(C.o_e1))
                return

        s.barrier()
        with ExitStack() as s2:
            s2.enter_context(C.kb.nc.named_scope("E2_l%d" % l))
            NW = 2
            wg = [kb.sb(s2, [128, 2048], BF16, "wg%d" % i) for i in range(NW)]
            wu = [kb.sb(s2, [128, 2048], BF16, "wu%d" % i) for i in range(NW)]
            wd = [kb.sb(s2, [128, 2048], BF16, "wd%d" % i) for i in range(NW)]
            NX = 3
            xg = [kb.sb(s2, [128, D], F32, "xg%d" % i) for i in range(NX)]
            for x_ in xg:
                s.op("pool", lambda e: e.memset(x_[:], 0.0), (), rd(x_))
            xT = [kb.sb(s2, [128, 8, 128], BF16, "xTe%d" % i) for i in range(2)]
            ga = [kb.sb(s2, [128, 256], F32, "ga%d" % i) for i in range(2)]
            a2 = [kb.sb(s2, [128, 256], F32, "a2%d" % i) for i in range(2)]
            aT = [kb.sb(s2, [128, 2, 128], BF16, "aTe%d" % i) for i in range(2)]
            eo = [kb.sb(s2, [128, D], BF16, "eo%d" % i) for i in range(2)]
            nblk_run = NBLK if C.dbg_stage != "E2s" else 8
            for blk in range(nblk_run):
                wb = blk % NW
                ix = IDXW[:, blk:blk + 1]
                s.idma(out=wg[wb][:], in_=C.WG[:, :], in_off=ix, reads=rd(C.WG, IDXW), writes=rd(wg[wb]), bounds_check=C.reg_tab, oob_is_err=False)
                s.idma(out=wu[wb][:], in_=C.WU[:, :], in_off=ix, reads=rd(C.WU, IDXW), writes=rd(wu[wb]), bounds_check=C.reg_tab, oob_is_err=False)
                s.idma(out=wd[wb][:], in_=C.WD[:, :], in_off=ix, reads=rd(C.WD, IDXW), writes=rd(wd[wb]), bounds_check=C.reg_tab, oob_is_err=False)
                for sti in range(2):
                    j = blk * 2 + sti
                    xb = xg[j % NX]
                    b2 = j % 2
                    s.idma(out=xb[:], in_=C.H2[:, :], in_off=IDXT[:, j:j + 1], reads=rd(C.H2, IDXT), writes=rd(xb), bounds_check=C.reg_nt, oob_is_err=False)
                    for kc in range(8):
                        pb = psb[kc // 4]
                        s.op("pe", lambda e: e.transpose(out=pb[:, (kc % 4) * 128:(kc % 4 + 1) * 128], in_=xb[:, kc * 128:(kc + 1) * 128],
                                                         identity=ident[:]), rd(xb, ident), rd(pb))
                    s.op("act", lambda e: e.copy(out=xT[b2][:, 0:4, :], in_=psb[0][:, :].rearrange("p (k t) -> p k t", k=4)), rd(psb[0]), rd(xT[b2]), nowaw=True)
                    s.op("dve", lambda e: e.tensor_copy(out=xT[b2][:, 4:8, :], in_=psb[1][:, :].rearrange("p (k t) -> p k t", k=4)), rd(psb[1]), rd(xT[b2]), nowaw=True)
                    for kc in range(8):
                        s.op("pe", lambda e: e.matmul(out=psb[2][:, 0:256], lhsT=xT[b2][:, kc, :], rhs=wg[wb][:, kc * 256:(kc + 1) * 256],
                                                      start=(kc == 0), stop=(kc == 7)), rd(xT[b2], wg[wb]), rd(psb[2]))
                    for kc in range(8):
                        s.op("pe", lambda e: e.matmul(out=psb[3][:, 0:256], lhsT=xT[b2][:, kc, :], rhs=wu[wb][:, kc * 256:(kc + 1) * 256],
                                                      start=(kc == 0), stop=(kc == 7)), rd(xT[b2], wu[wb]), rd(psb[3]))
                    s.op("act", lambda e: e.activation(out=ga[b2][:], in_=psb[2][:, 0:256], func=AF.Silu), rd(psb[2]), rd(ga[b2]))
                    s.op("dve", lambda e: e.tensor_tensor(out=a2[b2][:], in0=ga[b2][:], in1=psb[3][:, 0:256], op=ALU.mult), rd(ga[b2], psb[3]), rd(a2[b2]))
                    for k2 in range(2):
                        s.op("pe", lambda e: e.transpose(out=psb[4][:, k2 * 128:(k2 + 1) * 128], in_=a2[b2][:, k2 * 128:(k2 + 1) * 128],
                                                         identity=ident[:]), rd(a2[b2], ident), rd(psb[4]))
                    s.op("act", lambda e: e.copy(out=aT[b2][:], in_=psb[4][:, 0:256].rearrange("p (k t) -> p k t", k=2)), rd(psb[4]), rd(aT[b2]))
                    for cb in range(2):
                        for k2 in range(2):
                            s.op("pe", lambda e: e.matmul(out=psb[5 + cb][:, :], lhsT=aT[b2][:, k2, :],
                                                          rhs=wd[wb][:, k2 * 1024 + cb * 512:k2 * 1024 + (cb + 1) * 512],
                                                          start=(k2 == 0), stop=(k2 == 1)), rd(aT[b2], wd[wb]), rd(psb[5 + cb]))
                    s.op("act", lambda e: e.activation(out=eo[b2][:, 0:512], in_=psb[5][:, :], func=AF.Copy, scale=BT[:, j, 1:2]),
                         rd(psb[5], BT), rd(eo[b2]), nowaw=True)
                    s.op("dve", lambda e: e.tensor_scalar(out=eo[b2][:, 512:1024], in0=psb[6][:, :], scalar1=BT[:, j, 1:2], scalar2=None, op0=ALU.mult),
                         rd(psb[6], BT), rd(eo[b2]), nowaw=True)
                    s.dma("sp", C.EO[j * 128:(j + 1) * 128, :], eo[b2][:], rd(eo[b2]), rd(C.EO), nowaw=True)
            if C.dbg_stage in ("E2", "E2s"):
                return

        s.barrier()
        with ExitStack() as s3:
            s3.enter_context(C.kb.nc.named_scope("E3_l%d" % l))
            G2 = kb.sb(s3, [128, 2, D], F32, "G2")
            for r in range(2):
                bcast_row(C, s3, G2[:, r, :], G2, C.MODD[l, r:r + 1, 5120:6144], C.MODD, D, psb[7])
            LNW = kb.sb(s3, [128, D], F32, "LNW2")
            LNB = kb.sb(s3, [128, D], F32, "LNB2")
            bcast_row(C, s3, LNW[:], LNW, C.ln2_w[l:l + 1, :], C.ln2_w, D, psb[7])
            bcast_row(C, s3, LNB[:], LNB, C.ln2_b[l:l + 1, :], C.ln2_b, D, psb[7])
            gat = [kb.sb(s3, [128, D], BF16, "gat%d" % i) for i in range(4)]
            ffa = [kb.sb(s3, [128, D], F32, "ffa%d" % i) for i in range(2)]
            x1t = [kb.sb(s3, [128, D], F32, "x1t%d" % i) for i in range(2)]
            x2t = [kb.sb(s3, [128, D], F32, "x2t%d" % i) for i in range(2)]
            stats = [kb.sb(s3, [128, 2, 6], F32, "est%d" % i) for i in range(2)]
            mv = [kb.sb(s3, [128, 2], F32, "emv%d" % i) for i in range(2)]
            rstd = [kb.sb(s3, [128, 1], F32, "ers%d" % i) for i in range(2)]
            gi = 0
            for t in range(NTT):
                b = t % 2
                r = 0 if t < L // 128 else 1
                rows = slice(t * 128, (t + 1) * 128)
                s.dma("sp", ffa[b][:], C.SHO[rows, :], rd(C.SHO), rd(ffa[b]))
                s.dma("sp", x1t[b][:], C.X1[rows, :], rd(C.X1), rd(x1t[b]))
                for k in range(8):
                    g_ = gat[gi % 4]
                    gi += 1
                    s.idma(out=g_[:], in_=C.EO[:, :], in_off=SLOT8[:, t, k:k + 1], reads=rd(C.EO, SLOT8), writes=rd(g_))
                    eng = "dve"
                    s.op(eng, lambda e: e.tensor_tensor(out=ffa[b][:], in0=ffa[b][:], in1=g_[:], op=ALU.add), rd(ffa[b], g_), rd(ffa[b]))
                if C.dbg_stage == "E":
                    s.dma("sp", C.o_ff[rows, :], ffa[b][:], rd(ffa[b]), rd(C.o_ff))
                s.op("dve", lambda e: e.tensor_tensor(out=ffa[b][:], in0=ffa[b][:], in1=G2[:, r, :], op=ALU.mult), rd(ffa[b], G2), rd(ffa[b]))
                s.op("dve", lambda e: e.scalar_tensor_tensor(out=ffa[b][:], in0=x1t[b][:], scalar=ALPHA, in1=ffa[b][:],
                                                             op0=ALU.mult, op1=ALU.add), rd(x1t[b], ffa[b]), rd(ffa[b]))
                layer_norm_tile(C, ffa[b], x2t[b], stats[b], mv[b], rstd[b], LNW, LNB)
                s.dma("sp", C.X2[rows, :], x2t[b][:], rd(x2t[b]), rd(C.X2), nowaw=True)
                if l == DEPTH - 1 and t < L // 128 and C.out is not None:
                    s.dma("sp", C.out[rows, :], x2t[b][:], rd(x2t[b]), rd(C.out), nowaw=True)


def c_col(d, h):
    return d * 4 + h


USE_F32R = os.environ.get("F32R", "0") == "1"


def fr(ap):
    return ap.bitcast(mybir.dt.float32r) if USE_F32R else ap


def build(dbg_stage=None):
    nc = bass.Bass("TRN2", target_bir_lowering=False)
    es = ExitStack()
    kb = KB(nc, es)
    s = kb.s
    x_in = kb.dram("x", [NT, D], F32, kind="ExternalInput")
    c_in = kb.dram("c2", [2, D], F32, kind="ExternalInput")
    ada_w = kb.dram("ada_w", [DEPTH, D, 6 * D], F32, kind="ExternalInput")
    ada_b = kb.dram("ada_b", [DEPTH, 6 * D], F32, kind="ExternalInput")
    w_in = kb.dram("w_in", [DEPTH, D, N_IN], F32, kind="ExternalInput")
    ident_in = kb.dram("ident", [128, 128], F32, kind="ExternalInput")
    masks_in = kb.dram("masks", [8, 128, 128], F32, kind="ExternalInput")
    conv_w = kb.dram("conv_w", [DEPTH, 5, OFF_Z], F32, kind="ExternalInput")
    a_log = kb.dram("gdn_a_log", [DEPTH, 8], F32, kind="ExternalInput")
    dt_bias = kb.dram("gdn_dt_bias", [DEPTH, 8], F32, kind="ExternalInput")
    gdn_norm_w = kb.dram("gdn_norm_w", [DEPTH, 128], F32, kind="ExternalInput")
    q_norm_w = kb.dram("q_norm_w", [DEPTH, 128], F32, kind="ExternalInput")
    k_norm_w = kb.dram("k_norm_w", [DEPTH, 128], F32, kind="ExternalInput")
    rope_in = kb.dram("rope", [2, L, 384], F32, kind="ExternalInput")
    w_out = kb.dram("w_out", [DEPTH, D, D], F32, kind="ExternalInput")
    ln1_w = kb.dram("ln1_w", [DEPTH, D], F32, kind="ExternalInput")
    ln1_b = kb.dram("ln1_b", [DEPTH, D], F32, kind="ExternalInput")
    ln2_w = kb.dram("ln2_w", [DEPTH, D], F32, kind="ExternalInput")
    ln2_b = kb.dram("ln2_b", [DEPTH, D], F32, kind="ExternalInput")
    router_w = kb.dram("router_w", [DEPTH, D, 256], F32, kind="ExternalInput")
    router_bias = kb.dram("router_bias", [DEPTH, 256], F32, kind="ExternalInput")
    sh_w_gate = kb.dram("sh_w_gate", [DEPTH, D, 256], F32, kind="ExternalInput")
    sh_w_up = kb.dram("sh_w_up", [DEPTH, D, 256], F32, kind="ExternalInput")
    sh_w_down = kb.dram("sh_w_down", [DEPTH, 256, D], F32, kind="ExternalInput")
    iotas = kb.dram("iotas", [128, NBLK + 1 + NTT], F32, kind="ExternalInput")
    WG = WU = WD = None
    if dbg_stage in (None, "E1", "E2", "E2s", "E"):
        WG = kb.dram("WG", [TAB_LAYERS * 256 * 128, 2048], F32, kind="ExternalInput")
        WU = kb.dram("WU", [TAB_LAYERS * 256 * 128, 2048], F32, kind="ExternalInput")
        WD = kb.dram("WD", [TAB_LAYERS * 256 * 128, 2048], F32, kind="ExternalInput")
    outs = {}

    def dbg_out(name, shape, dt=F32):
        t = kb.dram(name, shape, dt, kind="ExternalOutput")
        outs[name] = t
        return t

    XS = kb.dram("XS", [NT, D], F32)
    MODD = kb.dram("MODD", [DEPTH, 2, 6 * D], F32)
    QKVT = kb.dram("QKVT", [OFF_Z, NT], F32)
    PTOK = kb.dram("PTOK", [NT, NTOKC], F32)
    OPSD = kb.dram("OPSD", [4, 2, NTT, 128, OPW], F32)
    OFB = [kb.dram("OFB%d" % d, [NT, 512], F32) for d in range(2)]
    MIXT = kb.dram("MIXT", [D, NT], BF16)
    X1 = kb.dram("X1", [NT, D], F32)
    X2 = kb.dram("X2", [NT, D], F32)
    H2 = kb.dram("H2", [NT, D], F32)
    SHO = kb.dram("SHO", [NT, D], F32)
    BUFTW = kb.dram("BUFTW", [NSLOT, 2], F32)
    EO = kb.dram("EO", [NSLOT, D], BF16)

    ces = es
    ident = kb.sb(ces, [128, 128], F32, "ident")
    s.dma("sp", ident[:], ident_in[:], reads=rd(ident_in), writes=rd(ident))
    identb = kb.sb(ces, [128, 128], BF16, "identb")
    s.op("dve", lambda e: e.tensor_copy(out=identb[:], in_=ident[:]), rd(ident), rd(identb))

    psb = [kb.ps(ces, [128, 512], F32, "bank%d" % i) for i in range(8)]
    for p_ in psb:
        p_.res.excl = True
    ones = kb.sb(ces, [128, 128], F32, "ones")
    s.op("pool", lambda e: e.memset(ones[:], 1.0), (), rd(ones))
    bcrow = kb.sb(ces, [128, D], F32, "bcrow")
    s.op("pool", lambda e: e.memset(bcrow[:], 0.0), (), rd(bcrow))
    masks = kb.sb(ces, [128, 8, 128], F32, "masks")
    ones256 = kb.sb(ces, [128, 256], F32, "ones256")
    s.op("pool", lambda e: e.memset(ones256[:], 1.0), (), rd(ones256))
    s.dma("sp", masks[:], masks_in.t.rearrange("m p f -> p m f"), rd(masks_in), rd(masks))

    final_out = dbg_out("out", [L, D]) if dbg_stage is None else None
    for l in range(DEPTH):
        with ExitStack() as st:
            st.enter_context(nc.named_scope("mod_l%d" % l))
            cT = kb.sb(st, [128, 8, 2], F32, "cT")
            craw = kb.sb(st, [2, D], F32, "craw")
            s.dma("sp", craw[:], c_in[:], rd(c_in), rd(craw))
            csil = kb.sb(st, [2, D], F32, "csil")
            s.op("act", lambda e: e.activation(out=csil[:], in_=craw[:], func=AF.Silu), rd(craw), rd(csil))
            for kc in range(8):
                s.op("pe", lambda e: e.transpose(out=psb[0][:, kc * 2:kc * 2 + 2], in_=csil[:, kc * 128:(kc + 1) * 128],
                                                 identity=ident[0:2, 0:2]), rd(csil, ident), rd(psb[0]))
            s.op("dve", lambda e: e.tensor_copy(out=cT[:].rearrange("p k r -> p (k r)"), in_=psb[0][:, 0:16]),
                 rd(psb[0]), rd(cT))
            modrow = kb.sb(st, [2, 6 * D], F32, "modrow")
            abrow = kb.sb(st, [2, 6 * D], F32, "abrow")
            for r in range(2):
                s.dma("sp", abrow[r:r + 1, :], ada_b[l:l + 1, :], rd(ada_b), rd(abrow))
            wch = [kb.sb(st, [128, 3072], F32, "adaw%d" % i) for i in range(2)]
            for half in range(2):
                for kc in range(8):
                    wt = wch[kc % 2]
                    s.dma("sp" if kc % 2 == 0 else "pool", wt[:],
                          ada_w[l, kc * 128:(kc + 1) * 128, half * 3072:(half + 1) * 3072], rd(ada_w), rd(wt))
                    for cb in range(6):
                        s.op("pe", lambda e: e.matmul(out=psb[cb][0:2, :], lhsT=cT[:, kc, :],
                                                      rhs=wt[:, cb * 512:(cb + 1) * 512],
                                                      start=(kc == 0), stop=(kc == 7)),
                             rd(cT, wt), rd(psb[cb]))
                for cb in range(6):
                    c0 = half * 3072 + cb * 512
                    s.op("dve", lambda e: e.tensor_tensor(out=modrow[:, c0:c0 + 512], in0=psb[cb][0:2, :],
                                                          in1=abrow[:, c0:c0 + 512], op=ALU.add),
                         rd(psb[cb], abrow), rd(modrow))
            s.dma("sp", MODD[l], modrow[:], rd(modrow), rd(MODD))
        if dbg_stage == "mod" and l == 0:
            break

        s.barrier()
        with ExitStack() as st:
            st.enter_context(nc.named_scope("A_l%d" % l))
            hT = kb.sb(st, [128, 8, NT], BF16, "hT")
            modT = kb.sb(st, [128, 48, 2], F32, "modT")
            for r in range(2):
                s.dma("sp", modT[:, :, r], MODD[l, r].rearrange("(c p) -> p c", p=128), rd(MODD), rd(modT),
                      allow_slow_non_contiguous=True)
            sc1p = kb.sb(st, [128, 8, 2], F32, "sc1p")
            s.op("dve", lambda e: e.tensor_scalar_add(out=sc1p[:], in0=modT[:, 8:16, :], scalar1=1.0), rd(modT), rd(sc1p))
            xin = [kb.sb(st, [128, D], F32, "xin%d" % i) for i in range(2)]
            xn = [kb.sb(st, [128, D], F32, "xn%d" % i) for i in range(2)]
            stats = [kb.sb(st, [128, 2, 6], F32, "bst%d" % i) for i in range(2)]
            mv = [kb.sb(st, [128, 2], F32, "mv%d" % i) for i in range(2)]
            rstd = [kb.sb(st, [128, 1], F32, "rstd%d" % i) for i in range(2)]
            src = x_in if l == 0 else X2
            for t in range(NTT):
                i = t % 2
                r = 0 if t < L // 128 else 1
                s.dma("sp", xin[i][:], src[t * 128:(t + 1) * 128, :], rd(src), rd(xin[i]))
                if l == 0:
                    for j in range(2):
                        s.op("dve", lambda e: e.bn_stats(out=stats[i][:, j, :], in_=xin[i][:, j * 512:(j + 1) * 512]),
                             rd(xin[i]), rd(stats[i]))
                    s.op("dve", lambda e: e.bn_aggr(out=mv[i][:], in_=stats[i][:].rearrange("p a b -> p (a b)")),
                         rd(stats[i]), rd(mv[i]))
                    s.op("dve", lambda e: e.tensor_scalar_add(out=rstd[i][:], in0=mv[i][:, 1:2], scalar1=LN_EPS),
                         rd(mv[i]), rd(rstd[i]))
                    s.op("act", lambda e: e.sqrt(out=rstd[i][:], in_=rstd[i][:]), rd(rstd[i]), rd(rstd[i]))
                    s.op("dve", lambda e: e.reciprocal(out=rstd[i][:], in_=rstd[i][:]), rd(rstd[i]), rd(rstd[i]))
                    s.op("dve", lambda e: e.tensor_scalar(out=xn[i][:], in0=xin[i][:], scalar1=mv[i][:, 0:1],
                                                          scalar2=rstd[i][:, 0:1], op0=ALU.subtract, op1=ALU.mult),
                         rd(xin[i], mv[i], rstd[i]), rd(xn[i]))
                    s.dma("pool", XS[t * 128:(t + 1) * 128, :], xn[i][:], rd(xn[i]), rd(XS), nowaw=True)
                    xs_t = xn[i]
                else:
                    xs_t = xin[i]
                for kc in range(8):
                    pb = psb[kc // 4]
                    s.op("pe", lambda e: e.transpose(out=pb[:, (kc % 4) * 128:(kc % 4 + 1) * 128],
                                                     in_=xs_t[:, kc * 128:(kc + 1) * 128], identity=ident[:]),
                         rd(xs_t, ident), rd(pb))
                for kc in range(8):
                    pb = psb[kc // 4]
                    eng = "act" if kc % 2 == 0 else "dve"
                    if eng == "act":
                        s.op("act", lambda e: e.activation(out=hT[:, kc, t * 128:(t + 1) * 128],
                                                           in_=pb[:, (kc % 4) * 128:(kc % 4 + 1) * 128],
                                                           func=AF.Identity, scale=sc1p[:, kc, r:r + 1],
                                                           bias=modT[:, kc, r:r + 1]),
                             rd(pb, sc1p, modT), rd(hT), nowaw=True)
                    else:
                        s.op("dve", lambda e: e.tensor_scalar(out=hT[:, kc, t * 128:(t + 1) * 128],
                                                              in0=pb[:, (kc % 4) * 128:(kc % 4 + 1) * 128],
                                                              scalar1=sc1p[:, kc, r:r + 1], scalar2=modT[:, kc, r:r + 1],
                                                              op0=ALU.mult, op1=ALU.add),
                             rd(pb, sc1p, modT), rd(hT), nowaw=True)
            wbf = kb.sb(st, [128, 8, N_IN], BF16, "wbf")
            for kc in range(8):
                for (c0, c1) in ((0, 1536), (1536, N_IN)):
                    s.dma("pool", wbf[:, kc, c0:c1], w_in[l, kc * 128:(kc + 1) * 128, c0:c1], rd(w_in), rd(wbf))
            ev = [kb.sb(st, [128, 512], F32, "ev%d" % i) for i in range(4)]
            n = 0
            for cc in range(12):
                for tt in range(0, NT, 512):
                    w = min(512, NT - tt)
                    pb = psb[2 + n % 4]
                    for kc in range(8):
                        s.op("pe", lambda e: e.matmul(out=pb[:, 0:w], lhsT=wbf[:, kc, cc * 128:(cc + 1) * 128],
                                                      rhs=hT[:, kc, tt:tt + w], start=(kc == 0), stop=(kc == 7)),
                             rd(wbf, hT), rd(pb))
                    e_ = ev[n % 4]
                    if n % 2 == 0:
                        s.op("act", lambda e: e.copy(out=e_[:, 0:w], in_=pb[:, 0:w]), rd(pb), rd(e_))
                    else:
                        s.op("dve", lambda e: e.tensor_copy(out=e_[:, 0:w], in_=pb[:, 0:w]), rd(pb), rd(e_))
                    s.dma("sp", QKVT[cc * 128:(cc + 1) * 128, tt:tt + w], e_[:, 0:w], rd(e_), rd(QKVT), nowaw=True)
                    n += 1
            for t in range(NTT):
                for c0 in range(OFF_Z, N_IN, 512):
                    w = min(512, N_IN - c0)
                    pb = psb[2 + n % 4]
                    for kc in range(8):
                        s.op("pe", lambda e: e.matmul(out=pb[:, 0:w], lhsT=hT[:, kc, t * 128:(t + 1) * 128],
                                                      rhs=wbf[:, kc, c0:c0 + w], start=(kc == 0), stop=(kc == 7)),
                             rd(wbf, hT), rd(pb))
                    e_ = ev[n % 4]
                    if n % 2 == 0:
                        s.op("act", lambda e: e.copy(out=e_[:, 0:w], in_=pb[:, 0:w]), rd(pb), rd(e_))
                    else:
                        s.op("dve", lambda e: e.tensor_copy(out=e_[:, 0:w], in_=pb[:, 0:w]), rd(pb), rd(e_))
                    s.dma("sp", PTOK[t * 128:(t + 1) * 128, c0 - OFF_Z:c0 - OFF_Z + w], e_[:, 0:w], rd(e_), rd(PTOK), nowaw=True)
                    n += 1
        if dbg_stage == "A":
            break
        s.barrier()
        C = NS()
        C.bcrow = bcrow
        C.kb = kb; C.ident = ident; C.identb = identb; C.ones = ones; C.masks = masks; C.psb = psb
        C.conv_w = conv_w; C.a_log = a_log; C.dt_bias = dt_bias; C.gdn_norm_w = gdn_norm_w
        C.QKVT = QKVT; C.PTOK = PTOK; C.OPSD = OPSD; C.OFB = OFB; C.MIXT = MIXT; C.dbg_stage = dbg_stage
        C.cut = int(os.environ.get("GCUT", "99"))
        if dbg_stage == "gdn":
            C.o_gdn = dbg_out("o_gdn", [NT, 512])
        if dbg_stage in ("gdnA", "gdnB"):
            C.o_dbg = dbg_out("o_dbg", [128, 1024])
        if dbg_stage == "gdnB":
            C.o_dbgB = dbg_out("o_dbgB", [4, 128, OPW])
        try:
            stage_gdn(C, l)
        except Cut:
            pass
        s.barrier()
        if dbg_stage in ("gdn", "gdnprep", "gdnscan", "gdnA", "gdnB"):
            break
        C.q_norm_w = q_norm_w; C.k_norm_w = k_norm_w; C.rope_in = rope_in
        if dbg_stage == "attq":
            C.o_dbg = dbg_out("o_dbg", [128, 1024])
        stage_att(C, l)
        s.barrier()
        if dbg_stage in ("att", "attq"):
            break
        C.w_out = w_out; C.ln1_w = ln1_w; C.ln1_b = ln1_b; C.ln2_w = ln2_w; C.ln2_b = ln2_b
        C.router_w = router_w; C.router_bias = router_bias; C.sh_w_gate = sh_w_gate; C.sh_w_up = sh_w_up
        C.sh_w_down = sh_w_down; C.MODD = MODD; C.XS = XS; C.X1 = X1; C.X2 = X2; C.H2 = H2; C.SHO = SHO
        if dbg_stage == "D":
            C.o_y = dbg_out("o_y", [NT, D])
        with ExitStack() as mst:
            M = NS()
            M.WFULL = kb.sb(mst, [128, NTT, 256], F32, "WFULL")
            stage_D(C, l, M)
            s.barrier()
            if dbg_stage != "D":
                C.iotas = iotas; C.WG = WG; C.WU = WU; C.WD = WD; C.BUFTW = BUFTW; C.EO = EO; C.ones256 = ones256
                C.out = final_out
                if not hasattr(kb, "reg_tab"):
                    kb.reg_tab = nc.gpsimd.to_reg(TAB_LAYERS * 32768 - 1)
                    kb.reg_nt = nc.gpsimd.to_reg(NT - 1)
                C.reg_tab = kb.reg_tab; C.reg_nt = kb.reg_nt
                if dbg_stage == "E1":
                    C.o_e1 = dbg_out("o_e1", [128, 3000])
                if dbg_stage == "E":
                    C.o_ff = dbg_out("o_ff", [NT, D])
                stage_E(C, l, M)
                s.barrier()
            if dbg_stage == "D":
                o2 = dbg_out("o_wfull", [128, NTT, 256])
                s.dma("sp", o2[:], M.WFULL[:], rd(M.WFULL), rd(o2))
                C.dfin = [o2]
        if dbg_stage in ("D", "E1", "E2", "E2s", "E"):
            break

    finals = []
    if dbg_stage == "mod":
        o = dbg_out("o_mod", [2, 6 * D])
        with ExitStack() as st:
            tmp = kb.sb(st, [2, 6 * D], F32, "dbgm")
            s.dma("sp", tmp[:], MODD[0], rd(MODD), rd(tmp))
            s.dma("sp", o[:], tmp[:], rd(tmp), rd(o))
        finals.append(o)
    if dbg_stage == "A":
        o1 = dbg_out("o_qkvt", [OFF_Z, NT])
        o2 = dbg_out("o_ptok", [NT, NTOKC])
        o3 = dbg_out("o_xs", [NT, D])
        s.dma("sp", o1[:], QKVT[:], rd(QKVT), rd(o1))
        s.dma("sp", o2[:], PTOK[:], rd(PTOK), rd(o2))
        s.dma("sp", o3[:], XS[:], rd(XS), rd(o3))
        finals += [o1, o2, o3]
    if dbg_stage is None:
        finals.append(final_out)
    if dbg_stage == "E1":
        finals.append(outs["o_e1"])
    if dbg_stage in ("E2", "E2s"):
        o1 = dbg_out("o_eo", [2048, D], BF16)
        s.dma("sp", o1[:], EO[0:2048, :], rd(EO), rd(o1))
        finals.append(o1)
    if dbg_stage == "E":
        o1 = dbg_out("o_x2", [NT, D])
        s.dma("sp", o1[:], X2[:], rd(X2), rd(o1))
        finals += [o1, outs["o_ff"]]
    if dbg_stage in ("gdn", "gdnscan"):
        o1 = dbg_out("o_of", [NT, 512])
        o2 = dbg_out("o_ob", [NT, 512])
        s.dma("sp", o1[:], OFB[0][:], rd(OFB[0]), rd(o1))
        s.dma("sp", o2[:], OFB[1][:], rd(OFB[1]), rd(o2))
        finals += [o1, o2]
        if dbg_stage == "gdn":
            finals.append(outs["o_gdn"])
    if dbg_stage in ("gdnA", "attq"):
        finals.append(outs["o_dbg"])
    if dbg_stage == "D":
        finals += C.dfin + [outs["o_y"]]
        for nm, tsrc in (("o_x1", X1), ("o_h2", H2), ("o_sho", SHO)):
            o1 = dbg_out(nm, [NT, D])
            s.dma("sp", o1[:], tsrc[:], rd(tsrc), rd(o1))
            finals.append(o1)
    if dbg_stage == "att":
        o1 = dbg_out("o_mixt", [D, NT], BF16)
        s.dma("sp", o1[:], MIXT[:], rd(MIXT), rd(o1))
        finals.append(o1)
    if dbg_stage == "gdnB":
        finals.append(outs["o_dbgB"])
    if dbg_stage == "gdnprep":
        o1 = dbg_out("o_ops", [4, 2, NTT, 128, OPW])
        s.dma("sp", o1[:], OPSD[:], rd(OPSD), rd(o1))
        finals.append(o1)
    s.finish("sp", [f.res for f in finals])
    es.close()
    return nc, list(outs.keys())


_r = np.arange(128)
_m4 = np.stack([(_r[:, None] <= _r[None, :]), (_r[:, None] >= _r[None, :]),
                (_r[:, None] > _r[None, :]), (_r[:, None] < _r[None, :])]).astype(np.float32)
MASKS = np.concatenate([_m4, (1.0 - _m4[2:4]) * 3.0e4, -(1.0 - _m4[0:2]) * 3.0e4]).astype(np.float32)


def _rope_tables():
    t = np.arange(L)
    row = (t // 64).astype(np.float32)
    col = (t % 64).astype(np.float32)
    inv = (np.float32(10000.0) ** (-np.arange(0, 64, 2, dtype=np.float32) / np.float32(64))).astype(np.float32)
    ang = np.stack([row[:, None] * inv, col[:, None] * inv], axis=1).astype(np.float32)
    cs = np.stack([np.cos(ang), np.sin(ang)]).reshape(2, L, 64).astype(np.float32)
    return np.ascontiguousarray(np.tile(cs, (1, 1, 6)))


ROPE = _rope_tables()
IOTAS = np.concatenate([np.tile(np.arange(NBLK, dtype=np.float32), (128, 1)), np.arange(128, dtype=np.float32)[:, None],
                        (np.arange(NTT, dtype=np.float32)[None, :] * 128 + np.arange(128, dtype=np.float32)[:, None])], axis=1)


def expert_tables(inputs):
    n = TAB_LAYERS
    g = inputs["exp_w_gate"][:n]; u = inputs["exp_w_up"][:n]; d = inputs["exp_w_down"][:n]
    WG = np.ascontiguousarray(g.reshape(n, 256, 8, 128, 256).transpose(0, 1, 3, 2, 4)).reshape(n * 256 * 128, 2048)
    WU = np.ascontiguousarray(u.reshape(n, 256, 8, 128, 256).transpose(0, 1, 3, 2, 4)).reshape(n * 256 * 128, 2048)
    WD = np.ascontiguousarray(d.reshape(n, 256, 2, 128, 1024).transpose(0, 1, 3, 2, 4)).reshape(n * 256 * 128, 2048)
    return WG, WU, WD


def make_inputs(inputs, b, tabs=None):
    xx = np.concatenate([inputs["x"][b], inputs["ctx"][b]], axis=0)
    c2 = np.stack([inputs["c"][b], inputs["c_ctx"]], axis=0)
    m = {
        "x": np.ascontiguousarray(xx, dtype=np.float32),
        "c2": np.ascontiguousarray(c2, dtype=np.float32),
        "ada_w": inputs["ada_w"], "ada_b": inputs["ada_b"], "w_in": inputs["w_in"],
        "ident": np.eye(128, dtype=np.float32),
        "masks": MASKS,
        "conv_w": inputs["conv_w"], "gdn_a_log": inputs["gdn_a_log"].reshape(DEPTH, 8),
        "gdn_dt_bias": inputs["gdn_dt_bias"].reshape(DEPTH, 8), "gdn_norm_w": inputs["gdn_norm_w"],
        "q_norm_w": inputs["q_norm_w"], "k_norm_w": inputs["k_norm_w"], "rope": ROPE,
        "w_out": inputs["w_out"], "ln1_w": inputs["ln1_w"], "ln1_b": inputs["ln1_b"],
        "ln2_w": inputs["ln2_w"], "ln2_b": inputs["ln2_b"], "router_w": inputs["router_w"],
        "router_bias": inputs["router_bias"], "sh_w_gate": inputs["sh_w_gate"], "sh_w_up": inputs["sh_w_up"],
        "sh_w_down": inputs["sh_w_down"],
        "iotas": IOTAS,
    }
    if tabs is not None:
        m["WG"], m["WU"], m["WD"] = tabs
    return m


def kernel(**inputs):
    nc, onames = build()
    tabs = expert_tables(inputs)
    in_maps = [make_inputs(inputs, b, tabs) for b in range(8)]
    res = run_bass_kernel_spmd(nc, in_maps, core_ids=list(range(8)))
    return np.stack([r["out"] for r in res.results], axis=0)
```
